# Optimizing a Trainium2 kernel written in Bass

```python
import math
import jax, jax.numpy as jnp
from jax import lax
import numpy as np

D_MODEL = 1024
BATCH = 16
SEQ = 2048
DEPTH = 1

PLE_DIM = 256
DA_HEADS = 4
DA_QK = 64
DA_V = 2 * DA_QK
DA_WIDTH = DA_HEADS * DA_V
RW_HEADS = 8
RW_N = 64
RW_WIDTH = RW_HEADS * RW_N
DECAY_LORA = 64
AAA_LORA = 64
GATE_LORA = 160
MIX_WIDTH = DA_WIDTH + RW_WIDTH
N_BUCKETS = 32
MAX_DISTANCE = 128
Q_BLOCK = 128
N_EXPERTS = 16
EC_FACTOR = 2
D_FF = 2048
NORM_EPS = 1e-6
RW_LN_EPS = 64e-5

DA_COLS = 3 * DA_WIDTH
RW_SIZES = [RW_WIDTH, RW_WIDTH, RW_WIDTH, 2 * DECAY_LORA, 2 * AAA_LORA, GATE_LORA]
RW_COLS = sum(RW_SIZES)
IN_COLS = DA_COLS + RW_COLS

kernel_name = "hybrid_diffattn_rwkv7_ecmoe_block"


def rmsnorm(x, g, eps=NORM_EPS):
    xf = x.astype(jnp.float32)
    y = xf * lax.rsqrt(jnp.mean(xf * xf, axis=-1, keepdims=True) + eps)
    return (y * g.astype(jnp.float32)).astype(x.dtype)


def t5_bucket(rel):
    half = N_BUCKETS // 2
    max_exact = half // 2
    ret = jnp.where(rel > 0, half, 0)
    n = jnp.abs(rel)
    nf = jnp.maximum(n, 1).astype(jnp.float32)
    large = max_exact + (jnp.log(nf / max_exact) / math.log(MAX_DISTANCE / max_exact)
                         * (half - max_exact)).astype(jnp.int32)
    large = jnp.minimum(large, half - 1)
    return ret + jnp.where(n < max_exact, n, large)


def diff_attention(q, k, v, lam, rel_bias):
    B, S, H, _, dq = q.shape
    dv = v.shape[-1]
    nb = S // Q_BLOCK
    qb = q.reshape(B, nb, Q_BLOCK, H, 2, dq).transpose(1, 0, 3, 4, 2, 5)
    kt = k.transpose(0, 2, 3, 1, 4)
    vt = v.transpose(0, 2, 1, 3)
    kpos = jnp.arange(S, dtype=jnp.int32)
    scale = DA_QK ** -0.5

    def block(args):
        qblk, start = args
        qpos = start + jnp.arange(Q_BLOCK, dtype=jnp.int32)
        bias = rel_bias[t5_bucket(kpos[None, :] - qpos[:, None])]
        bias = bias.astype(jnp.float32).transpose(2, 0, 1)[None, :, None]
        logits = jnp.einsum('bhcqd,bhckd->bhcqk', qblk, kt,
                            preferred_element_type=jnp.float32) * scale + bias
        probs = jax.nn.softmax(logits, axis=-1)
        attn = probs[:, :, 0] - lam * probs[:, :, 1]
        return jnp.einsum('bhqk,bhkd->bhqd', attn.astype(vt.dtype), vt)

    starts = jnp.arange(nb, dtype=jnp.int32) * Q_BLOCK
    out = lax.map(block, (qb, starts))
    return out.transpose(1, 0, 3, 2, 4).reshape(B, S, H, dv)


def diff_attn_mixer(u, q_g, k_g, lq1, lk1, lq2, lk2, subln_g, rel_bias, layer_idx):
    B, S, _ = u.shape
    q, k, v = jnp.split(u, 3, axis=-1)
    q = rmsnorm(q.reshape(B, S, DA_HEADS, 2, DA_QK), q_g)
    k = rmsnorm(k.reshape(B, S, DA_HEADS, 2, DA_QK), k_g)
    v = v.reshape(B, S, DA_HEADS, DA_V)
    lam_init = 0.8 - 0.6 * math.exp(-0.3 * layer_idx)
    lam = (jnp.exp(jnp.sum(lq1.astype(jnp.float32) * lk1.astype(jnp.float32)))
           - jnp.exp(jnp.sum(lq2.astype(jnp.float32) * lk2.astype(jnp.float32))) + lam_init)
    o = diff_attention(q, k, v, lam, rel_bias)
    o = rmsnorm(o, subln_g) * (1.0 - lam_init)
    return o.reshape(B, S, DA_WIDTH)


def centred_shift_mix(u, mu):
    zero = jnp.zeros_like(u[:, :1])
    prev = jnp.concatenate([zero, u[:, :-1]], axis=1)
    nxt = jnp.concatenate([u[:, 1:], zero], axis=1)
    return u + mu * (0.5 * (prev + nxt) - u)


def rwkv7_bidir_scan(r, w, k, v, kk, a):
    _, B, S, H, N = r.shape
    xs = tuple(jnp.moveaxis(t, 2, 0) for t in (r, w, k, v, kk, a))

    def step(state, inp):
        r_t, w_t, k_t, v_t, kk_t, a_t = inp
        sa = jnp.einsum('dbhvk,dbhk->dbhv', state, -kk_t)
        state = (state * w_t[..., None, :] + sa[..., None] * (kk_t * a_t)[..., None, :]
                 + v_t[..., :, None] * k_t[..., None, :])
        y = jnp.einsum('dbhvk,dbhk->dbhv', state, r_t)
        return state, y

    s0 = jnp.zeros((2, B, H, N, N), jnp.float32)
    _, ys = lax.scan(step, s0, xs)
    return jnp.moveaxis(ys, 0, 2)


def rwkv7_mixer(u, mu, w0, w2, a0, a2, g2, k_k, k_a, r_k, lnx_g, lnx_b):
    in_dtype = u.dtype
    u = centred_shift_mix(u.astype(jnp.float32), mu.astype(jnp.float32))
    B, S, _ = u.shape
    offs = list(np.cumsum(RW_SIZES)[:-1])
    r, k, v, wl, al, gl = jnp.split(u, offs, axis=-1)
    f32 = lambda t: t.astype(jnp.float32)
    wl = wl.reshape(B, S, 2, DECAY_LORA)
    al = al.reshape(B, S, 2, AAA_LORA)
    w_logit = f32(w0)[:, None, None, :] + jnp.einsum('bsdr,drc->dbsc', jnp.tanh(wl), f32(w2))
    decay = jnp.exp(-jnp.exp(-jax.nn.softplus(-w_logit) - 0.5))
    a = jax.nn.sigmoid(f32(a0)[:, None, None, :] + jnp.einsum('bsdr,drc->dbsc', al, f32(a2)))
    g = jax.nn.sigmoid(gl) @ f32(g2)
    kk = (k * f32(k_k)).reshape(B, S, RW_HEADS, RW_N)
    kk = kk / jnp.maximum(jnp.sqrt(jnp.sum(kk * kk, axis=-1, keepdims=True)), 1e-12)
    k_dir = k[None] * (1.0 + (a - 1.0) * f32(k_a))
    heads = lambda t: t.reshape(t.shape[:-1] + (RW_HEADS, RW_N))
    rh, vh = heads(r), heads(v)
    kdh, dh, ah = heads(k_dir), heads(decay), heads(a)
    both = lambda t: jnp.stack([t, t[:, ::-1]])
    orient = lambda t: jnp.stack([t[0], t[1][:, ::-1]])
    ys = rwkv7_bidir_scan(both(rh), orient(dh), orient(kdh), both(vh), both(kk), orient(ah))
    y = ys[0] + ys[1][:, ::-1]
    mean = jnp.mean(y, axis=-1, keepdims=True)
    var = jnp.mean(jnp.square(y - mean), axis=-1, keepdims=True)
    y = (y - mean) * lax.rsqrt(var + RW_LN_EPS)
    y = y * heads(f32(lnx_g)) + heads(f32(lnx_b))
    bonus = jnp.sum(rh * (kdh[0] + kdh[1]) * f32(r_k), axis=-1, keepdims=True) * vh
    out = (y + bonus).reshape(B, S, RW_WIDTH) * g
    return out.astype(in_dtype)


def expert_choice_ffn(x, w_router, w1, w3, w2):
    B, S, _ = x.shape
    cap = EC_FACTOR * S // N_EXPERTS
    aff = jax.nn.softmax((x @ w_router).astype(jnp.float32), axis=-1)
    gate, idx = lax.top_k(jnp.swapaxes(aff, 1, 2), cap)
    xe = jax.vmap(lambda xb, ib: xb[ib])(x, idx)
    hdn = jax.nn.silu(jnp.einsum('becd,edf->becf', xe, w1)) * jnp.einsum('becd,edf->becf', xe, w3)
    ye = jnp.einsum('becf,efd->becd', hdn, w2) * gate[..., None].astype(x.dtype)
    bidx = jnp.arange(B)[:, None, None]
    return jnp.zeros_like(x).at[bidx, idx].add(ye)


def per_layer_embedding(h, p_i, g_in, w_gate, w_pe, g_post):
    gate = jax.nn.sigmoid(rmsnorm(h, g_in) @ w_gate)
    e = rmsnorm(p_i @ w_pe, g_post)
    return h + gate * e


def setup_inputs(seed: int = 0) -> dict:
    key = jax.random.key(seed)
    ks = iter(jax.random.split(key, 48))
    f32 = jnp.float32
    nrm = lambda shape, scale: jax.random.normal(next(ks), shape, f32) * scale
    L = DEPTH
    return {
        "x": nrm((BATCH, SEQ, D_MODEL), 1.0),
        "p": nrm((DEPTH, BATCH, SEQ, PLE_DIM), 1.0),
        "rel_bias": nrm((N_BUCKETS, DA_HEADS), 0.5),
        "g_mix": 1.0 + nrm((L, D_MODEL), 0.05),
        "w_in": nrm((L, D_MODEL, IN_COLS), D_MODEL ** -0.5),
        "w_out": nrm((L, MIX_WIDTH, D_MODEL), MIX_WIDTH ** -0.5),
        "q_norm_g": 1.0 + nrm((L, DA_QK), 0.05),
        "k_norm_g": 1.0 + nrm((L, DA_QK), 0.05),
        "lam_q1": nrm((L, DA_QK), 0.1),
        "lam_k1": nrm((L, DA_QK), 0.1),
        "lam_q2": nrm((L, DA_QK), 0.1),
        "lam_k2": nrm((L, DA_QK), 0.1),
        "subln_g": 1.0 + nrm((L, DA_V), 0.05),
        "rw_mu": jax.random.uniform(next(ks), (L, RW_COLS), f32, 0.2, 0.8),
        "rw_w0": jax.random.uniform(next(ks), (L, 2, RW_WIDTH), f32, -6.0, -1.0),
        "rw_w2": nrm((L, 2, DECAY_LORA, RW_WIDTH), 0.1 * DECAY_LORA ** -0.5),
        "rw_a0": nrm((L, 2, RW_WIDTH), 0.1),
        "rw_a2": nrm((L, 2, AAA_LORA, RW_WIDTH), 0.1 * AAA_LORA ** -0.5),
        "rw_g2": nrm((L, GATE_LORA, RW_WIDTH), GATE_LORA ** -0.5),
        "rw_k_k": 0.85 + nrm((L, RW_WIDTH), 0.05),
        "rw_k_a": 1.0 + nrm((L, RW_WIDTH), 0.05),
        "rw_r_k": nrm((L, RW_HEADS, RW_N), 0.1),
        "rw_lnx_g": 1.0 + nrm((L, RW_WIDTH), 0.05),
        "rw_lnx_b": nrm((L, RW_WIDTH), 0.01),
        "g_ffn": 1.0 + nrm((L, D_MODEL), 0.05),
        "w_router": nrm((L, D_MODEL, N_EXPERTS), D_MODEL ** -0.5),
        "w1": nrm((L, N_EXPERTS, D_MODEL, D_FF), D_MODEL ** -0.5),
        "w3": nrm((L, N_EXPERTS, D_MODEL, D_FF), D_MODEL ** -0.5),
        "w2": nrm((L, N_EXPERTS, D_FF, D_MODEL), D_FF ** -0.5),
        "g_ple": 1.0 + nrm((L, D_MODEL), 0.05),
        "w_ple_gate": nrm((L, D_MODEL, D_MODEL), D_MODEL ** -0.5),
        "w_ple": nrm((L, PLE_DIM, D_MODEL), PLE_DIM ** -0.5),
        "g_ple_post": 1.0 + nrm((L, D_MODEL), 0.05),
    }


def reference(x, p, rel_bias, g_mix, w_in, w_out, q_norm_g, k_norm_g, lam_q1, lam_k1, lam_q2,
              lam_k2, subln_g, rw_mu, rw_w0, rw_w2, rw_a0, rw_a2, rw_g2, rw_k_k, rw_k_a, rw_r_k,
              rw_lnx_g, rw_lnx_b, g_ffn, w_router, w1, w3, w2, g_ple, w_ple_gate, w_ple,
              g_ple_post):
    h = x
    for i in range(DEPTH):
        u = rmsnorm(h, g_mix[i]) @ w_in[i]
        u_da, u_rw = u[..., :DA_COLS], u[..., DA_COLS:]
        o_da = diff_attn_mixer(u_da, q_norm_g[i], k_norm_g[i], lam_q1[i], lam_k1[i],
                               lam_q2[i], lam_k2[i], subln_g[i], rel_bias, i)
        o_rw = rwkv7_mixer(u_rw, rw_mu[i], rw_w0[i], rw_w2[i], rw_a0[i], rw_a2[i], rw_g2[i],
                           rw_k_k[i], rw_k_a[i], rw_r_k[i], rw_lnx_g[i], rw_lnx_b[i])
        h = h + jnp.concatenate([o_da, o_rw], axis=-1) @ w_out[i]
        h = h + expert_choice_ffn(rmsnorm(h, g_ffn[i]), w_router[i], w1[i], w3[i], w2[i])
        h = per_layer_embedding(h, p[i], g_ple[i], w_ple_gate[i], w_ple[i], g_ple_post[i])
    return h
```

```python
import math
import numpy as np
from contextlib import ExitStack
import concourse.bass as bass
import concourse.mybir as mybir
from concourse.bass_utils import run_bass_kernel_spmd

F32 = mybir.dt.float32
BF16 = mybir.dt.bfloat16
AF = mybir.ActivationFunctionType
ALU = mybir.AluOpType

NCORES = 8
SEQ = 2048
DM = 1024
NSEQ = 2
NT = SEQ // 128
IN_COLS = 3488
RW0 = 1536
NE = 16
CAP = 256
DFF = 2048
CDEC = 0.6065306597126334
LAM_INIT = 0.8 - 0.6 * math.exp(-0.3 * 0)
STRIP_W = 1152
ND = 1279


class Holder:
    def __init__(self, name, sem):
        self.name = name
        self.sem = sem
        self.count = 0


class Sched:
    def __init__(self, nc, stack):
        self.nc = nc
        self.stack = stack
        self.obj = {"pe": nc.tensor, "dve": nc.vector, "act": nc.scalar,
                    "pool": nc.gpsimd, "sp": nc.sync}
        self.eng = {}
        for n in self.obj:
            sem = stack.enter_context(nc.semaphore("s_" + n))
            self.eng[n] = Holder(n, sem)
        self.known = {n: {} for n in self.obj}
        self.last_w = {}
        self.readers = {}
        self.dma_sems = {}
        self.n_instr = 0

    def _deps(self, reads, writes, e=None):
        own = self.eng.get(e) if e in ("pe",) else None
        toks = []
        for r in reads:
            t = self.last_w.get(r)
            if t is not None:
                toks.append(t)
        for w in writes:
            t = self.last_w.get(w)
            if t is not None and t[0] is not own:
                toks.append(t)
            for h, v in self.readers.get(w, {}).items():
                if h is not own:
                    toks.append((h, v))
        return toks

    def _wait(self, e, toks):
        kn = self.known[e]
        need = {}
        for (h, v) in toks:
            if kn.get(h, 0) < v and need.get(h, 0) < v:
                need[h] = v
        for h, v in need.items():
            self.obj[e].wait_ge(h.sem, v)
            kn[h] = v
            self.n_instr += 1

    def _commit(self, tok, reads, writes):
        for r in reads:
            d = self.readers.setdefault(r, {})
            if d.get(tok[0], 0) < tok[1]:
                d[tok[0]] = tok[1]
        for w in writes:
            self.last_w[w] = tok
            self.readers[w] = {}

    def op(self, e, fn, reads=(), writes=()):
        toks = self._deps(reads, writes, e)
        self._wait(e, toks)
        ins = fn()
        h = self.eng[e]
        h.count += 1
        ins.then_inc(h.sem, 1)
        tok = (h, h.count)
        self._commit(tok, reads, writes)
        self.n_instr += 1
        return tok

    def dma(self, q, out, in_, semkey, reads=(), writes=(), **kw):
        toks = self._deps(reads, writes)
        self._wait(q, toks)
        if semkey not in self.dma_sems:
            sem = self.stack.enter_context(self.nc.semaphore("d_%d" % len(self.dma_sems)))
            self.dma_sems[semkey] = Holder("dma_" + str(semkey), sem)
        h = self.dma_sems[semkey]
        ins = self.obj[q].dma_start(out=out, in_=in_, **kw)
        ins.then_inc(h.sem, 16)
        h.count += 16
        tok = (h, h.count)
        self._commit(tok, reads, writes)
        self.n_instr += 1
        return tok

    def _all(self):
        toks = [(h, h.count) for h in self.eng.values() if h.count > 0]
        toks += [(h, h.count) for h in self.dma_sems.values() if h.count > 0]
        return toks

    def barrier(self):
        toks = self._all()
        for e in self.obj:
            self._wait(e, toks)

    def final_wait(self, e="sp"):
        self._wait(e, self._all())


def _t5_bucket(rel):
    half, max_exact = 16, 8
    ret = np.where(rel > 0, half, 0)
    n = np.abs(rel)
    nf = np.maximum(n, 1).astype(np.float32)
    large = max_exact + (np.log(nf / np.float32(max_exact)) / np.float32(math.log(128 / max_exact))
                         * np.float32(half - max_exact)).astype(np.int32)
    large = np.minimum(large, half - 1)
    return ret + np.where(n < max_exact, n, large)


class Pack:
    def __init__(self):
        self.cols = []
        self.off = {}
        self.n = 0

    def add(self, name, arr):
        arr = np.asarray(arr, np.float32)
        assert arr.shape[0] == 128
        if arr.ndim == 1:
            arr = arr[:, None]
        self.off[name] = (self.n, arr.shape[1])
        self.cols.append(arr)
        self.n += arr.shape[1]

    def build(self):
        return np.ascontiguousarray(np.concatenate(self.cols, axis=1))


def pc(v, nchunk):
    return np.ascontiguousarray(np.asarray(v, np.float32).reshape(nchunk, 128).T)


def make_cpack(inp):
    P = Pack()
    P.add("g_mix", pc(inp["g_mix"][0], 8))
    P.add("qg", np.tile(inp["q_norm_g"][0], 2))
    P.add("kg", np.tile(inp["k_norm_g"][0], 2))
    mu = np.zeros(16 * 128, np.float32)
    mu[:1952] = inp["rw_mu"][0]
    P.add("mu", pc(mu, 16))
    P.add("w0", pc(inp["rw_w0"][0].reshape(-1), 8))
    P.add("a0", pc(inp["rw_a0"][0].reshape(-1), 8))
    P.add("k_k", pc(inp["rw_k_k"][0], 4))
    P.add("k_a", pc(inp["rw_k_a"][0], 4))
    P.add("r_k", pc(inp["rw_r_k"][0].reshape(-1), 4))
    P.add("lnx_g", pc(inp["rw_lnx_g"][0], 4))
    P.add("lnx_b", pc(inp["rw_lnx_b"][0], 4))
    P.add("subln_g", inp["subln_g"][0])
    P.add("g_ple", pc(inp["g_ple"][0], 8))
    rb = inp["rel_bias"]
    far = np.stack([rb[15, :], rb[31, :]], axis=1).reshape(-1)
    P.add("rb_far", np.broadcast_to(far[None, :], (128, 8)))
    P.add("iota_p", np.arange(128, dtype=np.float32))
    P.add("iota_p1", np.arange(128, 256, dtype=np.float32))
    for k in ("lam_q1", "lam_k1", "lam_q2", "lam_k2"):
        P.add(k, np.broadcast_to(inp[k][0][None, :], (128, 64)))
    return P


def make_bpack(inp):
    P = Pack()
    P.add("g_ffn", np.broadcast_to(inp["g_ffn"][0][None, :], (128, DM)))
    P.add("g_post", np.broadcast_to(inp["g_ple_post"][0][None, :], (128, DM)))
    P.add("iota_row", np.broadcast_to(np.arange(CAP, dtype=np.float32)[None, :], (128, CAP)))
    return P


def make_struct():
    m = np.arange(ND)
    delta = 639 - m
    bk = _t5_bucket(delta.astype(np.int32))
    oh = np.zeros((32, ND), np.float32)
    oh[bk, m] = 1.0
    idx = np.arange(64)
    s = idx[:, None]
    t = idx[None, :]
    rw = np.zeros((2, 64, 192), np.float32)
    rw[0, :, 0:64] = (s < t)
    rw[0, :, 64:128] = (s <= t)
    rw[0, :, 128:192] = (t < s)
    rw[1, :, 0:64] = (s > t)
    rw[1, :, 64:128] = (s >= t)
    rw[1, :, 128:192] = (t > s)
    return {"onehot": oh, "rwmask": np.ascontiguousarray(rw.transpose(1, 0, 2).reshape(64, 384))}


def build_program(cp_off, bp_off, ncp, nbp, dbg=None, nseq=NSEQ, stop_after=None):
    dbg = dbg or {}
    nc = bass.Bass("TRN2", target_bir_lowering=False)
    D = {}

    def din(name, shape, dt=F32):
        D[name] = nc.dram_tensor(name, list(shape), dt, kind="ExternalInput").ap()
        return D[name]

    x_d = din("x", [NSEQ, SEQ, DM])
    p_d = din("p", [NSEQ, SEQ, 256])
    w_in_d = din("w_in", [DM, IN_COLS])
    w_out_d = din("w_out", [DM, DM])
    cpack_d = din("cpack", [128, ncp])
    bpack_d = din("bpack", [128, nbp])
    relb_d = din("rel_bias", [32, 4])
    onehot_d = din("onehot", [32, ND])
    rwmask_d = din("rwmask", [64, 384])
    w2cat_d = din("w2cat", [128, 512])
    a2cat_d = din("a2cat", [128, 512])
    g2_d = din("g2", [160, 512])
    wr_d = din("w_router", [DM, NE])
    w1_d = din("w1", [NE, DM, DFF])
    w3_d = din("w3", [NE, DM, DFF])
    w2_d = din("w2", [NE, DFF, DM])
    wg_d = din("w_ple_gate", [DM, DM])
    wpe_d = din("w_ple", [256, DM])
    out_d = nc.dram_tensor("out", [NSEQ, SEQ, DM], F32, kind="ExternalOutput").ap()
    gscr_t = nc.dram_tensor("gscr", [4, ND], F32)
    gscr_d = gscr_t.ap()
    dbg_d = {}
    for k, (shape, dt) in dbg.items():
        dbg_d[k] = nc.dram_tensor("dbg_" + k, list(shape), dt, kind="ExternalOutput").ap()

    with ExitStack() as top:
        S = Sched(nc, top)
        V, A, G, PE_ = nc.vector, nc.scalar, nc.gpsimd, nc.tensor

        uid = [0]

        def sb(st, name, shape, dt):
            uid[0] += 1
            return st.enter_context(nc.sbuf_tensor("%s_s%d" % (name, uid[0]), list(shape), dt))

        def ps(st, name, shape, dt=F32):
            uid[0] += 1
            return st.enter_context(nc.psum_tensor("%s_p%d" % (name, uid[0]), list(shape), dt))

        def C(name, j=0, w=1):
            o, n = cp_off[name]
            return cpack[:, o + j:o + j + w]

        def tap(name, src_ap, reads, idx=None):
            if name in dbg_d:
                dst = dbg_d[name] if idx is None else dbg_d[name][idx]
                S.dma("sp", dst, src_ap, "dbg", reads=reads)

        cpack = sb(top, "cpack", [128, ncp], F32)
        S.dma("sp", cpack[:], cpack_d, "c0", writes=["cpack"])
        kc = sb(top, "kc", [128, 8], F32)
        S.op("pool", lambda: G.memset(kc[:, 0:1], 1e-6), writes=["kc"])
        S.op("pool", lambda: G.memset(kc[:, 1:2], 64e-5), writes=["kc"])
        S.op("pool", lambda: G.memset(kc[:, 2:3], 0.0), writes=["kc"])
        S.op("pool", lambda: G.memset(kc[:, 3:4], 1e-18), writes=["kc"])
        EPS = kc[:, 0:1]
        EPSLN = kc[:, 1:2]
        ident_b = sb(top, "ident_b", [128, 128], BF16)
        ident_f = sb(top, "ident_f", [128, 128], F32)
        for idt, nm in ((ident_b, "ident_b"), (ident_f, "ident_f")):
            S.op("pool", lambda idt=idt: G.memset(idt[:], 1.0), writes=[nm])
            S.op("pool", lambda idt=idt: G.affine_select(
                out=idt[:], in_=idt[:], pattern=[[-1, 128]], compare_op=ALU.is_equal,
                fill=0.0, base=0, channel_multiplier=1), reads=[nm], writes=[nm])
        bd_b = sb(top, "bd_b", [128, 128], BF16)
        S.op("pool", lambda: G.memset(bd_b[:], 0.0), writes=["bd_b"])
        S.op("pool", lambda: G.memset(bd_b[0:64, 0:64], 1.0), reads=["bd_b"], writes=["bd_b"])
        S.op("pool", lambda: G.memset(bd_b[64:128, 64:128], 1.0), reads=["bd_b"], writes=["bd_b"])
        dc = sb(top, "dc", [128, 64], F32)
        S.op("dve", lambda: V.tensor_scalar(out=dc[:, 0:1], in0=C("qg"), scalar1=0.125, scalar2=None,
                                            op0=ALU.mult), reads=["cpack"], writes=["dc0"])
        S.op("dve", lambda: V.tensor_scalar(out=dc[:, 3:19], in0=C("mu", 0, 16), scalar1=-1.0, scalar2=1.0,
                                            op0=ALU.mult, op1=ALU.add), reads=["cpack"], writes=["dc_omm"])
        S.op("dve", lambda: V.tensor_scalar(out=dc[:, 19:35], in0=C("mu", 0, 16), scalar1=0.5, scalar2=None,
                                            op0=ALU.mult), reads=["cpack"], writes=["dc_hmu"])
        S.op("dve", lambda: V.tensor_scalar(out=dc[:, 35:39], in0=C("k_a", 0, 4), scalar1=-1.0, scalar2=1.0,
                                            op0=ALU.mult, op1=ALU.add), reads=["cpack"], writes=["dc_omka"])
        lt = sb(top, "lamtmp", [128, 64], F32)
        l2 = sb(top, "lam2", [128, 4], F32)
        for i, (a, b) in enumerate((("lam_q1", "lam_k1"), ("lam_q2", "lam_k2"))):
            oa, ob = cp_off[a][0], cp_off[b][0]
            S.op("dve", lambda oa=oa, ob=ob: V.tensor_tensor(out=lt[:], in0=cpack[:, oa:oa + 64],
                                                               in1=cpack[:, ob:ob + 64], op=ALU.mult),
                 reads=["cpack"], writes=["lamtmp"])
            S.op("dve", lambda i=i: V.reduce_sum(out=l2[:, i:i + 1], in_=lt[:], axis=mybir.AxisListType.X),
                 reads=["lamtmp"], writes=[("lam2", i)])
        S.op("act", lambda: A.activation(out=l2[:, 2:4], in_=l2[:, 0:2], func=AF.Exp),
             reads=[("lam2", 0), ("lam2", 1)], writes=["lam2e"])
        S.op("dve", lambda: V.tensor_tensor(out=dc[:, 1:2], in0=l2[:, 2:3], in1=l2[:, 3:4], op=ALU.subtract),
             reads=["lam2e"], writes=["dc1a"])
        S.op("dve", lambda: V.tensor_scalar(out=dc[:, 1:2], in0=dc[:, 1:2], scalar1=LAM_INIT, scalar2=None,
                                            op0=ALU.add), reads=["dc1a"], writes=["dc1"])
        S.op("dve", lambda: V.tensor_scalar(out=dc[:, 2:3], in0=dc[:, 1:2], scalar1=-1.0, scalar2=None,
                                            op0=ALU.mult), reads=["dc1"], writes=["dc2"])
        QGS = dc[:, 0:1]
        NLAM = dc[:, 2:3]

        with ExitStack() as st:
            rb_sb = sb(st, "rb_sb", [32, 4], F32)
            oh_sb = sb(st, "oh_sb", [32, ND], F32)
            g4 = sb(st, "g4", [4, ND], F32)
            gp = ps(st, "gp", [4, 512])
            S.dma("sp", rb_sb[:], relb_d, "c1", writes=["rb_sb"])
            S.dma("sp", oh_sb[:], onehot_d, "c2", writes=["oh_sb"])
            for b0 in range(0, ND, 512):
                n = min(512, ND - b0)
                S.op("pe", lambda b0=b0, n=n: PE_.matmul(gp[:, 0:n], lhsT=rb_sb[:], rhs=oh_sb[:, b0:b0 + n],
                                                           start=True, stop=True),
                     reads=["rb_sb", "oh_sb"], writes=["gp"])
                S.op("dve", lambda b0=b0, n=n: V.tensor_copy(out=g4[:, b0:b0 + n], in_=gp[:, 0:n]),
                     reads=["gp"], writes=["g4"])
            S.dma("sp", gscr_d, g4[:], "c3", reads=["g4"], writes=["gscr"])
            S.barrier()

        for s in range(nseq):
            with ExitStack() as sq:
                o_daT = sb(sq, "o_daT", [128, 4, SEQ], BF16)
                o_rwT = sb(sq, "o_rwT", [128, 4, SEQ], BF16)
                mixs = ExitStack()
                sq.callback(mixs.close)
                xnT = sb(mixs, "xnT", [128, 8, SEQ], BF16)
                with ExitStack() as st:
                    xt = [sb(st, "xt%d" % i, [128, DM], F32) for i in range(2)]
                    xs = [sb(st, "xs%d" % i, [128, DM], BF16) for i in range(2)]
                    junk = sb(st, "junk", [128, DM], BF16)
                    ssq = sb(st, "ssq", [128, 2 * NT], F32)
                    ptp = [ps(st, "ptp%d" % i, [128, 1024], BF16) for i in range(2)]
                    for tt in range(NT):
                        b = tt % 2
                        S.dma("sp", xt[b][:], x_d[s, tt * 128:(tt + 1) * 128, :], ("xt", b), writes=[("xt", b)])
                        S.op("act", lambda b=b, tt=tt: A.activation(out=junk[:], in_=xt[b][:], func=AF.Square,
                                                                     accum_out=ssq[:, tt:tt + 1]),
                             reads=[("xt", b)], writes=["junk", ("ssq", tt)])
                        S.op("act", lambda tt=tt: A.activation(out=ssq[:, NT + tt:NT + tt + 1], in_=ssq[:, tt:tt + 1],
                                                               func=AF.Sqrt, bias=EPS, scale=1.0 / DM),
                             reads=[("ssq", tt), "kc"], writes=[("ssd", tt)])
                        S.op("dve", lambda tt=tt: V.reciprocal(out=ssq[:, tt:tt + 1], in_=ssq[:, NT + tt:NT + tt + 1]),
                             reads=[("ssd", tt)], writes=[("rstd", tt)])
                        S.op("dve", lambda b=b, tt=tt: V.tensor_scalar(out=xs[b][:], in0=xt[b][:], scalar1=ssq[:, tt:tt + 1],
                                                                        scalar2=None, op0=ALU.mult),
                             reads=[("xt", b), ("rstd", tt)], writes=[("xs", b)])
                        for c in range(8):
                            S.op("pe", lambda b=b, c=c: PE_.transpose(out=ptp[b][:, c * 128:(c + 1) * 128],
                                                                       in_=xs[b][:, c * 128:(c + 1) * 128], identity=ident_b[:]),
                                 reads=[("xs", b), "ident_b"], writes=[("ptp", b)])
                        for c in range(8):
                            e = "act" if b % 2 else "dve"
                            if e == "dve":
                                S.op("dve", lambda b=b, c=c, tt=tt: V.tensor_scalar(
                                    out=xnT[:, c, tt * 128:(tt + 1) * 128], in0=ptp[b][:, c * 128:(c + 1) * 128],
                                    scalar1=C("g_mix", c), scalar2=None, op0=ALU.mult),
                                    reads=[("ptp", b), "cpack"], writes=[("xnT", tt)])
                            else:
                                S.op("act", lambda b=b, c=c, tt=tt: A.activation(
                                    out=xnT[:, c, tt * 128:(tt + 1) * 128], in_=ptp[b][:, c * 128:(c + 1) * 128],
                                    func=AF.Copy, scale=C("g_mix", c)),
                                    reads=[("ptp", b), "cpack"], writes=[("xnT", tt)])
                    S.barrier()
                tap("xnT", xnT[:], [("xnT", tt) for tt in range(NT)])
                XNT_ALL = [("xnT", tt) for tt in range(NT)]
                if stop_after == "p1":
                    S.barrier()
                    continue

                wlT = sb(mixs, "wlT", [128, SEQ], BF16)
                alT = sb(mixs, "alT", [128, SEQ], BF16)
                glT = sb(mixs, "glT", [128, SEQ], BF16)
                gl2T = sb(mixs, "gl2T", [32, SEQ], BF16)

                def shiftmix(uk, u, m, jc, outk, out, tmp, tmpk):
                    S.op("pool", lambda: G.tensor_tensor(out=tmp[:m, 1:SEQ - 1], in0=u[:m, 0:SEQ - 2], in1=u[:m, 2:SEQ],
                                                         op=ALU.add), reads=[uk], writes=[tmpk])
                    S.op("pool", lambda: G.tensor_copy(out=tmp[:m, 0:1], in_=u[:m, 1:2]), reads=[uk], writes=[tmpk])
                    S.op("pool", lambda: G.tensor_copy(out=tmp[:m, SEQ - 1:SEQ], in_=u[:m, SEQ - 2:SEQ - 1]),
                         reads=[uk], writes=[tmpk])
                    S.op("dve", lambda: V.tensor_scalar(out=tmp[:m, :], in0=tmp[:m, :], scalar1=dc[:m, 19 + jc:20 + jc],
                                                        scalar2=None, op0=ALU.mult),
                         reads=[tmpk, "dc_hmu"], writes=[tmpk])
                    S.op("dve", lambda: V.scalar_tensor_tensor(out=out[:m, :], in0=u[:m, :], scalar=dc[:m, 3 + jc:4 + jc],
                                                               in1=tmp[:m, :], op0=ALU.mult, op1=ALU.add),
                         reads=[uk, tmpk, "dc_omm"], writes=[outk])

                with ExitStack() as at:
                    qT = sb(at, "qT", [128, 4, SEQ], BF16)
                    kT = sb(at, "kT", [128, 4, SEQ], BF16)
                    vaug = sb(at, "vaug", [128, NT, 4, 130], BF16)
                    with ExitStack() as st:
                        wA = sb(st, "wA", [128, 8, 1952], BF16)
                        S.dma("pool", wA[:, :, 0:1536], w_in_d[:, 0:1536].rearrange("(c p) n -> p c n", p=128),
                              "wA", writes=["wA"])
                        S.dma("pool", wA[:, :, 1536:1952], w_in_d[:, 3072:3488].rearrange("(c p) n -> p c n", p=128),
                              "wA", writes=["wA"])
                        pu = [ps(st, "pu%d" % i, [128, 512]) for i in range(2)]
                        pss = [ps(st, "pss%d" % i, [128, 512]) for i in range(2)]
                        sqb = [sb(st, "sqb%d" % i, [128, 512], BF16) for i in range(2)]
                        usb = [sb(st, "usb%d" % i, [128, 512], F32) for i in range(2)]
                        sdb = [sb(st, "sdb%d" % i, [128, 512], F32) for i in range(2)]
                        S.op("pool", lambda: G.memset(vaug[:, :, :, 128:130], 1.0), writes=["vaug_ones"])

                        def proj_fm(pst, pk, c0, m, tb):
                            for dci in range(8):
                                S.op("pe", lambda dci=dci: PE_.matmul(pst[:m, :], lhsT=wA[:, dci, c0:c0 + m],
                                                                       rhs=xnT[:, dci, tb * 512:(tb + 1) * 512],
                                                                       start=(dci == 0), stop=(dci == 7)),
                                     reads=["wA"] + XNT_ALL[tb * 4:tb * 4 + 4], writes=[pk])

                        units = [(kind, h, tb) for kind in range(2) for h in range(4) for tb in range(4)]

                        def qk_front(i):
                            kind, h, tb = units[i]
                            b = i % 2
                            proj_fm(pu[b], ("pu", b), kind * 512 + h * 128, 128, tb)
                            S.op("dve", lambda: V.tensor_copy(out=usb[b][:], in_=pu[b][:]),
                                 reads=[("pu", b)], writes=[("usb", b)])
                            S.op("act", lambda: A.activation(out=sqb[b][:], in_=usb[b][:], func=AF.Square),
                                 reads=[("usb", b)], writes=[("sqb", b)])

                        def qk_back(i):
                            kind, h, tb = units[i]
                            b = i % 2
                            dst = (qT, kT)[kind]
                            gcol = QGS if kind == 0 else C("kg")
                            S.op("pe", lambda: PE_.matmul(pss[b][:], lhsT=bd_b[:], rhs=sqb[b][:], start=True, stop=True),
                                 reads=["bd_b", ("sqb", b)], writes=[("pss", b)])
                            S.op("act", lambda: A.activation(out=sdb[b][:], in_=pss[b][:], func=AF.Ln, bias=EPS,
                                                             scale=1.0 / 64),
                                 reads=[("pss", b), "kc"], writes=[("sdb", b)])
                            S.op("act", lambda: A.activation(out=sdb[b][:], in_=sdb[b][:], func=AF.Exp, scale=-0.5),
                                 reads=[("sdb", b)], writes=[("sdb", b)])
                            S.op("dve", lambda: V.scalar_tensor_tensor(
                                out=dst[:, h, tb * 512:(tb + 1) * 512], in0=usb[b][:], scalar=gcol, in1=sdb[b][:],
                                op0=ALU.mult, op1=ALU.mult),
                                reads=[("usb", b), ("sdb", b), "dc0", "cpack"], writes=[("qk", kind, h, tb)])

                        import os as _os
                        _cut = _os.environ.get("K_CUT", "")
                        if _cut.startswith("qkfront"):
                            proj_fm(pu[0], ("pu", 0), 0, 128, 0)
                            if "a" in _cut[7:]:
                                S.op("act", lambda: A.activation(out=sqb[0][:], in_=pu[0][:], func=AF.Square),
                                     reads=[("pu", 0)], writes=[("sqb", 0)])
                            if "d" in _cut[7:]:
                                S.op("dve", lambda: V.tensor_copy(out=usb[0][:], in_=pu[0][:]),
                                     reads=[("pu", 0)], writes=[("usb", 0)])
                            units = []
                        if _cut == "dmaonly":
                            units = []
                        if _cut == "mmonly":
                            proj_fm(pu[0], ("pu", 0), 0, 128, 0)
                            units = []
                        if _cut == "qk1":
                            units = units[:1]
                        for i in range(len(units)):
                            qk_front(i)
                            if i > 0:
                                qk_back(i - 1)
                        if units:
                            qk_back(len(units) - 1)
                        for tt in range(NT if _cut in ("", "v", "lora") else 0):
                            b = tt % 2
                            for dci in range(8):
                                S.op("pe", lambda dci=dci: PE_.matmul(pu[b][:], lhsT=xnT[:, dci, tt * 128:(tt + 1) * 128],
                                                                       rhs=wA[:, dci, 1024:1536],
                                                                       start=(dci == 0), stop=(dci == 7)),
                                     reads=["wA", ("xnT", tt)], writes=[("pu", b)])
                            src = pu[b][:].rearrange("p (h d) -> p h d", h=4)
                            if tt % 2:
                                S.op("act", lambda: A.activation(out=vaug[:, tt, :, 0:128], in_=src, func=AF.Copy),
                                     reads=[("pu", b)], writes=[("vaug", tt)])
                            else:
                                S.op("dve", lambda: V.tensor_copy(out=vaug[:, tt, :, 0:128], in_=src),
                                     reads=[("pu", b)], writes=[("vaug", tt)])
                        ush = sb(st, "ush", [128, SEQ], F32)
                        utmp = sb(st, "utmp", [128, SEQ], F32)
                        umix = sb(st, "umix", [128, SEQ], F32)
                        for ci, (c0, m, jc, func, dst) in enumerate((
                                (1536, 128, 12, AF.Tanh, wlT), (1664, 128, 13, AF.Copy, alT),
                                (1792, 128, 14, AF.Sigmoid, glT), (1920, 32, 15, AF.Sigmoid, gl2T))[:(4 if _cut in ("", "lora") else 0)]):
                            for tb in range(4):
                                b = tb % 2
                                proj_fm(pu[b], ("pu", b), c0, m, tb)
                                if tb % 2:
                                    S.op("act", lambda: A.activation(out=ush[:m, tb * 512:(tb + 1) * 512], in_=pu[b][:m, :],
                                                                     func=AF.Copy),
                                         reads=[("pu", b)], writes=["ush"])
                                else:
                                    S.op("dve", lambda: V.tensor_copy(out=ush[:m, tb * 512:(tb + 1) * 512], in_=pu[b][:m, :]),
                                         reads=[("pu", b)], writes=["ush"])
                            shiftmix("ush", ush, m, jc, "umix", umix, utmp, "utmp")
                            S.op("act", lambda: A.activation(out=dst[:m, :], in_=umix[:m, :], func=func),
                                 reads=["umix"], writes=[("lora_in", ci)])
                        S.barrier()
                    tap("qT", qT[:], [])
                    tap("kT", kT[:], [])
                    tap("vaug", vaug[:], [])
                    tap("wlT", wlT[:], [])
                    tap("glT", glT[:], [])
                    if stop_after == "p2a":
                        S.barrier()
                        continue

                    with ExitStack() as st:
                        strip = sb(st, "strip", [128, 4, STRIP_W], F32)
                        for i in range(128):
                            src = bass.AP(gscr_t, 127 - i, [[0, 1], [ND, 4], [1, STRIP_W]])
                            S.dma("sp", strip[i:i + 1, :, :], src, "c4", reads=["gscr"], writes=["strip"])
                        PT = [sb(st, "PT%d" % i, [128, NT, 512], BF16) for i in range(2)]
                        tmpb = [sb(st, "tmpb%d" % i, [128, 512], F32) for i in range(2)]
                        spp = [ps(st, "spp%d" % i, [128, 512]) for i in range(2)]
                        av = ps(st, "av", [128, 4, 512])
                        ptr = ps(st, "ptr", [128, 1024], BF16)
                        rin = sb(st, "rin", [128, 4, 2, 1], F32)
                        nl = sb(st, "nl", [128, 2, 2, 1], F32)
                        o0 = sb(st, "o0", [128, 128], F32)
                        osb = sb(st, "osb", [128, 4, 128], F32)
                        onb = sb(st, "onb", [128, 4, 128], BF16)
                        junk2 = sb(st, "junk2", [128, 128], BF16)
                        ss4 = sb(st, "ss4", [128, 8], F32)
                        aunits = [(h, qt, sub) for h in range(4) for qt in range(4) for sub in range(2)]
                        cnt = [0]

                        def qk_exp(u):
                            h, qt, sub = u
                            for kt in range(NT):
                                i = cnt[0]
                                cnt[0] += 1
                                b = i % 2
                                S.op("pe", lambda: PE_.matmul(spp[b][:], lhsT=kT[64 * sub:64 * sub + 64, h, kt * 128:(kt + 1) * 128],
                                                              rhs=qT[64 * sub:64 * sub + 64, h, qt * 512:(qt + 1) * 512],
                                                              start=True, stop=True),
                                     reads=[], writes=[("spp", b)])
                                Dk = kt * 128 - qt * 512
                                if -255 < Dk < 639:
                                    off = 512 - Dk
                                    S.op("dve", lambda: V.tensor_tensor(out=tmpb[b][:], in0=spp[b][:],
                                                                        in1=strip[:, h, off:off + 512], op=ALU.add),
                                         reads=[("spp", b), "strip"], writes=[("tmpb", b)])
                                    S.op("act", lambda: A.activation(out=PT[sub][:, kt, :], in_=tmpb[b][:], func=AF.Exp),
                                         reads=[("tmpb", b)], writes=[("PT", sub, kt)])
                                else:
                                    which = 1 if Dk > 0 else 0
                                    S.op("act", lambda: A.activation(out=PT[sub][:, kt, :], in_=spp[b][:], func=AF.Exp,
                                                                     bias=C("rb_far", h * 2 + which)),
                                         reads=[("spp", b)], writes=[("PT", sub, kt)])
                                yield

                        def accap(sub, qs, lo, hi):
                            return av[:, sub * 2 + qs // 2, (qs % 2) * 256 + lo:(qs % 2) * 256 + hi]

                        def av_mm(u):
                            h, qt, sub = u
                            for qs in range(4):
                                for kt in range(NT):
                                    S.op("pe", lambda: PE_.matmul(accap(sub, qs, 0, 129),
                                                                  lhsT=PT[sub][:, kt, qs * 128:(qs + 1) * 128],
                                                                  rhs=vaug[:, kt, h, 0:129],
                                                                  start=(kt == 0), stop=(kt == NT - 1)),
                                         reads=[("PT", sub, kt)], writes=[("av", sub)])
                                    if kt % 4 == 3:
                                        yield

                        def post_a(h, qt):
                            av4 = av[:].rearrange("p b (j w) -> p b j w", j=2)
                            S.op("dve", lambda: V.reciprocal(out=rin[:], in_=av4[:, :, :, 128:129]),
                                 reads=[("av", 0), ("av", 1)], writes=["rin"])
                            S.op("dve", lambda: V.tensor_scalar(out=nl[:], in0=rin[:, 2:4, :, :], scalar1=NLAM, scalar2=None,
                                                                op0=ALU.mult), reads=["rin", "dc2"], writes=["nl"])
                            for qs in range(4):
                                S.op("dve", lambda: V.tensor_scalar(out=o0[:], in0=accap(0, qs, 0, 128),
                                                                    scalar1=rin[:, qs // 2, qs % 2, :], scalar2=None,
                                                                    op0=ALU.mult),
                                     reads=[("av", 0), "rin"], writes=["o0"])
                                S.op("dve", lambda: V.scalar_tensor_tensor(out=osb[:, qs, :], in0=accap(1, qs, 0, 128),
                                                                           scalar=nl[:, qs // 2, qs % 2, :], in1=o0[:],
                                                                           op0=ALU.mult, op1=ALU.add),
                                     reads=[("av", 1), "nl", "o0"], writes=[("osb", qs)])

                        def post_b(h, qt):
                            for qs in range(4):
                                S.op("act", lambda: A.activation(out=junk2[:], in_=osb[:, qs, :], func=AF.Square,
                                                                 accum_out=ss4[:, qs:qs + 1]),
                                     reads=[("osb", qs)], writes=["junk2", ("ss4", qs)])
                                yield
                            S.op("act", lambda: A.activation(out=ss4[:, 4:8], in_=ss4[:, 0:4], func=AF.Sqrt, bias=EPS,
                                                             scale=1.0 / 128),
                                 reads=[("ss4", q_) for q_ in range(4)] + ["kc"], writes=["ss4b"])
                            yield
                            S.op("dve", lambda: V.reciprocal(out=ss4[:, 4:8], in_=ss4[:, 4:8]), reads=["ss4b"], writes=["ss4b"])
                            yield
                            for qs in range(4):
                                S.op("dve", lambda: V.tensor_scalar(out=onb[:, qs, :], in0=osb[:, qs, :],
                                                                    scalar1=ss4[:, 4 + qs:5 + qs], scalar2=1.0 - LAM_INIT,
                                                                    op0=ALU.mult, op1=ALU.mult),
                                     reads=[("osb", qs), "ss4b"], writes=[("onb", qs)])
                                S.op("pe", lambda: PE_.transpose(out=ptr[:, qs * 128:(qs + 1) * 128], in_=onb[:, qs, :],
                                                                 identity=ident_b[:]),
                                     reads=[("onb", qs), "ident_b"], writes=["ptr"])
                                yield
                            S.op("act", lambda: A.activation(out=o_daT[:, h, qt * 512:(qt + 1) * 512], in_=ptr[:, 0:512],
                                                             func=AF.Copy, scale=C("subln_g")),
                                 reads=["ptr", "cpack"], writes=[("o_daT", h, qt)])

                        n_u = len(aunits)
                        if _os.environ.get("K_SKIP_ATTN"):
                            n_u = 0
                        def rr(gens):
                            gens = [g for g in gens if g is not None]
                            while gens:
                                nxt = []
                                for g in gens:
                                    try:
                                        next(g)
                                        nxt.append(g)
                                    except StopIteration:
                                        pass
                                gens = nxt

                        pend = None
                        if n_u:
                            rr([qk_exp(aunits[0])])
                        for i in range(1, n_u):
                            rr([qk_exp(aunits[i]), av_mm(aunits[i - 1]), pend])
                            pend = None
                            if aunits[i - 1][2] == 1:
                                post_a(aunits[i - 1][0], aunits[i - 1][1])
                                pend = post_b(aunits[i - 1][0], aunits[i - 1][1])
                        if n_u:
                            rr([av_mm(aunits[-1]), pend])
                            post_a(aunits[-1][0], aunits[-1][1])
                            rr([post_b(aunits[-1][0], aunits[-1][1])])
                        S.barrier()
                tap("o_daT", o_daT[:], [])
                if stop_after == "attn":
                    S.barrier()
                    continue

                with ExitStack() as rw:
                    w2c = sb(rw, "w2c", [128, 512], BF16)
                    a2c = sb(rw, "a2c", [128, 512], BF16)
                    g2a = sb(rw, "g2a", [128, 512], BF16)
                    g2b = sb(rw, "g2b", [32, 512], BF16)
                    S.dma("pool", w2c[:], w2cat_d, "rwc", writes=["w2c"])
                    S.dma("pool", a2c[:], a2cat_d, "rwc", writes=["a2c"])
                    S.dma("pool", g2a[:], g2_d[0:128, :], "rwc", writes=["g2a"])
                    S.dma("pool", g2b[:], g2_d[128:160, :], "rwc", writes=["g2b"])
                    maskAB = sb(rw, "maskAB", [64, 4, 128], BF16)
                    maskN = sb(rw, "maskN", [64, 4, 64], BF16)
                    ident4 = sb(rw, "ident4", [64, 4, 64], BF16)
                    resetm = sb(rw, "resetm", [128, 256], F32)
                    bd_f = sb(rw, "bd_f", [128, 128], F32)
                    with ExitStack() as st:
                        rwm = sb(st, "rwm", [64, 384], F32)
                        S.dma("sp", rwm[:], rwmask_d, "rwc2", writes=["rwm"])
                        for inst in range(4):
                            d = inst % 2
                            S.op("dve", lambda: V.tensor_copy(out=maskAB[:, inst, :], in_=rwm[:, d * 192:d * 192 + 128]),
                                 reads=["rwm"], writes=["maskAB"])
                            S.op("dve", lambda: V.tensor_copy(out=maskN[:, inst, :], in_=rwm[:, d * 192 + 128:d * 192 + 192]),
                                 reads=["rwm"], writes=["maskN"])
                            S.op("dve", lambda: V.tensor_copy(out=ident4[:, inst, :], in_=ident_b[0:64, 0:64]),
                                 reads=["ident_b"], writes=["ident4"])
                        S.op("dve", lambda: V.memset(resetm[:], 1.0), writes=["resetm"])
                        S.op("dve", lambda: V.memset(resetm[:].rearrange("p (c l) -> p c l", l=64)[:, :, 0:1], 0.0),
                             reads=["resetm"], writes=["resetm"])
                        S.op("dve", lambda: V.memset(bd_f[:], 0.0), writes=["bd_f"])
                        S.op("dve", lambda: V.memset(bd_f[0:64, 0:64], 1.0), reads=["bd_f"], writes=["bd_f"])
                        S.op("dve", lambda: V.memset(bd_f[64:128, 64:128], 1.0), reads=["bd_f"], writes=["bd_f"])
                        S.barrier()

                    TB = 256
                    NB = SEQ // TB
                    CPB = TB // 64

                    def c3(ap):
                        return ap.rearrange("p (c l) -> p c l", l=64)

                    for j in range(4 if stop_after != "rw1" else 1):
                        with ExitStack() as pp:
                            AR = [sb(pp, "AR%d" % d, [128, 32, 2, 64], BF16) for d in range(2)]
                            bbar = [sb(pp, "bbar%d" % d, [128, SEQ], BF16) for d in range(2)]
                            kbar = [sb(pp, "kbar%d" % d, [128, SEQ], BF16) for d in range(2)]
                            vb = sb(pp, "vb", [128, SEQ], BF16)
                            rhi = [sb(pp, "rhi%d" % d, [64, 32, 64], BF16) for d in range(2)]
                            PLf = [sb(pp, "PLf%d" % d, [128, 32], F32) for d in range(2)]
                            PLhi = [sb(pp, "PLhi%d" % d, [64, 32], F32) for d in range(2)]
                            bonus = sb(pp, "bonus", [128, SEQ], BF16)
                            wBj = sb(pp, "wBj", [128, 8, 384], BF16)
                            yacc = sb(pp, "yacc", [64, 2, SEQ], F32)
                            for i3 in range(3):
                                cc0 = RW0 + i3 * 512 + j * 128
                                S.dma("pool", wBj[:, :, i3 * 128:(i3 + 1) * 128],
                                      w_in_d[:, cc0:cc0 + 128].rearrange("(c p) n -> p c n", p=128), "wBj", writes=["wBj"])
                            with ExitStack() as s1:
                                rf = sb(s1, "rf", [128, SEQ], F32)
                                kf = sb(s1, "kf", [128, SEQ], F32)
                                vf = sb(s1, "vf", [128, SEQ], F32)
                                ush = sb(s1, "ush2", [128, SEQ], F32)
                                utmp = sb(s1, "utmp2", [128, SEQ], F32)
                                sqb1 = sb(s1, "sqb1", [128, TB], BF16)
                                nct = sb(s1, "nct", [128, 2, CPB], F32)
                                pu2 = [ps(s1, "pu2%d" % i, [128, 512]) for i in range(2)]
                                pl2a = ps(s1, "pl2a", [128, 2, TB])
                                pl2b = ps(s1, "pl2b", [128, 2, TB])
                                pl2 = [pl2a[:, 0, :], pl2b[:, 0, :], pl2a[:, 1, :], pl2b[:, 1, :]]
                                pst_t = ps(s1, "pst", [128, 512])
                                pbn_t = ps(s1, "pbn", [128, 512])
                                pst = pst_t[:, 0:TB]
                                pbn = pbn_t[:, 0:TB]
                                for i3, (dst, dk) in enumerate(((rf, "rf"), (kf, "kf"), (vf, "vf"))):
                                    for tb in range(4):
                                        b = tb % 2
                                        for dci in range(8):
                                            S.op("pe", lambda dci=dci: PE_.matmul(
                                                pu2[b][:], lhsT=wBj[:, dci, i3 * 128:(i3 + 1) * 128],
                                                rhs=xnT[:, dci, tb * 512:(tb + 1) * 512], start=(dci == 0), stop=(dci == 7)),
                                                reads=["wBj"] + XNT_ALL[tb * 4:tb * 4 + 4], writes=[("pu2", b)])
                                        if tb % 2:
                                            S.op("act", lambda: A.activation(out=ush[:, tb * 512:(tb + 1) * 512], in_=pu2[b][:],
                                                                             func=AF.Copy),
                                                 reads=[("pu2", b)], writes=["ush2"])
                                        else:
                                            S.op("dve", lambda: V.tensor_copy(out=ush[:, tb * 512:(tb + 1) * 512], in_=pu2[b][:]),
                                                 reads=[("pu2", b)], writes=["ush2"])
                                    shiftmix("ush2", ush, 128, i3 * 4 + j, dk, dst, utmp, "utmp2")
                                S.op("act", lambda: A.activation(out=vb[:], in_=vf[:], func=AF.Copy), reads=["vf"], writes=["vb"])
                                S.barrier()
                                slots = [ush[:, i * TB:(i + 1) * TB] for i in range(8)] + [utmp[:, i * TB:(i + 1) * TB] for i in range(8)]
                                (t_sig0, t_sig1, t_a0, t_a1, t_kk, t_x, t_y, t_kd, t_be, t_cs, t_e1, t_e2, t_e3, t_e4, t_ks, t_z) = slots
                                t_sig = (t_sig0, t_sig1)
                                t_a = (t_a0, t_a1)
                                for tb in range(NB):
                                    sl = slice(tb * TB, (tb + 1) * TB)
                                    csl = slice(tb * CPB, (tb + 1) * CPB)
                                    for d in range(2):
                                        S.op("pe", lambda: PE_.matmul(pl2[d], lhsT=w2c[64 * d:64 * d + 64, j * 128:(j + 1) * 128],
                                                                      rhs=wlT[64 * d:64 * d + 64, sl], start=True, stop=True),
                                             reads=["w2c"], writes=[("pl2", d)])
                                        S.op("act", lambda: A.activation(out=t_sig[d], in_=pl2[d], func=AF.Sigmoid,
                                                                         bias=C("w0", d * 4 + j)),
                                             reads=[("pl2", d), "cpack"], writes=[("sig", d)])
                                        S.op("pe", lambda: PE_.matmul(pl2[2 + d], lhsT=a2c[64 * d:64 * d + 64, j * 128:(j + 1) * 128],
                                                                      rhs=alT[64 * d:64 * d + 64, sl], start=True, stop=True),
                                             reads=["a2c"], writes=[("pl2", d)])
                                        S.op("act", lambda: A.activation(out=t_a[d], in_=pl2[2 + d], func=AF.Sigmoid,
                                                                         bias=C("a0", d * 4 + j)),
                                             reads=[("pl2", d), "cpack"], writes=[("a", d)])
                                    S.op("dve", lambda: V.tensor_scalar(out=t_x, in0=kf[:, sl], scalar1=C("k_k", j), scalar2=None,
                                                                        op0=ALU.mult), reads=["kf", "cpack"], writes=["t_x"])
                                    S.op("act", lambda: A.activation(out=sqb1[:], in_=t_x, func=AF.Square),
                                         reads=["t_x"], writes=["sqb1"])
                                    S.op("pe", lambda: PE_.matmul(pst, lhsT=bd_b[:], rhs=sqb1[:], start=True, stop=True),
                                         reads=["sqb1", "bd_b"], writes=["pst"])
                                    S.op("act", lambda: A.activation(out=t_y, in_=pst, func=AF.Ln, bias=kc[:, 3:4]),
                                         reads=["pst", "kc"], writes=["t_y"])
                                    S.op("act", lambda: A.activation(out=t_y, in_=t_y, func=AF.Exp, scale=-0.5), reads=["t_y"], writes=["t_y"])
                                    S.op("dve", lambda: V.tensor_tensor(out=t_kk, in0=t_x, in1=t_y, op=ALU.mult),
                                         reads=["t_x", "t_y"], writes=["t_kk"])
                                    for d in range(2):
                                        S.op("dve", lambda: V.tensor_scalar(out=t_x, in0=t_a[d], scalar1=C("k_a", j),
                                                                            scalar2=dc[:, 35 + j:36 + j], op0=ALU.mult, op1=ALU.add),
                                             reads=[("a", d), "cpack", "dc_omka"], writes=["t_x"])
                                        S.op("dve", lambda: V.tensor_tensor(out=t_kd, in0=t_x, in1=kf[:, sl], op=ALU.mult),
                                             reads=["t_x", "kf"], writes=["t_kd"])
                                        S.op("dve", lambda: V.tensor_tensor(out=t_be, in0=t_kk, in1=t_a[d], op=ALU.mult),
                                             reads=["t_kk", ("a", d)], writes=["t_be"])
                                        S.op("dve", lambda: V.tensor_tensor_scan(out=t_cs, data0=resetm[:], data1=t_sig[d], initial=0.0,
                                                                                 op0=ALU.mult, op1=ALU.add),
                                             reads=["resetm", ("sig", d)], writes=["t_cs"])
                                        S.op("dve", lambda: V.tensor_tensor(out=t_sig[d], in0=t_cs, in1=t_sig[d], op=ALU.subtract),
                                             reads=["t_cs", ("sig", d)], writes=[("sig", d)])
                                        t_csm = t_sig[d]
                                        tot = c3(t_cs)[:, :, 63:64]
                                        S.op("dve", lambda: V.tensor_scalar(out=nct[:, 0, :].rearrange("p (c o) -> p c o", o=1), in0=tot,
                                                                            scalar1=-CDEC, scalar2=None, op0=ALU.mult),
                                             reads=["t_cs"], writes=["nct"])
                                        S.op("dve", lambda: V.tensor_scalar(out=nct[:, 1, :].rearrange("p (c o) -> p c o", o=1), in0=tot,
                                                                            scalar1=CDEC, scalar2=None, op0=ALU.mult),
                                             reads=["t_cs"], writes=["nct"])
                                        S.op("act", lambda: A.activation(out=PLf[d][:, csl], in_=nct[:, 0, :], func=AF.Exp),
                                             reads=["nct"], writes=[("PLf", d)])
                                        if d == 0:
                                            S.op("act", lambda: A.activation(out=t_e1, in_=t_cs, func=AF.Exp, scale=-CDEC),
                                                 reads=["t_cs"], writes=["t_e1"])
                                            S.op("act", lambda: A.activation(out=t_e2, in_=t_cs, func=AF.Exp, scale=CDEC),
                                                 reads=["t_cs"], writes=["t_e2"])
                                            S.op("act", lambda: A.activation(out=t_e3, in_=t_csm, func=AF.Exp, scale=-CDEC),
                                                 reads=[("sig", d)], writes=["t_e3"])
                                            e_r, e_bk, e_a = t_e1, t_e2, t_e3
                                        else:
                                            for c8 in range(CPB):
                                                cs8 = slice(c8 * 64, (c8 + 1) * 64)
                                                S.op("act", lambda: A.activation(out=t_e1[:, cs8], in_=t_csm[:, cs8], func=AF.Exp,
                                                                                 scale=CDEC, bias=nct[:, 0, c8:c8 + 1]),
                                                     reads=[("sig", d), "nct"], writes=["t_e1"])
                                                S.op("act", lambda: A.activation(out=t_e2[:, cs8], in_=t_csm[:, cs8], func=AF.Exp,
                                                                                 scale=-CDEC, bias=nct[:, 1, c8:c8 + 1]),
                                                     reads=[("sig", d), "nct"], writes=["t_e2"])
                                                S.op("act", lambda: A.activation(out=t_e4[:, cs8], in_=t_cs[:, cs8], func=AF.Exp,
                                                                                 scale=CDEC, bias=nct[:, 0, c8:c8 + 1]),
                                                     reads=["t_cs", "nct"], writes=["t_e4"])
                                            e_r, e_bk, e_a = t_e1, t_e2, t_e4
                                        S.op("dve", lambda: V.tensor_tensor(out=AR[d][:, csl, 1, :], in0=c3(rf[:, sl]), in1=c3(e_r),
                                                                            op=ALU.mult),
                                             reads=["rf", "t_e1"], writes=[("AR", d, tb)])
                                        S.op("dve", lambda: V.scalar_tensor_tensor(out=AR[d][:, csl, 0, :], in0=c3(t_kk), scalar=-1.0,
                                                                                   in1=c3(e_a), op0=ALU.mult, op1=ALU.mult),
                                             reads=["t_kk", "t_e3", "t_e4"], writes=[("AR", d, tb)])
                                        S.op("dve", lambda: V.tensor_tensor(out=bbar[d][:, sl], in0=t_be, in1=e_bk, op=ALU.mult),
                                             reads=["t_be", "t_e2"], writes=[("bbar", d, tb)])
                                        S.op("dve", lambda: V.tensor_tensor(out=kbar[d][:, sl], in0=t_kd, in1=e_bk, op=ALU.mult),
                                             reads=["t_kd", "t_e2"], writes=[("kbar", d, tb)])
                                        if d == 0:
                                            S.op("dve", lambda: V.tensor_copy(out=t_ks, in_=t_kd), reads=["t_kd"], writes=["t_ks"])
                                        else:
                                            S.op("dve", lambda: V.tensor_tensor(out=t_ks, in0=t_ks, in1=t_kd, op=ALU.add),
                                                 reads=["t_kd", "t_ks"], writes=["t_ks"])
                                    S.op("dve", lambda: V.scalar_tensor_tensor(out=t_z, in0=rf[:, sl], scalar=C("r_k", j), in1=t_ks,
                                                                               op0=ALU.mult, op1=ALU.mult),
                                         reads=["rf", "t_ks", "cpack"], writes=["t_z"])
                                    S.op("pe", lambda: PE_.matmul(pbn, lhsT=bd_f[:], rhs=t_z, start=True, stop=True),
                                         reads=["t_z", "bd_f"], writes=["pbn"])
                                    S.op("dve", lambda: V.tensor_tensor(out=bonus[:, sl], in0=pbn, in1=vf[:, sl], op=ALU.mult),
                                         reads=["pbn", "vf"], writes=[("bonus", tb)])
                                for d in range(2):
                                    S.dma("sp", rhi[d][:], AR[d][64:128, :, 1, :], "rhi",
                                          reads=[("AR", d, tb) for tb in range(NB)], writes=[("rhi", d)])
                                    S.dma("sp", PLhi[d][:], PLf[d][64:128, :], "rhi", reads=[("PLf", d)], writes=[("PLhi", d)])
                                S.barrier()
                            tap("AR0", AR[0][:], [])
                            tap("AR1", AR[1][:], [])
                            tap("bbar0", bbar[0][:], [])
                            tap("kbar1", kbar[1][:], [])
                            tap("bonus", bonus[:], [])
                            tap("PLf0", PLf[0][:], [])
                            if stop_after == "rw_s1":
                                S.barrier()
                                continue
                            with ExitStack() as s2:
                                tok = [sb(s2, "tok%d" % i, [64, 8, 128], BF16) for i in range(2)]
                                btl = [sb(s2, "btl%d" % i, [128, 2, 2, 64], BF16) for i in range(2)]
                                AbR = [sb(s2, "AbR%d" % i, [64, 4, 128], BF16) for i in range(2)]
                                AkR = [sb(s2, "AkR%d" % i, [64, 4, 128], BF16) for i in range(2)]
                                MNq = [sb(s2, "MNq%d" % i, [64, 2, 4, 64], BF16) for i in range(2)]
                                Rq = [sb(s2, "Rq%d" % i, [64, 4, 64], BF16) for i in range(2)]
                                N0 = sb(s2, "N0", [64, 4, 64], BF16)
                                WuT = [sb(s2, "WuT%d" % i, [64, 4, 64], BF16) for i in range(2)]
                                AVs = sb(s2, "AVs", [64, 4, 64], BF16)
                                Uv = [sb(s2, "Uv%d" % i, [64, 4, 64], F32) for i in range(2)]
                                Us = sb(s2, "Us", [64, 4, 64], BF16)
                                Sf = sb(s2, "Sf", [64, 4, 64], F32)
                                Sb_ = sb(s2, "Sb", [64, 4, 64], BF16)
                                ptok = ps(s2, "ptok", [64, 1024], BF16)
                                pAB = [ps(s2, "pAB%d" % i, [64, 2, 2, 128]) for i in range(2)]
                                pMN = ps(s2, "pMN", [64, 2, 4, 64])
                                pR0 = ps(s2, "pR0", [64, 2, 4, 64])
                                pWA = ps(s2, "pWA", [64, 2, 4, 64])
                                pUU = ps(s2, "pUU", [64, 2, 4, 64])
                                pYS = ps(s2, "pYS", [64, 2, 4, 64])
                                S.op("dve", lambda: V.memset(Sf[:], 0.0), writes=["Sf"])
                                S.op("dve", lambda: V.memset(Sb_[:], 0.0), writes=["Sb"])
                                def gen_pre(i):
                                    b = i % 2
                                    cd = (i, 31 - i)
                                    tq = tok[b]
                                    for d in range(2):
                                        c = cd[d]
                                        for q, src in enumerate((bbar[d], kbar[d])):
                                            S.op("dve", lambda: V.tensor_scalar(out=btl[b][:, d, q, :], in0=src[:, c * 64:(c + 1) * 64],
                                                                                scalar1=PLf[d][:, c:c + 1], scalar2=None, op0=ALU.mult),
                                                 reads=[("PLf", d)], writes=[("btl", b)])
                                    for inst in range(4):
                                        hh, d = inst // 2, inst % 2
                                        c = cd[d]
                                        p0 = 64 * hh
                                        lb = bbar[d][p0:p0 + 64, c * 64:(c + 1) * 64]
                                        lk = kbar[d][p0:p0 + 64, c * 64:(c + 1) * 64]
                                        rAR = AR[d][p0:p0 + 64, c, :, :].rearrange("p a b -> p (a b)")
                                        pn0 = pR0[:, 1, inst, :] if hh == 0 else pMN[:, 1, inst, :]
                                        S.op("pe", lambda: PE_.matmul(pAB[hh][:, d, 0, :], lhsT=lb, rhs=rAR, start=True, stop=True),
                                             reads=[], writes=[("pAB", hh)])
                                        S.op("pe", lambda: PE_.matmul(pn0, lhsT=AR[d][p0:p0 + 64, c, 0, :], rhs=lb,
                                                                      start=True, stop=True),
                                             reads=[], writes=["pR0" if hh == 0 else "pMN"])
                                        S.op("pe", lambda: PE_.matmul(pAB[hh][:, d, 1, :], lhsT=lk, rhs=rAR, start=True, stop=True),
                                             reads=[], writes=[("pAB", hh)])
                                    for hh in range(2):
                                        hi = slice(2 * hh, 2 * hh + 2)
                                        S.op("dve", lambda: V.tensor_tensor(out=AbR[b][:, hi, :], in0=pAB[hh][:, :, 0, :], in1=maskAB[:, hi, :],
                                                                            op=ALU.mult),
                                             reads=[("pAB", hh), "maskAB"], writes=[("AbR", b)])
                                    S.op("dve", lambda: V.tensor_tensor(out=N0[:, 0:2, :], in0=pR0[:, 1, 0:2, :], in1=maskN[:, 0:2, :], op=ALU.mult),
                                         reads=["pR0", "maskN"], writes=["N0"])
                                    S.op("dve", lambda: V.tensor_tensor(out=N0[:, 2:4, :], in0=pMN[:, 1, 2:4, :], in1=maskN[:, 2:4, :], op=ALU.mult),
                                         reads=["pMN", "maskN"], writes=["N0"])
                                    yield
                                    for d in range(2):
                                        c = cd[d]
                                        srcs = (AR[d][:, c, 0, :], btl[b][:, d, 0, :], btl[b][:, d, 1, :])
                                        for q, src in enumerate(srcs):
                                            S.op("pe", lambda: PE_.transpose(out=ptok[:, (d * 3 + q) * 128:(d * 3 + q + 1) * 128], in_=src,
                                                                             identity=ident_b[:]),
                                                 reads=[("btl", b), "ident_b"], writes=["ptok"])
                                        S.op("pe", lambda: PE_.transpose(out=ptok[:, (6 + d) * 128:(7 + d) * 128],
                                                                         in_=vb[:, c * 64:(c + 1) * 64], identity=ident_b[:]),
                                             reads=["vb", "ident_b"], writes=["ptok"])
                                    S.op("act", lambda: A.activation(out=tq[:].rearrange("p a b -> p (a b)"), in_=ptok[:], func=AF.Copy),
                                         reads=["ptok"], writes=[("tok", b)])
                                    yield

                                    def mn_mm(l_, Mp_, Np_, rk_):
                                        for inst in range(4):
                                            if l_ < 5:
                                                S.op("pe", lambda: PE_.matmul(pMN[:, 0, inst, :], lhsT=Np_[:, inst, :], rhs=Mp_[:, inst, :],
                                                                              start=True, stop=True),
                                                     reads=rk_, writes=["pMN"])
                                            S.op("pe", lambda: PE_.matmul(pWA[:, 0, inst, :], lhsT=Mp_[:, inst, :], rhs=Np_[:, inst, :],
                                                                          start=True, stop=True),
                                                 reads=rk_, writes=["pWA"])

                                    def mn_copy(l_):
                                        q_ = l_ % 2
                                        if l_ < 5:
                                            S.op("act", lambda: A.activation(out=MNq[q_][:, 0, :, :], in_=pMN[:, 0, :, :], func=AF.Copy),
                                                 reads=["pMN"], writes=[("Mq", q_)])
                                        S.op("dve", lambda: V.tensor_copy(out=MNq[q_][:, 1, :, :], in_=pWA[:, 0, :, :]),
                                             reads=["pWA"], writes=[("Nq", q_)])

                                    mn_mm(1, AbR[b][:, :, 0:64], N0[:], [("AbR", b), "N0"])
                                    mn_copy(1)
                                    S.op("dve", lambda: V.tensor_tensor(out=Rq[0][:], in0=AbR[b][:, :, 0:64], in1=ident4[:], op=ALU.add),
                                         reads=[("AbR", b), "ident4"], writes=[("Rq", 0)])
                                    for hh in range(2):
                                        hi = slice(2 * hh, 2 * hh + 2)
                                        S.op("dve", lambda: V.tensor_tensor(out=AkR[b][:, hi, :], in0=pAB[hh][:, :, 1, :], in1=maskAB[:, hi, :],
                                                                            op=ALU.mult),
                                             reads=[("pAB", hh), "maskAB"], writes=[("AkR", b)])
                                    yield
                                    for l in range(1, 6):
                                        lb_ = l % 2
                                        MK = [("Mq", lb_), ("Nq", lb_)]
                                        if l < 5:
                                            mn_mm(l + 1, MNq[lb_][:, 0, :, :], MNq[lb_][:, 1, :, :], MK)
                                        for inst in range(4):
                                            S.op("pe", lambda: PE_.matmul(pR0[:, 0, inst, :], lhsT=MNq[lb_][:, 1, inst, :],
                                                                          rhs=Rq[1 - lb_][:, inst, :], start=True, stop=True),
                                                 reads=[("Nq", lb_), ("Rq", 1 - lb_)], writes=["pR0"])
                                        if l == 1:
                                            for inst in range(4):
                                                hh, d = inst // 2, inst % 2
                                                hs = slice(hh * 64, hh * 64 + 64)
                                                S.op("pe", lambda: PE_.matmul(pUU[:, 0, inst, :], lhsT=AkR[b][:, inst, 0:64], rhs=tq[:, 6 + d, hs],
                                                                              start=True, stop=True),
                                                     reads=[("tok", b), ("AkR", b)], writes=["pUU"])
                                        if l < 5:
                                            mn_copy(l + 1)
                                        S.op("dve", lambda: V.tensor_tensor(out=Rq[lb_][:], in0=pR0[:, 0, :, :], in1=Rq[1 - lb_][:],
                                                                            op=ALU.add),
                                             reads=["pR0", ("Rq", 1 - lb_)], writes=[("Rq", lb_)])
                                        if l == 1:
                                            S.op("dve", lambda: V.tensor_copy(out=AVs[:], in_=pUU[:, 0, :, :]), reads=["pUU"], writes=["AVs"])
                                        yield
                                    R = Rq[1]
                                    for inst in range(4):
                                        hh, d = inst // 2, inst % 2
                                        hs = slice(hh * 64, hh * 64 + 64)
                                        S.op("pe", lambda: PE_.matmul(pWA[:, 0, inst, :], lhsT=tq[:, d * 3, hs], rhs=R[:, inst, :],
                                                                      start=True, stop=True),
                                             reads=[("tok", b), ("Rq", 1)], writes=["pWA"])
                                        S.op("pe", lambda: PE_.matmul(pR0[:, 1, inst, :], lhsT=R[:, inst, :], rhs=AVs[:, inst, :],
                                                                      start=True, stop=True),
                                             reads=[("Rq", 1), "AVs"], writes=["pR0"])
                                    yield
                                    S.op("act", lambda: A.activation(out=WuT[b][:], in_=pWA[:, 0, :, :], func=AF.Copy),
                                         reads=["pWA"], writes=[("WuT", b)])
                                    S.op("dve", lambda: V.tensor_copy(out=Uv[b][:], in_=pR0[:, 1, :, :]),
                                         reads=["pR0"], writes=[("Uv", b)])
                                    yield

                                def gen_loop(i):
                                    b = i % 2
                                    cd = (i, 31 - i)
                                    tq = tok[b]
                                    for inst in range(4):
                                        yield
                                        S.op("pe", lambda: PE_.matmul(pUU[:, 1, inst, :], lhsT=WuT[b][:, inst, :], rhs=Sb_[:, inst, :],
                                                                      start=True, stop=True),
                                             reads=[("WuT", b), "Sb"], writes=["pUU"])
                                    yield
                                    S.op("dve", lambda: V.tensor_tensor(out=Us[:], in0=pUU[:, 1, :, :], in1=Uv[b][:], op=ALU.add),
                                         reads=["pUU", ("Uv", b)], writes=["Us"])
                                    yield
                                    for inst in range(4):
                                        hh, d = inst // 2, inst % 2
                                        c = cd[d]
                                        hs = slice(hh * 64, hh * 64 + 64)
                                        rr = AR[d][0:64, c, 1, :] if hh == 0 else rhi[d][:, c, :]
                                        yield
                                        S.op("pe", lambda: PE_.matmul(pYS[:, 0, inst, :], lhsT=Sb_[:, inst, :], rhs=rr, start=True, stop=False),
                                             reads=["Sb", ("rhi", d)], writes=["pYS"])
                                        yield
                                        S.op("pe", lambda: PE_.matmul(pYS[:, 0, inst, :], lhsT=tq[:, 6 + d, hs], rhs=AkR[b][:, inst, 64:128],
                                                                      start=False, stop=False),
                                             reads=[("tok", b), ("AkR", b)], writes=["pYS"])
                                        yield
                                        S.op("pe", lambda: PE_.matmul(pYS[:, 0, inst, :], lhsT=Us[:, inst, :], rhs=AbR[b][:, inst, 64:128],
                                                                      start=False, stop=True),
                                             reads=["Us", ("AbR", b)], writes=["pYS"])
                                        yield
                                        S.op("pe", lambda: PE_.matmul(pYS[:, 1, inst, :], lhsT=tq[:, d * 3 + 1, hs], rhs=Us[:, inst, :],
                                                                      start=True, stop=False),
                                             reads=[("tok", b), "Us"], writes=["pYS"])
                                        yield
                                        S.op("pe", lambda: PE_.matmul(pYS[:, 1, inst, :], lhsT=tq[:, d * 3 + 2, hs], rhs=tq[:, 6 + d, hs],
                                                                      start=False, stop=True),
                                             reads=[("tok", b)], writes=["pYS"])
                                    yield
                                    for d in range(2):
                                        c = cd[d]
                                        dst = yacc[:, :, c * 64:(c + 1) * 64]
                                        src = pYS[:, 0, :, :].rearrange("p (h e) w -> p h e w", e=2)[:, :, d, :]
                                        if i < 16:
                                            S.op("dve", lambda: V.tensor_copy(out=dst, in_=src),
                                                 reads=["pYS"], writes=[("yacc", c)])
                                        else:
                                            S.op("dve", lambda: V.tensor_tensor(out=dst, in0=src, in1=dst, op=ALU.add),
                                                 reads=["pYS", ("yacc", c)], writes=[("yacc", c)])
                                    yield
                                    for inst in range(4):
                                        hh, d = inst // 2, inst % 2
                                        c = cd[d]
                                        plc = PLf[d][0:64, c:c + 1] if hh == 0 else PLhi[d][:, c:c + 1]
                                        yield
                                        S.op("dve", lambda: V.scalar_tensor_tensor(out=Sf[:, inst, :], in0=Sf[:, inst, :], scalar=plc,
                                                                                   in1=pYS[:, 1, inst, :], op0=ALU.mult, op1=ALU.add),
                                             reads=["pYS", "Sf", ("PLhi", d), ("PLf", d)], writes=["Sf"])
                                    yield
                                    S.op("act", lambda: A.activation(out=Sb_[:], in_=Sf[:], func=AF.Copy), reads=["Sf"], writes=["Sb"])
                                    yield

                                def drive(ga, gb):
                                    a_done = b_done = False
                                    while not (a_done and b_done):
                                        if not a_done:
                                            try:
                                                next(ga)
                                            except StopIteration:
                                                a_done = True
                                        if not b_done:
                                            try:
                                                next(gb)
                                            except StopIteration:
                                                b_done = True

                                for _ in gen_pre(0):
                                    pass
                                for i in range(32):
                                    drive(gen_loop(i), gen_pre(i + 1) if i + 1 < 32 else iter(()))
                                S.barrier()
                            with ExitStack() as s3:
                                y128 = sb(s3, "y128", [128, SEQ], F32)
                                fa = sb(s3, "fa", [128, 512], F32)
                                fb = sb(s3, "fb", [128, 512], F32)
                                pm = ps(s3, "pm", [128, 512])
                                pv = ps(s3, "pv", [128, 512])
                                pg = ps(s3, "pg", [128, 512])
                                S.op("act", lambda: A.activation(out=y128[0:64, :], in_=yacc[:, 0, :], func=AF.Copy),
                                     reads=[], writes=["y128a"])
                                S.dma("sp", y128[64:128, :], yacc[:, 1, :], "ymv", reads=[], writes=["y128b"])
                                tap("y128", y128[:], ["y128a", "y128b"])
                                for tb in range(4):
                                    sl = slice(tb * 512, (tb + 1) * 512)
                                    S.op("pe", lambda: PE_.matmul(pm[:], lhsT=bd_f[:], rhs=y128[:, sl], start=True, stop=True),
                                         reads=["y128a", "y128b", "bd_f"], writes=["pm"])
                                    S.op("dve", lambda: V.scalar_tensor_tensor(out=fa[:], in0=pm[:], scalar=-1.0 / 64, in1=y128[:, sl],
                                                                               op0=ALU.mult, op1=ALU.add),
                                         reads=["pm", "y128a", "y128b"], writes=["fa"])
                                    S.op("act", lambda: A.activation(out=fb[:], in_=fa[:], func=AF.Square), reads=["fa"], writes=["fb"])
                                    S.op("pe", lambda: PE_.matmul(pv[:], lhsT=bd_f[:], rhs=fb[:], start=True, stop=True),
                                         reads=["fb", "bd_f"], writes=["pv"])
                                    S.op("act", lambda: A.activation(out=fb[:], in_=pv[:], func=AF.Ln, bias=EPSLN, scale=1.0 / 64),
                                         reads=["pv", "kc"], writes=["fb"])
                                    S.op("act", lambda: A.activation(out=fb[:], in_=fb[:], func=AF.Exp, scale=-0.5), reads=["fb"], writes=["fb"])
                                    S.op("dve", lambda: V.tensor_tensor(out=fa[:], in0=fa[:], in1=fb[:], op=ALU.mult),
                                         reads=["fa", "fb"], writes=["fa"])
                                    S.op("dve", lambda: V.tensor_scalar(out=fa[:], in0=fa[:], scalar1=C("lnx_g", j), scalar2=C("lnx_b", j),
                                                                        op0=ALU.mult, op1=ALU.add),
                                         reads=["fa", "cpack"], writes=["fa"])
                                    S.op("dve", lambda: V.tensor_tensor(out=fa[:], in0=fa[:], in1=bonus[:, sl], op=ALU.add),
                                         reads=["fa"], writes=["fa"])
                                    S.op("pe", lambda: PE_.matmul(pg[:], lhsT=g2a[:, j * 128:(j + 1) * 128], rhs=glT[:, sl],
                                                                  start=True, stop=False), reads=["g2a"], writes=["pg"])
                                    S.op("pe", lambda: PE_.matmul(pg[:], lhsT=g2b[0:32, j * 128:(j + 1) * 128], rhs=gl2T[0:32, sl],
                                                                  start=False, stop=True), reads=["g2b"], writes=["pg"])
                                    S.op("dve", lambda: V.tensor_tensor(out=o_rwT[:, j, sl], in0=fa[:], in1=pg[:], op=ALU.mult),
                                         reads=["fa", "pg"], writes=[("o_rwT", j, tb)])
                                S.barrier()
                    S.barrier()
                tap("o_rwT", o_rwT[:] if stop_after != "rw1" else o_rwT[:, 0:1, :], [])
                S.barrier()
                mixs.close()
                if stop_after in ("attn", "rw", "rw1", "rw_s1", "p2a", "p1"):
                    S.barrier()
                    continue
                h = sb(sq, "h", [128, NT, DM], F32)
                HK = [("h", tt) for tt in range(NT)]
                with ExitStack() as st:
                    wo = sb(st, "wo", [128, 8, DM], BF16)
                    S.dma("pool", wo[:], w_out_d.rearrange("(c p) n -> p c n", p=128), "wo", writes=["wo"])
                    xt2 = [sb(st, "xt2%d" % i, [128, DM], F32) for i in range(2)]
                    po = [[ps(st, "po%d%d" % (i, k), [128, 512]) for k in range(2)] for i in range(2)]
                    for tt in range(NT):
                        b = tt % 2
                        S.dma("sp", xt2[b][:], x_d[s, tt * 128:(tt + 1) * 128, :], ("xt2", b), writes=[("xt2", b)])
                        for dh in range(2):
                            for c in range(8):
                                src = o_daT if c < 4 else o_rwT
                                S.op("pe", lambda: PE_.matmul(po[b][dh][:], lhsT=src[:, c % 4, tt * 128:(tt + 1) * 128],
                                                              rhs=wo[:, c, dh * 512:(dh + 1) * 512], start=(c == 0), stop=(c == 7)),
                                     reads=["wo"], writes=[("po", b, dh)])
                            S.op("dve", lambda: V.tensor_tensor(out=h[:, tt, dh * 512:(dh + 1) * 512], in0=po[b][dh][:],
                                                                in1=xt2[b][:, dh * 512:(dh + 1) * 512], op=ALU.add),
                                 reads=[("po", b, dh), ("xt2", b)], writes=[("h", tt)])
                    S.barrier()
                tap("h1", h[:], [])
                if stop_after == "h1":
                    S.barrier()
                    continue

                def load_bp(st_, name):
                    o, n = bp_off[name]
                    t_ = sb(st_, "bp_" + name, [128, n], F32)
                    S.dma("sp", t_[:], bpack_d[:, o:o + n], "bpk", writes=["bp_" + name])
                    return t_

                with ExitStack() as me:
                    Xn = sb(me, "Xn", [128, NT, DM], BF16)
                    aff_tok = sb(me, "aff_tok", [128, NT, NE], F32)
                    aff_hl = sb(me, "aff_hl", [128, NT, NE, 2], BF16)
                    posm_tok = sb(me, "posm_tok", [128, NT, NE], F32)
                    posm_b = sb(me, "posm_b", [NE, SEQ], BF16)
                    Eoh = sb(me, "Eoh", [NE, NE, 128], BF16)
                    S.op("dve", lambda: V.memset(Eoh[:], 1.0), writes=["Eoh"])
                    for e in range(NE):
                        S.op("dve", lambda: V.tensor_scalar(out=Eoh[:, e, :], in0=Eoh[:, e, :], scalar1=ident_f[0:NE, e:e + 1],
                                                            scalar2=None, op0=ALU.mult), reads=["Eoh", "ident_f"], writes=["Eoh"])
                    with ExitStack() as st:
                        gffn = load_bp(st, "g_ffn")
                        wr_f = sb(st, "wr_f", [128, 8, NE], F32)
                        S.dma("sp", wr_f[:], wr_d.rearrange("(c p) e -> p c e", p=128), "wrf", writes=["wr_f"])
                        affT = sb(st, "affT", [NE, SEQ], F32)
                        wk = sb(st, "wk", [NE, SEQ], F32)
                        maskT = sb(st, "maskT", [NE, SEQ], F32)
                        cumT = sb(st, "cumT", [NE, SEQ], F32)
                        ones16 = sb(st, "ones16", [NE, SEQ], F32)
                        m8 = sb(st, "m8", [NE, 8], F32)
                        xnf = [sb(st, "xnf%d" % i, [128, DM], F32) for i in range(2)]
                        xnTf = [sb(st, "xnTf%d" % i, [128, 8, 128], F32) for i in range(2)]
                        junk3 = sb(st, "junk3", [128, DM], BF16)
                        sst = sb(st, "sst", [128, 3 * NT], F32)
                        sm = sb(st, "sm", [128, 4 * NT], F32)
                        ex = sb(st, "ex", [128, NE], F32)
                        dtmp = sb(st, "dtmp", [128, NE], F32)
                        pT = [ps(st, "pT%d" % i, [128, 512]) for i in range(2)]
                        plg = ps(st, "plg", [128, 512])
                        paT = ps(st, "paT", [NE, 512])
                        ppm = ps(st, "ppm", [128, 512])
                        S.op("pool", lambda: G.memset(ones16[:], 1.0), writes=["ones16"])
                        for tt in range(NT):
                            b = tt % 2
                            S.op("act", lambda: A.activation(out=junk3[:], in_=h[:, tt, :], func=AF.Square,
                                                             accum_out=sst[:, tt:tt + 1]),
                                 reads=[("h", tt)], writes=["junk3", ("sst", tt)])
                            S.op("act", lambda: A.activation(out=sst[:, NT + tt:NT + tt + 1], in_=sst[:, tt:tt + 1], func=AF.Sqrt,
                                                             bias=EPS, scale=1.0 / DM), reads=[("sst", tt), "kc"], writes=[("sst1", tt)])
                            S.op("dve", lambda: V.reciprocal(out=sst[:, 2 * NT + tt:2 * NT + tt + 1], in_=sst[:, NT + tt:NT + tt + 1]),
                                 reads=[("sst1", tt)], writes=[("sst2", tt)])
                            S.op("dve", lambda: V.scalar_tensor_tensor(out=xnf[b][:], in0=h[:, tt, :],
                                                                       scalar=sst[:, 2 * NT + tt:2 * NT + tt + 1], in1=gffn[:],
                                                                       op0=ALU.mult, op1=ALU.mult),
                                 reads=[("h", tt), ("sst2", tt), "bp_g_ffn"], writes=[("xnf", b)])
                            S.op("act", lambda: A.activation(out=Xn[:, tt, :], in_=xnf[b][:], func=AF.Copy),
                                 reads=[("xnf", b)], writes=[("Xn", tt)])
                            for c in range(8):
                                S.op("pe", lambda: PE_.transpose(out=pT[c // 4][:, (c % 4) * 128:(c % 4 + 1) * 128],
                                                                 in_=xnf[b][:, c * 128:(c + 1) * 128], identity=ident_f[:]),
                                     reads=[("xnf", b), "ident_f"], writes=[("pT", c // 4)])
                            S.op("dve", lambda: V.tensor_copy(out=xnTf[b][:, 0:4, :].rearrange("p a b -> p (a b)"), in_=pT[0][:]),
                                 reads=[("pT", 0)], writes=[("xnTf", b, 0)])
                            S.op("act", lambda: A.activation(out=xnTf[b][:, 4:8, :].rearrange("p a b -> p (a b)"), in_=pT[1][:],
                                                             func=AF.Copy), reads=[("pT", 1)], writes=[("xnTf", b, 1)])
                            for c in range(8):
                                S.op("pe", lambda: PE_.matmul(plg[:, 0:NE], lhsT=xnTf[b][:, c, :], rhs=wr_f[:, c, :],
                                                              start=(c == 0), stop=(c == 7)),
                                     reads=[("xnTf", b, 0), ("xnTf", b, 1), "wr_f"], writes=["plg"])
                            S.op("dve", lambda: V.reduce_max(out=sm[:, tt:tt + 1], in_=plg[:, 0:NE], axis=mybir.AxisListType.X),
                                 reads=["plg"], writes=[("sm0", tt)])
                            S.op("dve", lambda: V.tensor_scalar(out=sm[:, NT + tt:NT + tt + 1], in0=sm[:, tt:tt + 1], scalar1=-1.0,
                                                                scalar2=None, op0=ALU.mult), reads=[("sm0", tt)], writes=[("sm1", tt)])
                            S.op("dve", lambda: V.tensor_scalar(out=ex[:], in0=plg[:, 0:NE], scalar1=sm[:, NT + tt:NT + tt + 1],
                                                                scalar2=None, op0=ALU.add), reads=["plg", ("sm1", tt)], writes=["exa"])
                            S.op("act", lambda: A.activation(out=ex[:], in_=ex[:], func=AF.Exp,
                                                             accum_out=sm[:, 2 * NT + tt:2 * NT + tt + 1]),
                                 reads=["exa"], writes=["ex", ("sm2", tt)])
                            S.op("dve", lambda: V.reciprocal(out=sm[:, 3 * NT + tt:3 * NT + tt + 1], in_=sm[:, 2 * NT + tt:2 * NT + tt + 1]),
                                 reads=[("sm2", tt)], writes=[("sm3", tt)])
                            S.op("dve", lambda: V.tensor_scalar(out=aff_tok[:, tt, :], in0=ex[:], scalar1=sm[:, 3 * NT + tt:3 * NT + tt + 1],
                                                                scalar2=None, op0=ALU.mult), reads=["ex", ("sm3", tt)], writes=[("aff", tt)])
                            S.op("dve", lambda: V.tensor_copy(out=aff_hl[:, tt, :, 0], in_=aff_tok[:, tt, :]),
                                 reads=[("aff", tt)], writes=[("affh", tt)])
                            S.op("dve", lambda: V.tensor_tensor(out=dtmp[:], in0=aff_tok[:, tt, :], in1=aff_hl[:, tt, :, 0], op=ALU.subtract),
                                 reads=[("aff", tt), ("affh", tt)], writes=["dtmp"])
                            S.op("dve", lambda: V.tensor_copy(out=aff_hl[:, tt, :, 1], in_=dtmp[:]), reads=["dtmp"], writes=[("affl", tt)])
                            S.op("pe", lambda: PE_.transpose(out=paT[:, (tt % 4) * 128:(tt % 4 + 1) * 128], in_=aff_tok[:, tt, :],
                                                             identity=ident_f[:]), reads=[("aff", tt), "ident_f"], writes=["paT"])
                            if tt % 4 == 3:
                                t0 = (tt - 3) * 128
                                S.op("dve", lambda: V.tensor_copy(out=affT[:, t0:t0 + 512], in_=paT[:]), reads=["paT"], writes=["affT"])
                        S.op("dve", lambda: V.tensor_copy(out=wk[:], in_=affT[:]), reads=["affT"], writes=["wk"])
                        for r in range(CAP // 8):
                            S.op("dve", lambda: V.max(out=m8[:], in_=wk[:]), reads=["wk"], writes=["m8"])
                            if r < CAP // 8 - 1:
                                S.op("dve", lambda: V.match_replace(out=wk[:], in_to_replace=m8[:], in_values=wk[:], imm_value=-1.0),
                                     reads=["wk", "m8"], writes=["wk"])
                        S.op("dve", lambda: V.tensor_scalar(out=maskT[:], in0=affT[:], scalar1=m8[:, 7:8], scalar2=None, op0=ALU.is_ge),
                             reads=["affT", "m8"], writes=["maskT"])
                        S.op("dve", lambda: V.tensor_tensor_scan(out=cumT[:], data0=ones16[:], data1=maskT[:], initial=0.0,
                                                                 op0=ALU.mult, op1=ALU.add), reads=["ones16", "maskT"], writes=["cumT"])
                        S.op("dve", lambda: V.tensor_tensor(out=cumT[:], in0=cumT[:], in1=maskT[:], op=ALU.mult),
                             reads=["cumT", "maskT"], writes=["cumT"])
                        S.op("dve", lambda: V.tensor_scalar(out=cumT[:], in0=cumT[:], scalar1=-1.0, scalar2=None, op0=ALU.add),
                             reads=["cumT"], writes=["cumT"])
                        S.op("dve", lambda: V.tensor_copy(out=posm_b[:], in_=cumT[:]), reads=["cumT"], writes=["posm_b"])
                        for tt in range(NT):
                            S.op("pe", lambda: PE_.transpose(out=ppm[:, (tt % 16) * NE:(tt % 16 + 1) * NE], in_=cumT[:, tt * 128:(tt + 1) * 128],
                                                             identity=ident_f[0:NE, 0:NE]), reads=["cumT", "ident_f"], writes=["ppm"])
                        S.op("dve", lambda: V.tensor_copy(out=posm_tok[:].rearrange("p a b -> p (a b)"), in_=ppm[:, 0:NT * NE]),
                             reads=["ppm"], writes=["posm_tok"])
                        S.barrier()
                    tap("aff_tok", aff_tok[:], [])
                    tap("posm_tok", posm_tok[:], [])
                    if stop_after == "router":
                        S.barrier()
                        continue
                    with ExitStack() as st:
                        iotar = load_bp(st, "iota_row")
                        Sel = sb(st, "Sel", [128, NT, CAP], BF16)
                        SelT = sb(st, "SelT", [128, 2, SEQ], BF16)
                        XeT = sb(st, "XeT", [128, 8, CAP], BF16)
                        HT = [sb(st, "HT%d" % i, [128, 2, CAP], BF16) for i in range(2)]
                        s1b = [sb(st, "s1b%d" % i, [128, CAP], F32) for i in range(2)]
                        Ysb = sb(st, "Ysb", [128, 2, DM], BF16)
                        gate = sb(st, "gate", [128, 2], F32)
                        gate4 = sb(st, "gate4", [128, 2, 2], F32)
                        w1b = [sb(st, "w1b%d" % i, [128, 8, 256], BF16) for i in range(2)]
                        w3b = [sb(st, "w3b%d" % i, [128, 8, 256], BF16) for i in range(2)]
                        w2b = [sb(st, "w2b%d" % i, [128, 2, DM], BF16) for i in range(2)]
                        pG = [ps(st, "pG%d" % i, [128, 2, CAP]) for i in range(2)]
                        pH = [ps(st, "pH%d" % i, [128, 2, CAP]) for i in range(2)]
                        pY = [[ps(st, "pY%d%d" % (i, k), [128, 512]) for k in range(2)] for i in range(2)]
                        wcnt = [0]

                        def load_w(e, fb):
                            wb_ = wcnt[0] % 2
                            wcnt[0] += 1
                            S.dma("pool", w1b[wb_][:], w1_d[e, :, fb * 256:(fb + 1) * 256].rearrange("(c p) n -> p c n", p=128),
                                  ("w1b", wb_), writes=[("w1b", wb_)])
                            S.dma("pool", w3b[wb_][:], w3_d[e, :, fb * 256:(fb + 1) * 256].rearrange("(c p) n -> p c n", p=128),
                                  ("w3b", wb_), writes=[("w3b", wb_)])
                            S.dma("pool", w2b[wb_][:], w2_d[e, fb * 256:(fb + 1) * 256, :].rearrange("(c p) n -> p c n", p=128),
                                  ("w2b", wb_), writes=[("w2b", wb_)])
                            return wb_

                        n_exp = NE if not _os.environ.get("K_NEXP") else int(_os.environ["K_NEXP"])
                        blocks = [(e, fb) for e in range(n_exp) for fb in range(8)]
                        wslot = {}
                        wslot[blocks[0]] = load_w(*blocks[0])
                        hcnt = [0]
                        for e in range(n_exp):
                            def build_sel(e_):
                                for tt in range(NT):
                                    S.op("dve", lambda: V.tensor_scalar(out=Sel[:, tt, :], in0=iotar[:], scalar1=posm_tok[:, tt, e_:e_ + 1],
                                                                        scalar2=None, op0=ALU.is_equal),
                                         reads=["posm_tok", "bp_iota_row"], writes=[("Sel", tt)])
                            if e == 0:
                                build_sel(0)
                            for tb in range(4):
                                pbc = pH[tb % 2][:].rearrange("p a b -> p (a b)")
                                S.op("pe", lambda: PE_.matmul(pbc, lhsT=Eoh[:, e, :], rhs=posm_b[:, tb * 512:(tb + 1) * 512],
                                                              start=True, stop=True), reads=["Eoh", "posm_b"], writes=[("pH", tb % 2)])
                                for cc in range(2):
                                    S.op("dve", lambda: V.tensor_scalar(out=SelT[:, cc, tb * 512:(tb + 1) * 512], in0=pbc,
                                                                        scalar1=C("iota_p" if cc == 0 else "iota_p1"), scalar2=None,
                                                                        op0=ALU.is_equal),
                                         reads=[("pH", tb % 2), "cpack"], writes=[("SelT", cc, tb)])
                            pgt = pH[0][:, 0, 0:4].rearrange("p (a b) -> p a b", b=2)
                            for cc in range(2):
                                for tt in range(NT):
                                    S.op("pe", lambda: PE_.matmul(pgt[:, cc, :], lhsT=Sel[:, tt, cc * 128:(cc + 1) * 128],
                                                                  rhs=aff_hl[:, tt, e, :], start=(tt == 0), stop=(tt == NT - 1)),
                                         reads=[("Sel", tt), ("affh", tt), ("affl", tt)], writes=[("pH", 0)])
                            S.op("dve", lambda: V.tensor_copy(out=gate4[:], in_=pgt), reads=[("pH", 0)], writes=["gate4"])
                            S.op("dve", lambda: V.tensor_tensor(out=gate[:], in0=gate4[:, :, 0], in1=gate4[:, :, 1], op=ALU.add),
                                 reads=["gate4"], writes=["gate"])
                            for dcb in range(4):
                                gb = dcb % 2
                                for k in range(2):
                                    dc_ = dcb * 2 + k
                                    for tt in range(NT):
                                        S.op("pe", lambda: PE_.matmul(pG[gb][:, k, :], lhsT=Xn[:, tt, dc_ * 128:(dc_ + 1) * 128],
                                                                      rhs=Sel[:, tt, :], start=(tt == 0), stop=(tt == NT - 1)),
                                             reads=[("Xn", tt), ("Sel", tt)], writes=[("pG", gb)])
                                if gb == 0:
                                    S.op("act", lambda: A.activation(out=XeT[:, dcb * 2:dcb * 2 + 2, :], in_=pG[gb][:], func=AF.Copy),
                                         reads=[("pG", gb)], writes=[("XeT", dcb)])
                                else:
                                    S.op("dve", lambda: V.tensor_copy(out=XeT[:, dcb * 2:dcb * 2 + 2, :], in_=pG[gb][:]),
                                         reads=[("pG", gb)], writes=[("XeT", dcb)])
                            XK = [("XeT", q) for q in range(4)]
                            if e + 1 < n_exp:
                                build_sel(e + 1)
                            for fb in range(8):
                                wb_ = wslot[(e, fb)]
                                nxt = blocks.index((e, fb)) + 1
                                if nxt < len(blocks):
                                    wslot[blocks[nxt]] = load_w(*blocks[nxt])
                                hb = hcnt[0] % 2
                                hcnt[0] += 1
                                for fc in range(2):
                                    pb_ = fc % 2
                                    for k, wsrc, wk_ in ((0, w1b, "w1b"), (1, w3b, "w3b")):
                                        for dc_ in range(8):
                                            S.op("pe", lambda: PE_.matmul(pH[pb_][:, k, :], lhsT=wsrc[wb_][:, dc_, fc * 128:(fc + 1) * 128],
                                                                          rhs=XeT[:, dc_, :], start=(dc_ == 0), stop=(dc_ == 7)),
                                                 reads=XK + [(wk_, wb_)], writes=[("pH", pb_)])
                                    S.op("act", lambda: A.activation(out=s1b[pb_][:], in_=pH[pb_][:, 0, :], func=AF.Silu),
                                         reads=[("pH", pb_)], writes=[("s1b", pb_)])
                                    S.op("dve", lambda: V.tensor_tensor(out=HT[hb][:, fc, :], in0=pH[pb_][:, 1, :], in1=s1b[pb_][:], op=ALU.mult),
                                         reads=[("pH", pb_), ("s1b", pb_)], writes=[("HT", hb, fc)])
                                for cc in range(2):
                                    for dh in range(2):
                                        for fc in range(2):
                                            S.op("pe", lambda: PE_.matmul(pY[cc][dh][:], lhsT=HT[hb][:, fc, cc * 128:(cc + 1) * 128],
                                                                          rhs=w2b[wb_][:, fc, dh * 512:(dh + 1) * 512],
                                                                          start=(fb == 0 and fc == 0), stop=(fb == 7 and fc == 1)),
                                                 reads=[("HT", hb, fc), ("w2b", wb_)], writes=[("pY", cc, dh)])
                            for cc in range(2):
                                for dh in range(2):
                                    S.op("act", lambda: A.activation(out=Ysb[:, cc, dh * 512:(dh + 1) * 512], in_=pY[cc][dh][:], func=AF.Copy,
                                                                     scale=gate[:, cc:cc + 1]),
                                         reads=[("pY", cc, dh), "gate"], writes=[("Ysb", cc)])
                            for tt in range(NT):
                                for dh in range(2):
                                    oi = (tt * 2 + dh) % 4
                                    pO = pY[oi // 2][oi % 2][:]
                                    ok_ = ("pY", oi // 2, oi % 2)
                                    for cc in range(2):
                                        S.op("pe", lambda: PE_.matmul(pO, lhsT=SelT[:, cc, tt * 128:(tt + 1) * 128],
                                                                      rhs=Ysb[:, cc, dh * 512:(dh + 1) * 512], start=(cc == 0), stop=(cc == 1)),
                                             reads=[("SelT", cc, tt // 4), ("Ysb", cc)], writes=[ok_])
                                    S.op("dve", lambda: V.tensor_tensor(out=h[:, tt, dh * 512:(dh + 1) * 512], in0=pO,
                                                                        in1=h[:, tt, dh * 512:(dh + 1) * 512], op=ALU.add),
                                         reads=[ok_, ("h", tt)], writes=[("h", tt)])
                        S.barrier()
                    S.barrier()
                tap("h2", h[:], [])
                if stop_after == "moe":
                    S.barrier()
                    continue

                with ExitStack() as pl:
                    gpost = load_bp(pl, "g_post")
                    hnT = sb(pl, "hnT", [128, 8, SEQ], BF16)
                    wg = sb(pl, "wg", [128, 8, DM], BF16)
                    wpe = sb(pl, "wpe", [128, 2, DM], BF16)
                    S.dma("pool", wg[:], wg_d.rearrange("(c p) n -> p c n", p=128), "wg", writes=["wg"])
                    S.dma("pool", wpe[:], wpe_d.rearrange("(c p) n -> p c n", p=128), "wpe", writes=["wpe"])
                    hs = [sb(pl, "hs%d" % i, [128, DM], BF16) for i in range(2)]
                    junk4 = sb(pl, "junk4", [128, DM], BF16)
                    st4 = sb(pl, "st4", [128, 8 * NT], F32)
                    pls = ExitStack()
                    pt4 = [ps(pls, "pt4%d" % i, [128, 1024], BF16) for i in range(2)]
                    for tt in range(NT):
                        b = tt % 2
                        S.op("act", lambda: A.activation(out=junk4[:], in_=h[:, tt, :], func=AF.Square, accum_out=st4[:, tt:tt + 1]),
                             reads=[("h", tt)], writes=["junk4", ("st4", tt)])
                        S.op("act", lambda: A.activation(out=st4[:, NT + tt:NT + tt + 1], in_=st4[:, tt:tt + 1], func=AF.Sqrt, bias=EPS,
                                                         scale=1.0 / DM), reads=[("st4", tt), "kc"], writes=[("st41", tt)])
                        S.op("dve", lambda: V.reciprocal(out=st4[:, 2 * NT + tt:2 * NT + tt + 1], in_=st4[:, NT + tt:NT + tt + 1]),
                             reads=[("st41", tt)], writes=[("st42", tt)])
                        S.op("dve", lambda: V.tensor_scalar(out=hs[b][:], in0=h[:, tt, :], scalar1=st4[:, 2 * NT + tt:2 * NT + tt + 1],
                                                            scalar2=None, op0=ALU.mult), reads=[("h", tt), ("st42", tt)], writes=[("hs", b)])
                        for c in range(8):
                            S.op("pe", lambda: PE_.transpose(out=pt4[b][:, c * 128:(c + 1) * 128], in_=hs[b][:, c * 128:(c + 1) * 128],
                                                             identity=ident_b[:]), reads=[("hs", b), "ident_b"], writes=[("pt4", b)])
                        for c in range(8):
                            if b == 0:
                                S.op("dve", lambda: V.tensor_scalar(out=hnT[:, c, tt * 128:(tt + 1) * 128], in0=pt4[b][:, c * 128:(c + 1) * 128],
                                                                    scalar1=C("g_ple", c), scalar2=None, op0=ALU.mult),
                                     reads=[("pt4", b), "cpack"], writes=[("hnT", tt)])
                            else:
                                S.op("act", lambda: A.activation(out=hnT[:, c, tt * 128:(tt + 1) * 128], in_=pt4[b][:, c * 128:(c + 1) * 128],
                                                                 func=AF.Copy, scale=C("g_ple", c)),
                                     reads=[("pt4", b), "cpack"], writes=[("hnT", tt)])
                    S.barrier()
                    pls.close()
                    with ExitStack() as st:
                        pgt2 = [[ps(st, "pgt2%d%d" % (i, k), [128, 512]) for k in range(2)] for i in range(2)]
                        pe2 = [ps(st, "pe2%d" % k, [128, 512]) for k in range(2)]
                        ppt = ps(st, "ppt", [128, 1024], BF16)
                        gsb = [sb(st, "gsb%d" % i, [128, DM], F32) for i in range(2)]
                        ptl = [sb(st, "ptl%d" % i, [128, 256], F32) for i in range(2)]
                        ptb = sb(st, "ptb", [128, 256], BF16)
                        pTb = sb(st, "pTb", [128, 2, 128], BF16)
                        en = [sb(st, "en%d" % i, [128, DM], F32) for i in range(2)]
                        junk5 = sb(st, "junk5", [128, 512], BF16)
                        for tt in range(NT):
                            b = tt % 2
                            S.dma("sp", ptl[b][:], p_d[s, tt * 128:(tt + 1) * 128, :], ("ptl", b), writes=[("ptl", b)])
                            for dh in range(2):
                                for c in range(8):
                                    S.op("pe", lambda: PE_.matmul(pgt2[b][dh][:], lhsT=hnT[:, c, tt * 128:(tt + 1) * 128],
                                                                  rhs=wg[:, c, dh * 512:(dh + 1) * 512], start=(c == 0), stop=(c == 7)),
                                         reads=[("hnT", tt), "wg"], writes=[("pgt2", b, dh)])
                                S.op("act", lambda: A.activation(out=gsb[b][:, dh * 512:(dh + 1) * 512], in_=pgt2[b][dh][:], func=AF.Sigmoid),
                                     reads=[("pgt2", b, dh)], writes=[("gsb", b, dh)])
                            S.op("dve", lambda: V.tensor_copy(out=ptb[:], in_=ptl[b][:]), reads=[("ptl", b)], writes=["ptb"])
                            for k in range(2):
                                S.op("pe", lambda: PE_.transpose(out=ppt[:, k * 128:(k + 1) * 128], in_=ptb[:, k * 128:(k + 1) * 128],
                                                                 identity=ident_b[:]), reads=["ptb", "ident_b"], writes=["ppt"])
                            S.op("dve", lambda: V.tensor_copy(out=pTb[:].rearrange("p a b -> p (a b)"), in_=ppt[:, 0:256]),
                                 reads=["ppt"], writes=["pTb"])
                            for dh in range(2):
                                for k in range(2):
                                    S.op("pe", lambda: PE_.matmul(pe2[dh][:], lhsT=pTb[:, k, :], rhs=wpe[:, k, dh * 512:(dh + 1) * 512],
                                                                  start=(k == 0), stop=(k == 1)), reads=["pTb", "wpe"], writes=[("pe2", dh)])
                                S.op("act", lambda: A.activation(out=junk5[:], in_=pe2[dh][:], func=AF.Square,
                                                                 accum_out=st4[:, 3 * NT + 2 * tt + dh:3 * NT + 2 * tt + dh + 1]),
                                     reads=[("pe2", dh)], writes=["junk5", ("st43", tt, dh)])
                            o5 = 5 * NT + tt
                            S.op("dve", lambda: V.tensor_tensor(out=st4[:, o5:o5 + 1], in0=st4[:, 3 * NT + 2 * tt:3 * NT + 2 * tt + 1],
                                                                in1=st4[:, 3 * NT + 2 * tt + 1:3 * NT + 2 * tt + 2], op=ALU.add),
                                 reads=[("st43", tt, 0), ("st43", tt, 1)], writes=[("st45", tt)])
                            S.op("act", lambda: A.activation(out=st4[:, 6 * NT + tt:6 * NT + tt + 1], in_=st4[:, o5:o5 + 1], func=AF.Sqrt,
                                                             bias=EPS, scale=1.0 / DM), reads=[("st45", tt), "kc"], writes=[("st46", tt)])
                            S.op("dve", lambda: V.reciprocal(out=st4[:, 7 * NT + tt:7 * NT + tt + 1], in_=st4[:, 6 * NT + tt:6 * NT + tt + 1]),
                                 reads=[("st46", tt)], writes=[("st47", tt)])
                            for dh in range(2):
                                hsl = slice(dh * 512, (dh + 1) * 512)
                                S.op("dve", lambda: V.scalar_tensor_tensor(out=en[b][:, hsl], in0=pe2[dh][:],
                                                                           scalar=st4[:, 7 * NT + tt:7 * NT + tt + 1],
                                                                           in1=gpost[:, dh * 512:(dh + 1) * 512], op0=ALU.mult, op1=ALU.mult),
                                     reads=[("pe2", dh), ("st47", tt), "bp_g_post"], writes=[("en", b, dh)])
                                S.op("dve", lambda: V.tensor_tensor(out=en[b][:, hsl], in0=en[b][:, hsl], in1=gsb[b][:, hsl], op=ALU.mult),
                                     reads=[("en", b, dh), ("gsb", b, dh)], writes=[("en", b, dh)])
                                S.op("dve", lambda: V.tensor_tensor(out=en[b][:, hsl], in0=en[b][:, hsl], in1=h[:, tt, hsl], op=ALU.add),
                                     reads=[("en", b, dh), ("h", tt)], writes=[("en", b, dh)])
                            S.dma("sp", out_d[s, tt * 128:(tt + 1) * 128, :], en[b][:], ("outd", b),
                                  reads=[("en", b, 0), ("en", b, 1)], writes=[])
                        S.barrier()
                    S.barrier()
                S.barrier()
        S.final_wait("sp")
        import os as _os2
        if _os2.environ.get("K_PRINTSEMS"):
            print("SEMS", [(i, k) for i, k in enumerate(S.dma_sems.keys())], "n_instr", S.n_instr)
    return nc


def _prep(inputs):
    inp = {k: np.asarray(v) for k, v in inputs.items()}
    cp = make_cpack(inp)
    bp = make_bpack(inp)
    st = make_struct()
    shared = {
        "w_in": np.ascontiguousarray(inp["w_in"][0]),
        "w_out": np.ascontiguousarray(inp["w_out"][0]),
        "cpack": cp.build(), "bpack": bp.build(),
        "rel_bias": np.ascontiguousarray(inp["rel_bias"]),
        "onehot": st["onehot"], "rwmask": st["rwmask"],
        "w2cat": np.ascontiguousarray(inp["rw_w2"][0].reshape(128, 512)),
        "a2cat": np.ascontiguousarray(inp["rw_a2"][0].reshape(128, 512)),
        "g2": np.ascontiguousarray(inp["rw_g2"][0]),
        "w_router": np.ascontiguousarray(inp["w_router"][0]),
        "w1": np.ascontiguousarray(inp["w1"][0]), "w3": np.ascontiguousarray(inp["w3"][0]),
        "w2": np.ascontiguousarray(inp["w2"][0]),
        "w_ple_gate": np.ascontiguousarray(inp["w_ple_gate"][0]),
        "w_ple": np.ascontiguousarray(inp["w_ple"][0]),
    }
    in_maps = []
    for c in range(NCORES):
        m = dict(shared)
        m["x"] = np.ascontiguousarray(inp["x"][c * NSEQ:(c + 1) * NSEQ])
        m["p"] = np.ascontiguousarray(inp["p"][0, c * NSEQ:(c + 1) * NSEQ])
        in_maps.append(m)
    return cp, bp, in_maps


def kernel(**inputs):
    cp, bp, in_maps = _prep(inputs)
    nc = build_program(cp.off, bp.off, cp.n, bp.n)
    res = run_bass_kernel_spmd(nc, in_maps, core_ids=list(range(NCORES)))
    return np.concatenate([r["out"] for r in res.results], axis=0).astype(np.float32)
```

```python
import math
import numpy as np
from contextlib import ExitStack
import concourse.bass as bass
import concourse.mybir as mybir
from concourse.bass_utils import run_bass_kernel_spmd

F32 = mybir.dt.float32
BF16 = mybir.dt.bfloat16
AF = mybir.ActivationFunctionType
ALU = mybir.AluOpType

NCORES = 8
SEQ = 2048
DM = 1024
NSEQ = 2
NT = SEQ // 128
IN_COLS = 3488
RW0 = 1536
NE = 16
CAP = 256
DFF = 2048
CDEC = 0.6065306597126334
LAM_INIT = 0.8 - 0.6 * math.exp(-0.3 * 0)
STRIP_W = 1152
ND = 1279


class Holder:
    def __init__(self, name, sem):
        self.name = name
        self.sem = sem
        self.count = 0


class Sched:
    def __init__(self, nc, stack):
        self.nc = nc
        self.stack = stack
        self.obj = {"pe": nc.tensor, "dve": nc.vector, "act": nc.scalar,
                    "pool": nc.gpsimd, "sp": nc.sync}
        self.eng = {}
        for n in self.obj:
            sem = stack.enter_context(nc.semaphore("s_" + n))
            self.eng[n] = Holder(n, sem)
        self.known = {n: {} for n in self.obj}
        self.last_w = {}
        self.readers = {}
        self.dma_sems = {}
        self.n_instr = 0

    def _deps(self, reads, writes, e=None):
        own = self.eng.get(e) if e in ("pe",) else None
        toks = []
        for r in reads:
            t = self.last_w.get(r)
            if t is not None:
                toks.append(t)
        for w in writes:
            t = self.last_w.get(w)
            if t is not None and t[0] is not own:
                toks.append(t)
            for h, v in self.readers.get(w, {}).items():
                if h is not own:
                    toks.append((h, v))
        return toks

    def _wait(self, e, toks, keep_one=False):
        kn = self.known[e]
        need = {}
        for (h, v) in toks:
            if kn.get(h, 0) < v and need.get(h, 0) < v:
                need[h] = v
        items = list(need.items())
        fused = None
        if keep_one and items:
            fused = items.pop()
        for h, v in items:
            self.obj[e].wait_ge(h.sem, v)
            kn[h] = v
            self.n_instr += 1
        if fused is not None:
            kn[fused[0]] = fused[1]
        return fused

    def _commit(self, tok, reads, writes):
        for r in reads:
            d = self.readers.setdefault(r, {})
            if d.get(tok[0], 0) < tok[1]:
                d[tok[0]] = tok[1]
        for w in writes:
            self.last_w[w] = tok
            self.readers[w] = {}

    def op(self, e, fn, reads=(), writes=(), fuse=True):
        toks = self._deps(reads, writes, e)
        fused = self._wait(e, toks, keep_one=fuse)
        ins = fn()
        if fused is not None:
            ins._wait_ge(fused[0].sem, fused[1])
        h = self.eng[e]
        h.count += 1
        ins.then_inc(h.sem, 1)
        tok = (h, h.count)
        self._commit(tok, reads, writes)
        self.n_instr += 1
        return tok

    def dma(self, q, out, in_, semkey, reads=(), writes=(), **kw):
        toks = self._deps(reads, writes)
        self._wait(q, toks)
        if semkey not in self.dma_sems:
            sem = self.stack.enter_context(self.nc.semaphore("d_%d" % len(self.dma_sems)))
            self.dma_sems[semkey] = Holder("dma_" + str(semkey), sem)
        h = self.dma_sems[semkey]
        ins = self.obj[q].dma_start(out=out, in_=in_, **kw)
        ins.then_inc(h.sem, 16)
        h.count += 16
        tok = (h, h.count)
        self._commit(tok, reads, writes)
        self.n_instr += 1
        return tok

    def _all(self):
        toks = [(h, h.count) for h in self.eng.values() if h.count > 0]
        toks += [(h, h.count) for h in self.dma_sems.values() if h.count > 0]
        return toks

    def barrier(self):
        toks = self._all()
        for e in self.obj:
            self._wait(e, toks)

    def final_wait(self, e="sp"):
        self._wait(e, self._all())


def _t5_bucket(rel):
    half, max_exact = 16, 8
    ret = np.where(rel > 0, half, 0)
    n = np.abs(rel)
    nf = np.maximum(n, 1).astype(np.float32)
    large = max_exact + (np.log(nf / np.float32(max_exact)) / np.float32(math.log(128 / max_exact))
                         * np.float32(half - max_exact)).astype(np.int32)
    large = np.minimum(large, half - 1)
    return ret + np.where(n < max_exact, n, large)


class Pack:
    def __init__(self):
        self.cols = []
        self.off = {}
        self.n = 0

    def add(self, name, arr):
        arr = np.asarray(arr, np.float32)
        assert arr.shape[0] == 128
        if arr.ndim == 1:
            arr = arr[:, None]
        self.off[name] = (self.n, arr.shape[1])
        self.cols.append(arr)
        self.n += arr.shape[1]

    def build(self):
        return np.ascontiguousarray(np.concatenate(self.cols, axis=1))


def pc(v, nchunk):
    return np.ascontiguousarray(np.asarray(v, np.float32).reshape(nchunk, 128).T)


def make_cpack(inp):
    P = Pack()
    P.add("g_mix", pc(inp["g_mix"][0], 8))
    P.add("qg", np.tile(inp["q_norm_g"][0], 2))
    P.add("kg", np.tile(inp["k_norm_g"][0], 2))
    mu = np.zeros(16 * 128, np.float32)
    mu[:1952] = inp["rw_mu"][0]
    P.add("mu", pc(mu, 16))
    P.add("w0", pc(inp["rw_w0"][0].reshape(-1), 8))
    P.add("a0", pc(inp["rw_a0"][0].reshape(-1), 8))
    P.add("k_k", pc(inp["rw_k_k"][0], 4))
    P.add("k_a", pc(inp["rw_k_a"][0], 4))
    P.add("r_k", pc(inp["rw_r_k"][0].reshape(-1), 4))
    P.add("lnx_g", pc(inp["rw_lnx_g"][0], 4))
    P.add("lnx_b", pc(inp["rw_lnx_b"][0], 4))
    P.add("subln_g", inp["subln_g"][0])
    P.add("g_ple", pc(inp["g_ple"][0], 8))
    rb = inp["rel_bias"]
    far = np.stack([rb[15, :], rb[31, :]], axis=1).reshape(-1)
    P.add("rb_far", np.broadcast_to(far[None, :], (128, 8)))
    P.add("iota_p", np.arange(128, dtype=np.float32))
    P.add("iota_p1", np.arange(128, 256, dtype=np.float32))
    for k in ("lam_q1", "lam_k1", "lam_q2", "lam_k2"):
        P.add(k, np.broadcast_to(inp[k][0][None, :], (128, 64)))
    return P


def make_bpack(inp):
    P = Pack()
    P.add("g_ffn", np.broadcast_to(inp["g_ffn"][0][None, :], (128, DM)))
    P.add("g_post", np.broadcast_to(inp["g_ple_post"][0][None, :], (128, DM)))
    P.add("iota_row", np.broadcast_to(np.arange(CAP, dtype=np.float32)[None, :], (128, CAP)))
    return P


def make_struct():
    m = np.arange(ND)
    delta = 639 - m
    bk = _t5_bucket(delta.astype(np.int32))
    oh = np.zeros((32, ND), np.float32)
    oh[bk, m] = 1.0
    idx = np.arange(64)
    s = idx[:, None]
    t = idx[None, :]
    rw = np.zeros((2, 64, 192), np.float32)
    rw[0, :, 0:64] = (s < t)
    rw[0, :, 64:128] = (s <= t)
    rw[0, :, 128:192] = (t < s)
    rw[1, :, 0:64] = (s > t)
    rw[1, :, 64:128] = (s >= t)
    rw[1, :, 128:192] = (t > s)
    return {"onehot": oh, "rwmask": np.ascontiguousarray(rw.transpose(1, 0, 2).reshape(64, 384))}


def build_program(cp_off, bp_off, ncp, nbp, dbg=None, nseq=NSEQ, stop_after=None):
    dbg = dbg or {}
    nc = bass.Bass("TRN2", target_bir_lowering=False)
    D = {}

    def din(name, shape, dt=F32):
        D[name] = nc.dram_tensor(name, list(shape), dt, kind="ExternalInput").ap()
        return D[name]

    x_d = din("x", [NSEQ, SEQ, DM])
    p_d = din("p", [NSEQ, SEQ, 256])
    w_in_d = din("w_in", [DM, IN_COLS])
    w_out_d = din("w_out", [DM, DM])
    cpack_d = din("cpack", [128, ncp])
    bpack_d = din("bpack", [128, nbp])
    relb_d = din("rel_bias", [32, 4])
    onehot_d = din("onehot", [32, ND])
    rwmask_d = din("rwmask", [64, 384])
    w2cat_d = din("w2cat", [128, 512])
    a2cat_d = din("a2cat", [128, 512])
    g2_d = din("g2", [160, 512])
    wr_d = din("w_router", [DM, NE])
    w1_d = din("w1", [NE, DM, DFF])
    w3_d = din("w3", [NE, DM, DFF])
    w2_d = din("w2", [NE, DFF, DM])
    wg_d = din("w_ple_gate", [DM, DM])
    wpe_d = din("w_ple", [256, DM])
    out_d = nc.dram_tensor("out", [NSEQ, SEQ, DM], F32, kind="ExternalOutput").ap()
    gscr_t = nc.dram_tensor("gscr", [4, ND], F32)
    gscr_d = gscr_t.ap()
    dbg_d = {}
    for k, (shape, dt) in dbg.items():
        dbg_d[k] = nc.dram_tensor("dbg_" + k, list(shape), dt, kind="ExternalOutput").ap()

    with ExitStack() as top:
        S = Sched(nc, top)
        V, A, G, PE_ = nc.vector, nc.scalar, nc.gpsimd, nc.tensor

        uid = [0]

        def sb(st, name, shape, dt):
            uid[0] += 1
            return st.enter_context(nc.sbuf_tensor("%s_s%d" % (name, uid[0]), list(shape), dt))

        def ps(st, name, shape, dt=F32):
            uid[0] += 1
            return st.enter_context(nc.psum_tensor("%s_p%d" % (name, uid[0]), list(shape), dt))

        def C(name, j=0, w=1):
            o, n = cp_off[name]
            return cpack[:, o + j:o + j + w]

        def tap(name, src_ap, reads, idx=None):
            if name in dbg_d:
                dst = dbg_d[name] if idx is None else dbg_d[name][idx]
                S.dma("sp", dst, src_ap, "dbg", reads=reads)

        cpack = sb(top, "cpack", [128, ncp], F32)
        S.dma("sp", cpack[:], cpack_d, "c0", writes=["cpack"])
        kc = sb(top, "kc", [128, 8], F32)
        S.op("pool", lambda: G.memset(kc[:, 0:1], 1e-6), writes=["kc"])
        S.op("pool", lambda: G.memset(kc[:, 1:2], 64e-5), writes=["kc"])
        S.op("pool", lambda: G.memset(kc[:, 2:3], 0.0), writes=["kc"])
        S.op("pool", lambda: G.memset(kc[:, 3:4], 1e-18), writes=["kc"])
        EPS = kc[:, 0:1]
        EPSLN = kc[:, 1:2]
        ident_b = sb(top, "ident_b", [128, 128], BF16)
        ident_f = sb(top, "ident_f", [128, 128], F32)
        for idt, nm in ((ident_b, "ident_b"), (ident_f, "ident_f")):
            S.op("pool", lambda idt=idt: G.memset(idt[:], 1.0), writes=[nm])
            S.op("pool", lambda idt=idt: G.affine_select(
                out=idt[:], in_=idt[:], pattern=[[-1, 128]], compare_op=ALU.is_equal,
                fill=0.0, base=0, channel_multiplier=1), reads=[nm], writes=[nm])
        bd_b = sb(top, "bd_b", [128, 128], BF16)
        S.op("pool", lambda: G.memset(bd_b[:], 0.0), writes=["bd_b"])
        S.op("pool", lambda: G.memset(bd_b[0:64, 0:64], 1.0), reads=["bd_b"], writes=["bd_b"])
        S.op("pool", lambda: G.memset(bd_b[64:128, 64:128], 1.0), reads=["bd_b"], writes=["bd_b"])
        dc = sb(top, "dc", [128, 64], F32)
        S.op("dve", lambda: V.tensor_scalar(out=dc[:, 0:1], in0=C("qg"), scalar1=0.125, scalar2=None,
                                            op0=ALU.mult), reads=["cpack"], writes=["dc0"])
        S.op("dve", lambda: V.tensor_scalar(out=dc[:, 3:19], in0=C("mu", 0, 16), scalar1=-1.0, scalar2=1.0,
                                            op0=ALU.mult, op1=ALU.add), reads=["cpack"], writes=["dc_omm"])
        S.op("dve", lambda: V.tensor_scalar(out=dc[:, 19:35], in0=C("mu", 0, 16), scalar1=0.5, scalar2=None,
                                            op0=ALU.mult), reads=["cpack"], writes=["dc_hmu"])
        S.op("dve", lambda: V.tensor_scalar(out=dc[:, 35:39], in0=C("k_a", 0, 4), scalar1=-1.0, scalar2=1.0,
                                            op0=ALU.mult, op1=ALU.add), reads=["cpack"], writes=["dc_omka"])
        lt = sb(top, "lamtmp", [128, 64], F32)
        l2 = sb(top, "lam2", [128, 4], F32)
        for i, (a, b) in enumerate((("lam_q1", "lam_k1"), ("lam_q2", "lam_k2"))):
            oa, ob = cp_off[a][0], cp_off[b][0]
            S.op("dve", lambda oa=oa, ob=ob: V.tensor_tensor(out=lt[:], in0=cpack[:, oa:oa + 64],
                                                               in1=cpack[:, ob:ob + 64], op=ALU.mult),
                 reads=["cpack"], writes=["lamtmp"])
            S.op("dve", lambda i=i: V.reduce_sum(out=l2[:, i:i + 1], in_=lt[:], axis=mybir.AxisListType.X),
                 reads=["lamtmp"], writes=[("lam2", i)])
        S.op("act", lambda: A.activation(out=l2[:, 2:4], in_=l2[:, 0:2], func=AF.Exp),
             reads=[("lam2", 0), ("lam2", 1)], writes=["lam2e"])
        S.op("dve", lambda: V.tensor_tensor(out=dc[:, 1:2], in0=l2[:, 2:3], in1=l2[:, 3:4], op=ALU.subtract),
             reads=["lam2e"], writes=["dc1a"])
        S.op("dve", lambda: V.tensor_scalar(out=dc[:, 1:2], in0=dc[:, 1:2], scalar1=LAM_INIT, scalar2=None,
                                            op0=ALU.add), reads=["dc1a"], writes=["dc1"])
        S.op("dve", lambda: V.tensor_scalar(out=dc[:, 2:3], in0=dc[:, 1:2], scalar1=-1.0, scalar2=None,
                                            op0=ALU.mult), reads=["dc1"], writes=["dc2"])
        QGS = dc[:, 0:1]
        NLAM = dc[:, 2:3]

        with ExitStack() as st:
            rb_sb = sb(st, "rb_sb", [32, 4], F32)
            oh_sb = sb(st, "oh_sb", [32, ND], F32)
            g4 = sb(st, "g4", [4, ND], F32)
            gp = ps(st, "gp", [4, 512])
            S.dma("sp", rb_sb[:], relb_d, "c1", writes=["rb_sb"])
            S.dma("sp", oh_sb[:], onehot_d, "c2", writes=["oh_sb"])
            for b0 in range(0, ND, 512):
                n = min(512, ND - b0)
                S.op("pe", lambda b0=b0, n=n: PE_.matmul(gp[:, 0:n], lhsT=rb_sb[:], rhs=oh_sb[:, b0:b0 + n],
                                                           start=True, stop=True),
                     reads=["rb_sb", "oh_sb"], writes=["gp"])
                S.op("dve", lambda b0=b0, n=n: V.tensor_copy(out=g4[:, b0:b0 + n], in_=gp[:, 0:n]),
                     reads=["gp"], writes=["g4"])
            S.dma("sp", gscr_d, g4[:], "c3", reads=["g4"], writes=["gscr"])
            S.barrier()

        for s in range(nseq):
            with ExitStack() as sq:
                o_daT = sb(sq, "o_daT", [128, 4, SEQ], BF16)
                o_rwT = sb(sq, "o_rwT", [128, 4, SEQ], BF16)
                mixs = ExitStack()
                sq.callback(mixs.close)
                xnT = sb(mixs, "xnT", [128, 8, SEQ], BF16)
                with ExitStack() as st:
                    xt = [sb(st, "xt%d" % i, [128, DM], F32) for i in range(2)]
                    xs = [sb(st, "xs%d" % i, [128, DM], BF16) for i in range(2)]
                    junk = sb(st, "junk", [128, DM], BF16)
                    ssq = sb(st, "ssq", [128, 2 * NT], F32)
                    ptp = [ps(st, "ptp%d" % i, [128, 1024], BF16) for i in range(2)]
                    for tt in range(NT):
                        b = tt % 2
                        S.dma("sp", xt[b][:], x_d[s, tt * 128:(tt + 1) * 128, :], ("xt", b), writes=[("xt", b)])
                        S.op("act", lambda b=b, tt=tt: A.activation(out=junk[:], in_=xt[b][:], func=AF.Square,
                                                                     accum_out=ssq[:, tt:tt + 1]),
                             reads=[("xt", b)], writes=["junk", ("ssq", tt)], fuse=False)
                        S.op("act", lambda tt=tt: A.activation(out=ssq[:, NT + tt:NT + tt + 1], in_=ssq[:, tt:tt + 1],
                                                               func=AF.Sqrt, bias=EPS, scale=1.0 / DM),
                             reads=[("ssq", tt), "kc"], writes=[("ssd", tt)])
                        S.op("dve", lambda tt=tt: V.reciprocal(out=ssq[:, tt:tt + 1], in_=ssq[:, NT + tt:NT + tt + 1]),
                             reads=[("ssd", tt)], writes=[("rstd", tt)])
                        S.op("dve", lambda b=b, tt=tt: V.tensor_scalar(out=xs[b][:], in0=xt[b][:], scalar1=ssq[:, tt:tt + 1],
                                                                        scalar2=None, op0=ALU.mult),
                             reads=[("xt", b), ("rstd", tt)], writes=[("xs", b)])
                        for c in range(8):
                            S.op("pe", lambda b=b, c=c: PE_.transpose(out=ptp[b][:, c * 128:(c + 1) * 128],
                                                                       in_=xs[b][:, c * 128:(c + 1) * 128], identity=ident_b[:]),
                                 reads=[("xs", b), "ident_b"], writes=[("ptp", b)])
                        for c in range(8):
                            e = "act" if b % 2 else "dve"
                            if e == "dve":
                                S.op("dve", lambda b=b, c=c, tt=tt: V.tensor_scalar(
                                    out=xnT[:, c, tt * 128:(tt + 1) * 128], in0=ptp[b][:, c * 128:(c + 1) * 128],
                                    scalar1=C("g_mix", c), scalar2=None, op0=ALU.mult),
                                    reads=[("ptp", b), "cpack"], writes=[("xnT", tt)])
                            else:
                                S.op("act", lambda b=b, c=c, tt=tt: A.activation(
                                    out=xnT[:, c, tt * 128:(tt + 1) * 128], in_=ptp[b][:, c * 128:(c + 1) * 128],
                                    func=AF.Copy, scale=C("g_mix", c)),
                                    reads=[("ptp", b), "cpack"], writes=[("xnT", tt)])
                    S.barrier()
                tap("xnT", xnT[:], [("xnT", tt) for tt in range(NT)])
                XNT_ALL = [("xnT", tt) for tt in range(NT)]
                if stop_after == "p1":
                    S.barrier()
                    continue

                wlT = sb(mixs, "wlT", [128, SEQ], BF16)
                alT = sb(mixs, "alT", [128, SEQ], BF16)
                glT = sb(mixs, "glT", [128, SEQ], BF16)
                gl2T = sb(mixs, "gl2T", [32, SEQ], BF16)

                def shiftmix(uk, u, m, jc, outk, out, tmp, tmpk):
                    S.op("pool", lambda: G.tensor_tensor(out=tmp[:m, 1:SEQ - 1], in0=u[:m, 0:SEQ - 2], in1=u[:m, 2:SEQ],
                                                         op=ALU.add), reads=[uk], writes=[tmpk])
                    S.op("pool", lambda: G.tensor_copy(out=tmp[:m, 0:1], in_=u[:m, 1:2]), reads=[uk], writes=[tmpk])
                    S.op("pool", lambda: G.tensor_copy(out=tmp[:m, SEQ - 1:SEQ], in_=u[:m, SEQ - 2:SEQ - 1]),
                         reads=[uk], writes=[tmpk])
                    S.op("dve", lambda: V.tensor_scalar(out=tmp[:m, :], in0=tmp[:m, :], scalar1=dc[:m, 19 + jc:20 + jc],
                                                        scalar2=None, op0=ALU.mult),
                         reads=[tmpk, "dc_hmu"], writes=[tmpk])
                    S.op("dve", lambda: V.scalar_tensor_tensor(out=out[:m, :], in0=u[:m, :], scalar=dc[:m, 3 + jc:4 + jc],
                                                               in1=tmp[:m, :], op0=ALU.mult, op1=ALU.add),
                         reads=[uk, tmpk, "dc_omm"], writes=[outk])

                with ExitStack() as at:
                    qT = sb(at, "qT", [128, 4, SEQ], BF16)
                    kT = sb(at, "kT", [128, 4, SEQ], BF16)
                    vaug = sb(at, "vaug", [128, NT, 4, 130], BF16)
                    with ExitStack() as st:
                        wA = sb(st, "wA", [128, 8, 1952], BF16)
                        S.dma("pool", wA[:, :, 0:1536], w_in_d[:, 0:1536].rearrange("(c p) n -> p c n", p=128),
                              "wA", writes=["wA"])
                        S.dma("pool", wA[:, :, 1536:1952], w_in_d[:, 3072:3488].rearrange("(c p) n -> p c n", p=128),
                              "wA", writes=["wA"])
                        pu = [ps(st, "pu%d" % i, [128, 512]) for i in range(2)]
                        pss = [ps(st, "pss%d" % i, [128, 512]) for i in range(2)]
                        sqb = [sb(st, "sqb%d" % i, [128, 512], BF16) for i in range(2)]
                        usb = [sb(st, "usb%d" % i, [128, 512], F32) for i in range(2)]
                        sdb = [sb(st, "sdb%d" % i, [128, 512], F32) for i in range(2)]
                        S.op("pool", lambda: G.memset(vaug[:, :, :, 128:130], 1.0), writes=["vaug_ones"])

                        def proj_fm(pst, pk, c0, m, tb):
                            for dci in range(8):
                                S.op("pe", lambda dci=dci: PE_.matmul(pst[:m, :], lhsT=wA[:, dci, c0:c0 + m],
                                                                       rhs=xnT[:, dci, tb * 512:(tb + 1) * 512],
                                                                       start=(dci == 0), stop=(dci == 7)),
                                     reads=["wA"] + XNT_ALL[tb * 4:tb * 4 + 4], writes=[pk])

                        units = [(kind, h, tb) for kind in range(2) for h in range(4) for tb in range(4)]

                        def qk_front(i):
                            kind, h, tb = units[i]
                            b = i % 2
                            proj_fm(pu[b], ("pu", b), kind * 512 + h * 128, 128, tb)
                            S.op("dve", lambda: V.tensor_copy(out=usb[b][:], in_=pu[b][:]),
                                 reads=[("pu", b)], writes=[("usb", b)])
                            S.op("act", lambda: A.activation(out=sqb[b][:], in_=usb[b][:], func=AF.Square),
                                 reads=[("usb", b)], writes=[("sqb", b)])

                        def qk_back(i):
                            kind, h, tb = units[i]
                            b = i % 2
                            dst = (qT, kT)[kind]
                            gcol = QGS if kind == 0 else C("kg")
                            S.op("pe", lambda: PE_.matmul(pss[b][:], lhsT=bd_b[:], rhs=sqb[b][:], start=True, stop=True),
                                 reads=["bd_b", ("sqb", b)], writes=[("pss", b)])
                            S.op("act", lambda: A.activation(out=sdb[b][:], in_=pss[b][:], func=AF.Ln, bias=EPS,
                                                             scale=1.0 / 64),
                                 reads=[("pss", b), "kc"], writes=[("sdb", b)])
                            S.op("act", lambda: A.activation(out=sdb[b][:], in_=sdb[b][:], func=AF.Exp, scale=-0.5),
                                 reads=[("sdb", b)], writes=[("sdb", b)])
                            S.op("dve", lambda: V.scalar_tensor_tensor(
                                out=dst[:, h, tb * 512:(tb + 1) * 512], in0=usb[b][:], scalar=gcol, in1=sdb[b][:],
                                op0=ALU.mult, op1=ALU.mult),
                                reads=[("usb", b), ("sdb", b), "dc0", "cpack"], writes=[("qk", kind, h, tb)])

                        import os as _os
                        _cut = _os.environ.get("K_CUT", "")
                        if _cut.startswith("qkfront"):
                            proj_fm(pu[0], ("pu", 0), 0, 128, 0)
                            if "a" in _cut[7:]:
                                S.op("act", lambda: A.activation(out=sqb[0][:], in_=pu[0][:], func=AF.Square),
                                     reads=[("pu", 0)], writes=[("sqb", 0)])
                            if "d" in _cut[7:]:
                                S.op("dve", lambda: V.tensor_copy(out=usb[0][:], in_=pu[0][:]),
                                     reads=[("pu", 0)], writes=[("usb", 0)])
                            units = []
                        if _cut == "dmaonly":
                            units = []
                        if _cut == "mmonly":
                            proj_fm(pu[0], ("pu", 0), 0, 128, 0)
                            units = []
                        if _cut == "qk1":
                            units = units[:1]
                        for i in range(len(units)):
                            qk_front(i)
                            if i > 0:
                                qk_back(i - 1)
                        if units:
                            qk_back(len(units) - 1)
                        for tt in range(NT if _cut in ("", "v", "lora") else 0):
                            b = tt % 2
                            for dci in range(8):
                                S.op("pe", lambda dci=dci: PE_.matmul(pu[b][:], lhsT=xnT[:, dci, tt * 128:(tt + 1) * 128],
                                                                       rhs=wA[:, dci, 1024:1536],
                                                                       start=(dci == 0), stop=(dci == 7)),
                                     reads=["wA", ("xnT", tt)], writes=[("pu", b)])
                            src = pu[b][:].rearrange("p (h d) -> p h d", h=4)
                            if tt % 2:
                                S.op("act", lambda: A.activation(out=vaug[:, tt, :, 0:128], in_=src, func=AF.Copy),
                                     reads=[("pu", b)], writes=[("vaug", tt)])
                            else:
                                S.op("dve", lambda: V.tensor_copy(out=vaug[:, tt, :, 0:128], in_=src),
                                     reads=[("pu", b)], writes=[("vaug", tt)])
                        ush = sb(st, "ush", [128, SEQ], F32)
                        utmp = sb(st, "utmp", [128, SEQ], F32)
                        umix = sb(st, "umix", [128, SEQ], F32)
                        for ci, (c0, m, jc, func, dst) in enumerate((
                                (1536, 128, 12, AF.Tanh, wlT), (1664, 128, 13, AF.Copy, alT),
                                (1792, 128, 14, AF.Sigmoid, glT), (1920, 32, 15, AF.Sigmoid, gl2T))[:(4 if _cut in ("", "lora") else 0)]):
                            for tb in range(4):
                                b = tb % 2
                                proj_fm(pu[b], ("pu", b), c0, m, tb)
                                if tb % 2:
                                    S.op("act", lambda: A.activation(out=ush[:m, tb * 512:(tb + 1) * 512], in_=pu[b][:m, :],
                                                                     func=AF.Copy),
                                         reads=[("pu", b)], writes=["ush"])
                                else:
                                    S.op("dve", lambda: V.tensor_copy(out=ush[:m, tb * 512:(tb + 1) * 512], in_=pu[b][:m, :]),
                                         reads=[("pu", b)], writes=["ush"])
                            shiftmix("ush", ush, m, jc, "umix", umix, utmp, "utmp")
                            S.op("act", lambda: A.activation(out=dst[:m, :], in_=umix[:m, :], func=func),
                                 reads=["umix"], writes=[("lora_in", ci)])
                        S.barrier()
                    tap("qT", qT[:], [])
                    tap("kT", kT[:], [])
                    tap("vaug", vaug[:], [])
                    tap("wlT", wlT[:], [])
                    tap("glT", glT[:], [])
                    if stop_after == "p2a":
                        S.barrier()
                        continue

                    with ExitStack() as st:
                        strip = sb(st, "strip", [128, 4, STRIP_W], F32)
                        for i in range(128):
                            src = bass.AP(gscr_t, 127 - i, [[0, 1], [ND, 4], [1, STRIP_W]])
                            S.dma("sp", strip[i:i + 1, :, :], src, "c4", reads=["gscr"], writes=["strip"])
                        PT = [sb(st, "PT%d" % i, [128, NT, 512], BF16) for i in range(2)]
                        tmpb = [sb(st, "tmpb%d" % i, [128, 512], F32) for i in range(2)]
                        spp = [ps(st, "spp%d" % i, [128, 512]) for i in range(2)]
                        av = ps(st, "av", [128, 4, 512])
                        ptr = ps(st, "ptr", [128, 1024], BF16)
                        rin = sb(st, "rin", [128, 4, 2, 1], F32)
                        nl = sb(st, "nl", [128, 2, 2, 1], F32)
                        o0 = sb(st, "o0", [128, 128], F32)
                        osb = sb(st, "osb", [128, 4, 128], F32)
                        onb = sb(st, "onb", [128, 4, 128], BF16)
                        junk2 = sb(st, "junk2", [128, 128], BF16)
                        ss4 = sb(st, "ss4", [128, 8], F32)
                        aunits = [(h, qt, sub) for h in range(4) for qt in range(4) for sub in range(2)]
                        cnt = [0]

                        def qk_exp(u):
                            h, qt, sub = u
                            for kt in range(NT):
                                i = cnt[0]
                                cnt[0] += 1
                                b = i % 2
                                S.op("pe", lambda: PE_.matmul(spp[b][:], lhsT=kT[64 * sub:64 * sub + 64, h, kt * 128:(kt + 1) * 128],
                                                              rhs=qT[64 * sub:64 * sub + 64, h, qt * 512:(qt + 1) * 512],
                                                              start=True, stop=True),
                                     reads=[], writes=[("spp", b)])
                                Dk = kt * 128 - qt * 512
                                if -255 < Dk < 639:
                                    off = 512 - Dk
                                    S.op("dve", lambda: V.tensor_tensor(out=tmpb[b][:], in0=spp[b][:],
                                                                        in1=strip[:, h, off:off + 512], op=ALU.add),
                                         reads=[("spp", b), "strip"], writes=[("tmpb", b)])
                                    S.op("act", lambda: A.activation(out=PT[sub][:, kt, :], in_=tmpb[b][:], func=AF.Exp),
                                         reads=[("tmpb", b)], writes=[("PT", sub, kt)])
                                else:
                                    which = 1 if Dk > 0 else 0
                                    S.op("act", lambda: A.activation(out=PT[sub][:, kt, :], in_=spp[b][:], func=AF.Exp,
                                                                     bias=C("rb_far", h * 2 + which)),
                                         reads=[("spp", b)], writes=[("PT", sub, kt)])
                                yield

                        def accap(sub, qs, lo, hi):
                            return av[:, sub * 2 + qs // 2, (qs % 2) * 256 + lo:(qs % 2) * 256 + hi]

                        def av_mm(u):
                            h, qt, sub = u
                            for qs in range(4):
                                for kt in range(NT):
                                    S.op("pe", lambda: PE_.matmul(accap(sub, qs, 0, 129),
                                                                  lhsT=PT[sub][:, kt, qs * 128:(qs + 1) * 128],
                                                                  rhs=vaug[:, kt, h, 0:129],
                                                                  start=(kt == 0), stop=(kt == NT - 1)),
                                         reads=[("PT", sub, kt)], writes=[("av", sub)])
                                    if kt % 4 == 3:
                                        yield

                        def post_a(h, qt):
                            av4 = av[:].rearrange("p b (j w) -> p b j w", j=2)
                            S.op("dve", lambda: V.reciprocal(out=rin[:], in_=av4[:, :, :, 128:129]),
                                 reads=[("av", 0), ("av", 1)], writes=["rin"])
                            S.op("dve", lambda: V.tensor_scalar(out=nl[:], in0=rin[:, 2:4, :, :], scalar1=NLAM, scalar2=None,
                                                                op0=ALU.mult), reads=["rin", "dc2"], writes=["nl"])
                            for qs in range(4):
                                S.op("dve", lambda: V.tensor_scalar(out=o0[:], in0=accap(0, qs, 0, 128),
                                                                    scalar1=rin[:, qs // 2, qs % 2, :], scalar2=None,
                                                                    op0=ALU.mult),
                                     reads=[("av", 0), "rin"], writes=["o0"])
                                S.op("dve", lambda: V.scalar_tensor_tensor(out=osb[:, qs, :], in0=accap(1, qs, 0, 128),
                                                                           scalar=nl[:, qs // 2, qs % 2, :], in1=o0[:],
                                                                           op0=ALU.mult, op1=ALU.add),
                                     reads=[("av", 1), "nl", "o0"], writes=[("osb", qs)])

                        def post_b(h, qt):
                            for qs in range(4):
                                S.op("act", lambda: A.activation(out=junk2[:], in_=osb[:, qs, :], func=AF.Square,
                                                                 accum_out=ss4[:, qs:qs + 1]),
                                     reads=[("osb", qs)], writes=["junk2", ("ss4", qs)], fuse=False)
                                yield
                            S.op("act", lambda: A.activation(out=ss4[:, 4:8], in_=ss4[:, 0:4], func=AF.Sqrt, bias=EPS,
                                                             scale=1.0 / 128),
                                 reads=[("ss4", q_) for q_ in range(4)] + ["kc"], writes=["ss4b"])
                            yield
                            S.op("dve", lambda: V.reciprocal(out=ss4[:, 4:8], in_=ss4[:, 4:8]), reads=["ss4b"], writes=["ss4b"])
                            yield
                            for qs in range(4):
                                S.op("dve", lambda: V.tensor_scalar(out=onb[:, qs, :], in0=osb[:, qs, :],
                                                                    scalar1=ss4[:, 4 + qs:5 + qs], scalar2=1.0 - LAM_INIT,
                                                                    op0=ALU.mult, op1=ALU.mult),
                                     reads=[("osb", qs), "ss4b"], writes=[("onb", qs)])
                                S.op("pe", lambda: PE_.transpose(out=ptr[:, qs * 128:(qs + 1) * 128], in_=onb[:, qs, :],
                                                                 identity=ident_b[:]),
                                     reads=[("onb", qs), "ident_b"], writes=["ptr"])
                                yield
                            S.op("act", lambda: A.activation(out=o_daT[:, h, qt * 512:(qt + 1) * 512], in_=ptr[:, 0:512],
                                                             func=AF.Copy, scale=C("subln_g")),
                                 reads=["ptr", "cpack"], writes=[("o_daT", h, qt)])

                        n_u = len(aunits)
                        if _os.environ.get("K_SKIP_ATTN"):
                            n_u = 0
                        def rr(gens):
                            gens = [g for g in gens if g is not None]
                            while gens:
                                nxt = []
                                for g in gens:
                                    try:
                                        next(g)
                                        nxt.append(g)
                                    except StopIteration:
                                        pass
                                gens = nxt

                        pend = None
                        if n_u:
                            rr([qk_exp(aunits[0])])
                        for i in range(1, n_u):
                            rr([qk_exp(aunits[i]), av_mm(aunits[i - 1]), pend])
                            pend = None
                            if aunits[i - 1][2] == 1:
                                post_a(aunits[i - 1][0], aunits[i - 1][1])
                                pend = post_b(aunits[i - 1][0], aunits[i - 1][1])
                        if n_u:
                            rr([av_mm(aunits[-1]), pend])
                            post_a(aunits[-1][0], aunits[-1][1])
                            rr([post_b(aunits[-1][0], aunits[-1][1])])
                        S.barrier()
                tap("o_daT", o_daT[:], [])
                if stop_after == "attn":
                    S.barrier()
                    continue

                with ExitStack() as rw:
                    w2c = sb(rw, "w2c", [128, 512], BF16)
                    a2c = sb(rw, "a2c", [128, 512], BF16)
                    g2a = sb(rw, "g2a", [128, 512], BF16)
                    g2b = sb(rw, "g2b", [32, 512], BF16)
                    S.dma("pool", w2c[:], w2cat_d, "rwc", writes=["w2c"])
                    S.dma("pool", a2c[:], a2cat_d, "rwc", writes=["a2c"])
                    S.dma("pool", g2a[:], g2_d[0:128, :], "rwc", writes=["g2a"])
                    S.dma("pool", g2b[:], g2_d[128:160, :], "rwc", writes=["g2b"])
                    maskAB = sb(rw, "maskAB", [64, 4, 128], BF16)
                    maskN = sb(rw, "maskN", [64, 4, 64], BF16)
                    ident4 = sb(rw, "ident4", [64, 4, 64], BF16)
                    resetm = sb(rw, "resetm", [128, 256], F32)
                    bd_f = sb(rw, "bd_f", [128, 128], F32)
                    with ExitStack() as st:
                        rwm = sb(st, "rwm", [64, 384], F32)
                        S.dma("sp", rwm[:], rwmask_d, "rwc2", writes=["rwm"])
                        for inst in range(4):
                            d = inst % 2
                            S.op("dve", lambda: V.tensor_copy(out=maskAB[:, inst, :], in_=rwm[:, d * 192:d * 192 + 128]),
                                 reads=["rwm"], writes=["maskAB"])
                            S.op("dve", lambda: V.tensor_copy(out=maskN[:, inst, :], in_=rwm[:, d * 192 + 128:d * 192 + 192]),
                                 reads=["rwm"], writes=["maskN"])
                            S.op("dve", lambda: V.tensor_copy(out=ident4[:, inst, :], in_=ident_b[0:64, 0:64]),
                                 reads=["ident_b"], writes=["ident4"])
                        S.op("dve", lambda: V.memset(resetm[:], 1.0), writes=["resetm"])
                        S.op("dve", lambda: V.memset(resetm[:].rearrange("p (c l) -> p c l", l=64)[:, :, 0:1], 0.0),
                             reads=["resetm"], writes=["resetm"])
                        S.op("dve", lambda: V.memset(bd_f[:], 0.0), writes=["bd_f"])
                        S.op("dve", lambda: V.memset(bd_f[0:64, 0:64], 1.0), reads=["bd_f"], writes=["bd_f"])
                        S.op("dve", lambda: V.memset(bd_f[64:128, 64:128], 1.0), reads=["bd_f"], writes=["bd_f"])
                        S.barrier()

                    TB = 256
                    NB = SEQ // TB
                    CPB = TB // 64

                    def c3(ap):
                        return ap.rearrange("p (c l) -> p c l", l=64)

                    for j in range(4 if stop_after != "rw1" else 1):
                        with ExitStack() as pp:
                            AR = [sb(pp, "AR%d" % d, [128, 32, 2, 64], BF16) for d in range(2)]
                            bbar = [sb(pp, "bbar%d" % d, [128, SEQ], BF16) for d in range(2)]
                            kbar = [sb(pp, "kbar%d" % d, [128, SEQ], BF16) for d in range(2)]
                            vb = sb(pp, "vb", [128, SEQ], BF16)
                            rhi = [sb(pp, "rhi%d" % d, [64, 32, 64], BF16) for d in range(2)]
                            PLf = [sb(pp, "PLf%d" % d, [128, 32], F32) for d in range(2)]
                            PLhi = [sb(pp, "PLhi%d" % d, [64, 32], F32) for d in range(2)]
                            bonus = sb(pp, "bonus", [128, SEQ], BF16)
                            wBj = sb(pp, "wBj", [128, 8, 384], BF16)
                            yacc = sb(pp, "yacc", [64, 2, SEQ], F32)
                            for i3 in range(3):
                                cc0 = RW0 + i3 * 512 + j * 128
                                S.dma("pool", wBj[:, :, i3 * 128:(i3 + 1) * 128],
                                      w_in_d[:, cc0:cc0 + 128].rearrange("(c p) n -> p c n", p=128), "wBj", writes=["wBj"])
                            with ExitStack() as s1:
                                rf = sb(s1, "rf", [128, SEQ], F32)
                                kf = sb(s1, "kf", [128, SEQ], F32)
                                vf = sb(s1, "vf", [128, SEQ], F32)
                                ush = sb(s1, "ush2", [128, SEQ], F32)
                                utmp = sb(s1, "utmp2", [128, SEQ], F32)
                                sqb1 = sb(s1, "sqb1", [128, TB], BF16)
                                nct = sb(s1, "nct", [128, 2, CPB], F32)
                                pu2 = [ps(s1, "pu2%d" % i, [128, 512]) for i in range(2)]
                                pl2a = ps(s1, "pl2a", [128, 2, TB])
                                pl2b = ps(s1, "pl2b", [128, 2, TB])
                                pl2 = [pl2a[:, 0, :], pl2b[:, 0, :], pl2a[:, 1, :], pl2b[:, 1, :]]
                                pst_t = ps(s1, "pst", [128, 512])
                                pbn_t = ps(s1, "pbn", [128, 512])
                                pst = pst_t[:, 0:TB]
                                pbn = pbn_t[:, 0:TB]
                                for i3, (dst, dk) in enumerate(((rf, "rf"), (kf, "kf"), (vf, "vf"))):
                                    for tb in range(4):
                                        b = tb % 2
                                        for dci in range(8):
                                            S.op("pe", lambda dci=dci: PE_.matmul(
                                                pu2[b][:], lhsT=wBj[:, dci, i3 * 128:(i3 + 1) * 128],
                                                rhs=xnT[:, dci, tb * 512:(tb + 1) * 512], start=(dci == 0), stop=(dci == 7)),
                                                reads=["wBj"] + XNT_ALL[tb * 4:tb * 4 + 4], writes=[("pu2", b)])
                                        if tb % 2:
                                            S.op("act", lambda: A.activation(out=ush[:, tb * 512:(tb + 1) * 512], in_=pu2[b][:],
                                                                             func=AF.Copy),
                                                 reads=[("pu2", b)], writes=["ush2"])
                                        else:
                                            S.op("dve", lambda: V.tensor_copy(out=ush[:, tb * 512:(tb + 1) * 512], in_=pu2[b][:]),
                                                 reads=[("pu2", b)], writes=["ush2"])
                                    shiftmix("ush2", ush, 128, i3 * 4 + j, dk, dst, utmp, "utmp2")
                                S.op("act", lambda: A.activation(out=vb[:], in_=vf[:], func=AF.Copy), reads=["vf"], writes=["vb"])
                                S.barrier()
                                slots = [ush[:, i * TB:(i + 1) * TB] for i in range(8)] + [utmp[:, i * TB:(i + 1) * TB] for i in range(8)]
                                (t_sig0, t_sig1, t_a0, t_a1, t_kk, t_x, t_y, t_kd, t_be, t_cs, t_e1, t_e2, t_e3, t_e4, t_ks, t_z) = slots
                                t_sig = (t_sig0, t_sig1)
                                t_a = (t_a0, t_a1)
                                for tb in range(NB):
                                    sl = slice(tb * TB, (tb + 1) * TB)
                                    csl = slice(tb * CPB, (tb + 1) * CPB)
                                    for d in range(2):
                                        S.op("pe", lambda: PE_.matmul(pl2[d], lhsT=w2c[64 * d:64 * d + 64, j * 128:(j + 1) * 128],
                                                                      rhs=wlT[64 * d:64 * d + 64, sl], start=True, stop=True),
                                             reads=["w2c"], writes=[("pl2", d)])
                                        S.op("act", lambda: A.activation(out=t_sig[d], in_=pl2[d], func=AF.Sigmoid,
                                                                         bias=C("w0", d * 4 + j)),
                                             reads=[("pl2", d), "cpack"], writes=[("sig", d)])
                                        S.op("pe", lambda: PE_.matmul(pl2[2 + d], lhsT=a2c[64 * d:64 * d + 64, j * 128:(j + 1) * 128],
                                                                      rhs=alT[64 * d:64 * d + 64, sl], start=True, stop=True),
                                             reads=["a2c"], writes=[("pl2", d)])
                                        S.op("act", lambda: A.activation(out=t_a[d], in_=pl2[2 + d], func=AF.Sigmoid,
                                                                         bias=C("a0", d * 4 + j)),
                                             reads=[("pl2", d), "cpack"], writes=[("a", d)])
                                    S.op("dve", lambda: V.tensor_scalar(out=t_x, in0=kf[:, sl], scalar1=C("k_k", j), scalar2=None,
                                                                        op0=ALU.mult), reads=["kf", "cpack"], writes=["t_x"])
                                    S.op("act", lambda: A.activation(out=sqb1[:], in_=t_x, func=AF.Square),
                                         reads=["t_x"], writes=["sqb1"])
                                    S.op("pe", lambda: PE_.matmul(pst, lhsT=bd_b[:], rhs=sqb1[:], start=True, stop=True),
                                         reads=["sqb1", "bd_b"], writes=["pst"])
                                    S.op("act", lambda: A.activation(out=t_y, in_=pst, func=AF.Ln, bias=kc[:, 3:4]),
                                         reads=["pst", "kc"], writes=["t_y"])
                                    S.op("act", lambda: A.activation(out=t_y, in_=t_y, func=AF.Exp, scale=-0.5), reads=["t_y"], writes=["t_y"])
                                    S.op("dve", lambda: V.tensor_tensor(out=t_kk, in0=t_x, in1=t_y, op=ALU.mult),
                                         reads=["t_x", "t_y"], writes=["t_kk"])
                                    for d in range(2):
                                        S.op("dve", lambda: V.tensor_scalar(out=t_x, in0=t_a[d], scalar1=C("k_a", j),
                                                                            scalar2=dc[:, 35 + j:36 + j], op0=ALU.mult, op1=ALU.add),
                                             reads=[("a", d), "cpack", "dc_omka"], writes=["t_x"])
                                        S.op("dve", lambda: V.tensor_tensor(out=t_kd, in0=t_x, in1=kf[:, sl], op=ALU.mult),
                                             reads=["t_x", "kf"], writes=["t_kd"])
                                        S.op("dve", lambda: V.tensor_tensor(out=t_be, in0=t_kk, in1=t_a[d], op=ALU.mult),
                                             reads=["t_kk", ("a", d)], writes=["t_be"])
                                        S.op("dve", lambda: V.tensor_tensor_scan(out=t_cs, data0=resetm[:], data1=t_sig[d], initial=0.0,
                                                                                 op0=ALU.mult, op1=ALU.add),
                                             reads=["resetm", ("sig", d)], writes=["t_cs"])
                                        S.op("dve", lambda: V.tensor_tensor(out=t_sig[d], in0=t_cs, in1=t_sig[d], op=ALU.subtract),
                                             reads=["t_cs", ("sig", d)], writes=[("sig", d)])
                                        t_csm = t_sig[d]
                                        tot = c3(t_cs)[:, :, 63:64]
                                        S.op("dve", lambda: V.tensor_scalar(out=nct[:, 0, :].rearrange("p (c o) -> p c o", o=1), in0=tot,
                                                                            scalar1=-CDEC, scalar2=None, op0=ALU.mult),
                                             reads=["t_cs"], writes=["nct"])
                                        S.op("dve", lambda: V.tensor_scalar(out=nct[:, 1, :].rearrange("p (c o) -> p c o", o=1), in0=tot,
                                                                            scalar1=CDEC, scalar2=None, op0=ALU.mult),
                                             reads=["t_cs"], writes=["nct"])
                                        S.op("act", lambda: A.activation(out=PLf[d][:, csl], in_=nct[:, 0, :], func=AF.Exp),
                                             reads=["nct"], writes=[("PLf", d)])
                                        if d == 0:
                                            S.op("act", lambda: A.activation(out=t_e1, in_=t_cs, func=AF.Exp, scale=-CDEC),
                                                 reads=["t_cs"], writes=["t_e1"])
                                            S.op("act", lambda: A.activation(out=t_e2, in_=t_cs, func=AF.Exp, scale=CDEC),
                                                 reads=["t_cs"], writes=["t_e2"])
                                            S.op("act", lambda: A.activation(out=t_e3, in_=t_csm, func=AF.Exp, scale=-CDEC),
                                                 reads=[("sig", d)], writes=["t_e3"])
                                            e_r, e_bk, e_a = t_e1, t_e2, t_e3
                                        else:
                                            for c8 in range(CPB):
                                                cs8 = slice(c8 * 64, (c8 + 1) * 64)
                                                S.op("act", lambda: A.activation(out=t_e1[:, cs8], in_=t_csm[:, cs8], func=AF.Exp,
                                                                                 scale=CDEC, bias=nct[:, 0, c8:c8 + 1]),
                                                     reads=[("sig", d), "nct"], writes=["t_e1"])
                                                S.op("act", lambda: A.activation(out=t_e2[:, cs8], in_=t_csm[:, cs8], func=AF.Exp,
                                                                                 scale=-CDEC, bias=nct[:, 1, c8:c8 + 1]),
                                                     reads=[("sig", d), "nct"], writes=["t_e2"])
                                                S.op("act", lambda: A.activation(out=t_e4[:, cs8], in_=t_cs[:, cs8], func=AF.Exp,
                                                                                 scale=CDEC, bias=nct[:, 0, c8:c8 + 1]),
                                                     reads=["t_cs", "nct"], writes=["t_e4"])
                                            e_r, e_bk, e_a = t_e1, t_e2, t_e4
                                        S.op("dve", lambda: V.tensor_tensor(out=AR[d][:, csl, 1, :], in0=c3(rf[:, sl]), in1=c3(e_r),
                                                                            op=ALU.mult),
                                             reads=["rf", "t_e1"], writes=[("AR", d, tb)])
                                        S.op("dve", lambda: V.scalar_tensor_tensor(out=AR[d][:, csl, 0, :], in0=c3(t_kk), scalar=-1.0,
                                                                                   in1=c3(e_a), op0=ALU.mult, op1=ALU.mult),
                                             reads=["t_kk", "t_e3", "t_e4"], writes=[("AR", d, tb)])
                                        S.op("dve", lambda: V.tensor_tensor(out=bbar[d][:, sl], in0=t_be, in1=e_bk, op=ALU.mult),
                                             reads=["t_be", "t_e2"], writes=[("bbar", d, tb)])
                                        S.op("dve", lambda: V.tensor_tensor(out=kbar[d][:, sl], in0=t_kd, in1=e_bk, op=ALU.mult),
                                             reads=["t_kd", "t_e2"], writes=[("kbar", d, tb)])
                                        if d == 0:
                                            S.op("dve", lambda: V.tensor_copy(out=t_ks, in_=t_kd), reads=["t_kd"], writes=["t_ks"])
                                        else:
                                            S.op("dve", lambda: V.tensor_tensor(out=t_ks, in0=t_ks, in1=t_kd, op=ALU.add),
                                                 reads=["t_kd", "t_ks"], writes=["t_ks"])
                                    S.op("dve", lambda: V.scalar_tensor_tensor(out=t_z, in0=rf[:, sl], scalar=C("r_k", j), in1=t_ks,
                                                                               op0=ALU.mult, op1=ALU.mult),
                                         reads=["rf", "t_ks", "cpack"], writes=["t_z"])
                                    S.op("pe", lambda: PE_.matmul(pbn, lhsT=bd_f[:], rhs=t_z, start=True, stop=True),
                                         reads=["t_z", "bd_f"], writes=["pbn"])
                                    S.op("dve", lambda: V.tensor_tensor(out=bonus[:, sl], in0=pbn, in1=vf[:, sl], op=ALU.mult),
                                         reads=["pbn", "vf"], writes=[("bonus", tb)])
                                for d in range(2):
                                    S.dma("sp", rhi[d][:], AR[d][64:128, :, 1, :], "rhi",
                                          reads=[("AR", d, tb) for tb in range(NB)], writes=[("rhi", d)])
                                    S.dma("sp", PLhi[d][:], PLf[d][64:128, :], "rhi", reads=[("PLf", d)], writes=[("PLhi", d)])
                                S.barrier()
                            tap("AR0", AR[0][:], [])
                            tap("AR1", AR[1][:], [])
                            tap("bbar0", bbar[0][:], [])
                            tap("kbar1", kbar[1][:], [])
                            tap("bonus", bonus[:], [])
                            tap("PLf0", PLf[0][:], [])
                            if stop_after == "rw_s1":
                                S.barrier()
                                continue
                            with ExitStack() as s2:
                                tok = [sb(s2, "tok%d" % i, [64, 8, 128], BF16) for i in range(2)]
                                btl = [sb(s2, "btl%d" % i, [128, 2, 2, 64], BF16) for i in range(2)]
                                AbR = [sb(s2, "AbR%d" % i, [64, 4, 128], BF16) for i in range(2)]
                                AkR = [sb(s2, "AkR%d" % i, [64, 4, 128], BF16) for i in range(2)]
                                MNq = [sb(s2, "MNq%d" % i, [64, 2, 4, 64], BF16) for i in range(2)]
                                Rq = [sb(s2, "Rq%d" % i, [64, 4, 64], BF16) for i in range(2)]
                                N0 = sb(s2, "N0", [64, 4, 64], BF16)
                                WuT = [sb(s2, "WuT%d" % i, [64, 4, 64], BF16) for i in range(2)]
                                AVs = sb(s2, "AVs", [64, 4, 64], BF16)
                                Uv = [sb(s2, "Uv%d" % i, [64, 4, 64], F32) for i in range(2)]
                                Us = sb(s2, "Us", [64, 4, 64], BF16)
                                Sf = sb(s2, "Sf", [64, 4, 64], F32)
                                Sb_ = sb(s2, "Sb", [64, 4, 64], BF16)
                                ptok = ps(s2, "ptok", [64, 1024], BF16)
                                pAB = [ps(s2, "pAB%d" % i, [64, 2, 2, 128]) for i in range(2)]
                                pMN = ps(s2, "pMN", [64, 2, 4, 64])
                                pR0 = ps(s2, "pR0", [64, 2, 4, 64])
                                pWA = ps(s2, "pWA", [64, 2, 4, 64])
                                pUU = ps(s2, "pUU", [64, 2, 4, 64])
                                pYS = ps(s2, "pYS", [64, 2, 4, 64])
                                S.op("dve", lambda: V.memset(Sf[:], 0.0), writes=["Sf"])
                                S.op("dve", lambda: V.memset(Sb_[:], 0.0), writes=["Sb"])
                                def gen_pre(i):
                                    b = i % 2
                                    cd = (i, 31 - i)
                                    tq = tok[b]
                                    for d in range(2):
                                        c = cd[d]
                                        for q, src in enumerate((bbar[d], kbar[d])):
                                            S.op("dve", lambda: V.tensor_scalar(out=btl[b][:, d, q, :], in0=src[:, c * 64:(c + 1) * 64],
                                                                                scalar1=PLf[d][:, c:c + 1], scalar2=None, op0=ALU.mult),
                                                 reads=[("PLf", d)], writes=[("btl", b)])
                                    for inst in range(4):
                                        hh, d = inst // 2, inst % 2
                                        c = cd[d]
                                        p0 = 64 * hh
                                        lb = bbar[d][p0:p0 + 64, c * 64:(c + 1) * 64]
                                        lk = kbar[d][p0:p0 + 64, c * 64:(c + 1) * 64]
                                        rAR = AR[d][p0:p0 + 64, c, :, :].rearrange("p a b -> p (a b)")
                                        pn0 = pR0[:, 1, inst, :] if hh == 0 else pMN[:, 1, inst, :]
                                        S.op("pe", lambda: PE_.matmul(pAB[hh][:, d, 0, :], lhsT=lb, rhs=rAR, start=True, stop=True),
                                             reads=[], writes=[("pAB", hh)])
                                        S.op("pe", lambda: PE_.matmul(pn0, lhsT=AR[d][p0:p0 + 64, c, 0, :], rhs=lb,
                                                                      start=True, stop=True),
                                             reads=[], writes=["pR0" if hh == 0 else "pMN"])
                                        S.op("pe", lambda: PE_.matmul(pAB[hh][:, d, 1, :], lhsT=lk, rhs=rAR, start=True, stop=True),
                                             reads=[], writes=[("pAB", hh)])
                                    for hh in range(2):
                                        hi = slice(2 * hh, 2 * hh + 2)
                                        S.op("dve", lambda: V.tensor_tensor(out=AbR[b][:, hi, :], in0=pAB[hh][:, :, 0, :], in1=maskAB[:, hi, :],
                                                                            op=ALU.mult),
                                             reads=[("pAB", hh), "maskAB"], writes=[("AbR", b)])
                                    S.op("dve", lambda: V.tensor_tensor(out=N0[:, 0:2, :], in0=pR0[:, 1, 0:2, :], in1=maskN[:, 0:2, :], op=ALU.mult),
                                         reads=["pR0", "maskN"], writes=["N0"])
                                    S.op("dve", lambda: V.tensor_tensor(out=N0[:, 2:4, :], in0=pMN[:, 1, 2:4, :], in1=maskN[:, 2:4, :], op=ALU.mult),
                                         reads=["pMN", "maskN"], writes=["N0"])
                                    yield
                                    for d in range(2):
                                        c = cd[d]
                                        srcs = (AR[d][:, c, 0, :], btl[b][:, d, 0, :], btl[b][:, d, 1, :])
                                        for q, src in enumerate(srcs):
                                            S.op("pe", lambda: PE_.transpose(out=ptok[:, (d * 3 + q) * 128:(d * 3 + q + 1) * 128], in_=src,
                                                                             identity=ident_b[:]),
                                                 reads=[("btl", b), "ident_b"], writes=["ptok"])
                                        S.op("pe", lambda: PE_.transpose(out=ptok[:, (6 + d) * 128:(7 + d) * 128],
                                                                         in_=vb[:, c * 64:(c + 1) * 64], identity=ident_b[:]),
                                             reads=["vb", "ident_b"], writes=["ptok"])
                                    S.op("act", lambda: A.activation(out=tq[:].rearrange("p a b -> p (a b)"), in_=ptok[:], func=AF.Copy),
                                         reads=["ptok"], writes=[("tok", b)])
                                    yield

                                    def mn_mm(l_, Mp_, Np_, rk_):
                                        for inst in range(4):
                                            if l_ < 5:
                                                S.op("pe", lambda: PE_.matmul(pMN[:, 0, inst, :], lhsT=Np_[:, inst, :], rhs=Mp_[:, inst, :],
                                                                              start=True, stop=True),
                                                     reads=rk_, writes=["pMN"])
                                            S.op("pe", lambda: PE_.matmul(pWA[:, 0, inst, :], lhsT=Mp_[:, inst, :], rhs=Np_[:, inst, :],
                                                                          start=True, stop=True),
                                                 reads=rk_, writes=["pWA"])

                                    def mn_copy(l_):
                                        q_ = l_ % 2
                                        if l_ < 5:
                                            S.op("act", lambda: A.activation(out=MNq[q_][:, 0, :, :], in_=pMN[:, 0, :, :], func=AF.Copy),
                                                 reads=["pMN"], writes=[("Mq", q_)])
                                        S.op("dve", lambda: V.tensor_copy(out=MNq[q_][:, 1, :, :], in_=pWA[:, 0, :, :]),
                                             reads=["pWA"], writes=[("Nq", q_)])

                                    mn_mm(1, AbR[b][:, :, 0:64], N0[:], [("AbR", b), "N0"])
                                    mn_copy(1)
                                    S.op("dve", lambda: V.tensor_tensor(out=Rq[0][:], in0=AbR[b][:, :, 0:64], in1=ident4[:], op=ALU.add),
                                         reads=[("AbR", b), "ident4"], writes=[("Rq", 0)])
                                    for hh in range(2):
                                        hi = slice(2 * hh, 2 * hh + 2)
                                        S.op("dve", lambda: V.tensor_tensor(out=AkR[b][:, hi, :], in0=pAB[hh][:, :, 1, :], in1=maskAB[:, hi, :],
                                                                            op=ALU.mult),
                                             reads=[("pAB", hh), "maskAB"], writes=[("AkR", b)])
                                    yield
                                    for l in range(1, 6):
                                        lb_ = l % 2
                                        MK = [("Mq", lb_), ("Nq", lb_)]
                                        if l < 5:
                                            mn_mm(l + 1, MNq[lb_][:, 0, :, :], MNq[lb_][:, 1, :, :], MK)
                                        for inst in range(4):
                                            S.op("pe", lambda: PE_.matmul(pR0[:, 0, inst, :], lhsT=MNq[lb_][:, 1, inst, :],
                                                                          rhs=Rq[1 - lb_][:, inst, :], start=True, stop=True),
                                                 reads=[("Nq", lb_), ("Rq", 1 - lb_)], writes=["pR0"])
                                        if l == 1:
                                            for inst in range(4):
                                                hh, d = inst // 2, inst % 2
                                                hs = slice(hh * 64, hh * 64 + 64)
                                                S.op("pe", lambda: PE_.matmul(pUU[:, 0, inst, :], lhsT=AkR[b][:, inst, 0:64], rhs=tq[:, 6 + d, hs],
                                                                              start=True, stop=True),
                                                     reads=[("tok", b), ("AkR", b)], writes=["pUU"])
                                        if l < 5:
                                            mn_copy(l + 1)
                                        S.op("dve", lambda: V.tensor_tensor(out=Rq[lb_][:], in0=pR0[:, 0, :, :], in1=Rq[1 - lb_][:],
                                                                            op=ALU.add),
                                             reads=["pR0", ("Rq", 1 - lb_)], writes=[("Rq", lb_)])
                                        if l == 1:
                                            S.op("dve", lambda: V.tensor_copy(out=AVs[:], in_=pUU[:, 0, :, :]), reads=["pUU"], writes=["AVs"])
                                        yield
                                    R = Rq[1]
                                    for inst in range(4):
                                        hh, d = inst // 2, inst % 2
                                        hs = slice(hh * 64, hh * 64 + 64)
                                        S.op("pe", lambda: PE_.matmul(pWA[:, 0, inst, :], lhsT=tq[:, d * 3, hs], rhs=R[:, inst, :],
                                                                      start=True, stop=True),
                                             reads=[("tok", b), ("Rq", 1)], writes=["pWA"])
                                        S.op("pe", lambda: PE_.matmul(pR0[:, 1, inst, :], lhsT=R[:, inst, :], rhs=AVs[:, inst, :],
                                                                      start=True, stop=True),
                                             reads=[("Rq", 1), "AVs"], writes=["pR0"])
                                    yield
                                    S.op("act", lambda: A.activation(out=WuT[b][:], in_=pWA[:, 0, :, :], func=AF.Copy),
                                         reads=["pWA"], writes=[("WuT", b)])
                                    S.op("dve", lambda: V.tensor_copy(out=Uv[b][:], in_=pR0[:, 1, :, :]),
                                         reads=["pR0"], writes=[("Uv", b)])
                                    yield

                                def gen_loop(i):
                                    b = i % 2
                                    cd = (i, 31 - i)
                                    tq = tok[b]
                                    for inst in range(4):
                                        yield
                                        S.op("pe", lambda: PE_.matmul(pUU[:, 1, inst, :], lhsT=WuT[b][:, inst, :], rhs=Sb_[:, inst, :],
                                                                      start=True, stop=True),
                                             reads=[("WuT", b), "Sb"], writes=["pUU"])
                                    yield
                                    S.op("dve", lambda: V.tensor_tensor(out=Us[:], in0=pUU[:, 1, :, :], in1=Uv[b][:], op=ALU.add),
                                         reads=["pUU", ("Uv", b)], writes=["Us"])
                                    yield
                                    for inst in range(4):
                                        hh, d = inst // 2, inst % 2
                                        c = cd[d]
                                        hs = slice(hh * 64, hh * 64 + 64)
                                        rr = AR[d][0:64, c, 1, :] if hh == 0 else rhi[d][:, c, :]
                                        yield
                                        S.op("pe", lambda: PE_.matmul(pYS[:, 0, inst, :], lhsT=Sb_[:, inst, :], rhs=rr, start=True, stop=False),
                                             reads=["Sb", ("rhi", d)], writes=["pYS"])
                                        yield
                                        S.op("pe", lambda: PE_.matmul(pYS[:, 0, inst, :], lhsT=tq[:, 6 + d, hs], rhs=AkR[b][:, inst, 64:128],
                                                                      start=False, stop=False),
                                             reads=[("tok", b), ("AkR", b)], writes=["pYS"])
                                        yield
                                        S.op("pe", lambda: PE_.matmul(pYS[:, 0, inst, :], lhsT=Us[:, inst, :], rhs=AbR[b][:, inst, 64:128],
                                                                      start=False, stop=True),
                                             reads=["Us", ("AbR", b)], writes=["pYS"])
                                        yield
                                        S.op("pe", lambda: PE_.matmul(pYS[:, 1, inst, :], lhsT=tq[:, d * 3 + 1, hs], rhs=Us[:, inst, :],
                                                                      start=True, stop=False),
                                             reads=[("tok", b), "Us"], writes=["pYS"])
                                        yield
                                        S.op("pe", lambda: PE_.matmul(pYS[:, 1, inst, :], lhsT=tq[:, d * 3 + 2, hs], rhs=tq[:, 6 + d, hs],
                                                                      start=False, stop=True),
                                             reads=[("tok", b)], writes=["pYS"])
                                    yield
                                    for d in range(2):
                                        c = cd[d]
                                        dst = yacc[:, :, c * 64:(c + 1) * 64]
                                        src = pYS[:, 0, :, :].rearrange("p (h e) w -> p h e w", e=2)[:, :, d, :]
                                        if i < 16:
                                            S.op("dve", lambda: V.tensor_copy(out=dst, in_=src),
                                                 reads=["pYS"], writes=[("yacc", c)])
                                        else:
                                            S.op("dve", lambda: V.tensor_tensor(out=dst, in0=src, in1=dst, op=ALU.add),
                                                 reads=["pYS", ("yacc", c)], writes=[("yacc", c)])
                                    yield
                                    for inst in range(4):
                                        hh, d = inst // 2, inst % 2
                                        c = cd[d]
                                        plc = PLf[d][0:64, c:c + 1] if hh == 0 else PLhi[d][:, c:c + 1]
                                        yield
                                        S.op("dve", lambda: V.scalar_tensor_tensor(out=Sf[:, inst, :], in0=Sf[:, inst, :], scalar=plc,
                                                                                   in1=pYS[:, 1, inst, :], op0=ALU.mult, op1=ALU.add),
                                             reads=["pYS", "Sf", ("PLhi", d), ("PLf", d)], writes=["Sf"])
                                    yield
                                    S.op("act", lambda: A.activation(out=Sb_[:], in_=Sf[:], func=AF.Copy), reads=["Sf"], writes=["Sb"])
                                    yield

                                def drive(ga, gb):
                                    a_done = b_done = False
                                    while not (a_done and b_done):
                                        if not a_done:
                                            try:
                                                next(ga)
                                            except StopIteration:
                                                a_done = True
                                        if not b_done:
                                            try:
                                                next(gb)
                                            except StopIteration:
                                                b_done = True

                                for _ in gen_pre(0):
                                    pass
                                for i in range(32):
                                    drive(gen_loop(i), gen_pre(i + 1) if i + 1 < 32 else iter(()))
                                S.barrier()
                            with ExitStack() as s3:
                                y128 = sb(s3, "y128", [128, SEQ], F32)
                                fa = sb(s3, "fa", [128, 512], F32)
                                fb = sb(s3, "fb", [128, 512], F32)
                                pm = ps(s3, "pm", [128, 512])
                                pv = ps(s3, "pv", [128, 512])
                                pg = ps(s3, "pg", [128, 512])
                                S.op("act", lambda: A.activation(out=y128[0:64, :], in_=yacc[:, 0, :], func=AF.Copy),
                                     reads=[], writes=["y128a"])
                                S.dma("sp", y128[64:128, :], yacc[:, 1, :], "ymv", reads=[], writes=["y128b"])
                                tap("y128", y128[:], ["y128a", "y128b"])
                                for tb in range(4):
                                    sl = slice(tb * 512, (tb + 1) * 512)
                                    S.op("pe", lambda: PE_.matmul(pm[:], lhsT=bd_f[:], rhs=y128[:, sl], start=True, stop=True),
                                         reads=["y128a", "y128b", "bd_f"], writes=["pm"])
                                    S.op("dve", lambda: V.scalar_tensor_tensor(out=fa[:], in0=pm[:], scalar=-1.0 / 64, in1=y128[:, sl],
                                                                               op0=ALU.mult, op1=ALU.add),
                                         reads=["pm", "y128a", "y128b"], writes=["fa"])
                                    S.op("act", lambda: A.activation(out=fb[:], in_=fa[:], func=AF.Square), reads=["fa"], writes=["fb"])
                                    S.op("pe", lambda: PE_.matmul(pv[:], lhsT=bd_f[:], rhs=fb[:], start=True, stop=True),
                                         reads=["fb", "bd_f"], writes=["pv"])
                                    S.op("act", lambda: A.activation(out=fb[:], in_=pv[:], func=AF.Ln, bias=EPSLN, scale=1.0 / 64),
                                         reads=["pv", "kc"], writes=["fb"])
                                    S.op("act", lambda: A.activation(out=fb[:], in_=fb[:], func=AF.Exp, scale=-0.5), reads=["fb"], writes=["fb"])
                                    S.op("dve", lambda: V.tensor_tensor(out=fa[:], in0=fa[:], in1=fb[:], op=ALU.mult),
                                         reads=["fa", "fb"], writes=["fa"])
                                    S.op("dve", lambda: V.tensor_scalar(out=fa[:], in0=fa[:], scalar1=C("lnx_g", j), scalar2=C("lnx_b", j),
                                                                        op0=ALU.mult, op1=ALU.add),
                                         reads=["fa", "cpack"], writes=["fa"])
                                    S.op("dve", lambda: V.tensor_tensor(out=fa[:], in0=fa[:], in1=bonus[:, sl], op=ALU.add),
                                         reads=["fa"], writes=["fa"])
                                    S.op("pe", lambda: PE_.matmul(pg[:], lhsT=g2a[:, j * 128:(j + 1) * 128], rhs=glT[:, sl],
                                                                  start=True, stop=False), reads=["g2a"], writes=["pg"])
                                    S.op("pe", lambda: PE_.matmul(pg[:], lhsT=g2b[0:32, j * 128:(j + 1) * 128], rhs=gl2T[0:32, sl],
                                                                  start=False, stop=True), reads=["g2b"], writes=["pg"])
                                    S.op("dve", lambda: V.tensor_tensor(out=o_rwT[:, j, sl], in0=fa[:], in1=pg[:], op=ALU.mult),
                                         reads=["fa", "pg"], writes=[("o_rwT", j, tb)])
                                S.barrier()
                    S.barrier()
                tap("o_rwT", o_rwT[:] if stop_after != "rw1" else o_rwT[:, 0:1, :], [])
                S.barrier()
                mixs.close()
                if stop_after in ("attn", "rw", "rw1", "rw_s1", "p2a", "p1"):
                    S.barrier()
                    continue
                h = sb(sq, "h", [128, NT, DM], F32)
                HK = [("h", tt) for tt in range(NT)]
                with ExitStack() as st:
                    wo = sb(st, "wo", [128, 8, DM], BF16)
                    S.dma("pool", wo[:], w_out_d.rearrange("(c p) n -> p c n", p=128), "wo", writes=["wo"])
                    xt2 = [sb(st, "xt2%d" % i, [128, DM], F32) for i in range(2)]
                    po = [[ps(st, "po%d%d" % (i, k), [128, 512]) for k in range(2)] for i in range(2)]
                    for tt in range(NT):
                        b = tt % 2
                        S.dma("sp", xt2[b][:], x_d[s, tt * 128:(tt + 1) * 128, :], ("xt2", b), writes=[("xt2", b)])
                        for dh in range(2):
                            for c in range(8):
                                src = o_daT if c < 4 else o_rwT
                                S.op("pe", lambda: PE_.matmul(po[b][dh][:], lhsT=src[:, c % 4, tt * 128:(tt + 1) * 128],
                                                              rhs=wo[:, c, dh * 512:(dh + 1) * 512], start=(c == 0), stop=(c == 7)),
                                     reads=["wo"], writes=[("po", b, dh)])
                            S.op("dve", lambda: V.tensor_tensor(out=h[:, tt, dh * 512:(dh + 1) * 512], in0=po[b][dh][:],
                                                                in1=xt2[b][:, dh * 512:(dh + 1) * 512], op=ALU.add),
                                 reads=[("po", b, dh), ("xt2", b)], writes=[("h", tt)])
                    S.barrier()
                tap("h1", h[:], [])
                if stop_after == "h1":
                    S.barrier()
                    continue

                def load_bp(st_, name):
                    o, n = bp_off[name]
                    t_ = sb(st_, "bp_" + name, [128, n], F32)
                    S.dma("sp", t_[:], bpack_d[:, o:o + n], "bpk", writes=["bp_" + name])
                    return t_

                with ExitStack() as me:
                    Xn = sb(me, "Xn", [128, NT, DM], BF16)
                    aff_tok = sb(me, "aff_tok", [128, NT, NE], F32)
                    aff_hl = sb(me, "aff_hl", [128, NT, NE, 2], BF16)
                    posm_tok = sb(me, "posm_tok", [128, NT, NE], F32)
                    posm_b = sb(me, "posm_b", [NE, SEQ], BF16)
                    Eoh = sb(me, "Eoh", [NE, NE, 128], BF16)
                    S.op("dve", lambda: V.memset(Eoh[:], 1.0), writes=["Eoh"])
                    for e in range(NE):
                        S.op("dve", lambda: V.tensor_scalar(out=Eoh[:, e, :], in0=Eoh[:, e, :], scalar1=ident_f[0:NE, e:e + 1],
                                                            scalar2=None, op0=ALU.mult), reads=["Eoh", "ident_f"], writes=["Eoh"])
                    with ExitStack() as st:
                        gffn = load_bp(st, "g_ffn")
                        wr_f = sb(st, "wr_f", [128, 8, NE], F32)
                        S.dma("sp", wr_f[:], wr_d.rearrange("(c p) e -> p c e", p=128), "wrf", writes=["wr_f"])
                        affT = sb(st, "affT", [NE, SEQ], F32)
                        wk = sb(st, "wk", [NE, SEQ], F32)
                        maskT = sb(st, "maskT", [NE, SEQ], F32)
                        cumT = sb(st, "cumT", [NE, SEQ], F32)
                        ones16 = sb(st, "ones16", [NE, SEQ], F32)
                        m8 = sb(st, "m8", [NE, 8], F32)
                        xnf = [sb(st, "xnf%d" % i, [128, DM], F32) for i in range(2)]
                        xnTf = [sb(st, "xnTf%d" % i, [128, 8, 128], F32) for i in range(2)]
                        junk3 = sb(st, "junk3", [128, DM], BF16)
                        sst = sb(st, "sst", [128, 3 * NT], F32)
                        sm = sb(st, "sm", [128, 4 * NT], F32)
                        ex = sb(st, "ex", [128, NE], F32)
                        dtmp = sb(st, "dtmp", [128, NE], F32)
                        pT = [ps(st, "pT%d" % i, [128, 512]) for i in range(2)]
                        plg = ps(st, "plg", [128, 512])
                        paT = ps(st, "paT", [NE, 512])
                        ppm = ps(st, "ppm", [128, 512])
                        S.op("pool", lambda: G.memset(ones16[:], 1.0), writes=["ones16"])
                        for tt in range(NT):
                            b = tt % 2
                            S.op("act", lambda: A.activation(out=junk3[:], in_=h[:, tt, :], func=AF.Square,
                                                             accum_out=sst[:, tt:tt + 1]),
                                 reads=[("h", tt)], writes=["junk3", ("sst", tt)], fuse=False)
                            S.op("act", lambda: A.activation(out=sst[:, NT + tt:NT + tt + 1], in_=sst[:, tt:tt + 1], func=AF.Sqrt,
                                                             bias=EPS, scale=1.0 / DM), reads=[("sst", tt), "kc"], writes=[("sst1", tt)])
                            S.op("dve", lambda: V.reciprocal(out=sst[:, 2 * NT + tt:2 * NT + tt + 1], in_=sst[:, NT + tt:NT + tt + 1]),
                                 reads=[("sst1", tt)], writes=[("sst2", tt)])
                            S.op("dve", lambda: V.scalar_tensor_tensor(out=xnf[b][:], in0=h[:, tt, :],
                                                                       scalar=sst[:, 2 * NT + tt:2 * NT + tt + 1], in1=gffn[:],
                                                                       op0=ALU.mult, op1=ALU.mult),
                                 reads=[("h", tt), ("sst2", tt), "bp_g_ffn"], writes=[("xnf", b)])
                            S.op("act", lambda: A.activation(out=Xn[:, tt, :], in_=xnf[b][:], func=AF.Copy),
                                 reads=[("xnf", b)], writes=[("Xn", tt)])
                            for c in range(8):
                                S.op("pe", lambda: PE_.transpose(out=pT[c // 4][:, (c % 4) * 128:(c % 4 + 1) * 128],
                                                                 in_=xnf[b][:, c * 128:(c + 1) * 128], identity=ident_f[:]),
                                     reads=[("xnf", b), "ident_f"], writes=[("pT", c // 4)])
                            S.op("dve", lambda: V.tensor_copy(out=xnTf[b][:, 0:4, :].rearrange("p a b -> p (a b)"), in_=pT[0][:]),
                                 reads=[("pT", 0)], writes=[("xnTf", b, 0)])
                            S.op("act", lambda: A.activation(out=xnTf[b][:, 4:8, :].rearrange("p a b -> p (a b)"), in_=pT[1][:],
                                                             func=AF.Copy), reads=[("pT", 1)], writes=[("xnTf", b, 1)])
                            for c in range(8):
                                S.op("pe", lambda: PE_.matmul(plg[:, 0:NE], lhsT=xnTf[b][:, c, :], rhs=wr_f[:, c, :],
                                                              start=(c == 0), stop=(c == 7)),
                                     reads=[("xnTf", b, 0), ("xnTf", b, 1), "wr_f"], writes=["plg"])
                            S.op("dve", lambda: V.reduce_max(out=sm[:, tt:tt + 1], in_=plg[:, 0:NE], axis=mybir.AxisListType.X),
                                 reads=["plg"], writes=[("sm0", tt)])
                            S.op("dve", lambda: V.tensor_scalar(out=sm[:, NT + tt:NT + tt + 1], in0=sm[:, tt:tt + 1], scalar1=-1.0,
                                                                scalar2=None, op0=ALU.mult), reads=[("sm0", tt)], writes=[("sm1", tt)])
                            S.op("dve", lambda: V.tensor_scalar(out=ex[:], in0=plg[:, 0:NE], scalar1=sm[:, NT + tt:NT + tt + 1],
                                                                scalar2=None, op0=ALU.add), reads=["plg", ("sm1", tt)], writes=["exa"])
                            S.op("act", lambda: A.activation(out=ex[:], in_=ex[:], func=AF.Exp,
                                                             accum_out=sm[:, 2 * NT + tt:2 * NT + tt + 1]),
                                 reads=["exa"], writes=["ex", ("sm2", tt)], fuse=False)
                            S.op("dve", lambda: V.reciprocal(out=sm[:, 3 * NT + tt:3 * NT + tt + 1], in_=sm[:, 2 * NT + tt:2 * NT + tt + 1]),
                                 reads=[("sm2", tt)], writes=[("sm3", tt)])
                            S.op("dve", lambda: V.tensor_scalar(out=aff_tok[:, tt, :], in0=ex[:], scalar1=sm[:, 3 * NT + tt:3 * NT + tt + 1],
                                                                scalar2=None, op0=ALU.mult), reads=["ex", ("sm3", tt)], writes=[("aff", tt)])
                            S.op("dve", lambda: V.tensor_copy(out=aff_hl[:, tt, :, 0], in_=aff_tok[:, tt, :]),
                                 reads=[("aff", tt)], writes=[("affh", tt)])
                            S.op("dve", lambda: V.tensor_tensor(out=dtmp[:], in0=aff_tok[:, tt, :], in1=aff_hl[:, tt, :, 0], op=ALU.subtract),
                                 reads=[("aff", tt), ("affh", tt)], writes=["dtmp"])
                            S.op("dve", lambda: V.tensor_copy(out=aff_hl[:, tt, :, 1], in_=dtmp[:]), reads=["dtmp"], writes=[("affl", tt)])
                            S.op("pe", lambda: PE_.transpose(out=paT[:, (tt % 4) * 128:(tt % 4 + 1) * 128], in_=aff_tok[:, tt, :],
                                                             identity=ident_f[:]), reads=[("aff", tt), "ident_f"], writes=["paT"])
                            if tt % 4 == 3:
                                t0 = (tt - 3) * 128
                                S.op("dve", lambda: V.tensor_copy(out=affT[:, t0:t0 + 512], in_=paT[:]), reads=["paT"], writes=["affT"])
                        S.op("dve", lambda: V.tensor_copy(out=wk[:], in_=affT[:]), reads=["affT"], writes=["wk"])
                        for r in range(CAP // 8):
                            S.op("dve", lambda: V.max(out=m8[:], in_=wk[:]), reads=["wk"], writes=["m8"], fuse=False)
                            if r < CAP // 8 - 1:
                                S.op("dve", lambda: V.match_replace(out=wk[:], in_to_replace=m8[:], in_values=wk[:], imm_value=-1.0),
                                     reads=["wk", "m8"], writes=["wk"], fuse=False)
                        S.op("dve", lambda: V.tensor_scalar(out=maskT[:], in0=affT[:], scalar1=m8[:, 7:8], scalar2=None, op0=ALU.is_ge),
                             reads=["affT", "m8"], writes=["maskT"])
                        S.op("dve", lambda: V.tensor_tensor_scan(out=cumT[:], data0=ones16[:], data1=maskT[:], initial=0.0,
                                                                 op0=ALU.mult, op1=ALU.add), reads=["ones16", "maskT"], writes=["cumT"])
                        S.op("dve", lambda: V.tensor_tensor(out=cumT[:], in0=cumT[:], in1=maskT[:], op=ALU.mult),
                             reads=["cumT", "maskT"], writes=["cumT"])
                        S.op("dve", lambda: V.tensor_scalar(out=cumT[:], in0=cumT[:], scalar1=-1.0, scalar2=None, op0=ALU.add),
                             reads=["cumT"], writes=["cumT"])
                        S.op("dve", lambda: V.tensor_copy(out=posm_b[:], in_=cumT[:]), reads=["cumT"], writes=["posm_b"])
                        for tt in range(NT):
                            S.op("pe", lambda: PE_.transpose(out=ppm[:, (tt % 16) * NE:(tt % 16 + 1) * NE], in_=cumT[:, tt * 128:(tt + 1) * 128],
                                                             identity=ident_f[0:NE, 0:NE]), reads=["cumT", "ident_f"], writes=["ppm"])
                        S.op("dve", lambda: V.tensor_copy(out=posm_tok[:].rearrange("p a b -> p (a b)"), in_=ppm[:, 0:NT * NE]),
                             reads=["ppm"], writes=["posm_tok"])
                        S.barrier()
                    tap("aff_tok", aff_tok[:], [])
                    tap("posm_tok", posm_tok[:], [])
                    if stop_after == "router":
                        S.barrier()
                        continue
                    with ExitStack() as st:
                        iotar = load_bp(st, "iota_row")
                        Sel = sb(st, "Sel", [128, NT, CAP], BF16)
                        SelT = sb(st, "SelT", [128, 2, SEQ], BF16)
                        XeT = sb(st, "XeT", [128, 8, CAP], BF16)
                        HT = [sb(st, "HT%d" % i, [128, 2, CAP], BF16) for i in range(2)]
                        s1b = [sb(st, "s1b%d" % i, [128, CAP], F32) for i in range(2)]
                        Ysb = sb(st, "Ysb", [128, 2, DM], BF16)
                        gate = sb(st, "gate", [128, 2], F32)
                        gate4 = sb(st, "gate4", [128, 2, 2], F32)
                        w1b = [sb(st, "w1b%d" % i, [128, 8, 256], BF16) for i in range(2)]
                        w3b = [sb(st, "w3b%d" % i, [128, 8, 256], BF16) for i in range(2)]
                        w2b = [sb(st, "w2b%d" % i, [128, 2, DM], BF16) for i in range(2)]
                        pG = [ps(st, "pG%d" % i, [128, 2, CAP]) for i in range(2)]
                        pH = [ps(st, "pH%d" % i, [128, 2, CAP]) for i in range(2)]
                        pY = [[ps(st, "pY%d%d" % (i, k), [128, 512]) for k in range(2)] for i in range(2)]
                        wcnt = [0]

                        def load_w(e, fb):
                            wb_ = wcnt[0] % 2
                            wcnt[0] += 1
                            S.dma("pool", w1b[wb_][:], w1_d[e, :, fb * 256:(fb + 1) * 256].rearrange("(c p) n -> p c n", p=128),
                                  ("w1b", wb_), writes=[("w1b", wb_)])
                            S.dma("pool", w3b[wb_][:], w3_d[e, :, fb * 256:(fb + 1) * 256].rearrange("(c p) n -> p c n", p=128),
                                  ("w3b", wb_), writes=[("w3b", wb_)])
                            S.dma("pool", w2b[wb_][:], w2_d[e, fb * 256:(fb + 1) * 256, :].rearrange("(c p) n -> p c n", p=128),
                                  ("w2b", wb_), writes=[("w2b", wb_)])
                            return wb_

                        n_exp = NE if not _os.environ.get("K_NEXP") else int(_os.environ["K_NEXP"])
                        blocks = [(e, fb) for e in range(n_exp) for fb in range(8)]
                        wslot = {}
                        wslot[blocks[0]] = load_w(*blocks[0])
                        hcnt = [0]
                        for e in range(n_exp):
                            def build_sel(e_):
                                for tt in range(NT):
                                    S.op("dve", lambda: V.tensor_scalar(out=Sel[:, tt, :], in0=iotar[:], scalar1=posm_tok[:, tt, e_:e_ + 1],
                                                                        scalar2=None, op0=ALU.is_equal),
                                         reads=["posm_tok", "bp_iota_row"], writes=[("Sel", tt)])
                            if e == 0:
                                build_sel(0)
                            for tb in range(4):
                                pbc = pH[tb % 2][:].rearrange("p a b -> p (a b)")
                                S.op("pe", lambda: PE_.matmul(pbc, lhsT=Eoh[:, e, :], rhs=posm_b[:, tb * 512:(tb + 1) * 512],
                                                              start=True, stop=True), reads=["Eoh", "posm_b"], writes=[("pH", tb % 2)])
                                for cc in range(2):
                                    S.op("dve", lambda: V.tensor_scalar(out=SelT[:, cc, tb * 512:(tb + 1) * 512], in0=pbc,
                                                                        scalar1=C("iota_p" if cc == 0 else "iota_p1"), scalar2=None,
                                                                        op0=ALU.is_equal),
                                         reads=[("pH", tb % 2), "cpack"], writes=[("SelT", cc, tb)])
                            pgt = pH[0][:, 0, 0:4].rearrange("p (a b) -> p a b", b=2)
                            for cc in range(2):
                                for tt in range(NT):
                                    S.op("pe", lambda: PE_.matmul(pgt[:, cc, :], lhsT=Sel[:, tt, cc * 128:(cc + 1) * 128],
                                                                  rhs=aff_hl[:, tt, e, :], start=(tt == 0), stop=(tt == NT - 1)),
                                         reads=[("Sel", tt), ("affh", tt), ("affl", tt)], writes=[("pH", 0)])
                            S.op("dve", lambda: V.tensor_copy(out=gate4[:], in_=pgt), reads=[("pH", 0)], writes=["gate4"])
                            S.op("dve", lambda: V.tensor_tensor(out=gate[:], in0=gate4[:, :, 0], in1=gate4[:, :, 1], op=ALU.add),
                                 reads=["gate4"], writes=["gate"])
                            for dcb in range(4):
                                gb = dcb % 2
                                for k in range(2):
                                    dc_ = dcb * 2 + k
                                    for tt in range(NT):
                                        S.op("pe", lambda: PE_.matmul(pG[gb][:, k, :], lhsT=Xn[:, tt, dc_ * 128:(dc_ + 1) * 128],
                                                                      rhs=Sel[:, tt, :], start=(tt == 0), stop=(tt == NT - 1)),
                                             reads=[("Xn", tt), ("Sel", tt)], writes=[("pG", gb)])
                                if gb == 0:
                                    S.op("act", lambda: A.activation(out=XeT[:, dcb * 2:dcb * 2 + 2, :], in_=pG[gb][:], func=AF.Copy),
                                         reads=[("pG", gb)], writes=[("XeT", dcb)])
                                else:
                                    S.op("dve", lambda: V.tensor_copy(out=XeT[:, dcb * 2:dcb * 2 + 2, :], in_=pG[gb][:]),
                                         reads=[("pG", gb)], writes=[("XeT", dcb)])
                            XK = [("XeT", q) for q in range(4)]
                            if e + 1 < n_exp:
                                build_sel(e + 1)
                            for fb in range(8):
                                wb_ = wslot[(e, fb)]
                                nxt = blocks.index((e, fb)) + 1
                                if nxt < len(blocks):
                                    wslot[blocks[nxt]] = load_w(*blocks[nxt])
                                hb = hcnt[0] % 2
                                hcnt[0] += 1
                                for fc in range(2):
                                    pb_ = fc % 2
                                    for k, wsrc, wk_ in ((0, w1b, "w1b"), (1, w3b, "w3b")):
                                        for dc_ in range(8):
                                            S.op("pe", lambda: PE_.matmul(pH[pb_][:, k, :], lhsT=wsrc[wb_][:, dc_, fc * 128:(fc + 1) * 128],
                                                                          rhs=XeT[:, dc_, :], start=(dc_ == 0), stop=(dc_ == 7)),
                                                 reads=XK + [(wk_, wb_)], writes=[("pH", pb_)])
                                    S.op("act", lambda: A.activation(out=s1b[pb_][:], in_=pH[pb_][:, 0, :], func=AF.Silu),
                                         reads=[("pH", pb_)], writes=[("s1b", pb_)])
                                    S.op("dve", lambda: V.tensor_tensor(out=HT[hb][:, fc, :], in0=pH[pb_][:, 1, :], in1=s1b[pb_][:], op=ALU.mult),
                                         reads=[("pH", pb_), ("s1b", pb_)], writes=[("HT", hb, fc)])
                                for cc in range(2):
                                    for dh in range(2):
                                        for fc in range(2):
                                            S.op("pe", lambda: PE_.matmul(pY[cc][dh][:], lhsT=HT[hb][:, fc, cc * 128:(cc + 1) * 128],
                                                                          rhs=w2b[wb_][:, fc, dh * 512:(dh + 1) * 512],
                                                                          start=(fb == 0 and fc == 0), stop=(fb == 7 and fc == 1)),
                                                 reads=[("HT", hb, fc), ("w2b", wb_)], writes=[("pY", cc, dh)])
                            for cc in range(2):
                                for dh in range(2):
                                    S.op("act", lambda: A.activation(out=Ysb[:, cc, dh * 512:(dh + 1) * 512], in_=pY[cc][dh][:], func=AF.Copy,
                                                                     scale=gate[:, cc:cc + 1]),
                                         reads=[("pY", cc, dh), "gate"], writes=[("Ysb", cc)])
                            for tt in range(NT):
                                for dh in range(2):
                                    oi = (tt * 2 + dh) % 4
                                    pO = pY[oi // 2][oi % 2][:]
                                    ok_ = ("pY", oi // 2, oi % 2)
                                    for cc in range(2):
                                        S.op("pe", lambda: PE_.matmul(pO, lhsT=SelT[:, cc, tt * 128:(tt + 1) * 128],
                                                                      rhs=Ysb[:, cc, dh * 512:(dh + 1) * 512], start=(cc == 0), stop=(cc == 1)),
                                             reads=[("SelT", cc, tt // 4), ("Ysb", cc)], writes=[ok_])
                                    S.op("dve", lambda: V.tensor_tensor(out=h[:, tt, dh * 512:(dh + 1) * 512], in0=pO,
                                                                        in1=h[:, tt, dh * 512:(dh + 1) * 512], op=ALU.add),
                                         reads=[ok_, ("h", tt)], writes=[("h", tt)])
                        S.barrier()
                    S.barrier()
                tap("h2", h[:], [])
                if stop_after == "moe":
                    S.barrier()
                    continue

                with ExitStack() as pl:
                    gpost = load_bp(pl, "g_post")
                    hnT = sb(pl, "hnT", [128, 8, SEQ], BF16)
                    wg = sb(pl, "wg", [128, 8, DM], BF16)
                    wpe = sb(pl, "wpe", [128, 2, DM], BF16)
                    S.dma("pool", wg[:], wg_d.rearrange("(c p) n -> p c n", p=128), "wg", writes=["wg"])
                    S.dma("pool", wpe[:], wpe_d.rearrange("(c p) n -> p c n", p=128), "wpe", writes=["wpe"])
                    hs = [sb(pl, "hs%d" % i, [128, DM], BF16) for i in range(2)]
                    junk4 = sb(pl, "junk4", [128, DM], BF16)
                    st4 = sb(pl, "st4", [128, 8 * NT], F32)
                    pls = ExitStack()
                    pt4 = [ps(pls, "pt4%d" % i, [128, 1024], BF16) for i in range(2)]
                    for tt in range(NT):
                        b = tt % 2
                        S.op("act", lambda: A.activation(out=junk4[:], in_=h[:, tt, :], func=AF.Square, accum_out=st4[:, tt:tt + 1]),
                             reads=[("h", tt)], writes=["junk4", ("st4", tt)], fuse=False)
                        S.op("act", lambda: A.activation(out=st4[:, NT + tt:NT + tt + 1], in_=st4[:, tt:tt + 1], func=AF.Sqrt, bias=EPS,
                                                         scale=1.0 / DM), reads=[("st4", tt), "kc"], writes=[("st41", tt)])
                        S.op("dve", lambda: V.reciprocal(out=st4[:, 2 * NT + tt:2 * NT + tt + 1], in_=st4[:, NT + tt:NT + tt + 1]),
                             reads=[("st41", tt)], writes=[("st42", tt)])
                        S.op("dve", lambda: V.tensor_scalar(out=hs[b][:], in0=h[:, tt, :], scalar1=st4[:, 2 * NT + tt:2 * NT + tt + 1],
                                                            scalar2=None, op0=ALU.mult), reads=[("h", tt), ("st42", tt)], writes=[("hs", b)])
                        for c in range(8):
                            S.op("pe", lambda: PE_.transpose(out=pt4[b][:, c * 128:(c + 1) * 128], in_=hs[b][:, c * 128:(c + 1) * 128],
                                                             identity=ident_b[:]), reads=[("hs", b), "ident_b"], writes=[("pt4", b)])
                        for c in range(8):
                            if b == 0:
                                S.op("dve", lambda: V.tensor_scalar(out=hnT[:, c, tt * 128:(tt + 1) * 128], in0=pt4[b][:, c * 128:(c + 1) * 128],
                                                                    scalar1=C("g_ple", c), scalar2=None, op0=ALU.mult),
                                     reads=[("pt4", b), "cpack"], writes=[("hnT", tt)])
                            else:
                                S.op("act", lambda: A.activation(out=hnT[:, c, tt * 128:(tt + 1) * 128], in_=pt4[b][:, c * 128:(c + 1) * 128],
                                                                 func=AF.Copy, scale=C("g_ple", c)),
                                     reads=[("pt4", b), "cpack"], writes=[("hnT", tt)])
                    S.barrier()
                    pls.close()
                    with ExitStack() as st:
                        pgt2 = [[ps(st, "pgt2%d%d" % (i, k), [128, 512]) for k in range(2)] for i in range(2)]
                        pe2 = [ps(st, "pe2%d" % k, [128, 512]) for k in range(2)]
                        ppt = ps(st, "ppt", [128, 1024], BF16)
                        gsb = [sb(st, "gsb%d" % i, [128, DM], F32) for i in range(2)]
                        ptl = [sb(st, "ptl%d" % i, [128, 256], F32) for i in range(2)]
                        ptb = sb(st, "ptb", [128, 256], BF16)
                        pTb = sb(st, "pTb", [128, 2, 128], BF16)
                        en = [sb(st, "en%d" % i, [128, DM], F32) for i in range(2)]
                        junk5 = sb(st, "junk5", [128, 512], BF16)
                        for tt in range(NT):
                            b = tt % 2
                            S.dma("sp", ptl[b][:], p_d[s, tt * 128:(tt + 1) * 128, :], ("ptl", b), writes=[("ptl", b)])
                            for dh in range(2):
                                for c in range(8):
                                    S.op("pe", lambda: PE_.matmul(pgt2[b][dh][:], lhsT=hnT[:, c, tt * 128:(tt + 1) * 128],
                                                                  rhs=wg[:, c, dh * 512:(dh + 1) * 512], start=(c == 0), stop=(c == 7)),
                                         reads=[("hnT", tt), "wg"], writes=[("pgt2", b, dh)])
                                S.op("act", lambda: A.activation(out=gsb[b][:, dh * 512:(dh + 1) * 512], in_=pgt2[b][dh][:], func=AF.Sigmoid),
                                     reads=[("pgt2", b, dh)], writes=[("gsb", b, dh)])
                            S.op("dve", lambda: V.tensor_copy(out=ptb[:], in_=ptl[b][:]), reads=[("ptl", b)], writes=["ptb"])
                            for k in range(2):
                                S.op("pe", lambda: PE_.transpose(out=ppt[:, k * 128:(k + 1) * 128], in_=ptb[:, k * 128:(k + 1) * 128],
                                                                 identity=ident_b[:]), reads=["ptb", "ident_b"], writes=["ppt"])
                            S.op("dve", lambda: V.tensor_copy(out=pTb[:].rearrange("p a b -> p (a b)"), in_=ppt[:, 0:256]),
                                 reads=["ppt"], writes=["pTb"])
                            for dh in range(2):
                                for k in range(2):
                                    S.op("pe", lambda: PE_.matmul(pe2[dh][:], lhsT=pTb[:, k, :], rhs=wpe[:, k, dh * 512:(dh + 1) * 512],
                                                                  start=(k == 0), stop=(k == 1)), reads=["pTb", "wpe"], writes=[("pe2", dh)])
                                S.op("act", lambda: A.activation(out=junk5[:], in_=pe2[dh][:], func=AF.Square,
                                                                 accum_out=st4[:, 3 * NT + 2 * tt + dh:3 * NT + 2 * tt + dh + 1]),
                                     reads=[("pe2", dh)], writes=["junk5", ("st43", tt, dh)], fuse=False)
                            o5 = 5 * NT + tt
                            S.op("dve", lambda: V.tensor_tensor(out=st4[:, o5:o5 + 1], in0=st4[:, 3 * NT + 2 * tt:3 * NT + 2 * tt + 1],
                                                                in1=st4[:, 3 * NT + 2 * tt + 1:3 * NT + 2 * tt + 2], op=ALU.add),
                                 reads=[("st43", tt, 0), ("st43", tt, 1)], writes=[("st45", tt)])
                            S.op("act", lambda: A.activation(out=st4[:, 6 * NT + tt:6 * NT + tt + 1], in_=st4[:, o5:o5 + 1], func=AF.Sqrt,
                                                             bias=EPS, scale=1.0 / DM), reads=[("st45", tt), "kc"], writes=[("st46", tt)])
                            S.op("dve", lambda: V.reciprocal(out=st4[:, 7 * NT + tt:7 * NT + tt + 1], in_=st4[:, 6 * NT + tt:6 * NT + tt + 1]),
                                 reads=[("st46", tt)], writes=[("st47", tt)])
                            for dh in range(2):
                                hsl = slice(dh * 512, (dh + 1) * 512)
                                S.op("dve", lambda: V.scalar_tensor_tensor(out=en[b][:, hsl], in0=pe2[dh][:],
                                                                           scalar=st4[:, 7 * NT + tt:7 * NT + tt + 1],
                                                                           in1=gpost[:, dh * 512:(dh + 1) * 512], op0=ALU.mult, op1=ALU.mult),
                                     reads=[("pe2", dh), ("st47", tt), "bp_g_post"], writes=[("en", b, dh)])
                                S.op("dve", lambda: V.tensor_tensor(out=en[b][:, hsl], in0=en[b][:, hsl], in1=gsb[b][:, hsl], op=ALU.mult),
                                     reads=[("en", b, dh), ("gsb", b, dh)], writes=[("en", b, dh)])
                                S.op("dve", lambda: V.tensor_tensor(out=en[b][:, hsl], in0=en[b][:, hsl], in1=h[:, tt, hsl], op=ALU.add),
                                     reads=[("en", b, dh), ("h", tt)], writes=[("en", b, dh)])
                            S.dma("sp", out_d[s, tt * 128:(tt + 1) * 128, :], en[b][:], ("outd", b),
                                  reads=[("en", b, 0), ("en", b, 1)], writes=[])
                        S.barrier()
                    S.barrier()
                S.barrier()
        S.final_wait("sp")
        import os as _os2
        if _os2.environ.get("K_PRINTSEMS"):
            print("SEMS", [(i, k) for i, k in enumerate(S.dma_sems.keys())], "n_instr", S.n_instr)
    return nc


def _prep(inputs):
    inp = {k: np.asarray(v) for k, v in inputs.items()}
    cp = make_cpack(inp)
    bp = make_bpack(inp)
    st = make_struct()
    shared = {
        "w_in": np.ascontiguousarray(inp["w_in"][0]),
        "w_out": np.ascontiguousarray(inp["w_out"][0]),
        "cpack": cp.build(), "bpack": bp.build(),
        "rel_bias": np.ascontiguousarray(inp["rel_bias"]),
        "onehot": st["onehot"], "rwmask": st["rwmask"],
        "w2cat": np.ascontiguousarray(inp["rw_w2"][0].reshape(128, 512)),
        "a2cat": np.ascontiguousarray(inp["rw_a2"][0].reshape(128, 512)),
        "g2": np.ascontiguousarray(inp["rw_g2"][0]),
        "w_router": np.ascontiguousarray(inp["w_router"][0]),
        "w1": np.ascontiguousarray(inp["w1"][0]), "w3": np.ascontiguousarray(inp["w3"][0]),
        "w2": np.ascontiguousarray(inp["w2"][0]),
        "w_ple_gate": np.ascontiguousarray(inp["w_ple_gate"][0]),
        "w_ple": np.ascontiguousarray(inp["w_ple"][0]),
    }
    in_maps = []
    for c in range(NCORES):
        m = dict(shared)
        m["x"] = np.ascontiguousarray(inp["x"][c * NSEQ:(c + 1) * NSEQ])
        m["p"] = np.ascontiguousarray(inp["p"][0, c * NSEQ:(c + 1) * NSEQ])
        in_maps.append(m)
    return cp, bp, in_maps


def kernel(**inputs):
    cp, bp, in_maps = _prep(inputs)
    nc = build_program(cp.off, bp.off, cp.n, bp.n)
    res = run_bass_kernel_spmd(nc, in_maps, core_ids=list(range(NCORES)))
    return np.concatenate([r["out"] for r in res.results], axis=0).astype(np.float32)
```

```python
import math
import numpy as np
from contextlib import ExitStack
import concourse.bass as bass
import concourse.mybir as mybir
from concourse.bass_utils import run_bass_kernel_spmd

F32 = mybir.dt.float32
BF16 = mybir.dt.bfloat16
AF = mybir.ActivationFunctionType
ALU = mybir.AluOpType

NCORES = 8
SEQ = 2048
DM = 1024
NSEQ = 2
NT = SEQ // 128
IN_COLS = 3488
RW0 = 1536
NE = 16
CAP = 256
DFF = 2048
CDEC = 0.6065306597126334
LAM_INIT = 0.8 - 0.6 * math.exp(-0.3 * 0)
STRIP_W = 1152
ND = 1279


class Holder:
    def __init__(self, name, sem):
        self.name = name
        self.sem = sem
        self.count = 0


class Sched:
    def __init__(self, nc, stack):
        self.nc = nc
        self.stack = stack
        self.obj = {"pe": nc.tensor, "dve": nc.vector, "act": nc.scalar,
                    "pool": nc.gpsimd, "sp": nc.sync}
        self.eng = {}
        for n in self.obj:
            sem = stack.enter_context(nc.semaphore("s_" + n))
            self.eng[n] = Holder(n, sem)
        self.known = {n: {} for n in self.obj}
        self.last_w = {}
        self.readers = {}
        self.dma_sems = {}
        self.n_instr = 0
        self.snap = {}
        self.seq = {}
        self.gseq = 0

    def _deps(self, reads, writes, e=None):
        own = self.eng.get(e) if e in ("pe",) else None
        toks = []
        for r in reads:
            t = self.last_w.get(r)
            if t is not None:
                toks.append(t)
        for w in writes:
            t = self.last_w.get(w)
            if t is not None and t[0] is not own:
                toks.append(t)
            for h, v in self.readers.get(w, {}).items():
                if h is not own:
                    toks.append((h, v))
        return toks

    def _wait(self, e, toks, keep_one=False):
        kn = self.known[e]
        need = {}
        for (h, v) in toks:
            if kn.get(h, 0) < v and need.get(h, 0) < v:
                need[h] = v
        order = sorted(need.items(), key=lambda kv: -self.seq.get((kv[0], kv[1]), 0))
        items = []
        for h, v in order:
            if kn.get(h, 0) >= v:
                continue
            items.append((h, v))
            kn[h] = v
            for h2, v2 in self.snap.get((h, v), {}).items():
                if kn.get(h2, 0) < v2:
                    kn[h2] = v2
        items.reverse()
        fused = None
        if keep_one and items:
            fused = items.pop()
        for h, v in items:
            self.obj[e].wait_ge(h.sem, v)
            kn[h] = v
            self.n_instr += 1
        if fused is not None:
            kn[fused[0]] = fused[1]
        return fused

    def _commit(self, tok, reads, writes):
        for r in reads:
            d = self.readers.setdefault(r, {})
            if d.get(tok[0], 0) < tok[1]:
                d[tok[0]] = tok[1]
        for w in writes:
            self.last_w[w] = tok
            self.readers[w] = {}

    def op(self, e, fn, reads=(), writes=(), fuse=True):
        toks = self._deps(reads, writes, e)
        fused = self._wait(e, toks, keep_one=fuse)
        ins = fn()
        if fused is not None:
            ins._wait_ge(fused[0].sem, fused[1])
        h = self.eng[e]
        h.count += 1
        ins.then_inc(h.sem, 1)
        tok = (h, h.count)
        self.gseq += 1
        self.seq[tok] = self.gseq
        self.snap[tok] = dict(self.known[e])
        self._commit(tok, reads, writes)
        self.n_instr += 1
        return tok

    def dma(self, q, out, in_, semkey, reads=(), writes=(), **kw):
        toks = self._deps(reads, writes)
        self._wait(q, toks)
        if semkey not in self.dma_sems:
            sem = self.stack.enter_context(self.nc.semaphore("d_%d" % len(self.dma_sems)))
            self.dma_sems[semkey] = Holder("dma_" + str(semkey), sem)
        h = self.dma_sems[semkey]
        ins = self.obj[q].dma_start(out=out, in_=in_, **kw)
        ins.then_inc(h.sem, 16)
        h.count += 16
        tok = (h, h.count)
        self.gseq += 1
        self.seq[tok] = self.gseq
        self.snap[tok] = dict(self.known[q])
        self._commit(tok, reads, writes)
        self.n_instr += 1
        return tok

    def _all(self):
        toks = [(h, h.count) for h in self.eng.values() if h.count > 0]
        toks += [(h, h.count) for h in self.dma_sems.values() if h.count > 0]
        return toks

    def barrier(self):
        toks = self._all()
        for e in self.obj:
            self._wait(e, toks)

    def final_wait(self, e="sp"):
        self._wait(e, self._all())


def _t5_bucket(rel):
    half, max_exact = 16, 8
    ret = np.where(rel > 0, half, 0)
    n = np.abs(rel)
    nf = np.maximum(n, 1).astype(np.float32)
    large = max_exact + (np.log(nf / np.float32(max_exact)) / np.float32(math.log(128 / max_exact))
                         * np.float32(half - max_exact)).astype(np.int32)
    large = np.minimum(large, half - 1)
    return ret + np.where(n < max_exact, n, large)


class Pack:
    def __init__(self):
        self.cols = []
        self.off = {}
        self.n = 0

    def add(self, name, arr):
        arr = np.asarray(arr, np.float32)
        assert arr.shape[0] == 128
        if arr.ndim == 1:
            arr = arr[:, None]
        self.off[name] = (self.n, arr.shape[1])
        self.cols.append(arr)
        self.n += arr.shape[1]

    def build(self):
        return np.ascontiguousarray(np.concatenate(self.cols, axis=1))


def pc(v, nchunk):
    return np.ascontiguousarray(np.asarray(v, np.float32).reshape(nchunk, 128).T)


def make_cpack(inp):
    P = Pack()
    P.add("g_mix", pc(inp["g_mix"][0], 8))
    P.add("qg", np.tile(inp["q_norm_g"][0], 2))
    P.add("kg", np.tile(inp["k_norm_g"][0], 2))
    mu = np.zeros(16 * 128, np.float32)
    mu[:1952] = inp["rw_mu"][0]
    P.add("mu", pc(mu, 16))
    P.add("w0", pc(inp["rw_w0"][0].reshape(-1), 8))
    P.add("a0", pc(inp["rw_a0"][0].reshape(-1), 8))
    P.add("k_k", pc(inp["rw_k_k"][0], 4))
    P.add("k_a", pc(inp["rw_k_a"][0], 4))
    P.add("r_k", pc(inp["rw_r_k"][0].reshape(-1), 4))
    P.add("lnx_g", pc(inp["rw_lnx_g"][0], 4))
    P.add("lnx_b", pc(inp["rw_lnx_b"][0], 4))
    P.add("subln_g", inp["subln_g"][0])
    P.add("g_ple", pc(inp["g_ple"][0], 8))
    rb = inp["rel_bias"]
    far = np.stack([rb[15, :], rb[31, :]], axis=1).reshape(-1)
    P.add("rb_far", np.broadcast_to(far[None, :], (128, 8)))
    P.add("iota_p", np.arange(128, dtype=np.float32))
    P.add("iota_p1", np.arange(128, 256, dtype=np.float32))
    for k in ("lam_q1", "lam_k1", "lam_q2", "lam_k2"):
        P.add(k, np.broadcast_to(inp[k][0][None, :], (128, 64)))
    return P


def make_bpack(inp):
    P = Pack()
    P.add("g_ffn", np.broadcast_to(inp["g_ffn"][0][None, :], (128, DM)))
    P.add("g_post", np.broadcast_to(inp["g_ple_post"][0][None, :], (128, DM)))
    P.add("iota_row", np.broadcast_to(np.arange(CAP, dtype=np.float32)[None, :], (128, CAP)))
    return P


def make_struct():
    m = np.arange(ND)
    delta = 639 - m
    bk = _t5_bucket(delta.astype(np.int32))
    oh = np.zeros((32, ND), np.float32)
    oh[bk, m] = 1.0
    idx = np.arange(64)
    s = idx[:, None]
    t = idx[None, :]
    rw = np.zeros((2, 64, 192), np.float32)
    rw[0, :, 0:64] = (s < t)
    rw[0, :, 64:128] = (s <= t)
    rw[0, :, 128:192] = (t < s)
    rw[1, :, 0:64] = (s > t)
    rw[1, :, 64:128] = (s >= t)
    rw[1, :, 128:192] = (t > s)
    return {"onehot": oh, "rwmask": np.ascontiguousarray(rw.transpose(1, 0, 2).reshape(64, 384))}


def build_program(cp_off, bp_off, ncp, nbp, dbg=None, nseq=NSEQ, stop_after=None):
    dbg = dbg or {}
    nc = bass.Bass("TRN2", target_bir_lowering=False)
    D = {}

    def din(name, shape, dt=F32):
        D[name] = nc.dram_tensor(name, list(shape), dt, kind="ExternalInput").ap()
        return D[name]

    x_d = din("x", [NSEQ, SEQ, DM])
    p_d = din("p", [NSEQ, SEQ, 256])
    w_in_d = din("w_in", [DM, IN_COLS])
    w_out_d = din("w_out", [DM, DM])
    cpack_d = din("cpack", [128, ncp])
    bpack_d = din("bpack", [128, nbp])
    relb_d = din("rel_bias", [32, 4])
    onehot_d = din("onehot", [32, ND])
    rwmask_d = din("rwmask", [64, 384])
    w2cat_d = din("w2cat", [128, 512])
    a2cat_d = din("a2cat", [128, 512])
    g2_d = din("g2", [160, 512])
    wr_d = din("w_router", [DM, NE])
    w1_d = din("w1", [NE, DM, DFF])
    w3_d = din("w3", [NE, DM, DFF])
    w2_d = din("w2", [NE, DFF, DM])
    wg_d = din("w_ple_gate", [DM, DM])
    wpe_d = din("w_ple", [256, DM])
    out_d = nc.dram_tensor("out", [NSEQ, SEQ, DM], F32, kind="ExternalOutput").ap()
    gscr_t = nc.dram_tensor("gscr", [4, ND], F32)
    gscr_d = gscr_t.ap()
    dbg_d = {}
    for k, (shape, dt) in dbg.items():
        dbg_d[k] = nc.dram_tensor("dbg_" + k, list(shape), dt, kind="ExternalOutput").ap()

    with ExitStack() as top:
        S = Sched(nc, top)
        V, A, G, PE_ = nc.vector, nc.scalar, nc.gpsimd, nc.tensor

        uid = [0]

        def sb(st, name, shape, dt):
            uid[0] += 1
            return st.enter_context(nc.sbuf_tensor("%s_s%d" % (name, uid[0]), list(shape), dt))

        def ps(st, name, shape, dt=F32):
            uid[0] += 1
            return st.enter_context(nc.psum_tensor("%s_p%d" % (name, uid[0]), list(shape), dt))

        def C(name, j=0, w=1):
            o, n = cp_off[name]
            return cpack[:, o + j:o + j + w]

        def tap(name, src_ap, reads, idx=None):
            if name in dbg_d:
                dst = dbg_d[name] if idx is None else dbg_d[name][idx]
                S.dma("sp", dst, src_ap, "dbg", reads=reads)

        cpack = sb(top, "cpack", [128, ncp], F32)
        S.dma("sp", cpack[:], cpack_d, "c0", writes=["cpack"])
        kc = sb(top, "kc", [128, 8], F32)
        S.op("pool", lambda: G.memset(kc[:, 0:1], 1e-6), writes=["kc"])
        S.op("pool", lambda: G.memset(kc[:, 1:2], 64e-5), writes=["kc"])
        S.op("pool", lambda: G.memset(kc[:, 2:3], 0.0), writes=["kc"])
        S.op("pool", lambda: G.memset(kc[:, 3:4], 1e-18), writes=["kc"])
        EPS = kc[:, 0:1]
        EPSLN = kc[:, 1:2]
        ident_b = sb(top, "ident_b", [128, 128], BF16)
        ident_f = sb(top, "ident_f", [128, 128], F32)
        for idt, nm in ((ident_b, "ident_b"), (ident_f, "ident_f")):
            S.op("pool", lambda idt=idt: G.memset(idt[:], 1.0), writes=[nm])
            S.op("pool", lambda idt=idt: G.affine_select(
                out=idt[:], in_=idt[:], pattern=[[-1, 128]], compare_op=ALU.is_equal,
                fill=0.0, base=0, channel_multiplier=1), reads=[nm], writes=[nm])
        bd_b = sb(top, "bd_b", [128, 128], BF16)
        S.op("pool", lambda: G.memset(bd_b[:], 0.0), writes=["bd_b"])
        S.op("pool", lambda: G.memset(bd_b[0:64, 0:64], 1.0), reads=["bd_b"], writes=["bd_b"])
        S.op("pool", lambda: G.memset(bd_b[64:128, 64:128], 1.0), reads=["bd_b"], writes=["bd_b"])
        dc = sb(top, "dc", [128, 64], F32)
        S.op("dve", lambda: V.tensor_scalar(out=dc[:, 0:1], in0=C("qg"), scalar1=0.125, scalar2=None,
                                            op0=ALU.mult), reads=["cpack"], writes=["dc0"])
        S.op("dve", lambda: V.tensor_scalar(out=dc[:, 3:19], in0=C("mu", 0, 16), scalar1=-1.0, scalar2=1.0,
                                            op0=ALU.mult, op1=ALU.add), reads=["cpack"], writes=["dc_omm"])
        S.op("dve", lambda: V.tensor_scalar(out=dc[:, 19:35], in0=C("mu", 0, 16), scalar1=0.5, scalar2=None,
                                            op0=ALU.mult), reads=["cpack"], writes=["dc_hmu"])
        S.op("dve", lambda: V.tensor_scalar(out=dc[:, 35:39], in0=C("k_a", 0, 4), scalar1=-1.0, scalar2=1.0,
                                            op0=ALU.mult, op1=ALU.add), reads=["cpack"], writes=["dc_omka"])
        lt = sb(top, "lamtmp", [128, 64], F32)
        l2 = sb(top, "lam2", [128, 4], F32)
        for i, (a, b) in enumerate((("lam_q1", "lam_k1"), ("lam_q2", "lam_k2"))):
            oa, ob = cp_off[a][0], cp_off[b][0]
            S.op("dve", lambda oa=oa, ob=ob: V.tensor_tensor(out=lt[:], in0=cpack[:, oa:oa + 64],
                                                               in1=cpack[:, ob:ob + 64], op=ALU.mult),
                 reads=["cpack"], writes=["lamtmp"])
            S.op("dve", lambda i=i: V.reduce_sum(out=l2[:, i:i + 1], in_=lt[:], axis=mybir.AxisListType.X),
                 reads=["lamtmp"], writes=[("lam2", i)])
        S.op("act", lambda: A.activation(out=l2[:, 2:4], in_=l2[:, 0:2], func=AF.Exp),
             reads=[("lam2", 0), ("lam2", 1)], writes=["lam2e"])
        S.op("dve", lambda: V.tensor_tensor(out=dc[:, 1:2], in0=l2[:, 2:3], in1=l2[:, 3:4], op=ALU.subtract),
             reads=["lam2e"], writes=["dc1a"])
        S.op("dve", lambda: V.tensor_scalar(out=dc[:, 1:2], in0=dc[:, 1:2], scalar1=LAM_INIT, scalar2=None,
                                            op0=ALU.add), reads=["dc1a"], writes=["dc1"])
        S.op("dve", lambda: V.tensor_scalar(out=dc[:, 2:3], in0=dc[:, 1:2], scalar1=-1.0, scalar2=None,
                                            op0=ALU.mult), reads=["dc1"], writes=["dc2"])
        QGS = dc[:, 0:1]
        NLAM = dc[:, 2:3]

        with ExitStack() as st:
            rb_sb = sb(st, "rb_sb", [32, 4], F32)
            oh_sb = sb(st, "oh_sb", [32, ND], F32)
            g4 = sb(st, "g4", [4, ND], F32)
            gp = ps(st, "gp", [4, 512])
            S.dma("sp", rb_sb[:], relb_d, "c1", writes=["rb_sb"])
            S.dma("sp", oh_sb[:], onehot_d, "c2", writes=["oh_sb"])
            for b0 in range(0, ND, 512):
                n = min(512, ND - b0)
                S.op("pe", lambda b0=b0, n=n: PE_.matmul(gp[:, 0:n], lhsT=rb_sb[:], rhs=oh_sb[:, b0:b0 + n],
                                                           start=True, stop=True),
                     reads=["rb_sb", "oh_sb"], writes=["gp"])
                S.op("dve", lambda b0=b0, n=n: V.tensor_copy(out=g4[:, b0:b0 + n], in_=gp[:, 0:n]),
                     reads=["gp"], writes=["g4"])
            S.dma("sp", gscr_d, g4[:], "c3", reads=["g4"], writes=["gscr"])
            S.barrier()

        for s in range(nseq):
            with ExitStack() as sq:
                o_daT = sb(sq, "o_daT", [128, 4, SEQ], BF16)
                o_rwT = sb(sq, "o_rwT", [128, 4, SEQ], BF16)
                mixs = ExitStack()
                sq.callback(mixs.close)
                xnT = sb(mixs, "xnT", [128, 8, SEQ], BF16)
                with ExitStack() as st:
                    xt = [sb(st, "xt%d" % i, [128, DM], F32) for i in range(2)]
                    xs = [sb(st, "xs%d" % i, [128, DM], BF16) for i in range(2)]
                    junk = sb(st, "junk", [128, DM], BF16)
                    ssq = sb(st, "ssq", [128, 2 * NT], F32)
                    ptp = [ps(st, "ptp%d" % i, [128, 1024], BF16) for i in range(2)]
                    for tt in range(NT):
                        b = tt % 2
                        S.dma("sp", xt[b][:], x_d[s, tt * 128:(tt + 1) * 128, :], ("xt", b), writes=[("xt", b)])
                        S.op("act", lambda b=b, tt=tt: A.activation(out=junk[:], in_=xt[b][:], func=AF.Square,
                                                                     accum_out=ssq[:, tt:tt + 1]),
                             reads=[("xt", b)], writes=["junk", ("ssq", tt)], fuse=False)
                        S.op("act", lambda tt=tt: A.activation(out=ssq[:, NT + tt:NT + tt + 1], in_=ssq[:, tt:tt + 1],
                                                               func=AF.Sqrt, bias=EPS, scale=1.0 / DM),
                             reads=[("ssq", tt), "kc"], writes=[("ssd", tt)])
                        S.op("dve", lambda tt=tt: V.reciprocal(out=ssq[:, tt:tt + 1], in_=ssq[:, NT + tt:NT + tt + 1]),
                             reads=[("ssd", tt)], writes=[("rstd", tt)])
                        S.op("dve", lambda b=b, tt=tt: V.tensor_scalar(out=xs[b][:], in0=xt[b][:], scalar1=ssq[:, tt:tt + 1],
                                                                        scalar2=None, op0=ALU.mult),
                             reads=[("xt", b), ("rstd", tt)], writes=[("xs", b)])
                        for c in range(8):
                            S.op("pe", lambda b=b, c=c: PE_.transpose(out=ptp[b][:, c * 128:(c + 1) * 128],
                                                                       in_=xs[b][:, c * 128:(c + 1) * 128], identity=ident_b[:]),
                                 reads=[("xs", b), "ident_b"], writes=[("ptp", b)])
                        for c in range(8):
                            e = "act" if b % 2 else "dve"
                            if e == "dve":
                                S.op("dve", lambda b=b, c=c, tt=tt: V.tensor_scalar(
                                    out=xnT[:, c, tt * 128:(tt + 1) * 128], in0=ptp[b][:, c * 128:(c + 1) * 128],
                                    scalar1=C("g_mix", c), scalar2=None, op0=ALU.mult),
                                    reads=[("ptp", b), "cpack"], writes=[("xnT", tt)])
                            else:
                                S.op("act", lambda b=b, c=c, tt=tt: A.activation(
                                    out=xnT[:, c, tt * 128:(tt + 1) * 128], in_=ptp[b][:, c * 128:(c + 1) * 128],
                                    func=AF.Copy, scale=C("g_mix", c)),
                                    reads=[("ptp", b), "cpack"], writes=[("xnT", tt)])
                    S.barrier()
                tap("xnT", xnT[:], [("xnT", tt) for tt in range(NT)])
                XNT_ALL = [("xnT", tt) for tt in range(NT)]
                if stop_after == "p1":
                    S.barrier()
                    continue

                wlT = sb(mixs, "wlT", [128, SEQ], BF16)
                alT = sb(mixs, "alT", [128, SEQ], BF16)
                glT = sb(mixs, "glT", [128, SEQ], BF16)
                gl2T = sb(mixs, "gl2T", [32, SEQ], BF16)

                def shiftmix(uk, u, m, jc, outk, out, tmp, tmpk):
                    S.op("pool", lambda: G.tensor_tensor(out=tmp[:m, 1:SEQ - 1], in0=u[:m, 0:SEQ - 2], in1=u[:m, 2:SEQ],
                                                         op=ALU.add), reads=[uk], writes=[tmpk])
                    S.op("pool", lambda: G.tensor_copy(out=tmp[:m, 0:1], in_=u[:m, 1:2]), reads=[uk], writes=[tmpk])
                    S.op("pool", lambda: G.tensor_copy(out=tmp[:m, SEQ - 1:SEQ], in_=u[:m, SEQ - 2:SEQ - 1]),
                         reads=[uk], writes=[tmpk])
                    S.op("dve", lambda: V.tensor_scalar(out=tmp[:m, :], in0=tmp[:m, :], scalar1=dc[:m, 19 + jc:20 + jc],
                                                        scalar2=None, op0=ALU.mult),
                         reads=[tmpk, "dc_hmu"], writes=[tmpk])
                    S.op("dve", lambda: V.scalar_tensor_tensor(out=out[:m, :], in0=u[:m, :], scalar=dc[:m, 3 + jc:4 + jc],
                                                               in1=tmp[:m, :], op0=ALU.mult, op1=ALU.add),
                         reads=[uk, tmpk, "dc_omm"], writes=[outk])

                with ExitStack() as at:
                    qT = sb(at, "qT", [128, 4, SEQ], BF16)
                    kT = sb(at, "kT", [128, 4, SEQ], BF16)
                    vaug = sb(at, "vaug", [128, NT, 4, 130], BF16)
                    with ExitStack() as st:
                        wA = sb(st, "wA", [128, 8, 1952], BF16)
                        S.dma("pool", wA[:, :, 0:1536], w_in_d[:, 0:1536].rearrange("(c p) n -> p c n", p=128),
                              "wA", writes=["wA"])
                        S.dma("pool", wA[:, :, 1536:1952], w_in_d[:, 3072:3488].rearrange("(c p) n -> p c n", p=128),
                              "wA", writes=["wA"])
                        pu = [ps(st, "pu%d" % i, [128, 512]) for i in range(2)]
                        pss = [ps(st, "pss%d" % i, [128, 512]) for i in range(2)]
                        sqb = [sb(st, "sqb%d" % i, [128, 512], BF16) for i in range(2)]
                        usb = [sb(st, "usb%d" % i, [128, 512], F32) for i in range(2)]
                        sdb = [sb(st, "sdb%d" % i, [128, 512], F32) for i in range(2)]
                        S.op("pool", lambda: G.memset(vaug[:, :, :, 128:130], 1.0), writes=["vaug_ones"])

                        def proj_fm(pst, pk, c0, m, tb):
                            for dci in range(8):
                                S.op("pe", lambda dci=dci: PE_.matmul(pst[:m, :], lhsT=wA[:, dci, c0:c0 + m],
                                                                       rhs=xnT[:, dci, tb * 512:(tb + 1) * 512],
                                                                       start=(dci == 0), stop=(dci == 7)),
                                     reads=["wA"] + XNT_ALL[tb * 4:tb * 4 + 4], writes=[pk])

                        units = [(kind, h, tb) for kind in range(2) for h in range(4) for tb in range(4)]

                        def qk_front(i):
                            kind, h, tb = units[i]
                            b = i % 2
                            proj_fm(pu[b], ("pu", b), kind * 512 + h * 128, 128, tb)
                            S.op("dve", lambda: V.tensor_copy(out=usb[b][:], in_=pu[b][:]),
                                 reads=[("pu", b)], writes=[("usb", b)])
                            S.op("act", lambda: A.activation(out=sqb[b][:], in_=usb[b][:], func=AF.Square),
                                 reads=[("usb", b)], writes=[("sqb", b)])

                        def qk_back(i):
                            kind, h, tb = units[i]
                            b = i % 2
                            dst = (qT, kT)[kind]
                            gcol = QGS if kind == 0 else C("kg")
                            S.op("pe", lambda: PE_.matmul(pss[b][:], lhsT=bd_b[:], rhs=sqb[b][:], start=True, stop=True),
                                 reads=["bd_b", ("sqb", b)], writes=[("pss", b)])
                            S.op("act", lambda: A.activation(out=sdb[b][:], in_=pss[b][:], func=AF.Ln, bias=EPS,
                                                             scale=1.0 / 64),
                                 reads=[("pss", b), "kc"], writes=[("sdb", b)])
                            S.op("act", lambda: A.activation(out=sdb[b][:], in_=sdb[b][:], func=AF.Exp, scale=-0.5),
                                 reads=[("sdb", b)], writes=[("sdb", b)])
                            S.op("dve", lambda: V.scalar_tensor_tensor(
                                out=dst[:, h, tb * 512:(tb + 1) * 512], in0=usb[b][:], scalar=gcol, in1=sdb[b][:],
                                op0=ALU.mult, op1=ALU.mult),
                                reads=[("usb", b), ("sdb", b), "dc0", "cpack"], writes=[("qk", kind, h, tb)])

                        import os as _os
                        _cut = _os.environ.get("K_CUT", "")
                        if _cut.startswith("qkfront"):
                            proj_fm(pu[0], ("pu", 0), 0, 128, 0)
                            if "a" in _cut[7:]:
                                S.op("act", lambda: A.activation(out=sqb[0][:], in_=pu[0][:], func=AF.Square),
                                     reads=[("pu", 0)], writes=[("sqb", 0)])
                            if "d" in _cut[7:]:
                                S.op("dve", lambda: V.tensor_copy(out=usb[0][:], in_=pu[0][:]),
                                     reads=[("pu", 0)], writes=[("usb", 0)])
                            units = []
                        if _cut == "dmaonly":
                            units = []
                        if _cut == "mmonly":
                            proj_fm(pu[0], ("pu", 0), 0, 128, 0)
                            units = []
                        if _cut == "qk1":
                            units = units[:1]
                        for i in range(len(units)):
                            qk_front(i)
                            if i > 0:
                                qk_back(i - 1)
                        if units:
                            qk_back(len(units) - 1)
                        for tt in range(NT if _cut in ("", "v", "lora") else 0):
                            b = tt % 2
                            for dci in range(8):
                                S.op("pe", lambda dci=dci: PE_.matmul(pu[b][:], lhsT=xnT[:, dci, tt * 128:(tt + 1) * 128],
                                                                       rhs=wA[:, dci, 1024:1536],
                                                                       start=(dci == 0), stop=(dci == 7)),
                                     reads=["wA", ("xnT", tt)], writes=[("pu", b)])
                            src = pu[b][:].rearrange("p (h d) -> p h d", h=4)
                            if tt % 2:
                                S.op("act", lambda: A.activation(out=vaug[:, tt, :, 0:128], in_=src, func=AF.Copy),
                                     reads=[("pu", b)], writes=[("vaug", tt)])
                            else:
                                S.op("dve", lambda: V.tensor_copy(out=vaug[:, tt, :, 0:128], in_=src),
                                     reads=[("pu", b)], writes=[("vaug", tt)])
                        ush = sb(st, "ush", [128, SEQ], F32)
                        utmp = sb(st, "utmp", [128, SEQ], F32)
                        umix = sb(st, "umix", [128, SEQ], F32)
                        for ci, (c0, m, jc, func, dst) in enumerate((
                                (1536, 128, 12, AF.Tanh, wlT), (1664, 128, 13, AF.Copy, alT),
                                (1792, 128, 14, AF.Sigmoid, glT), (1920, 32, 15, AF.Sigmoid, gl2T))[:(4 if _cut in ("", "lora") else 0)]):
                            for tb in range(4):
                                b = tb % 2
                                proj_fm(pu[b], ("pu", b), c0, m, tb)
                                if tb % 2:
                                    S.op("act", lambda: A.activation(out=ush[:m, tb * 512:(tb + 1) * 512], in_=pu[b][:m, :],
                                                                     func=AF.Copy),
                                         reads=[("pu", b)], writes=["ush"])
                                else:
                                    S.op("dve", lambda: V.tensor_copy(out=ush[:m, tb * 512:(tb + 1) * 512], in_=pu[b][:m, :]),
                                         reads=[("pu", b)], writes=["ush"])
                            shiftmix("ush", ush, m, jc, "umix", umix, utmp, "utmp")
                            S.op("act", lambda: A.activation(out=dst[:m, :], in_=umix[:m, :], func=func),
                                 reads=["umix"], writes=[("lora_in", ci)])
                        S.barrier()
                    tap("qT", qT[:], [])
                    tap("kT", kT[:], [])
                    tap("vaug", vaug[:], [])
                    tap("wlT", wlT[:], [])
                    tap("glT", glT[:], [])
                    if stop_after == "p2a":
                        S.barrier()
                        continue

                    with ExitStack() as st:
                        strip = sb(st, "strip", [128, 4, STRIP_W], F32)
                        for i in range(128):
                            src = bass.AP(gscr_t, 127 - i, [[0, 1], [ND, 4], [1, STRIP_W]])
                            S.dma("sp", strip[i:i + 1, :, :], src, "c4", reads=["gscr"], writes=["strip"])
                        PT = [sb(st, "PT%d" % i, [128, NT, 512], BF16) for i in range(2)]
                        tmpb = [sb(st, "tmpb%d" % i, [128, 512], F32) for i in range(2)]
                        spp = [ps(st, "spp%d" % i, [128, 512]) for i in range(2)]
                        av = ps(st, "av", [128, 4, 512])
                        ptr = ps(st, "ptr", [128, 1024], BF16)
                        rin = sb(st, "rin", [128, 4, 2, 1], F32)
                        nl = sb(st, "nl", [128, 2, 2, 1], F32)
                        o0 = sb(st, "o0", [128, 128], F32)
                        osb = sb(st, "osb", [128, 4, 128], F32)
                        onb = sb(st, "onb", [128, 4, 128], BF16)
                        junk2 = sb(st, "junk2", [128, 128], BF16)
                        ss4 = sb(st, "ss4", [128, 8], F32)
                        aunits = [(h, qt, sub) for h in range(4) for qt in range(4) for sub in range(2)]
                        cnt = [0]

                        def qk_exp(u):
                            h, qt, sub = u
                            for kt in range(NT):
                                i = cnt[0]
                                cnt[0] += 1
                                b = i % 2
                                S.op("pe", lambda: PE_.matmul(spp[b][:], lhsT=kT[64 * sub:64 * sub + 64, h, kt * 128:(kt + 1) * 128],
                                                              rhs=qT[64 * sub:64 * sub + 64, h, qt * 512:(qt + 1) * 512],
                                                              start=True, stop=True),
                                     reads=[], writes=[("spp", b)])
                                Dk = kt * 128 - qt * 512
                                if -255 < Dk < 639:
                                    off = 512 - Dk
                                    S.op("dve", lambda: V.tensor_tensor(out=tmpb[b][:], in0=spp[b][:],
                                                                        in1=strip[:, h, off:off + 512], op=ALU.add),
                                         reads=[("spp", b), "strip"], writes=[("tmpb", b)])
                                    S.op("act", lambda: A.activation(out=PT[sub][:, kt, :], in_=tmpb[b][:], func=AF.Exp),
                                         reads=[("tmpb", b)], writes=[("PT", sub, kt)])
                                else:
                                    which = 1 if Dk > 0 else 0
                                    S.op("act", lambda: A.activation(out=PT[sub][:, kt, :], in_=spp[b][:], func=AF.Exp,
                                                                     bias=C("rb_far", h * 2 + which)),
                                         reads=[("spp", b)], writes=[("PT", sub, kt)])
                                yield

                        def accap(sub, qs, lo, hi):
                            return av[:, sub * 2 + qs // 2, (qs % 2) * 256 + lo:(qs % 2) * 256 + hi]

                        def av_mm(u):
                            h, qt, sub = u
                            for qs in range(4):
                                for kt in range(NT):
                                    S.op("pe", lambda: PE_.matmul(accap(sub, qs, 0, 129),
                                                                  lhsT=PT[sub][:, kt, qs * 128:(qs + 1) * 128],
                                                                  rhs=vaug[:, kt, h, 0:129],
                                                                  start=(kt == 0), stop=(kt == NT - 1)),
                                         reads=[("PT", sub, kt)], writes=[("av", sub)])
                                    if kt % 4 == 3:
                                        yield

                        def post_a(h, qt):
                            av4 = av[:].rearrange("p b (j w) -> p b j w", j=2)
                            S.op("dve", lambda: V.reciprocal(out=rin[:], in_=av4[:, :, :, 128:129]),
                                 reads=[("av", 0), ("av", 1)], writes=["rin"])
                            S.op("dve", lambda: V.tensor_scalar(out=nl[:], in0=rin[:, 2:4, :, :], scalar1=NLAM, scalar2=None,
                                                                op0=ALU.mult), reads=["rin", "dc2"], writes=["nl"])
                            for qs in range(4):
                                S.op("dve", lambda: V.tensor_scalar(out=o0[:], in0=accap(0, qs, 0, 128),
                                                                    scalar1=rin[:, qs // 2, qs % 2, :], scalar2=None,
                                                                    op0=ALU.mult),
                                     reads=[("av", 0), "rin"], writes=["o0"])
                                S.op("dve", lambda: V.scalar_tensor_tensor(out=osb[:, qs, :], in0=accap(1, qs, 0, 128),
                                                                           scalar=nl[:, qs // 2, qs % 2, :], in1=o0[:],
                                                                           op0=ALU.mult, op1=ALU.add),
                                     reads=[("av", 1), "nl", "o0"], writes=[("osb", qs)])

                        def post_b(h, qt):
                            for qs in range(4):
                                S.op("act", lambda: A.activation(out=junk2[:], in_=osb[:, qs, :], func=AF.Square,
                                                                 accum_out=ss4[:, qs:qs + 1]),
                                     reads=[("osb", qs)], writes=["junk2", ("ss4", qs)], fuse=False)
                                yield
                            S.op("act", lambda: A.activation(out=ss4[:, 4:8], in_=ss4[:, 0:4], func=AF.Sqrt, bias=EPS,
                                                             scale=1.0 / 128),
                                 reads=[("ss4", q_) for q_ in range(4)] + ["kc"], writes=["ss4b"])
                            yield
                            S.op("dve", lambda: V.reciprocal(out=ss4[:, 4:8], in_=ss4[:, 4:8]), reads=["ss4b"], writes=["ss4b"])
                            yield
                            for qs in range(4):
                                S.op("dve", lambda: V.tensor_scalar(out=onb[:, qs, :], in0=osb[:, qs, :],
                                                                    scalar1=ss4[:, 4 + qs:5 + qs], scalar2=1.0 - LAM_INIT,
                                                                    op0=ALU.mult, op1=ALU.mult),
                                     reads=[("osb", qs), "ss4b"], writes=[("onb", qs)])
                                S.op("pe", lambda: PE_.transpose(out=ptr[:, qs * 128:(qs + 1) * 128], in_=onb[:, qs, :],
                                                                 identity=ident_b[:]),
                                     reads=[("onb", qs), "ident_b"], writes=["ptr"])
                                yield
                            S.op("act", lambda: A.activation(out=o_daT[:, h, qt * 512:(qt + 1) * 512], in_=ptr[:, 0:512],
                                                             func=AF.Copy, scale=C("subln_g")),
                                 reads=["ptr", "cpack"], writes=[("o_daT", h, qt)])

                        n_u = len(aunits)
                        if _os.environ.get("K_SKIP_ATTN"):
                            n_u = 0
                        def rr(gens):
                            gens = [g for g in gens if g is not None]
                            while gens:
                                nxt = []
                                for g in gens:
                                    try:
                                        next(g)
                                        nxt.append(g)
                                    except StopIteration:
                                        pass
                                gens = nxt

                        pend = None
                        if n_u:
                            rr([qk_exp(aunits[0])])
                        for i in range(1, n_u):
                            rr([qk_exp(aunits[i]), av_mm(aunits[i - 1]), pend])
                            pend = None
                            if aunits[i - 1][2] == 1:
                                post_a(aunits[i - 1][0], aunits[i - 1][1])
                                pend = post_b(aunits[i - 1][0], aunits[i - 1][1])
                        if n_u:
                            rr([av_mm(aunits[-1]), pend])
                            post_a(aunits[-1][0], aunits[-1][1])
                            rr([post_b(aunits[-1][0], aunits[-1][1])])
                        S.barrier()
                tap("o_daT", o_daT[:], [])
                if stop_after == "attn":
                    S.barrier()
                    continue

                with ExitStack() as rw:
                    w2c = sb(rw, "w2c", [128, 512], BF16)
                    a2c = sb(rw, "a2c", [128, 512], BF16)
                    g2a = sb(rw, "g2a", [128, 512], BF16)
                    g2b = sb(rw, "g2b", [32, 512], BF16)
                    S.dma("pool", w2c[:], w2cat_d, "rwc", writes=["w2c"])
                    S.dma("pool", a2c[:], a2cat_d, "rwc", writes=["a2c"])
                    S.dma("pool", g2a[:], g2_d[0:128, :], "rwc", writes=["g2a"])
                    S.dma("pool", g2b[:], g2_d[128:160, :], "rwc", writes=["g2b"])
                    maskAB = sb(rw, "maskAB", [64, 4, 128], BF16)
                    maskN = sb(rw, "maskN", [64, 4, 64], BF16)
                    ident4 = sb(rw, "ident4", [64, 4, 64], BF16)
                    resetm = sb(rw, "resetm", [128, 256], F32)
                    bd_f = sb(rw, "bd_f", [128, 128], F32)
                    with ExitStack() as st:
                        rwm = sb(st, "rwm", [64, 384], F32)
                        S.dma("sp", rwm[:], rwmask_d, "rwc2", writes=["rwm"])
                        for inst in range(4):
                            d = inst % 2
                            S.op("dve", lambda: V.tensor_copy(out=maskAB[:, inst, :], in_=rwm[:, d * 192:d * 192 + 128]),
                                 reads=["rwm"], writes=["maskAB"])
                            S.op("dve", lambda: V.tensor_copy(out=maskN[:, inst, :], in_=rwm[:, d * 192 + 128:d * 192 + 192]),
                                 reads=["rwm"], writes=["maskN"])
                            S.op("dve", lambda: V.tensor_copy(out=ident4[:, inst, :], in_=ident_b[0:64, 0:64]),
                                 reads=["ident_b"], writes=["ident4"])
                        S.op("dve", lambda: V.memset(resetm[:], 1.0), writes=["resetm"])
                        S.op("dve", lambda: V.memset(resetm[:].rearrange("p (c l) -> p c l", l=64)[:, :, 0:1], 0.0),
                             reads=["resetm"], writes=["resetm"])
                        S.op("dve", lambda: V.memset(bd_f[:], 0.0), writes=["bd_f"])
                        S.op("dve", lambda: V.memset(bd_f[0:64, 0:64], 1.0), reads=["bd_f"], writes=["bd_f"])
                        S.op("dve", lambda: V.memset(bd_f[64:128, 64:128], 1.0), reads=["bd_f"], writes=["bd_f"])
                        S.barrier()

                    TB = 256
                    NB = SEQ // TB
                    CPB = TB // 64

                    def c3(ap):
                        return ap.rearrange("p (c l) -> p c l", l=64)

                    for j in range(4 if stop_after != "rw1" else 1):
                        with ExitStack() as pp:
                            AR = [sb(pp, "AR%d" % d, [128, 32, 2, 64], BF16) for d in range(2)]
                            bbar = [sb(pp, "bbar%d" % d, [128, SEQ], BF16) for d in range(2)]
                            kbar = [sb(pp, "kbar%d" % d, [128, SEQ], BF16) for d in range(2)]
                            vb = sb(pp, "vb", [128, SEQ], BF16)
                            rhi = [sb(pp, "rhi%d" % d, [64, 32, 64], BF16) for d in range(2)]
                            PLf = [sb(pp, "PLf%d" % d, [128, 32], F32) for d in range(2)]
                            PLhi = [sb(pp, "PLhi%d" % d, [64, 32], F32) for d in range(2)]
                            bonus = sb(pp, "bonus", [128, SEQ], BF16)
                            wBj = sb(pp, "wBj", [128, 8, 384], BF16)
                            yacc = sb(pp, "yacc", [64, 2, SEQ], F32)
                            for i3 in range(3):
                                cc0 = RW0 + i3 * 512 + j * 128
                                S.dma("pool", wBj[:, :, i3 * 128:(i3 + 1) * 128],
                                      w_in_d[:, cc0:cc0 + 128].rearrange("(c p) n -> p c n", p=128), "wBj", writes=["wBj"])
                            with ExitStack() as s1:
                                rf = sb(s1, "rf", [128, SEQ], F32)
                                kf = sb(s1, "kf", [128, SEQ], F32)
                                vf = sb(s1, "vf", [128, SEQ], F32)
                                ush = sb(s1, "ush2", [128, SEQ], F32)
                                utmp = sb(s1, "utmp2", [128, SEQ], F32)
                                sqb1 = sb(s1, "sqb1", [128, TB], BF16)
                                nct = sb(s1, "nct", [128, 2, CPB], F32)
                                pu2 = [ps(s1, "pu2%d" % i, [128, 512]) for i in range(2)]
                                pl2a = ps(s1, "pl2a", [128, 2, TB])
                                pl2b = ps(s1, "pl2b", [128, 2, TB])
                                pl2 = [pl2a[:, 0, :], pl2b[:, 0, :], pl2a[:, 1, :], pl2b[:, 1, :]]
                                pst_t = ps(s1, "pst", [128, 512])
                                pbn_t = ps(s1, "pbn", [128, 512])
                                pst = pst_t[:, 0:TB]
                                pbn = pbn_t[:, 0:TB]
                                for i3, (dst, dk) in enumerate(((rf, "rf"), (kf, "kf"), (vf, "vf"))):
                                    for tb in range(4):
                                        b = tb % 2
                                        for dci in range(8):
                                            S.op("pe", lambda dci=dci: PE_.matmul(
                                                pu2[b][:], lhsT=wBj[:, dci, i3 * 128:(i3 + 1) * 128],
                                                rhs=xnT[:, dci, tb * 512:(tb + 1) * 512], start=(dci == 0), stop=(dci == 7)),
                                                reads=["wBj"] + XNT_ALL[tb * 4:tb * 4 + 4], writes=[("pu2", b)])
                                        if tb % 2:
                                            S.op("act", lambda: A.activation(out=ush[:, tb * 512:(tb + 1) * 512], in_=pu2[b][:],
                                                                             func=AF.Copy),
                                                 reads=[("pu2", b)], writes=["ush2"])
                                        else:
                                            S.op("dve", lambda: V.tensor_copy(out=ush[:, tb * 512:(tb + 1) * 512], in_=pu2[b][:]),
                                                 reads=[("pu2", b)], writes=["ush2"])
                                    shiftmix("ush2", ush, 128, i3 * 4 + j, dk, dst, utmp, "utmp2")
                                S.op("act", lambda: A.activation(out=vb[:], in_=vf[:], func=AF.Copy), reads=["vf"], writes=["vb"])
                                S.barrier()
                                slots = [ush[:, i * TB:(i + 1) * TB] for i in range(8)] + [utmp[:, i * TB:(i + 1) * TB] for i in range(8)]
                                (t_sig0, t_sig1, t_a0, t_a1, t_kk, t_x, t_y, t_kd, t_be, t_cs, t_e1, t_e2, t_e3, t_e4, t_ks, t_z) = slots
                                t_sig = (t_sig0, t_sig1)
                                t_a = (t_a0, t_a1)
                                for tb in range(NB):
                                    sl = slice(tb * TB, (tb + 1) * TB)
                                    csl = slice(tb * CPB, (tb + 1) * CPB)
                                    for d in range(2):
                                        S.op("pe", lambda: PE_.matmul(pl2[d], lhsT=w2c[64 * d:64 * d + 64, j * 128:(j + 1) * 128],
                                                                      rhs=wlT[64 * d:64 * d + 64, sl], start=True, stop=True),
                                             reads=["w2c"], writes=[("pl2", d)])
                                        S.op("act", lambda: A.activation(out=t_sig[d], in_=pl2[d], func=AF.Sigmoid,
                                                                         bias=C("w0", d * 4 + j)),
                                             reads=[("pl2", d), "cpack"], writes=[("sig", d)])
                                        S.op("pe", lambda: PE_.matmul(pl2[2 + d], lhsT=a2c[64 * d:64 * d + 64, j * 128:(j + 1) * 128],
                                                                      rhs=alT[64 * d:64 * d + 64, sl], start=True, stop=True),
                                             reads=["a2c"], writes=[("pl2", d)])
                                        S.op("act", lambda: A.activation(out=t_a[d], in_=pl2[2 + d], func=AF.Sigmoid,
                                                                         bias=C("a0", d * 4 + j)),
                                             reads=[("pl2", d), "cpack"], writes=[("a", d)])
                                    S.op("dve", lambda: V.tensor_scalar(out=t_x, in0=kf[:, sl], scalar1=C("k_k", j), scalar2=None,
                                                                        op0=ALU.mult), reads=["kf", "cpack"], writes=["t_x"])
                                    S.op("act", lambda: A.activation(out=sqb1[:], in_=t_x, func=AF.Square),
                                         reads=["t_x"], writes=["sqb1"])
                                    S.op("pe", lambda: PE_.matmul(pst, lhsT=bd_b[:], rhs=sqb1[:], start=True, stop=True),
                                         reads=["sqb1", "bd_b"], writes=["pst"])
                                    S.op("act", lambda: A.activation(out=t_y, in_=pst, func=AF.Ln, bias=kc[:, 3:4]),
                                         reads=["pst", "kc"], writes=["t_y"])
                                    S.op("act", lambda: A.activation(out=t_y, in_=t_y, func=AF.Exp, scale=-0.5), reads=["t_y"], writes=["t_y"])
                                    S.op("dve", lambda: V.tensor_tensor(out=t_kk, in0=t_x, in1=t_y, op=ALU.mult),
                                         reads=["t_x", "t_y"], writes=["t_kk"])
                                    for d in range(2):
                                        S.op("dve", lambda: V.tensor_scalar(out=t_x, in0=t_a[d], scalar1=C("k_a", j),
                                                                            scalar2=dc[:, 35 + j:36 + j], op0=ALU.mult, op1=ALU.add),
                                             reads=[("a", d), "cpack", "dc_omka"], writes=["t_x"])
                                        S.op("dve", lambda: V.tensor_tensor(out=t_kd, in0=t_x, in1=kf[:, sl], op=ALU.mult),
                                             reads=["t_x", "kf"], writes=["t_kd"])
                                        S.op("dve", lambda: V.tensor_tensor(out=t_be, in0=t_kk, in1=t_a[d], op=ALU.mult),
                                             reads=["t_kk", ("a", d)], writes=["t_be"])
                                        S.op("dve", lambda: V.tensor_tensor_scan(out=t_cs, data0=resetm[:], data1=t_sig[d], initial=0.0,
                                                                                 op0=ALU.mult, op1=ALU.add),
                                             reads=["resetm", ("sig", d)], writes=["t_cs"])
                                        S.op("dve", lambda: V.tensor_tensor(out=t_sig[d], in0=t_cs, in1=t_sig[d], op=ALU.subtract),
                                             reads=["t_cs", ("sig", d)], writes=[("sig", d)])
                                        t_csm = t_sig[d]
                                        tot = c3(t_cs)[:, :, 63:64]
                                        S.op("dve", lambda: V.tensor_scalar(out=nct[:, 0, :].rearrange("p (c o) -> p c o", o=1), in0=tot,
                                                                            scalar1=-CDEC, scalar2=None, op0=ALU.mult),
                                             reads=["t_cs"], writes=["nct"])
                                        S.op("dve", lambda: V.tensor_scalar(out=nct[:, 1, :].rearrange("p (c o) -> p c o", o=1), in0=tot,
                                                                            scalar1=CDEC, scalar2=None, op0=ALU.mult),
                                             reads=["t_cs"], writes=["nct"])
                                        S.op("act", lambda: A.activation(out=PLf[d][:, csl], in_=nct[:, 0, :], func=AF.Exp),
                                             reads=["nct"], writes=[("PLf", d)])
                                        if d == 0:
                                            S.op("act", lambda: A.activation(out=t_e1, in_=t_cs, func=AF.Exp, scale=-CDEC),
                                                 reads=["t_cs"], writes=["t_e1"])
                                            S.op("act", lambda: A.activation(out=t_e2, in_=t_cs, func=AF.Exp, scale=CDEC),
                                                 reads=["t_cs"], writes=["t_e2"])
                                            S.op("act", lambda: A.activation(out=t_e3, in_=t_csm, func=AF.Exp, scale=-CDEC),
                                                 reads=[("sig", d)], writes=["t_e3"])
                                            e_r, e_bk, e_a = t_e1, t_e2, t_e3
                                        else:
                                            for c8 in range(CPB):
                                                cs8 = slice(c8 * 64, (c8 + 1) * 64)
                                                S.op("act", lambda: A.activation(out=t_e1[:, cs8], in_=t_csm[:, cs8], func=AF.Exp,
                                                                                 scale=CDEC, bias=nct[:, 0, c8:c8 + 1]),
                                                     reads=[("sig", d), "nct"], writes=["t_e1"])
                                                S.op("act", lambda: A.activation(out=t_e2[:, cs8], in_=t_csm[:, cs8], func=AF.Exp,
                                                                                 scale=-CDEC, bias=nct[:, 1, c8:c8 + 1]),
                                                     reads=[("sig", d), "nct"], writes=["t_e2"])
                                                S.op("act", lambda: A.activation(out=t_e4[:, cs8], in_=t_cs[:, cs8], func=AF.Exp,
                                                                                 scale=CDEC, bias=nct[:, 0, c8:c8 + 1]),
                                                     reads=["t_cs", "nct"], writes=["t_e4"])
                                            e_r, e_bk, e_a = t_e1, t_e2, t_e4
                                        S.op("dve", lambda: V.tensor_tensor(out=AR[d][:, csl, 1, :], in0=c3(rf[:, sl]), in1=c3(e_r),
                                                                            op=ALU.mult),
                                             reads=["rf", "t_e1"], writes=[("AR", d, tb)])
                                        S.op("dve", lambda: V.scalar_tensor_tensor(out=AR[d][:, csl, 0, :], in0=c3(t_kk), scalar=-1.0,
                                                                                   in1=c3(e_a), op0=ALU.mult, op1=ALU.mult),
                                             reads=["t_kk", "t_e3", "t_e4"], writes=[("AR", d, tb)])
                                        S.op("dve", lambda: V.tensor_tensor(out=bbar[d][:, sl], in0=t_be, in1=e_bk, op=ALU.mult),
                                             reads=["t_be", "t_e2"], writes=[("bbar", d, tb)])
                                        S.op("dve", lambda: V.tensor_tensor(out=kbar[d][:, sl], in0=t_kd, in1=e_bk, op=ALU.mult),
                                             reads=["t_kd", "t_e2"], writes=[("kbar", d, tb)])
                                        if d == 0:
                                            S.op("dve", lambda: V.tensor_copy(out=t_ks, in_=t_kd), reads=["t_kd"], writes=["t_ks"])
                                        else:
                                            S.op("dve", lambda: V.tensor_tensor(out=t_ks, in0=t_ks, in1=t_kd, op=ALU.add),
                                                 reads=["t_kd", "t_ks"], writes=["t_ks"])
                                    S.op("dve", lambda: V.scalar_tensor_tensor(out=t_z, in0=rf[:, sl], scalar=C("r_k", j), in1=t_ks,
                                                                               op0=ALU.mult, op1=ALU.mult),
                                         reads=["rf", "t_ks", "cpack"], writes=["t_z"])
                                    S.op("pe", lambda: PE_.matmul(pbn, lhsT=bd_f[:], rhs=t_z, start=True, stop=True),
                                         reads=["t_z", "bd_f"], writes=["pbn"])
                                    S.op("dve", lambda: V.tensor_tensor(out=bonus[:, sl], in0=pbn, in1=vf[:, sl], op=ALU.mult),
                                         reads=["pbn", "vf"], writes=[("bonus", tb)])
                                for d in range(2):
                                    S.dma("sp", rhi[d][:], AR[d][64:128, :, 1, :], "rhi",
                                          reads=[("AR", d, tb) for tb in range(NB)], writes=[("rhi", d)])
                                    S.dma("sp", PLhi[d][:], PLf[d][64:128, :], "rhi", reads=[("PLf", d)], writes=[("PLhi", d)])
                                S.barrier()
                            tap("AR0", AR[0][:], [])
                            tap("AR1", AR[1][:], [])
                            tap("bbar0", bbar[0][:], [])
                            tap("kbar1", kbar[1][:], [])
                            tap("bonus", bonus[:], [])
                            tap("PLf0", PLf[0][:], [])
                            if stop_after == "rw_s1":
                                S.barrier()
                                continue
                            with ExitStack() as s2:
                                tok = [sb(s2, "tok%d" % i, [64, 8, 128], BF16) for i in range(2)]
                                btl = [sb(s2, "btl%d" % i, [128, 2, 2, 64], BF16) for i in range(2)]
                                AbR = [sb(s2, "AbR%d" % i, [64, 4, 128], BF16) for i in range(2)]
                                AkR = [sb(s2, "AkR%d" % i, [64, 4, 128], BF16) for i in range(2)]
                                MNq = [sb(s2, "MNq%d" % i, [64, 2, 4, 64], BF16) for i in range(2)]
                                Rq = [sb(s2, "Rq%d" % i, [64, 4, 64], BF16) for i in range(2)]
                                N0 = sb(s2, "N0", [64, 4, 64], BF16)
                                WuT = [sb(s2, "WuT%d" % i, [64, 4, 64], BF16) for i in range(2)]
                                AVs = sb(s2, "AVs", [64, 4, 64], BF16)
                                Uv = [sb(s2, "Uv%d" % i, [64, 4, 64], F32) for i in range(2)]
                                Us = sb(s2, "Us", [64, 4, 64], BF16)
                                Sf = sb(s2, "Sf", [64, 4, 64], F32)
                                Sb_ = sb(s2, "Sb", [64, 4, 64], BF16)
                                ptok = ps(s2, "ptok", [64, 1024], BF16)
                                pAB = [ps(s2, "pAB%d" % i, [64, 2, 2, 128]) for i in range(2)]
                                pMN = ps(s2, "pMN", [64, 2, 4, 64])
                                pR0 = ps(s2, "pR0", [64, 2, 4, 64])
                                pWA = ps(s2, "pWA", [64, 2, 4, 64])
                                pUU = ps(s2, "pUU", [64, 2, 4, 64])
                                pYS = ps(s2, "pYS", [64, 2, 4, 64])
                                S.op("dve", lambda: V.memset(Sf[:], 0.0), writes=["Sf"])
                                S.op("dve", lambda: V.memset(Sb_[:], 0.0), writes=["Sb"])
                                def gen_pre(i):
                                    b = i % 2
                                    cd = (i, 31 - i)
                                    tq = tok[b]
                                    for d in range(2):
                                        c = cd[d]
                                        for q, src in enumerate((bbar[d], kbar[d])):
                                            S.op("dve", lambda: V.tensor_scalar(out=btl[b][:, d, q, :], in0=src[:, c * 64:(c + 1) * 64],
                                                                                scalar1=PLf[d][:, c:c + 1], scalar2=None, op0=ALU.mult),
                                                 reads=[("PLf", d)], writes=[("btl", b)])
                                    for inst in range(4):
                                        hh, d = inst // 2, inst % 2
                                        c = cd[d]
                                        p0 = 64 * hh
                                        lb = bbar[d][p0:p0 + 64, c * 64:(c + 1) * 64]
                                        lk = kbar[d][p0:p0 + 64, c * 64:(c + 1) * 64]
                                        rAR = AR[d][p0:p0 + 64, c, :, :].rearrange("p a b -> p (a b)")
                                        pn0 = pR0[:, 1, inst, :] if hh == 0 else pMN[:, 1, inst, :]
                                        S.op("pe", lambda: PE_.matmul(pAB[hh][:, d, 0, :], lhsT=lb, rhs=rAR, start=True, stop=True),
                                             reads=[], writes=[("pAB", hh)])
                                        S.op("pe", lambda: PE_.matmul(pn0, lhsT=AR[d][p0:p0 + 64, c, 0, :], rhs=lb,
                                                                      start=True, stop=True),
                                             reads=[], writes=["pR0" if hh == 0 else "pMN"])
                                        S.op("pe", lambda: PE_.matmul(pAB[hh][:, d, 1, :], lhsT=lk, rhs=rAR, start=True, stop=True),
                                             reads=[], writes=[("pAB", hh)])
                                    for hh in range(2):
                                        hi = slice(2 * hh, 2 * hh + 2)
                                        S.op("dve", lambda: V.tensor_tensor(out=AbR[b][:, hi, :], in0=pAB[hh][:, :, 0, :], in1=maskAB[:, hi, :],
                                                                            op=ALU.mult),
                                             reads=[("pAB", hh), "maskAB"], writes=[("AbR", b)])
                                    S.op("dve", lambda: V.tensor_tensor(out=N0[:, 0:2, :], in0=pR0[:, 1, 0:2, :], in1=maskN[:, 0:2, :], op=ALU.mult),
                                         reads=["pR0", "maskN"], writes=["N0"])
                                    S.op("dve", lambda: V.tensor_tensor(out=N0[:, 2:4, :], in0=pMN[:, 1, 2:4, :], in1=maskN[:, 2:4, :], op=ALU.mult),
                                         reads=["pMN", "maskN"], writes=["N0"])
                                    yield
                                    for d in range(2):
                                        c = cd[d]
                                        srcs = (AR[d][:, c, 0, :], btl[b][:, d, 0, :], btl[b][:, d, 1, :])
                                        for q, src in enumerate(srcs):
                                            S.op("pe", lambda: PE_.transpose(out=ptok[:, (d * 3 + q) * 128:(d * 3 + q + 1) * 128], in_=src,
                                                                             identity=ident_b[:]),
                                                 reads=[("btl", b), "ident_b"], writes=["ptok"])
                                        S.op("pe", lambda: PE_.transpose(out=ptok[:, (6 + d) * 128:(7 + d) * 128],
                                                                         in_=vb[:, c * 64:(c + 1) * 64], identity=ident_b[:]),
                                             reads=["vb", "ident_b"], writes=["ptok"])
                                    S.op("act", lambda: A.activation(out=tq[:].rearrange("p a b -> p (a b)"), in_=ptok[:], func=AF.Copy),
                                         reads=["ptok"], writes=[("tok", b)])
                                    yield

                                    def mn_mm(l_, Mp_, Np_, rk_):
                                        for inst in range(4):
                                            if l_ < 5:
                                                S.op("pe", lambda: PE_.matmul(pMN[:, 0, inst, :], lhsT=Np_[:, inst, :], rhs=Mp_[:, inst, :],
                                                                              start=True, stop=True),
                                                     reads=rk_, writes=["pMN"])
                                            S.op("pe", lambda: PE_.matmul(pWA[:, 0, inst, :], lhsT=Mp_[:, inst, :], rhs=Np_[:, inst, :],
                                                                          start=True, stop=True),
                                                 reads=rk_, writes=["pWA"])

                                    def mn_copy(l_):
                                        q_ = l_ % 2
                                        if l_ < 5:
                                            S.op("act", lambda: A.activation(out=MNq[q_][:, 0, :, :], in_=pMN[:, 0, :, :], func=AF.Copy),
                                                 reads=["pMN"], writes=[("Mq", q_)])
                                        S.op("dve", lambda: V.tensor_copy(out=MNq[q_][:, 1, :, :], in_=pWA[:, 0, :, :]),
                                             reads=["pWA"], writes=[("Nq", q_)])

                                    mn_mm(1, AbR[b][:, :, 0:64], N0[:], [("AbR", b), "N0"])
                                    mn_copy(1)
                                    S.op("dve", lambda: V.tensor_tensor(out=Rq[0][:], in0=AbR[b][:, :, 0:64], in1=ident4[:], op=ALU.add),
                                         reads=[("AbR", b), "ident4"], writes=[("Rq", 0)])
                                    for hh in range(2):
                                        hi = slice(2 * hh, 2 * hh + 2)
                                        S.op("dve", lambda: V.tensor_tensor(out=AkR[b][:, hi, :], in0=pAB[hh][:, :, 1, :], in1=maskAB[:, hi, :],
                                                                            op=ALU.mult),
                                             reads=[("pAB", hh), "maskAB"], writes=[("AkR", b)])
                                    yield
                                    for l in range(1, 6):
                                        lb_ = l % 2
                                        MK = [("Mq", lb_), ("Nq", lb_)]
                                        if l < 5:
                                            mn_mm(l + 1, MNq[lb_][:, 0, :, :], MNq[lb_][:, 1, :, :], MK)
                                        for inst in range(4):
                                            S.op("pe", lambda: PE_.matmul(pR0[:, 0, inst, :], lhsT=MNq[lb_][:, 1, inst, :],
                                                                          rhs=Rq[1 - lb_][:, inst, :], start=True, stop=True),
                                                 reads=[("Nq", lb_), ("Rq", 1 - lb_)], writes=["pR0"])
                                        if l == 1:
                                            for inst in range(4):
                                                hh, d = inst // 2, inst % 2
                                                hs = slice(hh * 64, hh * 64 + 64)
                                                S.op("pe", lambda: PE_.matmul(pUU[:, 0, inst, :], lhsT=AkR[b][:, inst, 0:64], rhs=tq[:, 6 + d, hs],
                                                                              start=True, stop=True),
                                                     reads=[("tok", b), ("AkR", b)], writes=["pUU"])
                                        if l < 5:
                                            mn_copy(l + 1)
                                        S.op("dve", lambda: V.tensor_tensor(out=Rq[lb_][:], in0=pR0[:, 0, :, :], in1=Rq[1 - lb_][:],
                                                                            op=ALU.add),
                                             reads=["pR0", ("Rq", 1 - lb_)], writes=[("Rq", lb_)])
                                        if l == 1:
                                            S.op("dve", lambda: V.tensor_copy(out=AVs[:], in_=pUU[:, 0, :, :]), reads=["pUU"], writes=["AVs"])
                                        yield
                                    R = Rq[1]
                                    for inst in range(4):
                                        hh, d = inst // 2, inst % 2
                                        hs = slice(hh * 64, hh * 64 + 64)
                                        S.op("pe", lambda: PE_.matmul(pWA[:, 0, inst, :], lhsT=tq[:, d * 3, hs], rhs=R[:, inst, :],
                                                                      start=True, stop=True),
                                             reads=[("tok", b), ("Rq", 1)], writes=["pWA"])
                                        S.op("pe", lambda: PE_.matmul(pR0[:, 1, inst, :], lhsT=R[:, inst, :], rhs=AVs[:, inst, :],
                                                                      start=True, stop=True),
                                             reads=[("Rq", 1), "AVs"], writes=["pR0"])
                                    yield
                                    S.op("act", lambda: A.activation(out=WuT[b][:], in_=pWA[:, 0, :, :], func=AF.Copy),
                                         reads=["pWA"], writes=[("WuT", b)])
                                    S.op("dve", lambda: V.tensor_copy(out=Uv[b][:], in_=pR0[:, 1, :, :]),
                                         reads=["pR0"], writes=[("Uv", b)])
                                    yield

                                def gen_loop(i):
                                    b = i % 2
                                    cd = (i, 31 - i)
                                    tq = tok[b]
                                    for inst in range(4):
                                        yield
                                        S.op("pe", lambda: PE_.matmul(pUU[:, 1, inst, :], lhsT=WuT[b][:, inst, :], rhs=Sb_[:, inst, :],
                                                                      start=True, stop=True),
                                             reads=[("WuT", b), "Sb"], writes=["pUU"])
                                    yield
                                    S.op("dve", lambda: V.tensor_tensor(out=Us[:], in0=pUU[:, 1, :, :], in1=Uv[b][:], op=ALU.add),
                                         reads=["pUU", ("Uv", b)], writes=["Us"])
                                    yield
                                    for inst in range(4):
                                        hh, d = inst // 2, inst % 2
                                        c = cd[d]
                                        hs = slice(hh * 64, hh * 64 + 64)
                                        rr = AR[d][0:64, c, 1, :] if hh == 0 else rhi[d][:, c, :]
                                        yield
                                        S.op("pe", lambda: PE_.matmul(pYS[:, 0, inst, :], lhsT=Sb_[:, inst, :], rhs=rr, start=True, stop=False),
                                             reads=["Sb", ("rhi", d)], writes=["pYS"])
                                        yield
                                        S.op("pe", lambda: PE_.matmul(pYS[:, 0, inst, :], lhsT=tq[:, 6 + d, hs], rhs=AkR[b][:, inst, 64:128],
                                                                      start=False, stop=False),
                                             reads=[("tok", b), ("AkR", b)], writes=["pYS"])
                                        yield
                                        S.op("pe", lambda: PE_.matmul(pYS[:, 0, inst, :], lhsT=Us[:, inst, :], rhs=AbR[b][:, inst, 64:128],
                                                                      start=False, stop=True),
                                             reads=["Us", ("AbR", b)], writes=["pYS"])
                                        yield
                                        S.op("pe", lambda: PE_.matmul(pYS[:, 1, inst, :], lhsT=tq[:, d * 3 + 1, hs], rhs=Us[:, inst, :],
                                                                      start=True, stop=False),
                                             reads=[("tok", b), "Us"], writes=["pYS"])
                                        yield
                                        S.op("pe", lambda: PE_.matmul(pYS[:, 1, inst, :], lhsT=tq[:, d * 3 + 2, hs], rhs=tq[:, 6 + d, hs],
                                                                      start=False, stop=True),
                                             reads=[("tok", b)], writes=["pYS"])
                                    yield
                                    for d in range(2):
                                        c = cd[d]
                                        dst = yacc[:, :, c * 64:(c + 1) * 64]
                                        src = pYS[:, 0, :, :].rearrange("p (h e) w -> p h e w", e=2)[:, :, d, :]
                                        if i < 16:
                                            S.op("dve", lambda: V.tensor_copy(out=dst, in_=src),
                                                 reads=["pYS"], writes=[("yacc", c)])
                                        else:
                                            S.op("dve", lambda: V.tensor_tensor(out=dst, in0=src, in1=dst, op=ALU.add),
                                                 reads=["pYS", ("yacc", c)], writes=[("yacc", c)])
                                    yield
                                    for inst in range(4):
                                        hh, d = inst // 2, inst % 2
                                        c = cd[d]
                                        plc = PLf[d][0:64, c:c + 1] if hh == 0 else PLhi[d][:, c:c + 1]
                                        yield
                                        S.op("dve", lambda: V.scalar_tensor_tensor(out=Sf[:, inst, :], in0=Sf[:, inst, :], scalar=plc,
                                                                                   in1=pYS[:, 1, inst, :], op0=ALU.mult, op1=ALU.add),
                                             reads=["pYS", "Sf", ("PLhi", d), ("PLf", d)], writes=["Sf"])
                                    yield
                                    S.op("act", lambda: A.activation(out=Sb_[:], in_=Sf[:], func=AF.Copy), reads=["Sf"], writes=["Sb"])
                                    yield

                                def drive(ga, gb):
                                    a_done = b_done = False
                                    while not (a_done and b_done):
                                        if not a_done:
                                            try:
                                                next(ga)
                                            except StopIteration:
                                                a_done = True
                                        if not b_done:
                                            try:
                                                next(gb)
                                            except StopIteration:
                                                b_done = True

                                for _ in gen_pre(0):
                                    pass
                                for i in range(32):
                                    drive(gen_loop(i), gen_pre(i + 1) if i + 1 < 32 else iter(()))
                                S.barrier()
                            with ExitStack() as s3:
                                y128 = sb(s3, "y128", [128, SEQ], F32)
                                fa = sb(s3, "fa", [128, 512], F32)
                                fb = sb(s3, "fb", [128, 512], F32)
                                pm = ps(s3, "pm", [128, 512])
                                pv = ps(s3, "pv", [128, 512])
                                pg = ps(s3, "pg", [128, 512])
                                S.op("act", lambda: A.activation(out=y128[0:64, :], in_=yacc[:, 0, :], func=AF.Copy),
                                     reads=[], writes=["y128a"])
                                S.dma("sp", y128[64:128, :], yacc[:, 1, :], "ymv", reads=[], writes=["y128b"])
                                tap("y128", y128[:], ["y128a", "y128b"])
                                for tb in range(4):
                                    sl = slice(tb * 512, (tb + 1) * 512)
                                    S.op("pe", lambda: PE_.matmul(pm[:], lhsT=bd_f[:], rhs=y128[:, sl], start=True, stop=True),
                                         reads=["y128a", "y128b", "bd_f"], writes=["pm"])
                                    S.op("dve", lambda: V.scalar_tensor_tensor(out=fa[:], in0=pm[:], scalar=-1.0 / 64, in1=y128[:, sl],
                                                                               op0=ALU.mult, op1=ALU.add),
                                         reads=["pm", "y128a", "y128b"], writes=["fa"])
                                    S.op("act", lambda: A.activation(out=fb[:], in_=fa[:], func=AF.Square), reads=["fa"], writes=["fb"])
                                    S.op("pe", lambda: PE_.matmul(pv[:], lhsT=bd_f[:], rhs=fb[:], start=True, stop=True),
                                         reads=["fb", "bd_f"], writes=["pv"])
                                    S.op("act", lambda: A.activation(out=fb[:], in_=pv[:], func=AF.Ln, bias=EPSLN, scale=1.0 / 64),
                                         reads=["pv", "kc"], writes=["fb"])
                                    S.op("act", lambda: A.activation(out=fb[:], in_=fb[:], func=AF.Exp, scale=-0.5), reads=["fb"], writes=["fb"])
                                    S.op("dve", lambda: V.tensor_tensor(out=fa[:], in0=fa[:], in1=fb[:], op=ALU.mult),
                                         reads=["fa", "fb"], writes=["fa"])
                                    S.op("dve", lambda: V.tensor_scalar(out=fa[:], in0=fa[:], scalar1=C("lnx_g", j), scalar2=C("lnx_b", j),
                                                                        op0=ALU.mult, op1=ALU.add),
                                         reads=["fa", "cpack"], writes=["fa"])
                                    S.op("dve", lambda: V.tensor_tensor(out=fa[:], in0=fa[:], in1=bonus[:, sl], op=ALU.add),
                                         reads=["fa"], writes=["fa"])
                                    S.op("pe", lambda: PE_.matmul(pg[:], lhsT=g2a[:, j * 128:(j + 1) * 128], rhs=glT[:, sl],
                                                                  start=True, stop=False), reads=["g2a"], writes=["pg"])
                                    S.op("pe", lambda: PE_.matmul(pg[:], lhsT=g2b[0:32, j * 128:(j + 1) * 128], rhs=gl2T[0:32, sl],
                                                                  start=False, stop=True), reads=["g2b"], writes=["pg"])
                                    S.op("dve", lambda: V.tensor_tensor(out=o_rwT[:, j, sl], in0=fa[:], in1=pg[:], op=ALU.mult),
                                         reads=["fa", "pg"], writes=[("o_rwT", j, tb)])
                                S.barrier()
                    S.barrier()
                tap("o_rwT", o_rwT[:] if stop_after != "rw1" else o_rwT[:, 0:1, :], [])
                S.barrier()
                mixs.close()
                if stop_after in ("attn", "rw", "rw1", "rw_s1", "p2a", "p1"):
                    S.barrier()
                    continue
                h = sb(sq, "h", [128, NT, DM], F32)
                HK = [("h", tt) for tt in range(NT)]
                with ExitStack() as st:
                    wo = sb(st, "wo", [128, 8, DM], BF16)
                    S.dma("pool", wo[:], w_out_d.rearrange("(c p) n -> p c n", p=128), "wo", writes=["wo"])
                    xt2 = [sb(st, "xt2%d" % i, [128, DM], F32) for i in range(2)]
                    po = [[ps(st, "po%d%d" % (i, k), [128, 512]) for k in range(2)] for i in range(2)]
                    for tt in range(NT):
                        b = tt % 2
                        S.dma("sp", xt2[b][:], x_d[s, tt * 128:(tt + 1) * 128, :], ("xt2", b), writes=[("xt2", b)])
                        for dh in range(2):
                            for c in range(8):
                                src = o_daT if c < 4 else o_rwT
                                S.op("pe", lambda: PE_.matmul(po[b][dh][:], lhsT=src[:, c % 4, tt * 128:(tt + 1) * 128],
                                                              rhs=wo[:, c, dh * 512:(dh + 1) * 512], start=(c == 0), stop=(c == 7)),
                                     reads=["wo"], writes=[("po", b, dh)])
                            S.op("dve", lambda: V.tensor_tensor(out=h[:, tt, dh * 512:(dh + 1) * 512], in0=po[b][dh][:],
                                                                in1=xt2[b][:, dh * 512:(dh + 1) * 512], op=ALU.add),
                                 reads=[("po", b, dh), ("xt2", b)], writes=[("h", tt)])
                    S.barrier()
                tap("h1", h[:], [])
                if stop_after == "h1":
                    S.barrier()
                    continue

                def load_bp(st_, name):
                    o, n = bp_off[name]
                    t_ = sb(st_, "bp_" + name, [128, n], F32)
                    S.dma("sp", t_[:], bpack_d[:, o:o + n], "bpk", writes=["bp_" + name])
                    return t_

                with ExitStack() as me:
                    Xn = sb(me, "Xn", [128, NT, DM], BF16)
                    aff_tok = sb(me, "aff_tok", [128, NT, NE], F32)
                    aff_hl = sb(me, "aff_hl", [128, NT, NE, 2], BF16)
                    posm_tok = sb(me, "posm_tok", [128, NT, NE], F32)
                    posm_b = sb(me, "posm_b", [NE, SEQ], BF16)
                    Eoh = sb(me, "Eoh", [NE, NE, 128], BF16)
                    S.op("dve", lambda: V.memset(Eoh[:], 1.0), writes=["Eoh"])
                    for e in range(NE):
                        S.op("dve", lambda: V.tensor_scalar(out=Eoh[:, e, :], in0=Eoh[:, e, :], scalar1=ident_f[0:NE, e:e + 1],
                                                            scalar2=None, op0=ALU.mult), reads=["Eoh", "ident_f"], writes=["Eoh"])
                    with ExitStack() as st:
                        gffn = load_bp(st, "g_ffn")
                        wr_f = sb(st, "wr_f", [128, 8, NE], F32)
                        S.dma("sp", wr_f[:], wr_d.rearrange("(c p) e -> p c e", p=128), "wrf", writes=["wr_f"])
                        affT = sb(st, "affT", [NE, SEQ], F32)
                        wk = sb(st, "wk", [NE, SEQ], F32)
                        maskT = sb(st, "maskT", [NE, SEQ], F32)
                        cumT = sb(st, "cumT", [NE, SEQ], F32)
                        ones16 = sb(st, "ones16", [NE, SEQ], F32)
                        m8 = sb(st, "m8", [NE, 8], F32)
                        xnf = [sb(st, "xnf%d" % i, [128, DM], F32) for i in range(2)]
                        xnTf = [sb(st, "xnTf%d" % i, [128, 8, 128], F32) for i in range(2)]
                        junk3 = sb(st, "junk3", [128, DM], BF16)
                        sst = sb(st, "sst", [128, 3 * NT], F32)
                        sm = sb(st, "sm", [128, 4 * NT], F32)
                        ex = sb(st, "ex", [128, NE], F32)
                        dtmp = sb(st, "dtmp", [128, NE], F32)
                        pT = [ps(st, "pT%d" % i, [128, 512]) for i in range(2)]
                        plg = ps(st, "plg", [128, 512])
                        paT = ps(st, "paT", [NE, 512])
                        ppm = ps(st, "ppm", [128, 512])
                        S.op("pool", lambda: G.memset(ones16[:], 1.0), writes=["ones16"])
                        for tt in range(NT):
                            b = tt % 2
                            S.op("act", lambda: A.activation(out=junk3[:], in_=h[:, tt, :], func=AF.Square,
                                                             accum_out=sst[:, tt:tt + 1]),
                                 reads=[("h", tt)], writes=["junk3", ("sst", tt)], fuse=False)
                            S.op("act", lambda: A.activation(out=sst[:, NT + tt:NT + tt + 1], in_=sst[:, tt:tt + 1], func=AF.Sqrt,
                                                             bias=EPS, scale=1.0 / DM), reads=[("sst", tt), "kc"], writes=[("sst1", tt)])
                            S.op("dve", lambda: V.reciprocal(out=sst[:, 2 * NT + tt:2 * NT + tt + 1], in_=sst[:, NT + tt:NT + tt + 1]),
                                 reads=[("sst1", tt)], writes=[("sst2", tt)])
                            S.op("dve", lambda: V.scalar_tensor_tensor(out=xnf[b][:], in0=h[:, tt, :],
                                                                       scalar=sst[:, 2 * NT + tt:2 * NT + tt + 1], in1=gffn[:],
                                                                       op0=ALU.mult, op1=ALU.mult),
                                 reads=[("h", tt), ("sst2", tt), "bp_g_ffn"], writes=[("xnf", b)])
                            S.op("act", lambda: A.activation(out=Xn[:, tt, :], in_=xnf[b][:], func=AF.Copy),
                                 reads=[("xnf", b)], writes=[("Xn", tt)])
                            for c in range(8):
                                S.op("pe", lambda: PE_.transpose(out=pT[c // 4][:, (c % 4) * 128:(c % 4 + 1) * 128],
                                                                 in_=xnf[b][:, c * 128:(c + 1) * 128], identity=ident_f[:]),
                                     reads=[("xnf", b), "ident_f"], writes=[("pT", c // 4)])
                            S.op("dve", lambda: V.tensor_copy(out=xnTf[b][:, 0:4, :].rearrange("p a b -> p (a b)"), in_=pT[0][:]),
                                 reads=[("pT", 0)], writes=[("xnTf", b, 0)])
                            S.op("act", lambda: A.activation(out=xnTf[b][:, 4:8, :].rearrange("p a b -> p (a b)"), in_=pT[1][:],
                                                             func=AF.Copy), reads=[("pT", 1)], writes=[("xnTf", b, 1)])
                            for c in range(8):
                                S.op("pe", lambda: PE_.matmul(plg[:, 0:NE], lhsT=xnTf[b][:, c, :], rhs=wr_f[:, c, :],
                                                              start=(c == 0), stop=(c == 7)),
                                     reads=[("xnTf", b, 0), ("xnTf", b, 1), "wr_f"], writes=["plg"])
                            S.op("dve", lambda: V.reduce_max(out=sm[:, tt:tt + 1], in_=plg[:, 0:NE], axis=mybir.AxisListType.X),
                                 reads=["plg"], writes=[("sm0", tt)])
                            S.op("dve", lambda: V.tensor_scalar(out=sm[:, NT + tt:NT + tt + 1], in0=sm[:, tt:tt + 1], scalar1=-1.0,
                                                                scalar2=None, op0=ALU.mult), reads=[("sm0", tt)], writes=[("sm1", tt)])
                            S.op("dve", lambda: V.tensor_scalar(out=ex[:], in0=plg[:, 0:NE], scalar1=sm[:, NT + tt:NT + tt + 1],
                                                                scalar2=None, op0=ALU.add), reads=["plg", ("sm1", tt)], writes=["exa"])
                            S.op("act", lambda: A.activation(out=ex[:], in_=ex[:], func=AF.Exp,
                                                             accum_out=sm[:, 2 * NT + tt:2 * NT + tt + 1]),
                                 reads=["exa"], writes=["ex", ("sm2", tt)], fuse=False)
                            S.op("dve", lambda: V.reciprocal(out=sm[:, 3 * NT + tt:3 * NT + tt + 1], in_=sm[:, 2 * NT + tt:2 * NT + tt + 1]),
                                 reads=[("sm2", tt)], writes=[("sm3", tt)])
                            S.op("dve", lambda: V.tensor_scalar(out=aff_tok[:, tt, :], in0=ex[:], scalar1=sm[:, 3 * NT + tt:3 * NT + tt + 1],
                                                                scalar2=None, op0=ALU.mult), reads=["ex", ("sm3", tt)], writes=[("aff", tt)])
                            S.op("dve", lambda: V.tensor_copy(out=aff_hl[:, tt, :, 0], in_=aff_tok[:, tt, :]),
                                 reads=[("aff", tt)], writes=[("affh", tt)])
                            S.op("dve", lambda: V.tensor_tensor(out=dtmp[:], in0=aff_tok[:, tt, :], in1=aff_hl[:, tt, :, 0], op=ALU.subtract),
                                 reads=[("aff", tt), ("affh", tt)], writes=["dtmp"])
                            S.op("dve", lambda: V.tensor_copy(out=aff_hl[:, tt, :, 1], in_=dtmp[:]), reads=["dtmp"], writes=[("affl", tt)])
                            S.op("pe", lambda: PE_.transpose(out=paT[:, (tt % 4) * 128:(tt % 4 + 1) * 128], in_=aff_tok[:, tt, :],
                                                             identity=ident_f[:]), reads=[("aff", tt), "ident_f"], writes=["paT"])
                            if tt % 4 == 3:
                                t0 = (tt - 3) * 128
                                S.op("dve", lambda: V.tensor_copy(out=affT[:, t0:t0 + 512], in_=paT[:]), reads=["paT"], writes=["affT"])
                        S.op("dve", lambda: V.tensor_copy(out=wk[:], in_=affT[:]), reads=["affT"], writes=["wk"])
                        for r in range(CAP // 8):
                            S.op("dve", lambda: V.max(out=m8[:], in_=wk[:]), reads=["wk"], writes=["m8"], fuse=False)
                            if r < CAP // 8 - 1:
                                S.op("dve", lambda: V.match_replace(out=wk[:], in_to_replace=m8[:], in_values=wk[:], imm_value=-1.0),
                                     reads=["wk", "m8"], writes=["wk"], fuse=False)
                        S.op("dve", lambda: V.tensor_scalar(out=maskT[:], in0=affT[:], scalar1=m8[:, 7:8], scalar2=None, op0=ALU.is_ge),
                             reads=["affT", "m8"], writes=["maskT"])
                        S.op("dve", lambda: V.tensor_tensor_scan(out=cumT[:], data0=ones16[:], data1=maskT[:], initial=0.0,
                                                                 op0=ALU.mult, op1=ALU.add), reads=["ones16", "maskT"], writes=["cumT"])
                        S.op("dve", lambda: V.tensor_tensor(out=cumT[:], in0=cumT[:], in1=maskT[:], op=ALU.mult),
                             reads=["cumT", "maskT"], writes=["cumT"])
                        S.op("dve", lambda: V.tensor_scalar(out=cumT[:], in0=cumT[:], scalar1=-1.0, scalar2=None, op0=ALU.add),
                             reads=["cumT"], writes=["cumT"])
                        S.op("dve", lambda: V.tensor_copy(out=posm_b[:], in_=cumT[:]), reads=["cumT"], writes=["posm_b"])
                        for tt in range(NT):
                            S.op("pe", lambda: PE_.transpose(out=ppm[:, (tt % 16) * NE:(tt % 16 + 1) * NE], in_=cumT[:, tt * 128:(tt + 1) * 128],
                                                             identity=ident_f[0:NE, 0:NE]), reads=["cumT", "ident_f"], writes=["ppm"])
                        S.op("dve", lambda: V.tensor_copy(out=posm_tok[:].rearrange("p a b -> p (a b)"), in_=ppm[:, 0:NT * NE]),
                             reads=["ppm"], writes=["posm_tok"])
                        S.barrier()
                    tap("aff_tok", aff_tok[:], [])
                    tap("posm_tok", posm_tok[:], [])
                    if stop_after == "router":
                        S.barrier()
                        continue
                    with ExitStack() as st:
                        iotar = load_bp(st, "iota_row")
                        Sel = sb(st, "Sel", [128, NT, CAP], BF16)
                        SelT = sb(st, "SelT", [128, 2, SEQ], BF16)
                        XeT = sb(st, "XeT", [128, 8, CAP], BF16)
                        HT = [sb(st, "HT%d" % i, [128, 2, CAP], BF16) for i in range(2)]
                        s1b = [sb(st, "s1b%d" % i, [128, CAP], F32) for i in range(2)]
                        Ysb = sb(st, "Ysb", [128, 2, DM], BF16)
                        gate = sb(st, "gate", [128, 2], F32)
                        gate4 = sb(st, "gate4", [128, 2, 2], F32)
                        w1b = [sb(st, "w1b%d" % i, [128, 8, 256], BF16) for i in range(2)]
                        w3b = [sb(st, "w3b%d" % i, [128, 8, 256], BF16) for i in range(2)]
                        w2b = [sb(st, "w2b%d" % i, [128, 2, DM], BF16) for i in range(2)]
                        pG = [ps(st, "pG%d" % i, [128, 2, CAP]) for i in range(2)]
                        pH = [ps(st, "pH%d" % i, [128, 2, CAP]) for i in range(2)]
                        pY = [[ps(st, "pY%d%d" % (i, k), [128, 512]) for k in range(2)] for i in range(2)]
                        wcnt = [0]

                        def load_w(e, fb):
                            wb_ = wcnt[0] % 2
                            wcnt[0] += 1
                            S.dma("pool", w1b[wb_][:], w1_d[e, :, fb * 256:(fb + 1) * 256].rearrange("(c p) n -> p c n", p=128),
                                  ("w1b", wb_), writes=[("w1b", wb_)])
                            S.dma("pool", w3b[wb_][:], w3_d[e, :, fb * 256:(fb + 1) * 256].rearrange("(c p) n -> p c n", p=128),
                                  ("w3b", wb_), writes=[("w3b", wb_)])
                            S.dma("pool", w2b[wb_][:], w2_d[e, fb * 256:(fb + 1) * 256, :].rearrange("(c p) n -> p c n", p=128),
                                  ("w2b", wb_), writes=[("w2b", wb_)])
                            return wb_

                        n_exp = NE if not _os.environ.get("K_NEXP") else int(_os.environ["K_NEXP"])
                        blocks = [(e, fb) for e in range(n_exp) for fb in range(8)]
                        wslot = {}
                        wslot[blocks[0]] = load_w(*blocks[0])
                        hcnt = [0]
                        for e in range(n_exp):
                            def build_sel(e_):
                                for tt in range(NT):
                                    S.op("dve", lambda: V.tensor_scalar(out=Sel[:, tt, :], in0=iotar[:], scalar1=posm_tok[:, tt, e_:e_ + 1],
                                                                        scalar2=None, op0=ALU.is_equal),
                                         reads=["posm_tok", "bp_iota_row"], writes=[("Sel", tt)])
                            if e == 0:
                                build_sel(0)
                            for tb in range(4):
                                pbc = pH[tb % 2][:].rearrange("p a b -> p (a b)")
                                S.op("pe", lambda: PE_.matmul(pbc, lhsT=Eoh[:, e, :], rhs=posm_b[:, tb * 512:(tb + 1) * 512],
                                                              start=True, stop=True), reads=["Eoh", "posm_b"], writes=[("pH", tb % 2)])
                                for cc in range(2):
                                    S.op("dve", lambda: V.tensor_scalar(out=SelT[:, cc, tb * 512:(tb + 1) * 512], in0=pbc,
                                                                        scalar1=C("iota_p" if cc == 0 else "iota_p1"), scalar2=None,
                                                                        op0=ALU.is_equal),
                                         reads=[("pH", tb % 2), "cpack"], writes=[("SelT", cc, tb)])
                            pgt = pH[0][:, 0, 0:4].rearrange("p (a b) -> p a b", b=2)
                            for cc in range(2):
                                for tt in range(NT):
                                    S.op("pe", lambda: PE_.matmul(pgt[:, cc, :], lhsT=Sel[:, tt, cc * 128:(cc + 1) * 128],
                                                                  rhs=aff_hl[:, tt, e, :], start=(tt == 0), stop=(tt == NT - 1)),
                                         reads=[("Sel", tt), ("affh", tt), ("affl", tt)], writes=[("pH", 0)])
                            S.op("dve", lambda: V.tensor_copy(out=gate4[:], in_=pgt), reads=[("pH", 0)], writes=["gate4"])
                            S.op("dve", lambda: V.tensor_tensor(out=gate[:], in0=gate4[:, :, 0], in1=gate4[:, :, 1], op=ALU.add),
                                 reads=["gate4"], writes=["gate"])
                            for dcb in range(4):
                                gb = dcb % 2
                                for k in range(2):
                                    dc_ = dcb * 2 + k
                                    for tt in range(NT):
                                        S.op("pe", lambda: PE_.matmul(pG[gb][:, k, :], lhsT=Xn[:, tt, dc_ * 128:(dc_ + 1) * 128],
                                                                      rhs=Sel[:, tt, :], start=(tt == 0), stop=(tt == NT - 1)),
                                             reads=[("Xn", tt), ("Sel", tt)], writes=[("pG", gb)])
                                if gb == 0:
                                    S.op("act", lambda: A.activation(out=XeT[:, dcb * 2:dcb * 2 + 2, :], in_=pG[gb][:], func=AF.Copy),
                                         reads=[("pG", gb)], writes=[("XeT", dcb)])
                                else:
                                    S.op("dve", lambda: V.tensor_copy(out=XeT[:, dcb * 2:dcb * 2 + 2, :], in_=pG[gb][:]),
                                         reads=[("pG", gb)], writes=[("XeT", dcb)])
                            XK = [("XeT", q) for q in range(4)]
                            if e + 1 < n_exp:
                                build_sel(e + 1)
                            for fb in range(8):
                                wb_ = wslot[(e, fb)]
                                nxt = blocks.index((e, fb)) + 1
                                if nxt < len(blocks):
                                    wslot[blocks[nxt]] = load_w(*blocks[nxt])
                                hb = hcnt[0] % 2
                                hcnt[0] += 1
                                for fc in range(2):
                                    pb_ = fc % 2
                                    for k, wsrc, wk_ in ((0, w1b, "w1b"), (1, w3b, "w3b")):
                                        for dc_ in range(8):
                                            S.op("pe", lambda: PE_.matmul(pH[pb_][:, k, :], lhsT=wsrc[wb_][:, dc_, fc * 128:(fc + 1) * 128],
                                                                          rhs=XeT[:, dc_, :], start=(dc_ == 0), stop=(dc_ == 7)),
                                                 reads=XK + [(wk_, wb_)], writes=[("pH", pb_)])
                                    S.op("act", lambda: A.activation(out=s1b[pb_][:], in_=pH[pb_][:, 0, :], func=AF.Silu),
                                         reads=[("pH", pb_)], writes=[("s1b", pb_)])
                                    S.op("dve", lambda: V.tensor_tensor(out=HT[hb][:, fc, :], in0=pH[pb_][:, 1, :], in1=s1b[pb_][:], op=ALU.mult),
                                         reads=[("pH", pb_), ("s1b", pb_)], writes=[("HT", hb, fc)])
                                for cc in range(2):
                                    for dh in range(2):
                                        for fc in range(2):
                                            S.op("pe", lambda: PE_.matmul(pY[cc][dh][:], lhsT=HT[hb][:, fc, cc * 128:(cc + 1) * 128],
                                                                          rhs=w2b[wb_][:, fc, dh * 512:(dh + 1) * 512],
                                                                          start=(fb == 0 and fc == 0), stop=(fb == 7 and fc == 1)),
                                                 reads=[("HT", hb, fc), ("w2b", wb_)], writes=[("pY", cc, dh)])
                            for cc in range(2):
                                for dh in range(2):
                                    S.op("act", lambda: A.activation(out=Ysb[:, cc, dh * 512:(dh + 1) * 512], in_=pY[cc][dh][:], func=AF.Copy,
                                                                     scale=gate[:, cc:cc + 1]),
                                         reads=[("pY", cc, dh), "gate"], writes=[("Ysb", cc)])
                            for tt in range(NT):
                                for dh in range(2):
                                    oi = (tt * 2 + dh) % 4
                                    pO = pY[oi // 2][oi % 2][:]
                                    ok_ = ("pY", oi // 2, oi % 2)
                                    for cc in range(2):
                                        S.op("pe", lambda: PE_.matmul(pO, lhsT=SelT[:, cc, tt * 128:(tt + 1) * 128],
                                                                      rhs=Ysb[:, cc, dh * 512:(dh + 1) * 512], start=(cc == 0), stop=(cc == 1)),
                                             reads=[("SelT", cc, tt // 4), ("Ysb", cc)], writes=[ok_])
                                    S.op("dve", lambda: V.tensor_tensor(out=h[:, tt, dh * 512:(dh + 1) * 512], in0=pO,
                                                                        in1=h[:, tt, dh * 512:(dh + 1) * 512], op=ALU.add),
                                         reads=[ok_, ("h", tt)], writes=[("h", tt)])
                        S.barrier()
                    S.barrier()
                tap("h2", h[:], [])
                if stop_after == "moe":
                    S.barrier()
                    continue

                with ExitStack() as pl:
                    gpost = load_bp(pl, "g_post")
                    hnT = sb(pl, "hnT", [128, 8, SEQ], BF16)
                    wg = sb(pl, "wg", [128, 8, DM], BF16)
                    wpe = sb(pl, "wpe", [128, 2, DM], BF16)
                    S.dma("pool", wg[:], wg_d.rearrange("(c p) n -> p c n", p=128), "wg", writes=["wg"])
                    S.dma("pool", wpe[:], wpe_d.rearrange("(c p) n -> p c n", p=128), "wpe", writes=["wpe"])
                    hs = [sb(pl, "hs%d" % i, [128, DM], BF16) for i in range(2)]
                    junk4 = sb(pl, "junk4", [128, DM], BF16)
                    st4 = sb(pl, "st4", [128, 8 * NT], F32)
                    pls = ExitStack()
                    pt4 = [ps(pls, "pt4%d" % i, [128, 1024], BF16) for i in range(2)]
                    for tt in range(NT):
                        b = tt % 2
                        S.op("act", lambda: A.activation(out=junk4[:], in_=h[:, tt, :], func=AF.Square, accum_out=st4[:, tt:tt + 1]),
                             reads=[("h", tt)], writes=["junk4", ("st4", tt)], fuse=False)
                        S.op("act", lambda: A.activation(out=st4[:, NT + tt:NT + tt + 1], in_=st4[:, tt:tt + 1], func=AF.Sqrt, bias=EPS,
                                                         scale=1.0 / DM), reads=[("st4", tt), "kc"], writes=[("st41", tt)])
                        S.op("dve", lambda: V.reciprocal(out=st4[:, 2 * NT + tt:2 * NT + tt + 1], in_=st4[:, NT + tt:NT + tt + 1]),
                             reads=[("st41", tt)], writes=[("st42", tt)])
                        S.op("dve", lambda: V.tensor_scalar(out=hs[b][:], in0=h[:, tt, :], scalar1=st4[:, 2 * NT + tt:2 * NT + tt + 1],
                                                            scalar2=None, op0=ALU.mult), reads=[("h", tt), ("st42", tt)], writes=[("hs", b)])
                        for c in range(8):
                            S.op("pe", lambda: PE_.transpose(out=pt4[b][:, c * 128:(c + 1) * 128], in_=hs[b][:, c * 128:(c + 1) * 128],
                                                             identity=ident_b[:]), reads=[("hs", b), "ident_b"], writes=[("pt4", b)])
                        for c in range(8):
                            if b == 0:
                                S.op("dve", lambda: V.tensor_scalar(out=hnT[:, c, tt * 128:(tt + 1) * 128], in0=pt4[b][:, c * 128:(c + 1) * 128],
                                                                    scalar1=C("g_ple", c), scalar2=None, op0=ALU.mult),
                                     reads=[("pt4", b), "cpack"], writes=[("hnT", tt)])
                            else:
                                S.op("act", lambda: A.activation(out=hnT[:, c, tt * 128:(tt + 1) * 128], in_=pt4[b][:, c * 128:(c + 1) * 128],
                                                                 func=AF.Copy, scale=C("g_ple", c)),
                                     reads=[("pt4", b), "cpack"], writes=[("hnT", tt)])
                    S.barrier()
                    pls.close()
                    with ExitStack() as st:
                        pgt2 = [[ps(st, "pgt2%d%d" % (i, k), [128, 512]) for k in range(2)] for i in range(2)]
                        pe2 = [ps(st, "pe2%d" % k, [128, 512]) for k in range(2)]
                        ppt = ps(st, "ppt", [128, 1024], BF16)
                        gsb = [sb(st, "gsb%d" % i, [128, DM], F32) for i in range(2)]
                        ptl = [sb(st, "ptl%d" % i, [128, 256], F32) for i in range(2)]
                        ptb = sb(st, "ptb", [128, 256], BF16)
                        pTb = sb(st, "pTb", [128, 2, 128], BF16)
                        en = [sb(st, "en%d" % i, [128, DM], F32) for i in range(2)]
                        junk5 = sb(st, "junk5", [128, 512], BF16)
                        for tt in range(NT):
                            b = tt % 2
                            S.dma("sp", ptl[b][:], p_d[s, tt * 128:(tt + 1) * 128, :], ("ptl", b), writes=[("ptl", b)])
                            for dh in range(2):
                                for c in range(8):
                                    S.op("pe", lambda: PE_.matmul(pgt2[b][dh][:], lhsT=hnT[:, c, tt * 128:(tt + 1) * 128],
                                                                  rhs=wg[:, c, dh * 512:(dh + 1) * 512], start=(c == 0), stop=(c == 7)),
                                         reads=[("hnT", tt), "wg"], writes=[("pgt2", b, dh)])
                                S.op("act", lambda: A.activation(out=gsb[b][:, dh * 512:(dh + 1) * 512], in_=pgt2[b][dh][:], func=AF.Sigmoid),
                                     reads=[("pgt2", b, dh)], writes=[("gsb", b, dh)])
                            S.op("dve", lambda: V.tensor_copy(out=ptb[:], in_=ptl[b][:]), reads=[("ptl", b)], writes=["ptb"])
                            for k in range(2):
                                S.op("pe", lambda: PE_.transpose(out=ppt[:, k * 128:(k + 1) * 128], in_=ptb[:, k * 128:(k + 1) * 128],
                                                                 identity=ident_b[:]), reads=["ptb", "ident_b"], writes=["ppt"])
                            S.op("dve", lambda: V.tensor_copy(out=pTb[:].rearrange("p a b -> p (a b)"), in_=ppt[:, 0:256]),
                                 reads=["ppt"], writes=["pTb"])
                            for dh in range(2):
                                for k in range(2):
                                    S.op("pe", lambda: PE_.matmul(pe2[dh][:], lhsT=pTb[:, k, :], rhs=wpe[:, k, dh * 512:(dh + 1) * 512],
                                                                  start=(k == 0), stop=(k == 1)), reads=["pTb", "wpe"], writes=[("pe2", dh)])
                                S.op("act", lambda: A.activation(out=junk5[:], in_=pe2[dh][:], func=AF.Square,
                                                                 accum_out=st4[:, 3 * NT + 2 * tt + dh:3 * NT + 2 * tt + dh + 1]),
                                     reads=[("pe2", dh)], writes=["junk5", ("st43", tt, dh)], fuse=False)
                            o5 = 5 * NT + tt
                            S.op("dve", lambda: V.tensor_tensor(out=st4[:, o5:o5 + 1], in0=st4[:, 3 * NT + 2 * tt:3 * NT + 2 * tt + 1],
                                                                in1=st4[:, 3 * NT + 2 * tt + 1:3 * NT + 2 * tt + 2], op=ALU.add),
                                 reads=[("st43", tt, 0), ("st43", tt, 1)], writes=[("st45", tt)])
                            S.op("act", lambda: A.activation(out=st4[:, 6 * NT + tt:6 * NT + tt + 1], in_=st4[:, o5:o5 + 1], func=AF.Sqrt,
                                                             bias=EPS, scale=1.0 / DM), reads=[("st45", tt), "kc"], writes=[("st46", tt)])
                            S.op("dve", lambda: V.reciprocal(out=st4[:, 7 * NT + tt:7 * NT + tt + 1], in_=st4[:, 6 * NT + tt:6 * NT + tt + 1]),
                                 reads=[("st46", tt)], writes=[("st47", tt)])
                            for dh in range(2):
                                hsl = slice(dh * 512, (dh + 1) * 512)
                                S.op("dve", lambda: V.scalar_tensor_tensor(out=en[b][:, hsl], in0=pe2[dh][:],
                                                                           scalar=st4[:, 7 * NT + tt:7 * NT + tt + 1],
                                                                           in1=gpost[:, dh * 512:(dh + 1) * 512], op0=ALU.mult, op1=ALU.mult),
                                     reads=[("pe2", dh), ("st47", tt), "bp_g_post"], writes=[("en", b, dh)])
                                S.op("dve", lambda: V.tensor_tensor(out=en[b][:, hsl], in0=en[b][:, hsl], in1=gsb[b][:, hsl], op=ALU.mult),
                                     reads=[("en", b, dh), ("gsb", b, dh)], writes=[("en", b, dh)])
                                S.op("dve", lambda: V.tensor_tensor(out=en[b][:, hsl], in0=en[b][:, hsl], in1=h[:, tt, hsl], op=ALU.add),
                                     reads=[("en", b, dh), ("h", tt)], writes=[("en", b, dh)])
                            S.dma("sp", out_d[s, tt * 128:(tt + 1) * 128, :], en[b][:], ("outd", b),
                                  reads=[("en", b, 0), ("en", b, 1)], writes=[])
                        S.barrier()
                    S.barrier()
                S.barrier()
        S.final_wait("sp")
        import os as _os2
        if _os2.environ.get("K_PRINTSEMS"):
            print("SEMS", [(i, k) for i, k in enumerate(S.dma_sems.keys())], "n_instr", S.n_instr)
    return nc


def _prep(inputs):
    inp = {k: np.asarray(v) for k, v in inputs.items()}
    cp = make_cpack(inp)
    bp = make_bpack(inp)
    st = make_struct()
    shared = {
        "w_in": np.ascontiguousarray(inp["w_in"][0]),
        "w_out": np.ascontiguousarray(inp["w_out"][0]),
        "cpack": cp.build(), "bpack": bp.build(),
        "rel_bias": np.ascontiguousarray(inp["rel_bias"]),
        "onehot": st["onehot"], "rwmask": st["rwmask"],
        "w2cat": np.ascontiguousarray(inp["rw_w2"][0].reshape(128, 512)),
        "a2cat": np.ascontiguousarray(inp["rw_a2"][0].reshape(128, 512)),
        "g2": np.ascontiguousarray(inp["rw_g2"][0]),
        "w_router": np.ascontiguousarray(inp["w_router"][0]),
        "w1": np.ascontiguousarray(inp["w1"][0]), "w3": np.ascontiguousarray(inp["w3"][0]),
        "w2": np.ascontiguousarray(inp["w2"][0]),
        "w_ple_gate": np.ascontiguousarray(inp["w_ple_gate"][0]),
        "w_ple": np.ascontiguousarray(inp["w_ple"][0]),
    }
    in_maps = []
    for c in range(NCORES):
        m = dict(shared)
        m["x"] = np.ascontiguousarray(inp["x"][c * NSEQ:(c + 1) * NSEQ])
        m["p"] = np.ascontiguousarray(inp["p"][0, c * NSEQ:(c + 1) * NSEQ])
        in_maps.append(m)
    return cp, bp, in_maps


def kernel(**inputs):
    cp, bp, in_maps = _prep(inputs)
    nc = build_program(cp.off, bp.off, cp.n, bp.n)
    res = run_bass_kernel_spmd(nc, in_maps, core_ids=list(range(NCORES)))
    return np.concatenate([r["out"] for r in res.results], axis=0).astype(np.float32)
```

```python
import math
import numpy as np
from contextlib import ExitStack
import concourse.bass as bass
import concourse.mybir as mybir
from concourse.bass_utils import run_bass_kernel_spmd

F32 = mybir.dt.float32
BF16 = mybir.dt.bfloat16
AF = mybir.ActivationFunctionType
ALU = mybir.AluOpType

NCORES = 8
SEQ = 2048
DM = 1024
NSEQ = 2
NT = SEQ // 128
IN_COLS = 3488
RW0 = 1536
NE = 16
CAP = 256
DFF = 2048
CDEC = 0.6065306597126334
LAM_INIT = 0.8 - 0.6 * math.exp(-0.3 * 0)
STRIP_W = 1152
ND = 1279


class Holder:
    def __init__(self, name, sem):
        self.name = name
        self.sem = sem
        self.count = 0


class Sched:
    def __init__(self, nc, stack):
        self.nc = nc
        self.stack = stack
        self.obj = {"pe": nc.tensor, "dve": nc.vector, "act": nc.scalar,
                    "pool": nc.gpsimd, "sp": nc.sync}
        self.eng = {}
        for n in self.obj:
            sem = stack.enter_context(nc.semaphore("s_" + n))
            self.eng[n] = Holder(n, sem)
        self.known = {n: {} for n in self.obj}
        self.last_w = {}
        self.readers = {}
        self.dma_sems = {}
        self.n_instr = 0
        self.snap = {}
        self.seq = {}
        self.gseq = 0

    def _deps(self, reads, writes, e=None):
        own = self.eng.get(e) if e in ("pe",) else None
        toks = []
        for r in reads:
            t = self.last_w.get(r)
            if t is not None:
                toks.append(t)
        for w in writes:
            t = self.last_w.get(w)
            if t is not None and t[0] is not own:
                toks.append(t)
            for h, v in self.readers.get(w, {}).items():
                if h is not own:
                    toks.append((h, v))
        return toks

    def _wait(self, e, toks, keep_one=False):
        kn = self.known[e]
        need = {}
        for (h, v) in toks:
            if kn.get(h, 0) < v and need.get(h, 0) < v:
                need[h] = v
        order = sorted(need.items(), key=lambda kv: -self.seq.get((kv[0], kv[1]), 0))
        items = []
        for h, v in order:
            if kn.get(h, 0) >= v:
                continue
            items.append((h, v))
            kn[h] = v
            for h2, v2 in self.snap.get((h, v), {}).items():
                if kn.get(h2, 0) < v2:
                    kn[h2] = v2
        items.reverse()
        fused = None
        if keep_one and items:
            fused = items.pop()
        for h, v in items:
            self.obj[e].wait_ge(h.sem, v)
            kn[h] = v
            self.n_instr += 1
        if fused is not None:
            kn[fused[0]] = fused[1]
        return fused

    def _commit(self, tok, reads, writes):
        for r in reads:
            d = self.readers.setdefault(r, {})
            if d.get(tok[0], 0) < tok[1]:
                d[tok[0]] = tok[1]
        for w in writes:
            self.last_w[w] = tok
            self.readers[w] = {}

    def op(self, e, fn, reads=(), writes=(), fuse=True):
        toks = self._deps(reads, writes, e)
        fused = self._wait(e, toks, keep_one=fuse)
        ins = fn()
        if fused is not None:
            ins._wait_ge(fused[0].sem, fused[1])
        h = self.eng[e]
        h.count += 1
        ins.then_inc(h.sem, 1)
        tok = (h, h.count)
        self.gseq += 1
        self.seq[tok] = self.gseq
        self.snap[tok] = dict(self.known[e])
        self._commit(tok, reads, writes)
        self.n_instr += 1
        return tok

    def dma(self, q, out, in_, semkey, reads=(), writes=(), **kw):
        toks = self._deps(reads, writes)
        self._wait(q, toks)
        if semkey not in self.dma_sems:
            sem = self.stack.enter_context(self.nc.semaphore("d_%d" % len(self.dma_sems)))
            self.dma_sems[semkey] = Holder("dma_" + str(semkey), sem)
        h = self.dma_sems[semkey]
        ins = self.obj[q].dma_start(out=out, in_=in_, **kw)
        ins.then_inc(h.sem, 16)
        h.count += 16
        tok = (h, h.count)
        self.gseq += 1
        self.seq[tok] = self.gseq
        self.snap[tok] = dict(self.known[q])
        self._commit(tok, reads, writes)
        self.n_instr += 1
        return tok

    def _all(self):
        toks = [(h, h.count) for h in self.eng.values() if h.count > 0]
        toks += [(h, h.count) for h in self.dma_sems.values() if h.count > 0]
        return toks

    def barrier(self):
        toks = self._all()
        for e in self.obj:
            self._wait(e, toks)

    def final_wait(self, e="sp"):
        self._wait(e, self._all())


def _t5_bucket(rel):
    half, max_exact = 16, 8
    ret = np.where(rel > 0, half, 0)
    n = np.abs(rel)
    nf = np.maximum(n, 1).astype(np.float32)
    large = max_exact + (np.log(nf / np.float32(max_exact)) / np.float32(math.log(128 / max_exact))
                         * np.float32(half - max_exact)).astype(np.int32)
    large = np.minimum(large, half - 1)
    return ret + np.where(n < max_exact, n, large)


class Pack:
    def __init__(self):
        self.cols = []
        self.off = {}
        self.n = 0

    def add(self, name, arr):
        arr = np.asarray(arr, np.float32)
        assert arr.shape[0] == 128
        if arr.ndim == 1:
            arr = arr[:, None]
        self.off[name] = (self.n, arr.shape[1])
        self.cols.append(arr)
        self.n += arr.shape[1]

    def build(self):
        return np.ascontiguousarray(np.concatenate(self.cols, axis=1))


def pc(v, nchunk):
    return np.ascontiguousarray(np.asarray(v, np.float32).reshape(nchunk, 128).T)


def make_cpack(inp):
    P = Pack()
    P.add("g_mix", pc(inp["g_mix"][0], 8))
    P.add("qg", np.tile(inp["q_norm_g"][0], 2))
    P.add("kg", np.tile(inp["k_norm_g"][0], 2))
    mu = np.zeros(16 * 128, np.float32)
    mu[:1952] = inp["rw_mu"][0]
    P.add("mu", pc(mu, 16))
    P.add("w0", pc(inp["rw_w0"][0].reshape(-1), 8))
    P.add("a0", pc(inp["rw_a0"][0].reshape(-1), 8))
    P.add("k_k", pc(inp["rw_k_k"][0], 4))
    P.add("k_a", pc(inp["rw_k_a"][0], 4))
    P.add("r_k", pc(inp["rw_r_k"][0].reshape(-1), 4))
    P.add("lnx_g", pc(inp["rw_lnx_g"][0], 4))
    P.add("lnx_b", pc(inp["rw_lnx_b"][0], 4))
    P.add("subln_g", inp["subln_g"][0])
    P.add("g_ple", pc(inp["g_ple"][0], 8))
    rb = inp["rel_bias"]
    far = np.stack([rb[15, :], rb[31, :]], axis=1).reshape(-1)
    P.add("rb_far", np.broadcast_to(far[None, :], (128, 8)))
    P.add("iota_p", np.arange(128, dtype=np.float32))
    P.add("iota_p1", np.arange(128, 256, dtype=np.float32))
    for k in ("lam_q1", "lam_k1", "lam_q2", "lam_k2"):
        P.add(k, np.broadcast_to(inp[k][0][None, :], (128, 64)))
    return P


def make_bpack(inp):
    P = Pack()
    P.add("g_ffn", np.broadcast_to(inp["g_ffn"][0][None, :], (128, DM)))
    P.add("g_post", np.broadcast_to(inp["g_ple_post"][0][None, :], (128, DM)))
    P.add("iota_row", np.broadcast_to(np.arange(CAP, dtype=np.float32)[None, :], (128, CAP)))
    return P


def make_struct():
    m = np.arange(ND)
    delta = 639 - m
    bk = _t5_bucket(delta.astype(np.int32))
    oh = np.zeros((32, ND), np.float32)
    oh[bk, m] = 1.0
    idx = np.arange(64)
    s = idx[:, None]
    t = idx[None, :]
    rw = np.zeros((2, 64, 192), np.float32)
    rw[0, :, 0:64] = (s < t)
    rw[0, :, 64:128] = (s <= t)
    rw[0, :, 128:192] = (t < s)
    rw[1, :, 0:64] = (s > t)
    rw[1, :, 64:128] = (s >= t)
    rw[1, :, 128:192] = (t > s)
    return {"onehot": oh, "rwmask": np.ascontiguousarray(rw.transpose(1, 0, 2).reshape(64, 384))}


def build_program(cp_off, bp_off, ncp, nbp, dbg=None, nseq=NSEQ, stop_after=None):
    dbg = dbg or {}
    nc = bass.Bass("TRN2", target_bir_lowering=False)
    D = {}

    def din(name, shape, dt=F32):
        D[name] = nc.dram_tensor(name, list(shape), dt, kind="ExternalInput").ap()
        return D[name]

    x_d = din("x", [NSEQ, SEQ, DM])
    p_d = din("p", [NSEQ, SEQ, 256])
    w_in_d = din("w_in", [DM, IN_COLS])
    w_out_d = din("w_out", [DM, DM])
    cpack_d = din("cpack", [128, ncp])
    bpack_d = din("bpack", [128, nbp])
    relb_d = din("rel_bias", [32, 4])
    onehot_d = din("onehot", [32, ND])
    rwmask_d = din("rwmask", [64, 384])
    w2cat_d = din("w2cat", [128, 512])
    a2cat_d = din("a2cat", [128, 512])
    g2_d = din("g2", [160, 512])
    wr_d = din("w_router", [DM, NE])
    w1_d = din("w1", [NE, DM, DFF])
    w3_d = din("w3", [NE, DM, DFF])
    w2_d = din("w2", [NE, DFF, DM])
    wg_d = din("w_ple_gate", [DM, DM])
    wpe_d = din("w_ple", [256, DM])
    out_d = nc.dram_tensor("out", [NSEQ, SEQ, DM], F32, kind="ExternalOutput").ap()
    gscr_t = nc.dram_tensor("gscr", [4, ND], F32)
    gscr_d = gscr_t.ap()
    dbg_d = {}
    for k, (shape, dt) in dbg.items():
        dbg_d[k] = nc.dram_tensor("dbg_" + k, list(shape), dt, kind="ExternalOutput").ap()

    with ExitStack() as top:
        S = Sched(nc, top)
        V, A, G, PE_ = nc.vector, nc.scalar, nc.gpsimd, nc.tensor

        uid = [0]

        def sb(st, name, shape, dt):
            uid[0] += 1
            return st.enter_context(nc.sbuf_tensor("%s_s%d" % (name, uid[0]), list(shape), dt))

        def ps(st, name, shape, dt=F32):
            uid[0] += 1
            return st.enter_context(nc.psum_tensor("%s_p%d" % (name, uid[0]), list(shape), dt))

        def C(name, j=0, w=1):
            o, n = cp_off[name]
            return cpack[:, o + j:o + j + w]

        def tap(name, src_ap, reads, idx=None):
            if name in dbg_d:
                dst = dbg_d[name] if idx is None else dbg_d[name][idx]
                S.dma("sp", dst, src_ap, "dbg", reads=reads)

        cpack = sb(top, "cpack", [128, ncp], F32)
        S.dma("sp", cpack[:], cpack_d, "c0", writes=["cpack"])
        kc = sb(top, "kc", [128, 8], F32)
        S.op("pool", lambda: G.memset(kc[:, 0:1], 1e-6), writes=["kc"])
        S.op("pool", lambda: G.memset(kc[:, 1:2], 64e-5), writes=["kc"])
        S.op("pool", lambda: G.memset(kc[:, 2:3], 0.0), writes=["kc"])
        S.op("pool", lambda: G.memset(kc[:, 3:4], 1e-18), writes=["kc"])
        EPS = kc[:, 0:1]
        EPSLN = kc[:, 1:2]
        ident_b = sb(top, "ident_b", [128, 128], BF16)
        ident_f = sb(top, "ident_f", [128, 128], F32)
        for idt, nm in ((ident_b, "ident_b"), (ident_f, "ident_f")):
            S.op("pool", lambda idt=idt: G.memset(idt[:], 1.0), writes=[nm])
            S.op("pool", lambda idt=idt: G.affine_select(
                out=idt[:], in_=idt[:], pattern=[[-1, 128]], compare_op=ALU.is_equal,
                fill=0.0, base=0, channel_multiplier=1), reads=[nm], writes=[nm])
        bd_b = sb(top, "bd_b", [128, 128], BF16)
        S.op("pool", lambda: G.memset(bd_b[:], 0.0), writes=["bd_b"])
        S.op("pool", lambda: G.memset(bd_b[0:64, 0:64], 1.0), reads=["bd_b"], writes=["bd_b"])
        S.op("pool", lambda: G.memset(bd_b[64:128, 64:128], 1.0), reads=["bd_b"], writes=["bd_b"])
        dc = sb(top, "dc", [128, 64], F32)
        S.op("dve", lambda: V.tensor_scalar(out=dc[:, 0:1], in0=C("qg"), scalar1=0.125, scalar2=None,
                                            op0=ALU.mult), reads=["cpack"], writes=["dc0"])
        S.op("dve", lambda: V.tensor_scalar(out=dc[:, 3:19], in0=C("mu", 0, 16), scalar1=-1.0, scalar2=1.0,
                                            op0=ALU.mult, op1=ALU.add), reads=["cpack"], writes=["dc_omm"])
        S.op("dve", lambda: V.tensor_scalar(out=dc[:, 19:35], in0=C("mu", 0, 16), scalar1=0.5, scalar2=None,
                                            op0=ALU.mult), reads=["cpack"], writes=["dc_hmu"])
        S.op("dve", lambda: V.tensor_scalar(out=dc[:, 35:39], in0=C("k_a", 0, 4), scalar1=-1.0, scalar2=1.0,
                                            op0=ALU.mult, op1=ALU.add), reads=["cpack"], writes=["dc_omka"])
        lt = sb(top, "lamtmp", [128, 64], F32)
        l2 = sb(top, "lam2", [128, 4], F32)
        for i, (a, b) in enumerate((("lam_q1", "lam_k1"), ("lam_q2", "lam_k2"))):
            oa, ob = cp_off[a][0], cp_off[b][0]
            S.op("dve", lambda oa=oa, ob=ob: V.tensor_tensor(out=lt[:], in0=cpack[:, oa:oa + 64],
                                                               in1=cpack[:, ob:ob + 64], op=ALU.mult),
                 reads=["cpack"], writes=["lamtmp"])
            S.op("dve", lambda i=i: V.reduce_sum(out=l2[:, i:i + 1], in_=lt[:], axis=mybir.AxisListType.X),
                 reads=["lamtmp"], writes=[("lam2", i)])
        S.op("act", lambda: A.activation(out=l2[:, 2:4], in_=l2[:, 0:2], func=AF.Exp),
             reads=[("lam2", 0), ("lam2", 1)], writes=["lam2e"])
        S.op("dve", lambda: V.tensor_tensor(out=dc[:, 1:2], in0=l2[:, 2:3], in1=l2[:, 3:4], op=ALU.subtract),
             reads=["lam2e"], writes=["dc1a"])
        S.op("dve", lambda: V.tensor_scalar(out=dc[:, 1:2], in0=dc[:, 1:2], scalar1=LAM_INIT, scalar2=None,
                                            op0=ALU.add), reads=["dc1a"], writes=["dc1"])
        S.op("dve", lambda: V.tensor_scalar(out=dc[:, 2:3], in0=dc[:, 1:2], scalar1=-1.0, scalar2=None,
                                            op0=ALU.mult), reads=["dc1"], writes=["dc2"])
        QGS = dc[:, 0:1]
        NLAM = dc[:, 2:3]

        with ExitStack() as st:
            rb_sb = sb(st, "rb_sb", [32, 4], F32)
            oh_sb = sb(st, "oh_sb", [32, ND], F32)
            g4 = sb(st, "g4", [4, ND], F32)
            gp = ps(st, "gp", [4, 512])
            S.dma("sp", rb_sb[:], relb_d, "c1", writes=["rb_sb"])
            S.dma("sp", oh_sb[:], onehot_d, "c2", writes=["oh_sb"])
            for b0 in range(0, ND, 512):
                n = min(512, ND - b0)
                S.op("pe", lambda b0=b0, n=n: PE_.matmul(gp[:, 0:n], lhsT=rb_sb[:], rhs=oh_sb[:, b0:b0 + n],
                                                           start=True, stop=True),
                     reads=["rb_sb", "oh_sb"], writes=["gp"])
                S.op("dve", lambda b0=b0, n=n: V.tensor_copy(out=g4[:, b0:b0 + n], in_=gp[:, 0:n]),
                     reads=["gp"], writes=["g4"])
            S.dma("sp", gscr_d, g4[:], "c3", reads=["g4"], writes=["gscr"])
            S.barrier()

        for s in range(nseq):
            with ExitStack() as sq:
                o_daT = sb(sq, "o_daT", [128, 4, SEQ], BF16)
                o_rwT = sb(sq, "o_rwT", [128, 4, SEQ], BF16)
                mixs = ExitStack()
                sq.callback(mixs.close)
                xnT = sb(mixs, "xnT", [128, 8, SEQ], BF16)
                with ExitStack() as st:
                    xt = [sb(st, "xt%d" % i, [128, DM], F32) for i in range(2)]
                    xs = [sb(st, "xs%d" % i, [128, DM], BF16) for i in range(2)]
                    junk = sb(st, "junk", [128, DM], BF16)
                    ssq = sb(st, "ssq", [128, 2 * NT], F32)
                    ptp = [ps(st, "ptp%d" % i, [128, 1024], BF16) for i in range(2)]
                    for tt in range(NT):
                        b = tt % 2
                        S.dma("sp", xt[b][:], x_d[s, tt * 128:(tt + 1) * 128, :], ("xt", b), writes=[("xt", b)])
                        S.op("act", lambda b=b, tt=tt: A.activation(out=junk[:], in_=xt[b][:], func=AF.Square,
                                                                     accum_out=ssq[:, tt:tt + 1]),
                             reads=[("xt", b)], writes=["junk", ("ssq", tt)], fuse=False)
                        S.op("act", lambda tt=tt: A.activation(out=ssq[:, NT + tt:NT + tt + 1], in_=ssq[:, tt:tt + 1],
                                                               func=AF.Sqrt, bias=EPS, scale=1.0 / DM),
                             reads=[("ssq", tt), "kc"], writes=[("ssd", tt)])
                        S.op("dve", lambda tt=tt: V.reciprocal(out=ssq[:, tt:tt + 1], in_=ssq[:, NT + tt:NT + tt + 1]),
                             reads=[("ssd", tt)], writes=[("rstd", tt)])
                        S.op("dve", lambda b=b, tt=tt: V.tensor_scalar(out=xs[b][:], in0=xt[b][:], scalar1=ssq[:, tt:tt + 1],
                                                                        scalar2=None, op0=ALU.mult),
                             reads=[("xt", b), ("rstd", tt)], writes=[("xs", b)])
                        for c in range(8):
                            S.op("pe", lambda b=b, c=c: PE_.transpose(out=ptp[b][:, c * 128:(c + 1) * 128],
                                                                       in_=xs[b][:, c * 128:(c + 1) * 128], identity=ident_b[:]),
                                 reads=[("xs", b), "ident_b"], writes=[("ptp", b)])
                        for c in range(8):
                            e = "act" if b % 2 else "dve"
                            if e == "dve":
                                S.op("dve", lambda b=b, c=c, tt=tt: V.tensor_scalar(
                                    out=xnT[:, c, tt * 128:(tt + 1) * 128], in0=ptp[b][:, c * 128:(c + 1) * 128],
                                    scalar1=C("g_mix", c), scalar2=None, op0=ALU.mult),
                                    reads=[("ptp", b), "cpack"], writes=[("xnT", tt)])
                            else:
                                S.op("act", lambda b=b, c=c, tt=tt: A.activation(
                                    out=xnT[:, c, tt * 128:(tt + 1) * 128], in_=ptp[b][:, c * 128:(c + 1) * 128],
                                    func=AF.Copy, scale=C("g_mix", c)),
                                    reads=[("ptp", b), "cpack"], writes=[("xnT", tt)])
                    S.barrier()
                tap("xnT", xnT[:], [("xnT", tt) for tt in range(NT)])
                XNT_ALL = [("xnT", tt) for tt in range(NT)]
                if stop_after == "p1":
                    S.barrier()
                    continue

                wlT = sb(mixs, "wlT", [128, SEQ], BF16)
                alT = sb(mixs, "alT", [128, SEQ], BF16)
                glT = sb(mixs, "glT", [128, SEQ], BF16)
                gl2T = sb(mixs, "gl2T", [32, SEQ], BF16)

                def shiftmix(uk, u, m, jc, outk, out, tmp, tmpk):
                    S.op("pool", lambda: G.tensor_tensor(out=tmp[:m, 1:SEQ - 1], in0=u[:m, 0:SEQ - 2], in1=u[:m, 2:SEQ],
                                                         op=ALU.add), reads=[uk], writes=[tmpk])
                    S.op("pool", lambda: G.tensor_copy(out=tmp[:m, 0:1], in_=u[:m, 1:2]), reads=[uk], writes=[tmpk])
                    S.op("pool", lambda: G.tensor_copy(out=tmp[:m, SEQ - 1:SEQ], in_=u[:m, SEQ - 2:SEQ - 1]),
                         reads=[uk], writes=[tmpk])
                    S.op("dve", lambda: V.tensor_scalar(out=tmp[:m, :], in0=tmp[:m, :], scalar1=dc[:m, 19 + jc:20 + jc],
                                                        scalar2=None, op0=ALU.mult),
                         reads=[tmpk, "dc_hmu"], writes=[tmpk])
                    S.op("dve", lambda: V.scalar_tensor_tensor(out=out[:m, :], in0=u[:m, :], scalar=dc[:m, 3 + jc:4 + jc],
                                                               in1=tmp[:m, :], op0=ALU.mult, op1=ALU.add),
                         reads=[uk, tmpk, "dc_omm"], writes=[outk])

                with ExitStack() as at:
                    qT = sb(at, "qT", [128, 4, SEQ], BF16)
                    kT = sb(at, "kT", [128, 4, SEQ], BF16)
                    vaug = sb(at, "vaug", [128, NT, 4, 130], BF16)
                    with ExitStack() as st:
                        wA = sb(st, "wA", [128, 8, 1952], BF16)
                        S.dma("pool", wA[:, :, 0:1536], w_in_d[:, 0:1536].rearrange("(c p) n -> p c n", p=128),
                              "wA", writes=["wA"])
                        S.dma("pool", wA[:, :, 1536:1952], w_in_d[:, 3072:3488].rearrange("(c p) n -> p c n", p=128),
                              "wA", writes=["wA"])
                        pu = [ps(st, "pu%d" % i, [128, 512]) for i in range(2)]
                        pss = [ps(st, "pss%d" % i, [128, 512]) for i in range(2)]
                        sqb = [sb(st, "sqb%d" % i, [128, 512], BF16) for i in range(2)]
                        usb = [sb(st, "usb%d" % i, [128, 512], F32) for i in range(2)]
                        sdb = [sb(st, "sdb%d" % i, [128, 512], F32) for i in range(2)]
                        S.op("pool", lambda: G.memset(vaug[:, :, :, 128:130], 1.0), writes=["vaug_ones"])

                        def proj_fm(pst, pk, c0, m, tb):
                            for dci in range(8):
                                S.op("pe", lambda dci=dci: PE_.matmul(pst[:m, :], lhsT=wA[:, dci, c0:c0 + m],
                                                                       rhs=xnT[:, dci, tb * 512:(tb + 1) * 512],
                                                                       start=(dci == 0), stop=(dci == 7)),
                                     reads=["wA"] + XNT_ALL[tb * 4:tb * 4 + 4], writes=[pk])

                        units = [(kind, h, tb) for kind in range(2) for h in range(4) for tb in range(4)]

                        def qk_front(i):
                            kind, h, tb = units[i]
                            b = i % 2
                            proj_fm(pu[b], ("pu", b), kind * 512 + h * 128, 128, tb)
                            S.op("dve", lambda: V.tensor_copy(out=usb[b][:], in_=pu[b][:]),
                                 reads=[("pu", b)], writes=[("usb", b)])
                            S.op("act", lambda: A.activation(out=sqb[b][:], in_=usb[b][:], func=AF.Square),
                                 reads=[("usb", b)], writes=[("sqb", b)])

                        def qk_back(i):
                            kind, h, tb = units[i]
                            b = i % 2
                            dst = (qT, kT)[kind]
                            gcol = QGS if kind == 0 else C("kg")
                            S.op("pe", lambda: PE_.matmul(pss[b][:], lhsT=bd_b[:], rhs=sqb[b][:], start=True, stop=True),
                                 reads=["bd_b", ("sqb", b)], writes=[("pss", b)])
                            S.op("act", lambda: A.activation(out=sdb[b][:], in_=pss[b][:], func=AF.Ln, bias=EPS,
                                                             scale=1.0 / 64),
                                 reads=[("pss", b), "kc"], writes=[("sdb", b)])
                            S.op("act", lambda: A.activation(out=sdb[b][:], in_=sdb[b][:], func=AF.Exp, scale=-0.5),
                                 reads=[("sdb", b)], writes=[("sdb", b)])
                            S.op("dve", lambda: V.scalar_tensor_tensor(
                                out=dst[:, h, tb * 512:(tb + 1) * 512], in0=usb[b][:], scalar=gcol, in1=sdb[b][:],
                                op0=ALU.mult, op1=ALU.mult),
                                reads=[("usb", b), ("sdb", b), "dc0", "cpack"], writes=[("qk", kind, h, tb)])

                        import os as _os
                        _cut = _os.environ.get("K_CUT", "")
                        if _cut.startswith("qkfront"):
                            proj_fm(pu[0], ("pu", 0), 0, 128, 0)
                            if "a" in _cut[7:]:
                                S.op("act", lambda: A.activation(out=sqb[0][:], in_=pu[0][:], func=AF.Square),
                                     reads=[("pu", 0)], writes=[("sqb", 0)])
                            if "d" in _cut[7:]:
                                S.op("dve", lambda: V.tensor_copy(out=usb[0][:], in_=pu[0][:]),
                                     reads=[("pu", 0)], writes=[("usb", 0)])
                            units = []
                        if _cut == "dmaonly":
                            units = []
                        if _cut == "mmonly":
                            proj_fm(pu[0], ("pu", 0), 0, 128, 0)
                            units = []
                        if _cut == "qk1":
                            units = units[:1]
                        for i in range(len(units)):
                            qk_front(i)
                            if i > 0:
                                qk_back(i - 1)
                        if units:
                            qk_back(len(units) - 1)
                        for tt in range(NT if _cut in ("", "v", "lora") else 0):
                            b = tt % 2
                            for dci in range(8):
                                S.op("pe", lambda dci=dci: PE_.matmul(pu[b][:], lhsT=xnT[:, dci, tt * 128:(tt + 1) * 128],
                                                                       rhs=wA[:, dci, 1024:1536],
                                                                       start=(dci == 0), stop=(dci == 7)),
                                     reads=["wA", ("xnT", tt)], writes=[("pu", b)])
                            src = pu[b][:].rearrange("p (h d) -> p h d", h=4)
                            if tt % 2:
                                S.op("act", lambda: A.activation(out=vaug[:, tt, :, 0:128], in_=src, func=AF.Copy),
                                     reads=[("pu", b)], writes=[("vaug", tt)])
                            else:
                                S.op("dve", lambda: V.tensor_copy(out=vaug[:, tt, :, 0:128], in_=src),
                                     reads=[("pu", b)], writes=[("vaug", tt)])
                        ush = sb(st, "ush", [128, SEQ], F32)
                        utmp = sb(st, "utmp", [128, SEQ], F32)
                        umix = sb(st, "umix", [128, SEQ], F32)
                        for ci, (c0, m, jc, func, dst) in enumerate((
                                (1536, 128, 12, AF.Tanh, wlT), (1664, 128, 13, AF.Copy, alT),
                                (1792, 128, 14, AF.Sigmoid, glT), (1920, 32, 15, AF.Sigmoid, gl2T))[:(4 if _cut in ("", "lora") else 0)]):
                            for tb in range(4):
                                b = tb % 2
                                proj_fm(pu[b], ("pu", b), c0, m, tb)
                                if tb % 2:
                                    S.op("act", lambda: A.activation(out=ush[:m, tb * 512:(tb + 1) * 512], in_=pu[b][:m, :],
                                                                     func=AF.Copy),
                                         reads=[("pu", b)], writes=["ush"])
                                else:
                                    S.op("dve", lambda: V.tensor_copy(out=ush[:m, tb * 512:(tb + 1) * 512], in_=pu[b][:m, :]),
                                         reads=[("pu", b)], writes=["ush"])
                            shiftmix("ush", ush, m, jc, "umix", umix, utmp, "utmp")
                            S.op("act", lambda: A.activation(out=dst[:m, :], in_=umix[:m, :], func=func),
                                 reads=["umix"], writes=[("lora_in", ci)])
                        S.barrier()
                    tap("qT", qT[:], [])
                    tap("kT", kT[:], [])
                    tap("vaug", vaug[:], [])
                    tap("wlT", wlT[:], [])
                    tap("glT", glT[:], [])
                    if stop_after == "p2a":
                        S.barrier()
                        continue

                    with ExitStack() as st:
                        strip = sb(st, "strip", [128, 4, STRIP_W], F32)
                        for i in range(128):
                            src = bass.AP(gscr_t, 127 - i, [[0, 1], [ND, 4], [1, STRIP_W]])
                            S.dma("sp", strip[i:i + 1, :, :], src, "c4", reads=["gscr"], writes=["strip"])
                        PT = [sb(st, "PT%d" % i, [128, NT, 512], BF16) for i in range(2)]
                        tmpb = [sb(st, "tmpb%d" % i, [128, 512], F32) for i in range(2)]
                        spp = [ps(st, "spp%d" % i, [128, 512]) for i in range(2)]
                        av = ps(st, "av", [128, 4, 512])
                        ptr = ps(st, "ptr", [128, 1024], BF16)
                        rin = sb(st, "rin", [128, 4, 2, 1], F32)
                        nl = sb(st, "nl", [128, 2, 2, 1], F32)
                        o0 = sb(st, "o0", [128, 128], F32)
                        osb = sb(st, "osb", [128, 4, 128], F32)
                        onb = sb(st, "onb", [128, 4, 128], BF16)
                        junk2 = sb(st, "junk2", [128, 128], BF16)
                        ss4 = sb(st, "ss4", [128, 8], F32)
                        aunits = [(h, qt, sub) for h in range(4) for qt in range(4) for sub in range(2)]
                        cnt = [0]

                        def qk_exp(u):
                            h, qt, sub = u
                            for kt in range(NT):
                                i = cnt[0]
                                cnt[0] += 1
                                b = i % 2
                                S.op("pe", lambda: PE_.matmul(spp[b][:], lhsT=kT[64 * sub:64 * sub + 64, h, kt * 128:(kt + 1) * 128],
                                                              rhs=qT[64 * sub:64 * sub + 64, h, qt * 512:(qt + 1) * 512],
                                                              start=True, stop=True),
                                     reads=[], writes=[("spp", b)])
                                Dk = kt * 128 - qt * 512
                                if -255 < Dk < 639:
                                    off = 512 - Dk
                                    S.op("dve", lambda: V.tensor_tensor(out=tmpb[b][:], in0=spp[b][:],
                                                                        in1=strip[:, h, off:off + 512], op=ALU.add),
                                         reads=[("spp", b), "strip"], writes=[("tmpb", b)])
                                    S.op("act", lambda: A.activation(out=PT[sub][:, kt, :], in_=tmpb[b][:], func=AF.Exp),
                                         reads=[("tmpb", b)], writes=[("PT", sub, kt)])
                                else:
                                    which = 1 if Dk > 0 else 0
                                    S.op("act", lambda: A.activation(out=PT[sub][:, kt, :], in_=spp[b][:], func=AF.Exp,
                                                                     bias=C("rb_far", h * 2 + which)),
                                         reads=[("spp", b)], writes=[("PT", sub, kt)])
                                yield

                        def accap(sub, qs, lo, hi):
                            return av[:, sub * 2 + qs // 2, (qs % 2) * 256 + lo:(qs % 2) * 256 + hi]

                        def av_mm(u):
                            h, qt, sub = u
                            for qs in range(4):
                                for kt in range(NT):
                                    S.op("pe", lambda: PE_.matmul(accap(sub, qs, 0, 129),
                                                                  lhsT=PT[sub][:, kt, qs * 128:(qs + 1) * 128],
                                                                  rhs=vaug[:, kt, h, 0:129],
                                                                  start=(kt == 0), stop=(kt == NT - 1)),
                                         reads=[("PT", sub, kt)], writes=[("av", sub)])
                                    if kt % 4 == 3:
                                        yield

                        def post_a(h, qt):
                            av4 = av[:].rearrange("p b (j w) -> p b j w", j=2)
                            S.op("dve", lambda: V.reciprocal(out=rin[:], in_=av4[:, :, :, 128:129]),
                                 reads=[("av", 0), ("av", 1)], writes=["rin"])
                            S.op("dve", lambda: V.tensor_scalar(out=nl[:], in0=rin[:, 2:4, :, :], scalar1=NLAM, scalar2=None,
                                                                op0=ALU.mult), reads=["rin", "dc2"], writes=["nl"])
                            for qs in range(4):
                                S.op("dve", lambda: V.tensor_scalar(out=o0[:], in0=accap(0, qs, 0, 128),
                                                                    scalar1=rin[:, qs // 2, qs % 2, :], scalar2=None,
                                                                    op0=ALU.mult),
                                     reads=[("av", 0), "rin"], writes=["o0"])
                                S.op("dve", lambda: V.scalar_tensor_tensor(out=osb[:, qs, :], in0=accap(1, qs, 0, 128),
                                                                           scalar=nl[:, qs // 2, qs % 2, :], in1=o0[:],
                                                                           op0=ALU.mult, op1=ALU.add),
                                     reads=[("av", 1), "nl", "o0"], writes=[("osb", qs)])

                        def post_b(h, qt):
                            for qs in range(4):
                                S.op("act", lambda: A.activation(out=junk2[:], in_=osb[:, qs, :], func=AF.Square,
                                                                 accum_out=ss4[:, qs:qs + 1]),
                                     reads=[("osb", qs)], writes=["junk2", ("ss4", qs)], fuse=False)
                                yield
                            S.op("act", lambda: A.activation(out=ss4[:, 4:8], in_=ss4[:, 0:4], func=AF.Ln, bias=EPS,
                                                             scale=1.0 / 128),
                                 reads=[("ss4", q_) for q_ in range(4)] + ["kc"], writes=["ss4b"])
                            yield
                            S.op("act", lambda: A.activation(out=ss4[:, 4:8], in_=ss4[:, 4:8], func=AF.Exp, scale=-0.5),
                                 reads=["ss4b"], writes=["ss4b"])
                            yield
                            for qs in range(4):
                                S.op("dve", lambda: V.tensor_scalar(out=onb[:, qs, :], in0=osb[:, qs, :],
                                                                    scalar1=ss4[:, 4 + qs:5 + qs], scalar2=1.0 - LAM_INIT,
                                                                    op0=ALU.mult, op1=ALU.mult),
                                     reads=[("osb", qs), "ss4b"], writes=[("onb", qs)])
                                S.op("pe", lambda: PE_.transpose(out=ptr[:, qs * 128:(qs + 1) * 128], in_=onb[:, qs, :],
                                                                 identity=ident_b[:]),
                                     reads=[("onb", qs), "ident_b"], writes=["ptr"])
                                yield
                            S.op("act", lambda: A.activation(out=o_daT[:, h, qt * 512:(qt + 1) * 512], in_=ptr[:, 0:512],
                                                             func=AF.Copy, scale=C("subln_g")),
                                 reads=["ptr", "cpack"], writes=[("o_daT", h, qt)])

                        n_u = len(aunits)
                        if _os.environ.get("K_SKIP_ATTN"):
                            n_u = 0
                        def rr(gens):
                            gens = [g for g in gens if g is not None]
                            while gens:
                                nxt = []
                                for g in gens:
                                    try:
                                        next(g)
                                        nxt.append(g)
                                    except StopIteration:
                                        pass
                                gens = nxt

                        pend = None
                        if n_u:
                            rr([qk_exp(aunits[0])])
                        for i in range(1, n_u):
                            rr([qk_exp(aunits[i]), av_mm(aunits[i - 1]), pend])
                            pend = None
                            if aunits[i - 1][2] == 1:
                                post_a(aunits[i - 1][0], aunits[i - 1][1])
                                pend = post_b(aunits[i - 1][0], aunits[i - 1][1])
                        if n_u:
                            rr([av_mm(aunits[-1]), pend])
                            post_a(aunits[-1][0], aunits[-1][1])
                            rr([post_b(aunits[-1][0], aunits[-1][1])])
                        S.barrier()
                tap("o_daT", o_daT[:], [])
                if stop_after == "attn":
                    S.barrier()
                    continue

                with ExitStack() as rw:
                    w2c = sb(rw, "w2c", [128, 512], BF16)
                    a2c = sb(rw, "a2c", [128, 512], BF16)
                    g2a = sb(rw, "g2a", [128, 512], BF16)
                    g2b = sb(rw, "g2b", [32, 512], BF16)
                    S.dma("pool", w2c[:], w2cat_d, "rwc", writes=["w2c"])
                    S.dma("pool", a2c[:], a2cat_d, "rwc", writes=["a2c"])
                    S.dma("pool", g2a[:], g2_d[0:128, :], "rwc", writes=["g2a"])
                    S.dma("pool", g2b[:], g2_d[128:160, :], "rwc", writes=["g2b"])
                    maskAB = sb(rw, "maskAB", [64, 4, 128], BF16)
                    maskN = sb(rw, "maskN", [64, 4, 64], BF16)
                    ident4 = sb(rw, "ident4", [64, 4, 64], BF16)
                    resetm = sb(rw, "resetm", [128, 256], F32)
                    bd_f = sb(rw, "bd_f", [128, 128], F32)
                    with ExitStack() as st:
                        rwm = sb(st, "rwm", [64, 384], F32)
                        S.dma("sp", rwm[:], rwmask_d, "rwc2", writes=["rwm"])
                        for inst in range(4):
                            d = inst % 2
                            S.op("dve", lambda: V.tensor_copy(out=maskAB[:, inst, :], in_=rwm[:, d * 192:d * 192 + 128]),
                                 reads=["rwm"], writes=["maskAB"])
                            S.op("dve", lambda: V.tensor_copy(out=maskN[:, inst, :], in_=rwm[:, d * 192 + 128:d * 192 + 192]),
                                 reads=["rwm"], writes=["maskN"])
                            S.op("dve", lambda: V.tensor_copy(out=ident4[:, inst, :], in_=ident_b[0:64, 0:64]),
                                 reads=["ident_b"], writes=["ident4"])
                        S.op("dve", lambda: V.memset(resetm[:], 1.0), writes=["resetm"])
                        S.op("dve", lambda: V.memset(resetm[:].rearrange("p (c l) -> p c l", l=64)[:, :, 0:1], 0.0),
                             reads=["resetm"], writes=["resetm"])
                        S.op("dve", lambda: V.memset(bd_f[:], 0.0), writes=["bd_f"])
                        S.op("dve", lambda: V.memset(bd_f[0:64, 0:64], 1.0), reads=["bd_f"], writes=["bd_f"])
                        S.op("dve", lambda: V.memset(bd_f[64:128, 64:128], 1.0), reads=["bd_f"], writes=["bd_f"])
                        S.barrier()

                    TB = 256
                    NB = SEQ // TB
                    CPB = TB // 64

                    def c3(ap):
                        return ap.rearrange("p (c l) -> p c l", l=64)

                    for j in range(4 if stop_after != "rw1" else 1):
                        with ExitStack() as pp:
                            AR = [sb(pp, "AR%d" % d, [128, 32, 2, 64], BF16) for d in range(2)]
                            bbar = [sb(pp, "bbar%d" % d, [128, SEQ], BF16) for d in range(2)]
                            kbar = [sb(pp, "kbar%d" % d, [128, SEQ], BF16) for d in range(2)]
                            vb = sb(pp, "vb", [128, SEQ], BF16)
                            rhi = [sb(pp, "rhi%d" % d, [64, 32, 64], BF16) for d in range(2)]
                            PLf = [sb(pp, "PLf%d" % d, [128, 32], F32) for d in range(2)]
                            PLhi = [sb(pp, "PLhi%d" % d, [64, 32], F32) for d in range(2)]
                            bonus = sb(pp, "bonus", [128, SEQ], BF16)
                            wBj = sb(pp, "wBj", [128, 8, 384], BF16)
                            yacc = sb(pp, "yacc", [64, 2, SEQ], F32)
                            for i3 in range(3):
                                cc0 = RW0 + i3 * 512 + j * 128
                                S.dma("pool", wBj[:, :, i3 * 128:(i3 + 1) * 128],
                                      w_in_d[:, cc0:cc0 + 128].rearrange("(c p) n -> p c n", p=128), "wBj", writes=["wBj"])
                            with ExitStack() as s1:
                                rf = sb(s1, "rf", [128, SEQ], F32)
                                kf = sb(s1, "kf", [128, SEQ], F32)
                                vf = sb(s1, "vf", [128, SEQ], F32)
                                ush = sb(s1, "ush2", [128, SEQ], F32)
                                utmp = sb(s1, "utmp2", [128, SEQ], F32)
                                sqb1 = sb(s1, "sqb1", [128, TB], BF16)
                                nct = sb(s1, "nct", [128, 2, CPB], F32)
                                pu2 = [ps(s1, "pu2%d" % i, [128, 512]) for i in range(2)]
                                pl2a = ps(s1, "pl2a", [128, 2, TB])
                                pl2b = ps(s1, "pl2b", [128, 2, TB])
                                pl2 = [pl2a[:, 0, :], pl2b[:, 0, :], pl2a[:, 1, :], pl2b[:, 1, :]]
                                pst_t = ps(s1, "pst", [128, 512])
                                pbn_t = ps(s1, "pbn", [128, 512])
                                pst = pst_t[:, 0:TB]
                                pbn = pbn_t[:, 0:TB]
                                for i3, (dst, dk) in enumerate(((rf, "rf"), (kf, "kf"), (vf, "vf"))):
                                    for tb in range(4):
                                        b = tb % 2
                                        for dci in range(8):
                                            S.op("pe", lambda dci=dci: PE_.matmul(
                                                pu2[b][:], lhsT=wBj[:, dci, i3 * 128:(i3 + 1) * 128],
                                                rhs=xnT[:, dci, tb * 512:(tb + 1) * 512], start=(dci == 0), stop=(dci == 7)),
                                                reads=["wBj"] + XNT_ALL[tb * 4:tb * 4 + 4], writes=[("pu2", b)])
                                        if tb % 2:
                                            S.op("act", lambda: A.activation(out=ush[:, tb * 512:(tb + 1) * 512], in_=pu2[b][:],
                                                                             func=AF.Copy),
                                                 reads=[("pu2", b)], writes=["ush2"])
                                        else:
                                            S.op("dve", lambda: V.tensor_copy(out=ush[:, tb * 512:(tb + 1) * 512], in_=pu2[b][:]),
                                                 reads=[("pu2", b)], writes=["ush2"])
                                    shiftmix("ush2", ush, 128, i3 * 4 + j, dk, dst, utmp, "utmp2")
                                S.op("act", lambda: A.activation(out=vb[:], in_=vf[:], func=AF.Copy), reads=["vf"], writes=["vb"])
                                S.barrier()
                                slots = [ush[:, i * TB:(i + 1) * TB] for i in range(8)] + [utmp[:, i * TB:(i + 1) * TB] for i in range(8)]
                                (t_sig0, t_sig1, t_a0, t_a1, t_kk, t_x, t_y, t_kd, t_be, t_cs, t_e1, t_e2, t_e3, t_e4, t_ks, t_z) = slots
                                t_sig = (t_sig0, t_sig1)
                                t_a = (t_a0, t_a1)
                                for tb in range(NB):
                                    sl = slice(tb * TB, (tb + 1) * TB)
                                    csl = slice(tb * CPB, (tb + 1) * CPB)
                                    for d in range(2):
                                        S.op("pe", lambda: PE_.matmul(pl2[d], lhsT=w2c[64 * d:64 * d + 64, j * 128:(j + 1) * 128],
                                                                      rhs=wlT[64 * d:64 * d + 64, sl], start=True, stop=True),
                                             reads=["w2c"], writes=[("pl2", d)])
                                        S.op("act", lambda: A.activation(out=t_sig[d], in_=pl2[d], func=AF.Sigmoid,
                                                                         bias=C("w0", d * 4 + j)),
                                             reads=[("pl2", d), "cpack"], writes=[("sig", d)])
                                        S.op("pe", lambda: PE_.matmul(pl2[2 + d], lhsT=a2c[64 * d:64 * d + 64, j * 128:(j + 1) * 128],
                                                                      rhs=alT[64 * d:64 * d + 64, sl], start=True, stop=True),
                                             reads=["a2c"], writes=[("pl2", d)])
                                        S.op("act", lambda: A.activation(out=t_a[d], in_=pl2[2 + d], func=AF.Sigmoid,
                                                                         bias=C("a0", d * 4 + j)),
                                             reads=[("pl2", d), "cpack"], writes=[("a", d)])
                                    S.op("dve", lambda: V.tensor_scalar(out=t_x, in0=kf[:, sl], scalar1=C("k_k", j), scalar2=None,
                                                                        op0=ALU.mult), reads=["kf", "cpack"], writes=["t_x"])
                                    S.op("act", lambda: A.activation(out=sqb1[:], in_=t_x, func=AF.Square),
                                         reads=["t_x"], writes=["sqb1"])
                                    S.op("pe", lambda: PE_.matmul(pst, lhsT=bd_b[:], rhs=sqb1[:], start=True, stop=True),
                                         reads=["sqb1", "bd_b"], writes=["pst"])
                                    S.op("act", lambda: A.activation(out=t_y, in_=pst, func=AF.Ln, bias=kc[:, 3:4]),
                                         reads=["pst", "kc"], writes=["t_y"])
                                    S.op("act", lambda: A.activation(out=t_y, in_=t_y, func=AF.Exp, scale=-0.5), reads=["t_y"], writes=["t_y"])
                                    S.op("dve", lambda: V.tensor_tensor(out=t_kk, in0=t_x, in1=t_y, op=ALU.mult),
                                         reads=["t_x", "t_y"], writes=["t_kk"])
                                    for d in range(2):
                                        S.op("dve", lambda: V.tensor_scalar(out=t_x, in0=t_a[d], scalar1=C("k_a", j),
                                                                            scalar2=dc[:, 35 + j:36 + j], op0=ALU.mult, op1=ALU.add),
                                             reads=[("a", d), "cpack", "dc_omka"], writes=["t_x"])
                                        S.op("dve", lambda: V.tensor_tensor(out=t_kd, in0=t_x, in1=kf[:, sl], op=ALU.mult),
                                             reads=["t_x", "kf"], writes=["t_kd"])
                                        S.op("dve", lambda: V.tensor_tensor(out=t_be, in0=t_kk, in1=t_a[d], op=ALU.mult),
                                             reads=["t_kk", ("a", d)], writes=["t_be"])
                                        S.op("dve", lambda: V.tensor_tensor_scan(out=t_cs, data0=resetm[:], data1=t_sig[d], initial=0.0,
                                                                                 op0=ALU.mult, op1=ALU.add),
                                             reads=["resetm", ("sig", d)], writes=["t_cs"])
                                        S.op("dve", lambda: V.tensor_tensor(out=t_sig[d], in0=t_cs, in1=t_sig[d], op=ALU.subtract),
                                             reads=["t_cs", ("sig", d)], writes=[("sig", d)])
                                        t_csm = t_sig[d]
                                        tot = c3(t_cs)[:, :, 63:64]
                                        S.op("dve", lambda: V.tensor_scalar(out=nct[:, 0, :].rearrange("p (c o) -> p c o", o=1), in0=tot,
                                                                            scalar1=-CDEC, scalar2=None, op0=ALU.mult),
                                             reads=["t_cs"], writes=["nct"])
                                        S.op("dve", lambda: V.tensor_scalar(out=nct[:, 1, :].rearrange("p (c o) -> p c o", o=1), in0=tot,
                                                                            scalar1=CDEC, scalar2=None, op0=ALU.mult),
                                             reads=["t_cs"], writes=["nct"])
                                        S.op("act", lambda: A.activation(out=PLf[d][:, csl], in_=nct[:, 0, :], func=AF.Exp),
                                             reads=["nct"], writes=[("PLf", d)])
                                        if d == 0:
                                            S.op("act", lambda: A.activation(out=t_e1, in_=t_cs, func=AF.Exp, scale=-CDEC),
                                                 reads=["t_cs"], writes=["t_e1"])
                                            S.op("act", lambda: A.activation(out=t_e2, in_=t_cs, func=AF.Exp, scale=CDEC),
                                                 reads=["t_cs"], writes=["t_e2"])
                                            S.op("act", lambda: A.activation(out=t_e3, in_=t_csm, func=AF.Exp, scale=-CDEC),
                                                 reads=[("sig", d)], writes=["t_e3"])
                                            e_r, e_bk, e_a = t_e1, t_e2, t_e3
                                        else:
                                            for c8 in range(CPB):
                                                cs8 = slice(c8 * 64, (c8 + 1) * 64)
                                                S.op("act", lambda: A.activation(out=t_e1[:, cs8], in_=t_csm[:, cs8], func=AF.Exp,
                                                                                 scale=CDEC, bias=nct[:, 0, c8:c8 + 1]),
                                                     reads=[("sig", d), "nct"], writes=["t_e1"])
                                                S.op("act", lambda: A.activation(out=t_e2[:, cs8], in_=t_csm[:, cs8], func=AF.Exp,
                                                                                 scale=-CDEC, bias=nct[:, 1, c8:c8 + 1]),
                                                     reads=[("sig", d), "nct"], writes=["t_e2"])
                                                S.op("act", lambda: A.activation(out=t_e4[:, cs8], in_=t_cs[:, cs8], func=AF.Exp,
                                                                                 scale=CDEC, bias=nct[:, 0, c8:c8 + 1]),
                                                     reads=["t_cs", "nct"], writes=["t_e4"])
                                            e_r, e_bk, e_a = t_e1, t_e2, t_e4
                                        S.op("dve", lambda: V.tensor_tensor(out=AR[d][:, csl, 1, :], in0=c3(rf[:, sl]), in1=c3(e_r),
                                                                            op=ALU.mult),
                                             reads=["rf", "t_e1"], writes=[("AR", d, tb)])
                                        S.op("dve", lambda: V.scalar_tensor_tensor(out=AR[d][:, csl, 0, :], in0=c3(t_kk), scalar=-1.0,
                                                                                   in1=c3(e_a), op0=ALU.mult, op1=ALU.mult),
                                             reads=["t_kk", "t_e3", "t_e4"], writes=[("AR", d, tb)])
                                        S.op("dve", lambda: V.tensor_tensor(out=bbar[d][:, sl], in0=t_be, in1=e_bk, op=ALU.mult),
                                             reads=["t_be", "t_e2"], writes=[("bbar", d, tb)])
                                        S.op("dve", lambda: V.tensor_tensor(out=kbar[d][:, sl], in0=t_kd, in1=e_bk, op=ALU.mult),
                                             reads=["t_kd", "t_e2"], writes=[("kbar", d, tb)])
                                        if d == 0:
                                            S.op("dve", lambda: V.tensor_copy(out=t_ks, in_=t_kd), reads=["t_kd"], writes=["t_ks"])
                                        else:
                                            S.op("dve", lambda: V.tensor_tensor(out=t_ks, in0=t_ks, in1=t_kd, op=ALU.add),
                                                 reads=["t_kd", "t_ks"], writes=["t_ks"])
                                    S.op("dve", lambda: V.scalar_tensor_tensor(out=t_z, in0=rf[:, sl], scalar=C("r_k", j), in1=t_ks,
                                                                               op0=ALU.mult, op1=ALU.mult),
                                         reads=["rf", "t_ks", "cpack"], writes=["t_z"])
                                    S.op("pe", lambda: PE_.matmul(pbn, lhsT=bd_f[:], rhs=t_z, start=True, stop=True),
                                         reads=["t_z", "bd_f"], writes=["pbn"])
                                    S.op("dve", lambda: V.tensor_tensor(out=bonus[:, sl], in0=pbn, in1=vf[:, sl], op=ALU.mult),
                                         reads=["pbn", "vf"], writes=[("bonus", tb)])
                                for d in range(2):
                                    S.dma("sp", rhi[d][:], AR[d][64:128, :, 1, :], "rhi",
                                          reads=[("AR", d, tb) for tb in range(NB)], writes=[("rhi", d)])
                                    S.dma("sp", PLhi[d][:], PLf[d][64:128, :], "rhi", reads=[("PLf", d)], writes=[("PLhi", d)])
                                S.barrier()
                            tap("AR0", AR[0][:], [])
                            tap("AR1", AR[1][:], [])
                            tap("bbar0", bbar[0][:], [])
                            tap("kbar1", kbar[1][:], [])
                            tap("bonus", bonus[:], [])
                            tap("PLf0", PLf[0][:], [])
                            if stop_after == "rw_s1":
                                S.barrier()
                                continue
                            with ExitStack() as s2:
                                tok = [sb(s2, "tok%d" % i, [64, 8, 128], BF16) for i in range(2)]
                                btl = [sb(s2, "btl%d" % i, [128, 2, 2, 64], BF16) for i in range(2)]
                                AbR = [sb(s2, "AbR%d" % i, [64, 4, 128], BF16) for i in range(2)]
                                AkR = [sb(s2, "AkR%d" % i, [64, 4, 128], BF16) for i in range(2)]
                                MNq = [sb(s2, "MNq%d" % i, [64, 2, 4, 64], BF16) for i in range(2)]
                                Rq = [sb(s2, "Rq%d" % i, [64, 4, 64], BF16) for i in range(2)]
                                N0 = sb(s2, "N0", [64, 4, 64], BF16)
                                WuT = [sb(s2, "WuT%d" % i, [64, 4, 64], BF16) for i in range(2)]
                                AVs = sb(s2, "AVs", [64, 4, 64], BF16)
                                Uv = [sb(s2, "Uv%d" % i, [64, 4, 64], F32) for i in range(2)]
                                Us = sb(s2, "Us", [64, 4, 64], BF16)
                                Sf = sb(s2, "Sf", [64, 4, 64], F32)
                                Sb_ = sb(s2, "Sb", [64, 4, 64], BF16)
                                ptok = ps(s2, "ptok", [64, 1024], BF16)
                                pAB = [ps(s2, "pAB%d" % i, [64, 2, 2, 128]) for i in range(2)]
                                pMN = ps(s2, "pMN", [64, 2, 4, 64])
                                pR0 = ps(s2, "pR0", [64, 2, 4, 64])
                                pWA = ps(s2, "pWA", [64, 2, 4, 64])
                                pUU = ps(s2, "pUU", [64, 2, 4, 64])
                                pYS = ps(s2, "pYS", [64, 2, 4, 64])
                                S.op("dve", lambda: V.memset(Sf[:], 0.0), writes=["Sf"])
                                S.op("dve", lambda: V.memset(Sb_[:], 0.0), writes=["Sb"])
                                def gen_pre(i):
                                    b = i % 2
                                    cd = (i, 31 - i)
                                    tq = tok[b]
                                    for d in range(2):
                                        c = cd[d]
                                        for q, src in enumerate((bbar[d], kbar[d])):
                                            S.op("dve", lambda: V.tensor_scalar(out=btl[b][:, d, q, :], in0=src[:, c * 64:(c + 1) * 64],
                                                                                scalar1=PLf[d][:, c:c + 1], scalar2=None, op0=ALU.mult),
                                                 reads=[("PLf", d)], writes=[("btl", b)])
                                    for inst in range(4):
                                        hh, d = inst // 2, inst % 2
                                        c = cd[d]
                                        p0 = 64 * hh
                                        lb = bbar[d][p0:p0 + 64, c * 64:(c + 1) * 64]
                                        lk = kbar[d][p0:p0 + 64, c * 64:(c + 1) * 64]
                                        rAR = AR[d][p0:p0 + 64, c, :, :].rearrange("p a b -> p (a b)")
                                        pn0 = pR0[:, 1, inst, :] if hh == 0 else pMN[:, 1, inst, :]
                                        S.op("pe", lambda: PE_.matmul(pAB[hh][:, d, 0, :], lhsT=lb, rhs=rAR, start=True, stop=True),
                                             reads=[], writes=[("pAB", hh)])
                                        S.op("pe", lambda: PE_.matmul(pn0, lhsT=AR[d][p0:p0 + 64, c, 0, :], rhs=lb,
                                                                      start=True, stop=True),
                                             reads=[], writes=["pR0" if hh == 0 else "pMN"])
                                        S.op("pe", lambda: PE_.matmul(pAB[hh][:, d, 1, :], lhsT=lk, rhs=rAR, start=True, stop=True),
                                             reads=[], writes=[("pAB", hh)])
                                    for hh in range(2):
                                        hi = slice(2 * hh, 2 * hh + 2)
                                        S.op("dve", lambda: V.tensor_tensor(out=AbR[b][:, hi, :], in0=pAB[hh][:, :, 0, :], in1=maskAB[:, hi, :],
                                                                            op=ALU.mult),
                                             reads=[("pAB", hh), "maskAB"], writes=[("AbR", b)])
                                    S.op("dve", lambda: V.tensor_tensor(out=N0[:, 0:2, :], in0=pR0[:, 1, 0:2, :], in1=maskN[:, 0:2, :], op=ALU.mult),
                                         reads=["pR0", "maskN"], writes=["N0"])
                                    S.op("dve", lambda: V.tensor_tensor(out=N0[:, 2:4, :], in0=pMN[:, 1, 2:4, :], in1=maskN[:, 2:4, :], op=ALU.mult),
                                         reads=["pMN", "maskN"], writes=["N0"])
                                    yield
                                    for d in range(2):
                                        c = cd[d]
                                        srcs = (AR[d][:, c, 0, :], btl[b][:, d, 0, :], btl[b][:, d, 1, :])
                                        for q, src in enumerate(srcs):
                                            S.op("pe", lambda: PE_.transpose(out=ptok[:, (d * 3 + q) * 128:(d * 3 + q + 1) * 128], in_=src,
                                                                             identity=ident_b[:]),
                                                 reads=[("btl", b), "ident_b"], writes=["ptok"])
                                        S.op("pe", lambda: PE_.transpose(out=ptok[:, (6 + d) * 128:(7 + d) * 128],
                                                                         in_=vb[:, c * 64:(c + 1) * 64], identity=ident_b[:]),
                                             reads=["vb", "ident_b"], writes=["ptok"])
                                    S.op("act", lambda: A.activation(out=tq[:].rearrange("p a b -> p (a b)"), in_=ptok[:], func=AF.Copy),
                                         reads=["ptok"], writes=[("tok", b)])
                                    yield

                                    def mn_mm(l_, Mp_, Np_, rk_):
                                        for inst in range(4):
                                            if l_ < 5:
                                                S.op("pe", lambda: PE_.matmul(pMN[:, 0, inst, :], lhsT=Np_[:, inst, :], rhs=Mp_[:, inst, :],
                                                                              start=True, stop=True),
                                                     reads=rk_, writes=["pMN"])
                                            S.op("pe", lambda: PE_.matmul(pWA[:, 0, inst, :], lhsT=Mp_[:, inst, :], rhs=Np_[:, inst, :],
                                                                          start=True, stop=True),
                                                 reads=rk_, writes=["pWA"])

                                    def mn_copy(l_):
                                        q_ = l_ % 2
                                        if l_ < 5:
                                            S.op("act", lambda: A.activation(out=MNq[q_][:, 0, :, :], in_=pMN[:, 0, :, :], func=AF.Copy),
                                                 reads=["pMN"], writes=[("Mq", q_)])
                                        S.op("dve", lambda: V.tensor_copy(out=MNq[q_][:, 1, :, :], in_=pWA[:, 0, :, :]),
                                             reads=["pWA"], writes=[("Nq", q_)])

                                    mn_mm(1, AbR[b][:, :, 0:64], N0[:], [("AbR", b), "N0"])
                                    mn_copy(1)
                                    S.op("dve", lambda: V.tensor_tensor(out=Rq[0][:], in0=AbR[b][:, :, 0:64], in1=ident4[:], op=ALU.add),
                                         reads=[("AbR", b), "ident4"], writes=[("Rq", 0)])
                                    for hh in range(2):
                                        hi = slice(2 * hh, 2 * hh + 2)
                                        S.op("dve", lambda: V.tensor_tensor(out=AkR[b][:, hi, :], in0=pAB[hh][:, :, 1, :], in1=maskAB[:, hi, :],
                                                                            op=ALU.mult),
                                             reads=[("pAB", hh), "maskAB"], writes=[("AkR", b)])
                                    yield
                                    for l in range(1, 6):
                                        lb_ = l % 2
                                        MK = [("Mq", lb_), ("Nq", lb_)]
                                        if l < 5:
                                            mn_mm(l + 1, MNq[lb_][:, 0, :, :], MNq[lb_][:, 1, :, :], MK)
                                        for inst in range(4):
                                            S.op("pe", lambda: PE_.matmul(pR0[:, 0, inst, :], lhsT=MNq[lb_][:, 1, inst, :],
                                                                          rhs=Rq[1 - lb_][:, inst, :], start=True, stop=True),
                                                 reads=[("Nq", lb_), ("Rq", 1 - lb_)], writes=["pR0"])
                                        if l == 1:
                                            for inst in range(4):
                                                hh, d = inst // 2, inst % 2
                                                hs = slice(hh * 64, hh * 64 + 64)
                                                S.op("pe", lambda: PE_.matmul(pUU[:, 0, inst, :], lhsT=AkR[b][:, inst, 0:64], rhs=tq[:, 6 + d, hs],
                                                                              start=True, stop=True),
                                                     reads=[("tok", b), ("AkR", b)], writes=["pUU"])
                                        if l < 5:
                                            mn_copy(l + 1)
                                        S.op("dve", lambda: V.tensor_tensor(out=Rq[lb_][:], in0=pR0[:, 0, :, :], in1=Rq[1 - lb_][:],
                                                                            op=ALU.add),
                                             reads=["pR0", ("Rq", 1 - lb_)], writes=[("Rq", lb_)])
                                        if l == 1:
                                            S.op("dve", lambda: V.tensor_copy(out=AVs[:], in_=pUU[:, 0, :, :]), reads=["pUU"], writes=["AVs"])
                                        yield
                                    R = Rq[1]
                                    for inst in range(4):
                                        hh, d = inst // 2, inst % 2
                                        hs = slice(hh * 64, hh * 64 + 64)
                                        S.op("pe", lambda: PE_.matmul(pWA[:, 0, inst, :], lhsT=tq[:, d * 3, hs], rhs=R[:, inst, :],
                                                                      start=True, stop=True),
                                             reads=[("tok", b), ("Rq", 1)], writes=["pWA"])
                                        S.op("pe", lambda: PE_.matmul(pR0[:, 1, inst, :], lhsT=R[:, inst, :], rhs=AVs[:, inst, :],
                                                                      start=True, stop=True),
                                             reads=[("Rq", 1), "AVs"], writes=["pR0"])
                                    yield
                                    S.op("act", lambda: A.activation(out=WuT[b][:], in_=pWA[:, 0, :, :], func=AF.Copy),
                                         reads=["pWA"], writes=[("WuT", b)])
                                    S.op("dve", lambda: V.tensor_copy(out=Uv[b][:], in_=pR0[:, 1, :, :]),
                                         reads=["pR0"], writes=[("Uv", b)])
                                    yield

                                def gen_loop(i):
                                    b = i % 2
                                    cd = (i, 31 - i)
                                    tq = tok[b]
                                    for inst in range(4):
                                        yield
                                        S.op("pe", lambda: PE_.matmul(pUU[:, 1, inst, :], lhsT=WuT[b][:, inst, :], rhs=Sb_[:, inst, :],
                                                                      start=True, stop=True),
                                             reads=[("WuT", b), "Sb"], writes=["pUU"])
                                    yield
                                    S.op("dve", lambda: V.tensor_tensor(out=Us[:], in0=pUU[:, 1, :, :], in1=Uv[b][:], op=ALU.add),
                                         reads=["pUU", ("Uv", b)], writes=["Us"])
                                    yield
                                    for inst in range(4):
                                        hh, d = inst // 2, inst % 2
                                        c = cd[d]
                                        hs = slice(hh * 64, hh * 64 + 64)
                                        rr = AR[d][0:64, c, 1, :] if hh == 0 else rhi[d][:, c, :]
                                        yield
                                        S.op("pe", lambda: PE_.matmul(pYS[:, 0, inst, :], lhsT=Sb_[:, inst, :], rhs=rr, start=True, stop=False),
                                             reads=["Sb", ("rhi", d)], writes=["pYS"])
                                        yield
                                        S.op("pe", lambda: PE_.matmul(pYS[:, 0, inst, :], lhsT=tq[:, 6 + d, hs], rhs=AkR[b][:, inst, 64:128],
                                                                      start=False, stop=False),
                                             reads=[("tok", b), ("AkR", b)], writes=["pYS"])
                                        yield
                                        S.op("pe", lambda: PE_.matmul(pYS[:, 0, inst, :], lhsT=Us[:, inst, :], rhs=AbR[b][:, inst, 64:128],
                                                                      start=False, stop=True),
                                             reads=["Us", ("AbR", b)], writes=["pYS"])
                                        yield
                                        S.op("pe", lambda: PE_.matmul(pYS[:, 1, inst, :], lhsT=tq[:, d * 3 + 1, hs], rhs=Us[:, inst, :],
                                                                      start=True, stop=False),
                                             reads=[("tok", b), "Us"], writes=["pYS"])
                                        yield
                                        S.op("pe", lambda: PE_.matmul(pYS[:, 1, inst, :], lhsT=tq[:, d * 3 + 2, hs], rhs=tq[:, 6 + d, hs],
                                                                      start=False, stop=True),
                                             reads=[("tok", b)], writes=["pYS"])
                                    yield
                                    for d in range(2):
                                        c = cd[d]
                                        dst = yacc[:, :, c * 64:(c + 1) * 64]
                                        src = pYS[:, 0, :, :].rearrange("p (h e) w -> p h e w", e=2)[:, :, d, :]
                                        if i < 16:
                                            S.op("dve", lambda: V.tensor_copy(out=dst, in_=src),
                                                 reads=["pYS"], writes=[("yacc", c)])
                                        else:
                                            S.op("dve", lambda: V.tensor_tensor(out=dst, in0=src, in1=dst, op=ALU.add),
                                                 reads=["pYS", ("yacc", c)], writes=[("yacc", c)])
                                    yield
                                    for inst in range(4):
                                        hh, d = inst // 2, inst % 2
                                        c = cd[d]
                                        plc = PLf[d][0:64, c:c + 1] if hh == 0 else PLhi[d][:, c:c + 1]
                                        yield
                                        S.op("dve", lambda: V.scalar_tensor_tensor(out=Sf[:, inst, :], in0=Sf[:, inst, :], scalar=plc,
                                                                                   in1=pYS[:, 1, inst, :], op0=ALU.mult, op1=ALU.add),
                                             reads=["pYS", "Sf", ("PLhi", d), ("PLf", d)], writes=["Sf"])
                                    yield
                                    S.op("act", lambda: A.activation(out=Sb_[:], in_=Sf[:], func=AF.Copy), reads=["Sf"], writes=["Sb"])
                                    yield

                                def drive(ga, gb):
                                    a_done = b_done = False
                                    while not (a_done and b_done):
                                        if not a_done:
                                            try:
                                                next(ga)
                                            except StopIteration:
                                                a_done = True
                                        if not b_done:
                                            try:
                                                next(gb)
                                            except StopIteration:
                                                b_done = True

                                for _ in gen_pre(0):
                                    pass
                                for i in range(32):
                                    drive(gen_loop(i), gen_pre(i + 1) if i + 1 < 32 else iter(()))
                                S.barrier()
                            with ExitStack() as s3:
                                y128 = sb(s3, "y128", [128, SEQ], F32)
                                fa = sb(s3, "fa", [128, 512], F32)
                                fb = sb(s3, "fb", [128, 512], F32)
                                pm = ps(s3, "pm", [128, 512])
                                pv = ps(s3, "pv", [128, 512])
                                pg = ps(s3, "pg", [128, 512])
                                S.op("act", lambda: A.activation(out=y128[0:64, :], in_=yacc[:, 0, :], func=AF.Copy),
                                     reads=[], writes=["y128a"])
                                S.dma("sp", y128[64:128, :], yacc[:, 1, :], "ymv", reads=[], writes=["y128b"])
                                tap("y128", y128[:], ["y128a", "y128b"])
                                for tb in range(4):
                                    sl = slice(tb * 512, (tb + 1) * 512)
                                    S.op("pe", lambda: PE_.matmul(pm[:], lhsT=bd_f[:], rhs=y128[:, sl], start=True, stop=True),
                                         reads=["y128a", "y128b", "bd_f"], writes=["pm"])
                                    S.op("dve", lambda: V.scalar_tensor_tensor(out=fa[:], in0=pm[:], scalar=-1.0 / 64, in1=y128[:, sl],
                                                                               op0=ALU.mult, op1=ALU.add),
                                         reads=["pm", "y128a", "y128b"], writes=["fa"])
                                    S.op("act", lambda: A.activation(out=fb[:], in_=fa[:], func=AF.Square), reads=["fa"], writes=["fb"])
                                    S.op("pe", lambda: PE_.matmul(pv[:], lhsT=bd_f[:], rhs=fb[:], start=True, stop=True),
                                         reads=["fb", "bd_f"], writes=["pv"])
                                    S.op("act", lambda: A.activation(out=fb[:], in_=pv[:], func=AF.Ln, bias=EPSLN, scale=1.0 / 64),
                                         reads=["pv", "kc"], writes=["fb"])
                                    S.op("act", lambda: A.activation(out=fb[:], in_=fb[:], func=AF.Exp, scale=-0.5), reads=["fb"], writes=["fb"])
                                    S.op("dve", lambda: V.tensor_tensor(out=fa[:], in0=fa[:], in1=fb[:], op=ALU.mult),
                                         reads=["fa", "fb"], writes=["fa"])
                                    S.op("dve", lambda: V.tensor_scalar(out=fa[:], in0=fa[:], scalar1=C("lnx_g", j), scalar2=C("lnx_b", j),
                                                                        op0=ALU.mult, op1=ALU.add),
                                         reads=["fa", "cpack"], writes=["fa"])
                                    S.op("dve", lambda: V.tensor_tensor(out=fa[:], in0=fa[:], in1=bonus[:, sl], op=ALU.add),
                                         reads=["fa"], writes=["fa"])
                                    S.op("pe", lambda: PE_.matmul(pg[:], lhsT=g2a[:, j * 128:(j + 1) * 128], rhs=glT[:, sl],
                                                                  start=True, stop=False), reads=["g2a"], writes=["pg"])
                                    S.op("pe", lambda: PE_.matmul(pg[:], lhsT=g2b[0:32, j * 128:(j + 1) * 128], rhs=gl2T[0:32, sl],
                                                                  start=False, stop=True), reads=["g2b"], writes=["pg"])
                                    S.op("dve", lambda: V.tensor_tensor(out=o_rwT[:, j, sl], in0=fa[:], in1=pg[:], op=ALU.mult),
                                         reads=["fa", "pg"], writes=[("o_rwT", j, tb)])
                                S.barrier()
                    S.barrier()
                tap("o_rwT", o_rwT[:] if stop_after != "rw1" else o_rwT[:, 0:1, :], [])
                S.barrier()
                mixs.close()
                if stop_after in ("attn", "rw", "rw1", "rw_s1", "p2a", "p1"):
                    S.barrier()
                    continue
                h = sb(sq, "h", [128, NT, DM], F32)
                HK = [("h", tt) for tt in range(NT)]
                with ExitStack() as st:
                    wo = sb(st, "wo", [128, 8, DM], BF16)
                    S.dma("pool", wo[:], w_out_d.rearrange("(c p) n -> p c n", p=128), "wo", writes=["wo"])
                    xt2 = [sb(st, "xt2%d" % i, [128, DM], F32) for i in range(2)]
                    po = [[ps(st, "po%d%d" % (i, k), [128, 512]) for k in range(2)] for i in range(2)]
                    for tt in range(NT):
                        b = tt % 2
                        S.dma("sp", xt2[b][:], x_d[s, tt * 128:(tt + 1) * 128, :], ("xt2", b), writes=[("xt2", b)])
                        for dh in range(2):
                            for c in range(8):
                                src = o_daT if c < 4 else o_rwT
                                S.op("pe", lambda: PE_.matmul(po[b][dh][:], lhsT=src[:, c % 4, tt * 128:(tt + 1) * 128],
                                                              rhs=wo[:, c, dh * 512:(dh + 1) * 512], start=(c == 0), stop=(c == 7)),
                                     reads=["wo"], writes=[("po", b, dh)])
                            S.op("dve", lambda: V.tensor_tensor(out=h[:, tt, dh * 512:(dh + 1) * 512], in0=po[b][dh][:],
                                                                in1=xt2[b][:, dh * 512:(dh + 1) * 512], op=ALU.add),
                                 reads=[("po", b, dh), ("xt2", b)], writes=[("h", tt)])
                    S.barrier()
                tap("h1", h[:], [])
                if stop_after == "h1":
                    S.barrier()
                    continue

                def load_bp(st_, name):
                    o, n = bp_off[name]
                    t_ = sb(st_, "bp_" + name, [128, n], F32)
                    S.dma("sp", t_[:], bpack_d[:, o:o + n], "bpk", writes=["bp_" + name])
                    return t_

                with ExitStack() as me:
                    Xn = sb(me, "Xn", [128, NT, DM], BF16)
                    aff_tok = sb(me, "aff_tok", [128, NT, NE], F32)
                    aff_hl = sb(me, "aff_hl", [128, NT, NE, 2], BF16)
                    posm_tok = sb(me, "posm_tok", [128, NT, NE], F32)
                    posm_b = sb(me, "posm_b", [NE, SEQ], BF16)
                    Eoh = sb(me, "Eoh", [NE, NE, 128], BF16)
                    S.op("dve", lambda: V.memset(Eoh[:], 1.0), writes=["Eoh"])
                    for e in range(NE):
                        S.op("dve", lambda: V.tensor_scalar(out=Eoh[:, e, :], in0=Eoh[:, e, :], scalar1=ident_f[0:NE, e:e + 1],
                                                            scalar2=None, op0=ALU.mult), reads=["Eoh", "ident_f"], writes=["Eoh"])
                    with ExitStack() as st:
                        gffn = load_bp(st, "g_ffn")
                        wr_f = sb(st, "wr_f", [128, 8, NE], F32)
                        S.dma("sp", wr_f[:], wr_d.rearrange("(c p) e -> p c e", p=128), "wrf", writes=["wr_f"])
                        affT = sb(st, "affT", [NE, SEQ], F32)
                        wk = sb(st, "wk", [NE, SEQ], F32)
                        maskT = sb(st, "maskT", [NE, SEQ], F32)
                        cumT = sb(st, "cumT", [NE, SEQ], F32)
                        ones16 = sb(st, "ones16", [NE, SEQ], F32)
                        m8 = sb(st, "m8", [NE, 8], F32)
                        xnf = [sb(st, "xnf%d" % i, [128, DM], F32) for i in range(2)]
                        xnTf = [sb(st, "xnTf%d" % i, [128, 8, 128], F32) for i in range(2)]
                        junk3 = sb(st, "junk3", [128, DM], BF16)
                        sst = sb(st, "sst", [128, 3 * NT], F32)
                        sm = sb(st, "sm", [128, 4 * NT], F32)
                        ex = sb(st, "ex", [128, NE], F32)
                        dtmp = sb(st, "dtmp", [128, NE], F32)
                        pT = [ps(st, "pT%d" % i, [128, 512]) for i in range(2)]
                        plg = ps(st, "plg", [128, 512])
                        paT = ps(st, "paT", [NE, 512])
                        ppm = ps(st, "ppm", [128, 512])
                        S.op("pool", lambda: G.memset(ones16[:], 1.0), writes=["ones16"])
                        for tt in range(NT):
                            b = tt % 2
                            S.op("act", lambda: A.activation(out=junk3[:], in_=h[:, tt, :], func=AF.Square,
                                                             accum_out=sst[:, tt:tt + 1]),
                                 reads=[("h", tt)], writes=["junk3", ("sst", tt)], fuse=False)
                            S.op("act", lambda: A.activation(out=sst[:, NT + tt:NT + tt + 1], in_=sst[:, tt:tt + 1], func=AF.Ln,
                                                             bias=EPS, scale=1.0 / DM), reads=[("sst", tt), "kc"], writes=[("sst1", tt)])
                            S.op("act", lambda: A.activation(out=sst[:, 2 * NT + tt:2 * NT + tt + 1], in_=sst[:, NT + tt:NT + tt + 1],
                                                             func=AF.Exp, scale=-0.5),
                                 reads=[("sst1", tt)], writes=[("sst2", tt)])
                            S.op("dve", lambda: V.scalar_tensor_tensor(out=xnf[b][:], in0=h[:, tt, :],
                                                                       scalar=sst[:, 2 * NT + tt:2 * NT + tt + 1], in1=gffn[:],
                                                                       op0=ALU.mult, op1=ALU.mult),
                                 reads=[("h", tt), ("sst2", tt), "bp_g_ffn"], writes=[("xnf", b)])
                            S.op("act", lambda: A.activation(out=Xn[:, tt, :], in_=xnf[b][:], func=AF.Copy),
                                 reads=[("xnf", b)], writes=[("Xn", tt)])
                            for c in range(8):
                                S.op("pe", lambda: PE_.transpose(out=pT[c // 4][:, (c % 4) * 128:(c % 4 + 1) * 128],
                                                                 in_=xnf[b][:, c * 128:(c + 1) * 128], identity=ident_f[:]),
                                     reads=[("xnf", b), "ident_f"], writes=[("pT", c // 4)])
                            S.op("dve", lambda: V.tensor_copy(out=xnTf[b][:, 0:4, :].rearrange("p a b -> p (a b)"), in_=pT[0][:]),
                                 reads=[("pT", 0)], writes=[("xnTf", b, 0)])
                            S.op("act", lambda: A.activation(out=xnTf[b][:, 4:8, :].rearrange("p a b -> p (a b)"), in_=pT[1][:],
                                                             func=AF.Copy), reads=[("pT", 1)], writes=[("xnTf", b, 1)])
                            for c in range(8):
                                S.op("pe", lambda: PE_.matmul(plg[:, 0:NE], lhsT=xnTf[b][:, c, :], rhs=wr_f[:, c, :],
                                                              start=(c == 0), stop=(c == 7)),
                                     reads=[("xnTf", b, 0), ("xnTf", b, 1), "wr_f"], writes=["plg"])
                            S.op("dve", lambda: V.reduce_max(out=sm[:, tt:tt + 1], in_=plg[:, 0:NE], axis=mybir.AxisListType.X),
                                 reads=["plg"], writes=[("sm0", tt)])
                            S.op("dve", lambda: V.tensor_scalar(out=sm[:, NT + tt:NT + tt + 1], in0=sm[:, tt:tt + 1], scalar1=-1.0,
                                                                scalar2=None, op0=ALU.mult), reads=[("sm0", tt)], writes=[("sm1", tt)])
                            S.op("dve", lambda: V.tensor_scalar(out=ex[:], in0=plg[:, 0:NE], scalar1=sm[:, NT + tt:NT + tt + 1],
                                                                scalar2=None, op0=ALU.add), reads=["plg", ("sm1", tt)], writes=["exa"])
                            S.op("act", lambda: A.activation(out=ex[:], in_=ex[:], func=AF.Exp,
                                                             accum_out=sm[:, 2 * NT + tt:2 * NT + tt + 1]),
                                 reads=["exa"], writes=["ex", ("sm2", tt)], fuse=False)
                            S.op("dve", lambda: V.reciprocal(out=sm[:, 3 * NT + tt:3 * NT + tt + 1], in_=sm[:, 2 * NT + tt:2 * NT + tt + 1]),
                                 reads=[("sm2", tt)], writes=[("sm3", tt)])
                            S.op("dve", lambda: V.tensor_scalar(out=aff_tok[:, tt, :], in0=ex[:], scalar1=sm[:, 3 * NT + tt:3 * NT + tt + 1],
                                                                scalar2=None, op0=ALU.mult), reads=["ex", ("sm3", tt)], writes=[("aff", tt)])
                            S.op("dve", lambda: V.tensor_copy(out=aff_hl[:, tt, :, 0], in_=aff_tok[:, tt, :]),
                                 reads=[("aff", tt)], writes=[("affh", tt)])
                            S.op("dve", lambda: V.tensor_tensor(out=dtmp[:], in0=aff_tok[:, tt, :], in1=aff_hl[:, tt, :, 0], op=ALU.subtract),
                                 reads=[("aff", tt), ("affh", tt)], writes=["dtmp"])
                            S.op("dve", lambda: V.tensor_copy(out=aff_hl[:, tt, :, 1], in_=dtmp[:]), reads=["dtmp"], writes=[("affl", tt)])
                            S.op("pe", lambda: PE_.transpose(out=paT[:, (tt % 4) * 128:(tt % 4 + 1) * 128], in_=aff_tok[:, tt, :],
                                                             identity=ident_f[:]), reads=[("aff", tt), "ident_f"], writes=["paT"])
                            if tt % 4 == 3:
                                t0 = (tt - 3) * 128
                                S.op("dve", lambda: V.tensor_copy(out=affT[:, t0:t0 + 512], in_=paT[:]), reads=["paT"], writes=["affT"])
                        S.op("dve", lambda: V.tensor_copy(out=wk[:], in_=affT[:]), reads=["affT"], writes=["wk"])
                        for r in range(CAP // 8):
                            S.op("dve", lambda: V.max(out=m8[:], in_=wk[:]), reads=["wk"], writes=["m8"], fuse=False)
                            if r < CAP // 8 - 1:
                                S.op("dve", lambda: V.match_replace(out=wk[:], in_to_replace=m8[:], in_values=wk[:], imm_value=-1.0),
                                     reads=["wk", "m8"], writes=["wk"], fuse=False)
                        S.op("dve", lambda: V.tensor_scalar(out=maskT[:], in0=affT[:], scalar1=m8[:, 7:8], scalar2=None, op0=ALU.is_ge),
                             reads=["affT", "m8"], writes=["maskT"])
                        S.op("dve", lambda: V.tensor_tensor_scan(out=cumT[:], data0=ones16[:], data1=maskT[:], initial=0.0,
                                                                 op0=ALU.mult, op1=ALU.add), reads=["ones16", "maskT"], writes=["cumT"])
                        S.op("dve", lambda: V.tensor_tensor(out=cumT[:], in0=cumT[:], in1=maskT[:], op=ALU.mult),
                             reads=["cumT", "maskT"], writes=["cumT"])
                        S.op("dve", lambda: V.tensor_scalar(out=cumT[:], in0=cumT[:], scalar1=-1.0, scalar2=None, op0=ALU.add),
                             reads=["cumT"], writes=["cumT"])
                        S.op("dve", lambda: V.tensor_copy(out=posm_b[:], in_=cumT[:]), reads=["cumT"], writes=["posm_b"])
                        for tt in range(NT):
                            S.op("pe", lambda: PE_.transpose(out=ppm[:, (tt % 16) * NE:(tt % 16 + 1) * NE], in_=cumT[:, tt * 128:(tt + 1) * 128],
                                                             identity=ident_f[0:NE, 0:NE]), reads=["cumT", "ident_f"], writes=["ppm"])
                        S.op("dve", lambda: V.tensor_copy(out=posm_tok[:].rearrange("p a b -> p (a b)"), in_=ppm[:, 0:NT * NE]),
                             reads=["ppm"], writes=["posm_tok"])
                        S.barrier()
                    tap("aff_tok", aff_tok[:], [])
                    tap("posm_tok", posm_tok[:], [])
                    if stop_after == "router":
                        S.barrier()
                        continue
                    with ExitStack() as st:
                        iotar = load_bp(st, "iota_row")
                        Sel = sb(st, "Sel", [128, NT, CAP], BF16)
                        SelT = sb(st, "SelT", [128, 2, SEQ], BF16)
                        XeT = sb(st, "XeT", [128, 8, CAP], BF16)
                        HT = [sb(st, "HT%d" % i, [128, 2, CAP], BF16) for i in range(2)]
                        s1b = [sb(st, "s1b%d" % i, [128, CAP], F32) for i in range(2)]
                        Ysb = sb(st, "Ysb", [128, 2, DM], BF16)
                        gate = sb(st, "gate", [128, 2], F32)
                        gate4 = sb(st, "gate4", [128, 2, 2], F32)
                        w1b = [sb(st, "w1b%d" % i, [128, 8, 256], BF16) for i in range(2)]
                        w3b = [sb(st, "w3b%d" % i, [128, 8, 256], BF16) for i in range(2)]
                        w2b = [sb(st, "w2b%d" % i, [128, 2, DM], BF16) for i in range(2)]
                        pG = [ps(st, "pG%d" % i, [128, 2, CAP]) for i in range(2)]
                        pH = [ps(st, "pH%d" % i, [128, 2, CAP]) for i in range(2)]
                        pY = [[ps(st, "pY%d%d" % (i, k), [128, 512]) for k in range(2)] for i in range(2)]
                        wcnt = [0]

                        def load_w(e, fb):
                            wb_ = wcnt[0] % 2
                            wcnt[0] += 1
                            S.dma("pool", w1b[wb_][:], w1_d[e, :, fb * 256:(fb + 1) * 256].rearrange("(c p) n -> p c n", p=128),
                                  ("w1b", wb_), writes=[("w1b", wb_)])
                            S.dma("pool", w3b[wb_][:], w3_d[e, :, fb * 256:(fb + 1) * 256].rearrange("(c p) n -> p c n", p=128),
                                  ("w3b", wb_), writes=[("w3b", wb_)])
                            S.dma("pool", w2b[wb_][:], w2_d[e, fb * 256:(fb + 1) * 256, :].rearrange("(c p) n -> p c n", p=128),
                                  ("w2b", wb_), writes=[("w2b", wb_)])
                            return wb_

                        n_exp = NE if not _os.environ.get("K_NEXP") else int(_os.environ["K_NEXP"])
                        blocks = [(e, fb) for e in range(n_exp) for fb in range(8)]
                        wslot = {}
                        wslot[blocks[0]] = load_w(*blocks[0])
                        hcnt = [0]
                        for e in range(n_exp):
                            def build_sel(e_):
                                for tt in range(NT):
                                    S.op("dve", lambda: V.tensor_scalar(out=Sel[:, tt, :], in0=iotar[:], scalar1=posm_tok[:, tt, e_:e_ + 1],
                                                                        scalar2=None, op0=ALU.is_equal),
                                         reads=["posm_tok", "bp_iota_row"], writes=[("Sel", tt)])
                            if e == 0:
                                build_sel(0)
                            for tb in range(4):
                                pbc = pH[tb % 2][:].rearrange("p a b -> p (a b)")
                                S.op("pe", lambda: PE_.matmul(pbc, lhsT=Eoh[:, e, :], rhs=posm_b[:, tb * 512:(tb + 1) * 512],
                                                              start=True, stop=True), reads=["Eoh", "posm_b"], writes=[("pH", tb % 2)])
                                for cc in range(2):
                                    S.op("dve", lambda: V.tensor_scalar(out=SelT[:, cc, tb * 512:(tb + 1) * 512], in0=pbc,
                                                                        scalar1=C("iota_p" if cc == 0 else "iota_p1"), scalar2=None,
                                                                        op0=ALU.is_equal),
                                         reads=[("pH", tb % 2), "cpack"], writes=[("SelT", cc, tb)])
                            pgt = pH[0][:, 0, 0:4].rearrange("p (a b) -> p a b", b=2)
                            for cc in range(2):
                                for tt in range(NT):
                                    S.op("pe", lambda: PE_.matmul(pgt[:, cc, :], lhsT=Sel[:, tt, cc * 128:(cc + 1) * 128],
                                                                  rhs=aff_hl[:, tt, e, :], start=(tt == 0), stop=(tt == NT - 1)),
                                         reads=[("Sel", tt), ("affh", tt), ("affl", tt)], writes=[("pH", 0)])
                            S.op("dve", lambda: V.tensor_copy(out=gate4[:], in_=pgt), reads=[("pH", 0)], writes=["gate4"])
                            S.op("dve", lambda: V.tensor_tensor(out=gate[:], in0=gate4[:, :, 0], in1=gate4[:, :, 1], op=ALU.add),
                                 reads=["gate4"], writes=["gate"])
                            for dcb in range(4):
                                gb = dcb % 2
                                for k in range(2):
                                    dc_ = dcb * 2 + k
                                    for tt in range(NT):
                                        S.op("pe", lambda: PE_.matmul(pG[gb][:, k, :], lhsT=Xn[:, tt, dc_ * 128:(dc_ + 1) * 128],
                                                                      rhs=Sel[:, tt, :], start=(tt == 0), stop=(tt == NT - 1)),
                                             reads=[("Xn", tt), ("Sel", tt)], writes=[("pG", gb)])
                                if gb == 0:
                                    S.op("act", lambda: A.activation(out=XeT[:, dcb * 2:dcb * 2 + 2, :], in_=pG[gb][:], func=AF.Copy),
                                         reads=[("pG", gb)], writes=[("XeT", dcb)])
                                else:
                                    S.op("dve", lambda: V.tensor_copy(out=XeT[:, dcb * 2:dcb * 2 + 2, :], in_=pG[gb][:]),
                                         reads=[("pG", gb)], writes=[("XeT", dcb)])
                            XK = [("XeT", q) for q in range(4)]
                            if e + 1 < n_exp:
                                build_sel(e + 1)
                            for fb in range(8):
                                wb_ = wslot[(e, fb)]
                                nxt = blocks.index((e, fb)) + 1
                                if nxt < len(blocks):
                                    wslot[blocks[nxt]] = load_w(*blocks[nxt])
                                hb = hcnt[0] % 2
                                hcnt[0] += 1
                                for fc in range(2):
                                    pb_ = fc % 2
                                    for k, wsrc, wk_ in ((0, w1b, "w1b"), (1, w3b, "w3b")):
                                        for dc_ in range(8):
                                            S.op("pe", lambda: PE_.matmul(pH[pb_][:, k, :], lhsT=wsrc[wb_][:, dc_, fc * 128:(fc + 1) * 128],
                                                                          rhs=XeT[:, dc_, :], start=(dc_ == 0), stop=(dc_ == 7)),
                                                 reads=XK + [(wk_, wb_)], writes=[("pH", pb_)])
                                    S.op("act", lambda: A.activation(out=s1b[pb_][:], in_=pH[pb_][:, 0, :], func=AF.Silu),
                                         reads=[("pH", pb_)], writes=[("s1b", pb_)])
                                    S.op("dve", lambda: V.tensor_tensor(out=HT[hb][:, fc, :], in0=pH[pb_][:, 1, :], in1=s1b[pb_][:], op=ALU.mult),
                                         reads=[("pH", pb_), ("s1b", pb_)], writes=[("HT", hb, fc)])
                                for cc in range(2):
                                    for dh in range(2):
                                        for fc in range(2):
                                            S.op("pe", lambda: PE_.matmul(pY[cc][dh][:], lhsT=HT[hb][:, fc, cc * 128:(cc + 1) * 128],
                                                                          rhs=w2b[wb_][:, fc, dh * 512:(dh + 1) * 512],
                                                                          start=(fb == 0 and fc == 0), stop=(fb == 7 and fc == 1)),
                                                 reads=[("HT", hb, fc), ("w2b", wb_)], writes=[("pY", cc, dh)])
                            for cc in range(2):
                                for dh in range(2):
                                    S.op("act", lambda: A.activation(out=Ysb[:, cc, dh * 512:(dh + 1) * 512], in_=pY[cc][dh][:], func=AF.Copy,
                                                                     scale=gate[:, cc:cc + 1]),
                                         reads=[("pY", cc, dh), "gate"], writes=[("Ysb", cc)])
                            for tt in range(NT):
                                for dh in range(2):
                                    oi = (tt * 2 + dh) % 4
                                    pO = pY[oi // 2][oi % 2][:]
                                    ok_ = ("pY", oi // 2, oi % 2)
                                    for cc in range(2):
                                        S.op("pe", lambda: PE_.matmul(pO, lhsT=SelT[:, cc, tt * 128:(tt + 1) * 128],
                                                                      rhs=Ysb[:, cc, dh * 512:(dh + 1) * 512], start=(cc == 0), stop=(cc == 1)),
                                             reads=[("SelT", cc, tt // 4), ("Ysb", cc)], writes=[ok_])
                                    S.op("dve", lambda: V.tensor_tensor(out=h[:, tt, dh * 512:(dh + 1) * 512], in0=pO,
                                                                        in1=h[:, tt, dh * 512:(dh + 1) * 512], op=ALU.add),
                                         reads=[ok_, ("h", tt)], writes=[("h", tt)])
                        S.barrier()
                    S.barrier()
                tap("h2", h[:], [])
                if stop_after == "moe":
                    S.barrier()
                    continue

                with ExitStack() as pl:
                    gpost = load_bp(pl, "g_post")
                    hnT = sb(pl, "hnT", [128, 8, SEQ], BF16)
                    wg = sb(pl, "wg", [128, 8, DM], BF16)
                    wpe = sb(pl, "wpe", [128, 2, DM], BF16)
                    S.dma("pool", wg[:], wg_d.rearrange("(c p) n -> p c n", p=128), "wg", writes=["wg"])
                    S.dma("pool", wpe[:], wpe_d.rearrange("(c p) n -> p c n", p=128), "wpe", writes=["wpe"])
                    hs = [sb(pl, "hs%d" % i, [128, DM], BF16) for i in range(2)]
                    junk4 = sb(pl, "junk4", [128, DM], BF16)
                    st4 = sb(pl, "st4", [128, 8 * NT], F32)
                    pls = ExitStack()
                    pt4 = [ps(pls, "pt4%d" % i, [128, 1024], BF16) for i in range(2)]
                    for tt in range(NT):
                        b = tt % 2
                        S.op("act", lambda: A.activation(out=junk4[:], in_=h[:, tt, :], func=AF.Square, accum_out=st4[:, tt:tt + 1]),
                             reads=[("h", tt)], writes=["junk4", ("st4", tt)], fuse=False)
                        S.op("act", lambda: A.activation(out=st4[:, NT + tt:NT + tt + 1], in_=st4[:, tt:tt + 1], func=AF.Sqrt, bias=EPS,
                                                         scale=1.0 / DM), reads=[("st4", tt), "kc"], writes=[("st41", tt)])
                        S.op("dve", lambda: V.reciprocal(out=st4[:, 2 * NT + tt:2 * NT + tt + 1], in_=st4[:, NT + tt:NT + tt + 1]),
                             reads=[("st41", tt)], writes=[("st42", tt)])
                        S.op("dve", lambda: V.tensor_scalar(out=hs[b][:], in0=h[:, tt, :], scalar1=st4[:, 2 * NT + tt:2 * NT + tt + 1],
                                                            scalar2=None, op0=ALU.mult), reads=[("h", tt), ("st42", tt)], writes=[("hs", b)])
                        for c in range(8):
                            S.op("pe", lambda: PE_.transpose(out=pt4[b][:, c * 128:(c + 1) * 128], in_=hs[b][:, c * 128:(c + 1) * 128],
                                                             identity=ident_b[:]), reads=[("hs", b), "ident_b"], writes=[("pt4", b)])
                        for c in range(8):
                            if b == 0:
                                S.op("dve", lambda: V.tensor_scalar(out=hnT[:, c, tt * 128:(tt + 1) * 128], in0=pt4[b][:, c * 128:(c + 1) * 128],
                                                                    scalar1=C("g_ple", c), scalar2=None, op0=ALU.mult),
                                     reads=[("pt4", b), "cpack"], writes=[("hnT", tt)])
                            else:
                                S.op("act", lambda: A.activation(out=hnT[:, c, tt * 128:(tt + 1) * 128], in_=pt4[b][:, c * 128:(c + 1) * 128],
                                                                 func=AF.Copy, scale=C("g_ple", c)),
                                     reads=[("pt4", b), "cpack"], writes=[("hnT", tt)])
                    S.barrier()
                    pls.close()
                    with ExitStack() as st:
                        pgt2 = [[ps(st, "pgt2%d%d" % (i, k), [128, 512]) for k in range(2)] for i in range(2)]
                        pe2 = [ps(st, "pe2%d" % k, [128, 512]) for k in range(2)]
                        ppt = ps(st, "ppt", [128, 1024], BF16)
                        gsb = [sb(st, "gsb%d" % i, [128, DM], F32) for i in range(2)]
                        ptl = [sb(st, "ptl%d" % i, [128, 256], F32) for i in range(2)]
                        ptb = sb(st, "ptb", [128, 256], BF16)
                        pTb = sb(st, "pTb", [128, 2, 128], BF16)
                        en = [sb(st, "en%d" % i, [128, DM], F32) for i in range(2)]
                        junk5 = sb(st, "junk5", [128, 512], BF16)
                        for tt in range(NT):
                            b = tt % 2
                            S.dma("sp", ptl[b][:], p_d[s, tt * 128:(tt + 1) * 128, :], ("ptl", b), writes=[("ptl", b)])
                            for dh in range(2):
                                for c in range(8):
                                    S.op("pe", lambda: PE_.matmul(pgt2[b][dh][:], lhsT=hnT[:, c, tt * 128:(tt + 1) * 128],
                                                                  rhs=wg[:, c, dh * 512:(dh + 1) * 512], start=(c == 0), stop=(c == 7)),
                                         reads=[("hnT", tt), "wg"], writes=[("pgt2", b, dh)])
                                S.op("act", lambda: A.activation(out=gsb[b][:, dh * 512:(dh + 1) * 512], in_=pgt2[b][dh][:], func=AF.Sigmoid),
                                     reads=[("pgt2", b, dh)], writes=[("gsb", b, dh)])
                            S.op("dve", lambda: V.tensor_copy(out=ptb[:], in_=ptl[b][:]), reads=[("ptl", b)], writes=["ptb"])
                            for k in range(2):
                                S.op("pe", lambda: PE_.transpose(out=ppt[:, k * 128:(k + 1) * 128], in_=ptb[:, k * 128:(k + 1) * 128],
                                                                 identity=ident_b[:]), reads=["ptb", "ident_b"], writes=["ppt"])
                            S.op("dve", lambda: V.tensor_copy(out=pTb[:].rearrange("p a b -> p (a b)"), in_=ppt[:, 0:256]),
                                 reads=["ppt"], writes=["pTb"])
                            for dh in range(2):
                                for k in range(2):
                                    S.op("pe", lambda: PE_.matmul(pe2[dh][:], lhsT=pTb[:, k, :], rhs=wpe[:, k, dh * 512:(dh + 1) * 512],
                                                                  start=(k == 0), stop=(k == 1)), reads=["pTb", "wpe"], writes=[("pe2", dh)])
                                S.op("act", lambda: A.activation(out=junk5[:], in_=pe2[dh][:], func=AF.Square,
                                                                 accum_out=st4[:, 3 * NT + 2 * tt + dh:3 * NT + 2 * tt + dh + 1]),
                                     reads=[("pe2", dh)], writes=["junk5", ("st43", tt, dh)], fuse=False)
                            o5 = 5 * NT + tt
                            S.op("dve", lambda: V.tensor_tensor(out=st4[:, o5:o5 + 1], in0=st4[:, 3 * NT + 2 * tt:3 * NT + 2 * tt + 1],
                                                                in1=st4[:, 3 * NT + 2 * tt + 1:3 * NT + 2 * tt + 2], op=ALU.add),
                                 reads=[("st43", tt, 0), ("st43", tt, 1)], writes=[("st45", tt)])
                            S.op("act", lambda: A.activation(out=st4[:, 6 * NT + tt:6 * NT + tt + 1], in_=st4[:, o5:o5 + 1], func=AF.Sqrt,
                                                             bias=EPS, scale=1.0 / DM), reads=[("st45", tt), "kc"], writes=[("st46", tt)])
                            S.op("dve", lambda: V.reciprocal(out=st4[:, 7 * NT + tt:7 * NT + tt + 1], in_=st4[:, 6 * NT + tt:6 * NT + tt + 1]),
                                 reads=[("st46", tt)], writes=[("st47", tt)])
                            for dh in range(2):
                                hsl = slice(dh * 512, (dh + 1) * 512)
                                S.op("dve", lambda: V.scalar_tensor_tensor(out=en[b][:, hsl], in0=pe2[dh][:],
                                                                           scalar=st4[:, 7 * NT + tt:7 * NT + tt + 1],
                                                                           in1=gpost[:, dh * 512:(dh + 1) * 512], op0=ALU.mult, op1=ALU.mult),
                                     reads=[("pe2", dh), ("st47", tt), "bp_g_post"], writes=[("en", b, dh)])
                                S.op("dve", lambda: V.tensor_tensor(out=en[b][:, hsl], in0=en[b][:, hsl], in1=gsb[b][:, hsl], op=ALU.mult),
                                     reads=[("en", b, dh), ("gsb", b, dh)], writes=[("en", b, dh)])
                                S.op("dve", lambda: V.tensor_tensor(out=en[b][:, hsl], in0=en[b][:, hsl], in1=h[:, tt, hsl], op=ALU.add),
                                     reads=[("en", b, dh), ("h", tt)], writes=[("en", b, dh)])
                            S.dma("sp", out_d[s, tt * 128:(tt + 1) * 128, :], en[b][:], ("outd", b),
                                  reads=[("en", b, 0), ("en", b, 1)], writes=[])
                        S.barrier()
                    S.barrier()
                S.barrier()
        S.final_wait("sp")
        import os as _os2
        if _os2.environ.get("K_PRINTSEMS"):
            print("SEMS", [(i, k) for i, k in enumerate(S.dma_sems.keys())], "n_instr", S.n_instr)
    return nc


def _prep(inputs):
    inp = {k: np.asarray(v) for k, v in inputs.items()}
    cp = make_cpack(inp)
    bp = make_bpack(inp)
    st = make_struct()
    shared = {
        "w_in": np.ascontiguousarray(inp["w_in"][0]),
        "w_out": np.ascontiguousarray(inp["w_out"][0]),
        "cpack": cp.build(), "bpack": bp.build(),
        "rel_bias": np.ascontiguousarray(inp["rel_bias"]),
        "onehot": st["onehot"], "rwmask": st["rwmask"],
        "w2cat": np.ascontiguousarray(inp["rw_w2"][0].reshape(128, 512)),
        "a2cat": np.ascontiguousarray(inp["rw_a2"][0].reshape(128, 512)),
        "g2": np.ascontiguousarray(inp["rw_g2"][0]),
        "w_router": np.ascontiguousarray(inp["w_router"][0]),
        "w1": np.ascontiguousarray(inp["w1"][0]), "w3": np.ascontiguousarray(inp["w3"][0]),
        "w2": np.ascontiguousarray(inp["w2"][0]),
        "w_ple_gate": np.ascontiguousarray(inp["w_ple_gate"][0]),
        "w_ple": np.ascontiguousarray(inp["w_ple"][0]),
    }
    in_maps = []
    for c in range(NCORES):
        m = dict(shared)
        m["x"] = np.ascontiguousarray(inp["x"][c * NSEQ:(c + 1) * NSEQ])
        m["p"] = np.ascontiguousarray(inp["p"][0, c * NSEQ:(c + 1) * NSEQ])
        in_maps.append(m)
    return cp, bp, in_maps


def kernel(**inputs):
    cp, bp, in_maps = _prep(inputs)
    nc = build_program(cp.off, bp.off, cp.n, bp.n)
    res = run_bass_kernel_spmd(nc, in_maps, core_ids=list(range(NCORES)))
    return np.concatenate([r["out"] for r in res.results], axis=0).astype(np.float32)
```
